# Optimizing a Trainium2 kernel written in Bass

```python
import math
import jax, jax.numpy as jnp
from jax import lax
import numpy as np

D_MODEL = 1024
BATCH = 4
SEQ = 8192
DEPTH = 4

MIX_WIDTH = D_MODEL
HEAD_DIM = 64
CONV_WIDTH = D_MODEL // 4
NSA_HEADS = 8
NSA_KV_GROUPS = 2
NSA_WIDTH = NSA_HEADS * HEAD_DIM
KV_WIDTH = NSA_KV_GROUPS * HEAD_DIM
MEM_HEADS = 4
MEM_WIDTH = MEM_HEADS * HEAD_DIM
N_MEM = 256
CONV_K = 3
CMP_BLOCK = 32
CMP_STRIDE = 16
CMP_HIDDEN = 256
SEL_BLOCK = 64
SEL_TOPK = 16
WINDOW = 512
Q_BLOCK = 128
N_BRANCH = 3
RMS_EPS = 1e-6
FORCE_SCORE = 1e4
IN_SPLITS = (CONV_WIDTH, CONV_WIDTH, CONV_WIDTH, CONV_WIDTH,
             NSA_WIDTH, KV_WIDTH, KV_WIDTH, KV_WIDTH, KV_WIDTH, KV_WIDTH, KV_WIDTH,
             N_BRANCH * NSA_HEADS, NSA_WIDTH,
             MEM_WIDTH, MEM_WIDTH)
IN_WIDTH = sum(IN_SPLITS)

kernel_name = "hymba_conv_nsa_memory_trunk"


def rmsnorm(x, g):
    xf = x.astype(jnp.float32)
    y = xf * lax.rsqrt(jnp.mean(xf * xf, axis=-1, keepdims=True) + RMS_EPS)
    return (y * g.astype(jnp.float32)).astype(x.dtype)


def alibi_slopes(n):
    return jnp.exp2(-8.0 * jnp.arange(1, n + 1, dtype=jnp.float32) / n)


def masked_softmax(s, mask):
    s = jnp.where(mask, s.astype(jnp.float32), -jnp.inf)
    m = jnp.max(s, axis=-1, keepdims=True)
    m = jnp.where(jnp.isfinite(m), m, 0.0)
    p = jnp.exp(s - m)
    return p / jnp.maximum(jnp.sum(p, axis=-1, keepdims=True), 1e-30)


def short_conv(b, c, h, w, bias):
    T = h.shape[1]
    up = jnp.pad(c * h, ((0, 0), (CONV_K - 1, 0), (0, 0)))
    y = bias + up[:, 0:T] * w[0]
    for k in range(1, CONV_K):
        y = y + up[:, k:k + T] * w[k]
    return b * y


def compress_kv(k, pos, w1, w2):
    B, G, T, HD = k.shape
    ch = k.reshape(B, G, T // CMP_STRIDE, CMP_STRIDE, HD)
    blocks = jnp.concatenate([ch[:, :, :-1], ch[:, :, 1:]], axis=3) + pos
    flat = blocks.reshape(B, G, blocks.shape[2], CMP_BLOCK * HD)
    return jax.nn.gelu(flat @ w1) @ w2


def nsa_attention(q, k_cmp, v_cmp, k_sel, v_sel, k_win, v_win, gate_logits,
                  pos_k, w1_k, w2_k, pos_v, w1_v, w2_v):
    B, T, _ = q.shape
    G, R, HD = NSA_KV_GROUPS, NSA_HEADS // NSA_KV_GROUPS, HEAD_DIM
    n_cmp = T // CMP_STRIDE - 1
    n_sel = T // SEL_BLOCK
    top_k = min(SEL_TOPK, n_sel)
    n_qb = T // Q_BLOCK

    def to_heads(t, n):
        return t.reshape(B, T, n, HD).transpose(0, 2, 1, 3)

    qh = (to_heads(q, NSA_HEADS) * HD ** -0.5).reshape(B, G, R, T, HD)
    kc = compress_kv(to_heads(k_cmp, G), pos_k, w1_k, w2_k)
    vc = compress_kv(to_heads(v_cmp, G), pos_v, w1_v, w2_v)
    ks_blk = to_heads(k_sel, G).reshape(B, G, n_sel, SEL_BLOCK, HD)
    vs_blk = to_heads(v_sel, G).reshape(B, G, n_sel, SEL_BLOCK, HD)
    wpad = ((0, 0), (0, 0), (WINDOW, 0), (0, 0))
    kw = jnp.pad(to_heads(k_win, G), wpad)
    vw = jnp.pad(to_heads(v_win, G), wpad)
    gates = jax.nn.sigmoid(gate_logits.astype(jnp.float32))
    gates = gates.reshape(B, T, N_BRANCH, G, R).transpose(2, 0, 3, 4, 1)[..., None]
    slopes = alibi_slopes(NSA_HEADS).reshape(1, G, R, 1, 1)
    cmp_end = jnp.arange(n_cmp) * CMP_STRIDE + (CMP_BLOCK - 1)
    blk = jnp.arange(n_sel)
    b_ix = jnp.arange(B)[:, None, None, None]
    g_ix = jnp.arange(G)[None, :, None, None]

    def step(qb):
        q0 = qb * Q_BLOCK
        qi = lax.dynamic_slice_in_dim(qh, q0, Q_BLOCK, axis=3)
        gi = lax.dynamic_slice_in_dim(gates, q0, Q_BLOCK, axis=4)
        t = q0 + jnp.arange(Q_BLOCK)
        d_c = (t[:, None] - cmp_end[None, :]).astype(jnp.float32)
        s_c = jnp.einsum('bgrqd,bgnd->bgrqn', qi, kc) - slopes * d_c
        p_c = masked_softmax(s_c, d_c >= 0)
        o_cmp = jnp.einsum('bgrqn,bgnd->bgrqd', p_c, vc)
        imp = jnp.sum(p_c, axis=2)
        chunk = (jnp.pad(imp, ((0, 0), (0, 0), (0, 0), (0, 1)))
                 + jnp.pad(imp, ((0, 0), (0, 0), (0, 0), (1, 0))))
        imp_sel = chunk.reshape(B, G, Q_BLOCK, n_sel, SEL_BLOCK // CMP_STRIDE).sum(-1)
        cur = t // SEL_BLOCK
        forced = ((blk[None, :] == 0) | (blk[None, :] == cur[:, None])
                  | (blk[None, :] == cur[:, None] - 1))
        future = blk[None, :] > cur[:, None]
        imp_sel = jnp.where(forced, FORCE_SCORE, jnp.where(future, -1.0, imp_sel))
        _, idx = lax.top_k(imp_sel, top_k)
        k_g = ks_blk[b_ix, g_ix, idx]
        v_g = vs_blk[b_ix, g_ix, idx]
        spos = idx[..., None] * SEL_BLOCK + jnp.arange(SEL_BLOCK)
        d_s = (t[:, None, None] - spos).astype(jnp.float32)[:, :, None]
        s_s = jnp.einsum('bgrqd,bgqnsd->bgrqns', qi, k_g) - slopes[..., None] * d_s
        m_s = jnp.broadcast_to(d_s >= 0, s_s.shape)
        shp = s_s.shape
        p_s = masked_softmax(s_s.reshape(shp[:4] + (-1,)), m_s.reshape(shp[:4] + (-1,))).reshape(shp)
        o_sel = jnp.einsum('bgrqns,bgqnsd->bgrqd', p_s, v_g)
        kwi = lax.dynamic_slice_in_dim(kw, q0, Q_BLOCK + WINDOW, axis=2)
        vwi = lax.dynamic_slice_in_dim(vw, q0, Q_BLOCK + WINDOW, axis=2)
        wpos = q0 - WINDOW + jnp.arange(Q_BLOCK + WINDOW)
        d_w = t[:, None] - wpos[None, :]
        m_w = (d_w >= 0) & (d_w < WINDOW) & (wpos[None, :] >= 0)
        s_w = jnp.einsum('bgrqd,bgsd->bgrqs', qi, kwi) - slopes * d_w.astype(jnp.float32)
        o_win = jnp.einsum('bgrqs,bgsd->bgrqd', masked_softmax(s_w, m_w), vwi)
        return gi[0] * o_cmp + gi[1] * o_sel + gi[2] * o_win

    out = lax.map(step, jnp.arange(n_qb))
    return out.transpose(1, 0, 4, 2, 3, 5).reshape(B, T, NSA_WIDTH).astype(q.dtype)


def memory_attention(q, mem_n, w_mem_kv):
    B, T, _ = q.shape
    kv = mem_n @ w_mem_kv
    k, v = jnp.split(kv, 2, axis=-1)
    qh = q.reshape(B, T, MEM_HEADS, HEAD_DIM) * HEAD_DIM ** -0.5
    kh = k.reshape(B, -1, MEM_HEADS, HEAD_DIM)
    vh = v.reshape(B, -1, MEM_HEADS, HEAD_DIM)
    p = jax.nn.softmax(jnp.einsum('bthd,bmhd->bhtm', qh, kh).astype(jnp.float32), axis=-1)
    o = jnp.einsum('bhtm,bmhd->bthd', p, vh)
    return o.reshape(B, T, MEM_WIDTH).astype(q.dtype)


def setup_inputs(seed: int = 0) -> dict:
    key = jax.random.key(seed)
    ks = jax.random.split(key, 20)
    L, D = DEPTH, D_MODEL
    f32 = jnp.float32

    def nrm(k, shape, fan_in):
        return jax.random.normal(k, shape, f32) * fan_in ** -0.5

    def gain(k):
        return 1.0 + 0.02 * jax.random.normal(k, (L, D), f32)

    return {
        "x": jax.random.normal(ks[0], (BATCH, SEQ, D), f32),
        "mem": jax.random.normal(ks[1], (BATCH, N_MEM, D), f32),
        "pre_norm_g": gain(ks[2]),
        "post_norm_g": gain(ks[3]),
        "mem_norm_g": gain(ks[4]),
        "w_in": nrm(ks[5], (L, D, IN_WIDTH), D),
        "b_gate": 0.01 * jax.random.normal(ks[6], (L, N_BRANCH * NSA_HEADS), f32),
        "conv_w": nrm(ks[7], (L, CONV_K, CONV_WIDTH), CONV_K),
        "conv_b": 0.01 * jax.random.normal(ks[8], (L, CONV_WIDTH), f32),
        "cmp_pos_k": 0.02 * jax.random.normal(ks[9], (L, CMP_BLOCK, HEAD_DIM), f32),
        "cmp_w1_k": nrm(ks[10], (L, CMP_BLOCK * HEAD_DIM, CMP_HIDDEN), CMP_BLOCK * HEAD_DIM),
        "cmp_w2_k": nrm(ks[11], (L, CMP_HIDDEN, HEAD_DIM), CMP_HIDDEN),
        "cmp_pos_v": 0.02 * jax.random.normal(ks[12], (L, CMP_BLOCK, HEAD_DIM), f32),
        "cmp_w1_v": nrm(ks[13], (L, CMP_BLOCK * HEAD_DIM, CMP_HIDDEN), CMP_BLOCK * HEAD_DIM),
        "cmp_w2_v": nrm(ks[14], (L, CMP_HIDDEN, HEAD_DIM), CMP_HIDDEN),
        "w_mem_kv": nrm(ks[15], (L, D, 2 * MEM_WIDTH), D),
        "w_out": nrm(ks[16], (L, MIX_WIDTH, D), MIX_WIDTH),
    }


def reference(x, mem, pre_norm_g, post_norm_g, mem_norm_g, w_in, b_gate, conv_w, conv_b,
              cmp_pos_k, cmp_w1_k, cmp_w2_k, cmp_pos_v, cmp_w1_v, cmp_w2_v, w_mem_kv, w_out):
    offs = np.cumsum(IN_SPLITS)[:-1].tolist()
    for l in range(DEPTH):
        h = rmsnorm(x, pre_norm_g[l])
        proj = h @ w_in[l]
        (c_b, c_c, c_h, c_gate, n_q, n_kc, n_vc, n_ks, n_vs, n_kw, n_vw,
         n_glog, n_gate, m_q, m_gate) = jnp.split(proj, offs, axis=-1)
        y_conv = short_conv(c_b, c_c, c_h, conv_w[l], conv_b[l]) * jax.nn.silu(c_gate)
        y_nsa = nsa_attention(n_q, n_kc, n_vc, n_ks, n_vs, n_kw, n_vw, n_glog + b_gate[l],
                              cmp_pos_k[l], cmp_w1_k[l], cmp_w2_k[l],
                              cmp_pos_v[l], cmp_w1_v[l], cmp_w2_v[l]) * jax.nn.silu(n_gate)
        y_mem = memory_attention(m_q, rmsnorm(mem, mem_norm_g[l]), w_mem_kv[l]) * jax.nn.silu(m_gate)
        y = jnp.concatenate([y_conv, y_nsa, y_mem], axis=-1) @ w_out[l]
        x = x + rmsnorm(y, post_norm_g[l])
    return x
```

```python
import numpy as np
import ml_dtypes
from contextlib import ExitStack
import concourse.bass as bass
import concourse.mybir as mybir
from concourse.bass_utils import run_bass_kernel_spmd

F32 = mybir.dt.float32
BF16 = mybir.dt.bfloat16
AF = mybir.ActivationFunctionType
ALU = mybir.AluOpType
AX = mybir.AxisListType
NPBF = ml_dtypes.bfloat16

D = 1024
NEG = -1.0e6
SLOPES = [2.0 ** -(h + 1) for h in range(8)]
CH = 60


class Op:
    __slots__ = ("eng", "pos", "fn", "waits", "signal", "token", "is_dma", "lane", "lane_val", "snap")


class Sched:
    def __init__(self, nc, n_lanes=24, same_engine_sync=True):
        self.nc = nc
        self.eng = {"pe": nc.tensor, "act": nc.scalar, "dve": nc.vector, "pool": nc.gpsimd, "sp": nc.sync}
        self.ops = {e: [] for e in self.eng}
        self.lw = {}
        self.rd = {}
        self.known = {e: {} for e in self.eng}
        self.n_lanes = n_lanes
        self.lane_last = [None] * n_lanes
        self.lane_cnt = [0] * n_lanes
        self.next_lane = 0
        self.same_engine_sync = same_engine_sync
        self.nwaits = 0

    def _need(self, op, d):
        e = op.eng
        kn = self.known[e]
        if d.is_dma:
            key = ("L", d.lane)
            val = d.lane_val
        else:
            if d.eng == e:
                if e == "pe" or not self.same_engine_sync:
                    return
            key = d.eng
            val = d.pos
        if kn.get(key, -1) >= val:
            return
        kn[key] = val
        op.waits.append(d)
        d.signal = True
        self.nwaits += 1
        if d.snap is not None:
            for k, v in d.snap:
                if kn.get(k, -1) < v:
                    kn[k] = v

    def add(self, eng, fn, reads=(), writes=(), dma=False):
        op = Op()
        op.eng = eng
        op.fn = fn
        op.waits = []
        op.signal = False
        op.token = None
        op.is_dma = dma
        op.lane = None
        op.lane_val = None
        op.pos = len(self.ops[eng])
        deps = []
        for r in reads:
            w = self.lw.get(r)
            if w is not None:
                deps.append(w)
        for r in writes:
            w = self.lw.get(r)
            if w is not None:
                deps.append(w)
            rr = self.rd.get(r)
            if rr:
                deps.extend(rr.values())
        seen = set()
        for d in deps:
            if id(d) in seen:
                continue
            seen.add(id(d))
            self._need(op, d)
        if dma:
            lane = self.next_lane
            self.next_lane = (self.next_lane + 1) % self.n_lanes
            prev = self.lane_last[lane]
            if prev is not None:
                self._need(op, prev)
            self.lane_cnt[lane] += 1
            op.lane = lane
            op.lane_val = self.lane_cnt[lane]
            self.lane_last[lane] = op
        kn = self.known[eng]
        op.snap = tuple((k, v) for k, v in kn.items() if not isinstance(k, tuple))
        self.ops[eng].append(op)
        for r in reads:
            dd = self.rd.setdefault(r, {})
            dd[("D", id(op)) if dma else eng] = op
        for r in writes:
            self.lw[r] = op
            self.rd[r] = {}
        return op

    def emit(self, ctx):
        nc = self.nc
        esem = {e: ctx.enter_context(nc.semaphore("s_" + e)) for e in self.eng}
        lsem = [ctx.enter_context(nc.semaphore("l_%d" % i)) for i in range(self.n_lanes)]
        for e, lst in self.ops.items():
            c = 0
            for op in lst:
                if (not op.is_dma) and op.signal:
                    c += 1
                    op.token = c
        block = ctx.enter_context(nc.Block())
        reg = {"pe": block.tensor, "act": block.scalar, "dve": block.vector, "pool": block.gpsimd, "sp": block.sync}

        def make(e):
            def body(engh):
                for op in self.ops[e]:
                    for d in op.waits:
                        if d.is_dma:
                            engh.wait_ge(lsem[d.lane], 16 * d.lane_val)
                        else:
                            engh.wait_ge(esem[d.eng], d.token)
                    ins = op.fn(engh)
                    if op.is_dma:
                        ins.then_inc(lsem[op.lane], 16)
                    elif op.signal:
                        ins.then_inc(esem[e], 1)
                if e == "sp":
                    for i in range(self.n_lanes):
                        if self.lane_cnt[i]:
                            engh.wait_ge(lsem[i], 16 * self.lane_cnt[i])
            return body

        for e in self.eng:
            reg[e](make(e))


OFF = dict(cB=0, cC=256, ch=512, cg=768, q=1024, kc=1536, vc=1664, ks=1792, vs=1920, kw=2048, vw=2176,
           gl=2304, ng=2328, mq=2840, mg=3096)
NBLK = 13
BW = 288


def _block_cols():
    r = lambda a, n: list(range(a, a + n))
    blocks = [
        r(OFF["q"], 256), r(OFF["q"] + 256, 256),
        r(OFF["ks"], 128) + r(OFF["kw"], 128),
        r(OFF["kc"], 128) + r(OFF["vc"], 128),
        r(OFF["mq"], 256),
        r(OFF["cB"], 256), r(OFF["cC"], 256), r(OFF["ch"], 256), r(OFF["cg"], 256),
        r(OFF["vs"], 128) + r(OFF["vw"], 128) + r(OFF["gl"], 24),
        r(OFF["ng"], 256), r(OFF["ng"] + 256, 256),
        r(OFF["mg"], 256),
    ]
    return blocks


def host_consts(T):
    k = np.arange(128)[:, None]
    q = np.arange(512)[None, :]
    mc = np.zeros((128, 4, 512), np.float32)
    ml = np.zeros((128, 4, 512), np.float32)
    for kr in range(4):
        mc[:, kr, :] = np.where(128 * kr + k > q, NEG, 0.0)
        ml[:, kr, :] = np.where(128 * kr + k <= q, NEG, 0.0)
    mcmp = np.zeros((128, 5, 512), np.float32)
    for v in range(4):
        for rr in range(32):
            mcmp[32 * v + rr, v, :] = np.where(16 * rr + 15 > q[0], NEG, 0.0)
    mcmp[:, 4, :] = mcmp[:, 0, :]
    mcmp[0, 4, :] = NEG
    mq = np.zeros((128, 2, 4, 32), np.float32)
    p = np.arange(128)[:, None]
    rr = np.arange(32)[None, :]
    for s in range(4):
        mq[:, 0, s, :] = np.where(16 * rr + 15 > 128 * s + p, NEG, 0.0)
    mq[:, 1] = mq[:, 0]
    mq[:, 1, :, 0] = NEG
    pos = np.arange(T)
    kexts = np.zeros((64, T), np.float32)
    kexts[0] = pos // 128
    kexts[1] = pos % 128
    kexts[2] = 1.0
    kexts[3] = 1.0
    blk = (pos // 64) % CH
    for r_ in range(CH):
        kexts[4 + r_] = (blk == r_)
    sl = np.arange(512)
    pc = 16 * sl + 15
    kextc = np.stack([pc // 128, pc % 128, np.ones(512), np.ones(512)]).astype(np.float32)
    tq = np.arange(512)
    qext = np.stack([np.full(512, 128.0), np.ones(512), -128.0 * (tq // 128), -1.0 * (tq % 128)]).astype(np.float32)
    colab = np.zeros((128, 2), np.float32)
    colab[:, 0] = np.where(np.arange(128) >= 64, 1e4, -1.0)
    colab[:, 1] = np.where(np.arange(128) < 64, 1e4, -1.0)
    bf = lambda a: np.ascontiguousarray(a).astype(NPBF)
    return dict(mc=bf(mc.reshape(128, -1)), ml=bf(ml.reshape(128, -1)), mcmp=bf(mcmp.reshape(128, -1)),
                mq=bf(mq.reshape(128, -1)), kexts=bf(kexts), kextc=bf(kextc), qext=bf(qext), colab=colab)


def host_weights(L, w_in, w_out, w1k, w1v, w2k, w2v, posk, posv, wm, convw, convb):
    blocks = _block_cols()
    w_in_p = np.zeros((L, NBLK, 128, 8, BW), np.float32)
    for b, cols in enumerate(blocks):
        sub = w_in[:, :, cols]
        w_in_p[:, b, :, :, :len(cols)] = sub.reshape(L, 8, 128, len(cols)).transpose(0, 2, 1, 3)
    w_out_p = w_out.reshape(L, 8, 128, 4, 256).transpose(0, 3, 2, 1, 4)
    w1 = np.stack([w1k, w1v], axis=1)
    w1_p = w1.reshape(L, 2, 16, 128, 2, 128).transpose(0, 1, 4, 3, 2, 5)
    w2 = np.stack([w2k, w2v], axis=1)
    w2_p = w2.reshape(L, 2, 2, 128, 64).transpose(0, 1, 3, 2, 4)
    pos = np.stack([posk, posv], axis=1)
    pos_p = pos.reshape(L, 2, 16, 2, 64).transpose(0, 1, 3, 4, 2).reshape(L, 2, 128, 16)
    wm_p = wm.reshape(L, 8, 128, 2, 256).transpose(0, 3, 2, 1, 4)
    convw_t = convw.reshape(L, 3, 2, 128).transpose(3, 0, 2, 1)
    convb_t = convb.reshape(L, 2, 128).transpose(2, 0, 1)
    c = np.ascontiguousarray
    return dict(w_in_p=c(w_in_p.reshape(L, NBLK, 128, 8 * BW)), w_out_p=c(w_out_p.reshape(L, 4, 128, 2048)),
                w1_p=c(w1_p.reshape(L, 2, 2, 128, 2048)), w2_p=c(w2_p.reshape(L, 2, 128, 128)),
                pos_p=c(pos_p), wm_p=c(wm_p.reshape(L, 2, 128, 2048)),
                convw_t=c(convw_t.reshape(128, L * 6)), convb_t=c(convb_t.reshape(128, L * 2)))


def build(T=8192, L=4, same_engine_sync=True):
    NT = T // 512
    NKT = T // 128
    nc = bass.Bass("TRN2", target_bir_lowering=False)
    dram = lambda name, shape, dt_, kind: nc.dram_tensor(name, shape, dt_, kind=kind).ap()
    EI, EO, IN = "ExternalInput", "ExternalOutput", "Internal"
    x_d = dram("x", [T, D], F32, EI)
    mem_d = dram("mem", [256, D], F32, EI)
    win_d = dram("w_in_p", [L, NBLK, 128, 8 * BW], F32, EI)
    wout_d = dram("w_out_p", [L, 4, 128, 2048], F32, EI)
    w1_d = dram("w1_p", [L, 2, 2, 128, 2048], F32, EI)
    w2_d = dram("w2_p", [L, 2, 128, 128], F32, EI)
    pos_d = dram("pos_p", [L, 2, 128, 16], F32, EI)
    wm_d = dram("wm_p", [L, 2, 128, 2048], F32, EI)
    gpre_d = dram("gpre", [L, D], F32, EI)
    gpost_d = dram("gpost", [L, D], F32, EI)
    gmem_d = dram("gmem", [L, D], F32, EI)
    convw_d = dram("convw_t", [128, L * 6], F32, EI)
    convb_d = dram("convb_t", [128, L * 2], F32, EI)
    bgate_d = dram("bgate", [L, 24], F32, EI)
    mc_d = dram("mc", [128, 2048], BF16, EI)
    ml_d = dram("ml", [128, 2048], BF16, EI)
    mcmp_d = dram("mcmp", [128, 2560], BF16, EI)
    mq_d = dram("mq", [128, 256], BF16, EI)
    kexts_d = dram("kexts", [64, T], BF16, EI)
    kextc_d = dram("kextc", [4, 512], BF16, EI)
    qext_d = dram("qext", [4, 512], BF16, EI)
    colab_d = dram("colab", [128, 2], F32, EI)
    out_d = dram("out", [T, D], F32, EO)
    xs_d = dram("xs", [T, D], F32, IN)
    WIN_s = dram("WIN_s", [L, NBLK, 128, 8 * BW], BF16, IN)
    WOUT_s = dram("WOUT_s", [L, 4, 128, 2048], BF16, IN)
    W1_s = dram("W1_s", [L, 2, 2, 128, 2048], BF16, IN)
    WM_s = dram("WM_s", [L, 2, 128, 2048], BF16, IN)

    ctx = ExitStack()
    S = Sched(nc, same_engine_sync=same_engine_sync)
    sb = lambda name, shape, dt_=F32: nc.alloc_sbuf_tensor(name, shape, dt_)
    KXs = sb("KXs", [128, 2, T], BF16)
    Vs = sb("Vs", [128, NKT, 2, 65], BF16)
    KXw = sb("KXw", [128, 2, 1024], BF16)
    Vw = sb("Vw", [128, 8, 2, 65], BF16)
    KXc = sb("KXc", [128, 2, 512], BF16)
    VC = sb("VC", [128, 4, 2, 65], BF16)
    KC2 = sb("KC2", [128, 2, 2, 544], BF16)
    KM = sb("KM", [128, 4, 256], BF16)
    VM = sb("VM", [128, 2, 4, 65], BF16)
    QX = sb("QX", [128, 8, 512], BF16)
    QXm = sb("QXm", [128, 4, 512], BF16)
    PX = sb("PX", [128, 2, 3, 512], BF16)
    MC = sb("MC", [128, 4, 512], BF16)
    ML = sb("ML", [128, 4, 512], BF16)
    MCMP = sb("MCMP", [128, 5, 512], BF16)
    MQ = sb("MQ", [128, 2, 4, 32], BF16)
    identb = sb("identb", [128, 128], BF16)
    identf = sb("identf", [128, 128], F32)
    gpre_b = sb("gpre_b", [128, D], F32)
    gpost_b = sb("gpost_b", [128, D], F32)
    convw_t = sb("convw_sb", [128, L * 6], F32)
    convb_t = sb("convb_sb", [128, L * 2], F32)
    bgate_b = sb("bgate_b", [128, L * 24], F32)
    colab = sb("colab_sb", [128, 2], F32)
    mhalf = sb("mhalf", [128, 4], F32)
    cbias = sb("cbias", [128, 256], F32)
    w2b = sb("w2b", [128, 2, 128], BF16)
    pos2b = sb("pos2b", [128, 2, 16], BF16)
    xb = [sb("xb%d" % i, [128, D], F32) for i in range(2)]
    hT = sb("hT", [128, 8, 512], BF16)
    NWB = 3
    wbuf = [sb("wbuf%d" % i, [128, 8 * BW], BF16) for i in range(NWB)]
    cbuf = sb("cbuf", [128, 4, 512], F32)
    ubuf = sb("ubuf", [128, 2, 514], F32)
    tmpA = sb("tmpA", [128, 1024], F32)
    abuf = tmpA[:, 0:512]
    thb = tmpA[:, 512:1024]
    hidf = tmpA[:, 0:768].rearrange("p (a b) -> p a b", b=256)
    ycp = sb("ycp", [128, D], F32)
    yT = sb("yT", [128, 8, 512], BF16)
    GN = sb("GN", [128, 4, 512], BF16)
    GM = sb("GM", [128, 4, 256], BF16)
    GL2 = sb("GL2", [128, 4, 24], F32)
    pbuf = [sb("pbuf%d" % i, [128, 512], BF16) for i in range(4)]
    OTs = [sb("OTs%d" % i, [128, 512], F32) for i in range(2)]
    acc = sb("acc", [128, 4, 512], F32)
    accm = sb("accm", [128, 4, 256], F32)
    tmpc = sb("tmpc", [128, 4, 64], F32)
    ebuf = [sb("ebuf%d" % i, [128, 512], F32) for i in range(2)]
    imp = sb("imp", [128, 516], F32)
    selv = sb("selv", [128, 128], F32)
    selv2 = sb("selv2", [128, 128], F32)
    Zc = sb("Zc", [128, 3, 128], F32)
    m8 = sb("m8", [128, 16], F32)
    st = sb("st", [128, 32], F32)
    hidb = sb("hidb", [128, 256], BF16)
    vcst = sb("vcst", [32, 2, 64], BF16)

    ps = [nc.alloc_psum_tensor("ps%d" % i, [128, 512], F32) for i in range(8)]
    PSN = ["ps%d" % i for i in range(8)]

    def dma(out, in_, r, w, q="sp"):
        S.add(q, lambda e: e.dma_start(out=out, in_=in_), r, w, dma=True)

    def mm(out, lhsT, rhs, start, stop, r, w):
        S.add("pe", lambda e: e.matmul(out, lhsT=lhsT, rhs=rhs, start=start, stop=stop), r, w)

    def trn(out, in_, ident, r, w):
        S.add("pe", lambda e: e.transpose(out=out, in_=in_, identity=ident), r, w)

    def actv(out, in_, func, r, w, scale=1.0, bias=0.0, accum=None):
        if accum is None:
            S.add("act", lambda e: e.activation(out=out, in_=in_, func=func, bias=bias, scale=scale), r, w)
        else:
            S.add("act", lambda e: e.activation(out=out, in_=in_, func=func, bias=bias, scale=scale, accum_out=accum), r, w)

    def cp(out, in_, r, w, eng="dve"):
        S.add(eng, lambda e: e.tensor_copy(out=out, in_=in_), r, w)

    def ts(out, in0, s1, s2, op0, op1, r, w, eng="dve"):
        if op1 is None:
            S.add(eng, lambda e: e.tensor_scalar(out=out, in0=in0, scalar1=s1, scalar2=None, op0=op0), r, w)
        else:
            S.add(eng, lambda e: e.tensor_scalar(out=out, in0=in0, scalar1=s1, scalar2=s2, op0=op0, op1=op1), r, w)

    def tt(out, in0, in1, op, r, w, eng="dve"):
        S.add(eng, lambda e: e.tensor_tensor(out=out, in0=in0, in1=in1, op=op), r, w)

    def stt(out, in0, scalar, in1, op0, op1, r, w, accum=None):
        if accum is None:
            S.add("dve", lambda e: e.scalar_tensor_tensor(out=out, in0=in0, scalar=scalar, in1=in1, op0=op0, op1=op1), r, w)
        else:
            S.add("dve", lambda e: e.scalar_tensor_tensor(out=out, in0=in0, scalar=scalar, in1=in1, op0=op0, op1=op1, accum_out=accum), r, w)

    def mset(ap, val, w, eng="pool"):
        S.add(eng, lambda e: e.memset(ap, val), (), w)

    dma(MC[:].rearrange("p a b -> p (a b)"), mc_d, [], ["MC"])
    dma(ML[:].rearrange("p a b -> p (a b)"), ml_d, [], ["ML"])
    dma(MCMP[:].rearrange("p a b -> p (a b)"), mcmp_d, [], ["MCMP"])
    dma(MQ[:].rearrange("p a b c -> p (a b c)"), mq_d, [], ["MQ"])
    dma(colab[:], colab_d, [], ["colab"])
    dma(convw_t[:], convw_d, [], ["convw"])
    dma(convb_t[:], convb_d, [], ["convb"])
    dma(bgate_b[:], bgate_d.rearrange("l n -> (l n)").partition_broadcast(128), [], ["bgate"])
    for g in range(2):
        dma(KXs[64:128, g, :], kexts_d, [], [("KXs", g, "x")])
        dma(KXc[64:68, g, :], kextc_d, [], [("KXc", g, "x")])
    for h in range(8):
        dma(QX[64:68, h, :], qext_d, [], [("QX", h, "x")])
    mset(identf[:], 0.0, ["identf"])
    S.add("pool", lambda e: e.affine_select(out=identf[:], in_=identf[:], pattern=[[-1, 128]], compare_op=ALU.not_equal,
                                            fill=1.0, base=0, channel_multiplier=1), ["identf"], ["identf"])
    cp(identb[:], identf[:], ["identf"], ["identb"])
    mset(mhalf[:], -0.5, ["mhalf"])
    mset(Vs[:].rearrange("p a b c -> p (a b c)"), 1.0, [("Vs", "all")])
    mset(Vw[:].rearrange("p a b c -> p (a b c)"), 1.0, [("Vw", "all")])
    mset(VC[:].rearrange("p a b c -> p (a b c)"), 1.0, [("VC", "all")])
    mset(VM[:].rearrange("p a b c -> p (a b c)"), 1.0, [("VM", "all")])
    mset(KC2[:].rearrange("p a b c -> p (a b c)"), 0.0, ["KC2"])
    mset(KXc[0:64, :, :].rearrange("p a b -> p (a b)"), 0.0, [("KXc", 0, "k"), ("KXc", 1, "k")])
    mset(PX[:].rearrange("p a b c -> p (a b c)"), 0.0, ["PXinit"])
    mset(Zc[:].rearrange("p a b -> p (a b)"), 0.0, ["Zc"])
    mset(imp[:], 0.0, ["imp"])

    wb_i = [0]

    def next_wb():
        i = wb_i[0]
        wb_i[0] = (i + 1) % NWB
        return i

    def prep(src, dst, n, last, dst_key):
        i = next_wb()
        wv = wbuf[i][:, 0:n].rearrange("p (c n) -> p c n", n=last)
        dma(wv, src.rearrange("p (c n) -> p c n", n=last), [], [("wbuf", i)], q="pool")
        dma(dst, wbuf[i][:, 0:n], [("wbuf", i)], [dst_key])

    for l in range(L):
        for b in range(NBLK):
            prep(win_d[l, b], WIN_s[l, b], 8 * BW, BW, ("WIN", l, b))
        for b in range(4):
            prep(wout_d[l, b], WOUT_s[l, b], 2048, 256, ("WOUT", l, b))
        for kv in range(2):
            for hf in range(2):
                prep(w1_d[l, kv, hf], W1_s[l, kv, hf], 2048, 128, ("W1", l, kv, hf))
        for b in range(2):
            prep(wm_d[l, b], WM_s[l, b], 2048, 256, ("WM", l, b))

    def wload(src, n, key):
        i = next_wb()
        dma(wbuf[i][:, 0:n], src, [key], [("wbuf", i)])
        return i

    gen_i = [0]

    def gen_bank():
        i = 5 + gen_i[0]
        gen_i[0] ^= 1
        return i

    def norm_transpose(xt, xkey, gtile, gkey, col0):
        stt(cbuf[:, 0:2, :].rearrange("p a b -> p (a b)"), xt[:], 1.0, xt[:], ALU.mult, ALU.mult,
            [xkey], ["cbuf", "st0"], accum=st[:, 0:1])
        ts(st[:, 1:2], st[:, 0:1], 1.0 / D, 1e-6, ALU.mult, ALU.add, ["st0"], ["st1"])
        tt(st[:, 2:3], st[:, 1:2], mhalf[:, 0:1], ALU.pow, ["st1", "mhalf"], ["st2"], eng="pool")
        stt(xt[:], xt[:], st[:, 2:3], gtile[:], ALU.mult, ALU.mult, [xkey, "st2", gkey], [xkey])
        for half in range(2):
            for c4 in range(4):
                c = half * 4 + c4
                trn(ps[7][:, c4 * 128:(c4 + 1) * 128], xt[:, c * 128:(c + 1) * 128], identf[:], [xkey, "identf"], [PSN[7]])
            cp(hT[:, half * 4:half * 4 + 4, col0:col0 + 128], ps[7][:].rearrange("p (a b) -> p a b", b=128),
               [PSN[7]], ["hT"])

    class Unit:
        pass

    def run_units(units):
        n = len(units)
        LA = 2
        pend = []
        for i in range(n + LA):
            if i < n:
                u = units[i]
                sbk = i % 3
                nmm = len(u.mm1)
                for k, (lh, rh, rd_) in enumerate(u.mm1):
                    mm(ps[sbk][0:u.M, :], lh, rh, k == 0, k == nmm - 1, rd_, [PSN[sbk]])
                pb = i % 4
                actv(pbuf[pb][0:u.M, :], ps[sbk][0:u.M, :], AF.Exp, [PSN[sbk]], [("pbuf", pb)], scale=u.scale, bias=u.bias)
            k2 = i - LA
            if k2 >= 0:
                u = units[k2]
                pb = k2 % 4
                mm(ps[u.ob][0:65, :], u.v, pbuf[pb][0:u.M, :], u.first, u.last, [("pbuf", pb), u.vkey], [PSN[u.ob]])
                if u.last:
                    pend.append([k2 + 3, u])
            while pend and (pend[0][0] <= i or i == n + LA - 1):
                _, u = pend.pop(0)
                u.fin(u)

    ob_i = [0]

    def next_ob():
        i = 3 + ob_i[0]
        ob_i[0] ^= 1
        return i

    ot_i = [0]

    def finalize(u, gate_ap, const, dst, dkey, first_write):
        k = ot_i[0]
        ot_i[0] ^= 1
        cp(OTs[k][0:65, :], ps[u.ob][0:65, :], [PSN[u.ob]], [("OTs", k)])
        for s in range(4):
            trn(ps[7][:, s * 65:(s + 1) * 65], OTs[k][0:65, s * 128:(s + 1) * 128], identf[0:65, 0:65],
                [("OTs", k), "identf"], [PSN[7]])
        pv = ps[7][:, 0:260].rearrange("p (s c) -> p s c", c=65)
        ts(st[:, 20:24], pv[:, :, 64], 1e-30, None, ALU.max, None, [PSN[7]], ["st20"])
        S.add("dve", lambda e: e.reciprocal(out=st[:, 8:12], in_=st[:, 20:24]), ["st20"], ["st8"])
        if gate_ap is None:
            ts(st[:, 12:16], st[:, 8:12], const, None, ALU.mult, None, ["st8"], ["st12"])
        else:
            stt(st[:, 12:16], st[:, 8:12], const, gate_ap, ALU.mult, ALU.mult, ["st8", "GL2"], ["st12"])
        fb = st[:, 12:16].unsqueeze(2).to_broadcast([128, 4, 64])
        if first_write:
            tt(dst, pv[:, :, 0:64], fb, ALU.mult, [PSN[7], "st12"], [dkey])
        else:
            tt(tmpc[:], pv[:, :, 0:64], fb, ALU.mult, [PSN[7], "st12"], ["tmpc"])
            tt(dst, dst, tmpc[:], ALU.add, [dkey, "tmpc"], [dkey], eng="pool")

    def mk_unit(mm1, M, scale, bias, v, vkey, ob, first, last, fin):
        u = Unit()
        u.mm1, u.M, u.scale, u.bias, u.v, u.vkey, u.ob, u.first, u.last, u.fin = mm1, M, scale, bias, v, vkey, ob, first, last, fin
        return u

    for l in range(L):
        dma(gpre_b[:], gpre_d[l:l + 1, :].rearrange("a n -> (a n)").partition_broadcast(128), [], ["gpre"])
        dma(gpost_b[:], gpost_d[l:l + 1, :].rearrange("a n -> (a n)").partition_broadcast(128), [], ["gpost"])
        for kv in range(2):
            dma(w2b[:, kv, :], w2_d[l, kv], [], ["w2b"], q="pool")
            dma(pos2b[:, kv, :], pos_d[l, kv], [], ["pos2b"], q="pool")
        gb = gen_bank()
        for kv in range(2):
            for hf in range(2):
                i = wload(W1_s[l, kv, hf], 2048, ("W1", l, kv, hf))
                wv = wbuf[i][:, 0:2048].rearrange("p (c n) -> p c n", n=128)
                col = kv * 2 + hf
                for pp in range(16):
                    mm(ps[gb][:, col:col + 1], wv[:, pp, :], pos2b[:, kv, pp:pp + 1], pp == 0, pp == 15,
                       [("wbuf", i), "pos2b"], [PSN[gb]])
        cbv = cbias[:].rearrange("p (kv g hf r) -> p kv g hf r", kv=2, g=2, hf=2)
        for kv in range(2):
            for g in range(2):
                for hf in range(2):
                    col = kv * 2 + hf
                    cp(cbv[:, kv, g, hf, :], ps[gb][:, col:col + 1].to_broadcast([128, 32]), [PSN[gb]], ["cbias"])
        gmem_b = cbuf[:, 2:4, :].rearrange("p a b -> p (a b)")
        dma(gmem_b, gmem_d[l:l + 1, :].rearrange("a n -> (a n)").partition_broadcast(128), [], ["cb23"])
        for mt in range(2):
            dma(xb[mt][:], mem_d[mt * 128:(mt + 1) * 128, :], [], [("xb", mt)])
            norm_transpose(xb[mt], ("xb", mt), gmem_b, "cb23", mt * 128)
        ik = wload(WM_s[l, 0], 2048, ("WM", l, 0))
        wk = wbuf[ik][:, 0:2048].rearrange("p (c n) -> p c n", n=256)
        for pr in range(2):
            gb = gen_bank()
            for c in range(8):
                mm(ps[gb][:, 0:256], wk[:, c, pr * 128:(pr + 1) * 128], hT[:, c, 0:256], c == 0, c == 7,
                   [("wbuf", ik), "hT"], [PSN[gb]])
            cp(KM[0:64, 2 * pr, :], ps[gb][0:64, 0:256], [PSN[gb]], ["KM"])
            cp(KM[0:64, 2 * pr + 1, :], ps[gb][64:128, 0:256], [PSN[gb]], ["KM"])
        iv = wload(WM_s[l, 1], 2048, ("WM", l, 1))
        wv_ = wbuf[iv][:, 0:2048].rearrange("p (c n) -> p c n", n=256)
        for mt in range(2):
            gb = gen_bank()
            for c in range(8):
                mm(ps[gb][:, 0:256], hT[:, c, mt * 128:(mt + 1) * 128], wv_[:, c, :], c == 0, c == 7,
                   [("wbuf", iv), "hT"], [PSN[gb]])
            cp(VM[:, mt, :, 0:64], ps[gb][:, 0:256].rearrange("p (h d) -> p h d", d=64), [PSN[gb], ("VM", "all")], ["VM"])
        mset(ubuf[:, :, 0:2], 0.0, ["ubuf0", "ubuf1"])

        src_d = x_d if l == 0 else xs_d
        dst_d = out_d if l == L - 1 else xs_d

        for j in range(NT):
            t0 = 512 * j
            slot = j % 2
            pslot = 1 - slot
            xk = lambda s, j=j: ("xrow", j * 4 + s)
            for s in range(4):
                b_ = s % 2
                dma(xb[b_][:], src_d[t0 + s * 128:t0 + (s + 1) * 128, :], [xk(s)], [("xb", b_)])
                norm_transpose(xb[b_], ("xb", b_), gpre_b[:], "gpre", s * 128)
            for g in range(2):
                dma(KXw[64:68, g, slot * 512:(slot + 1) * 512], kexts_d[0:4, t0:t0 + 512], [], [("KXw", g, slot, "x")])

            def fm_block(b):
                i = wload(WIN_s[l, b], 8 * BW, ("WIN", l, b))
                wv = wbuf[i][:, :].rearrange("p (c n) -> p c n", n=BW)
                for grp in range(2):
                    gb = gen_bank()
                    for c in range(8):
                        mm(ps[gb][:, :], wv[:, c, grp * 128:(grp + 1) * 128], hT[:, c, :], c == 0, c == 7,
                           [("wbuf", i), "hT"], [PSN[gb]])
                    yield gb, grp

            for b in range(2):
                for gb, grp in fm_block(b):
                    for hh in range(2):
                        h = b * 4 + grp * 2 + hh
                        ts(QX[0:64, h, :], ps[gb][hh * 64:(hh + 1) * 64, :], 1.0 / (8.0 * SLOPES[h]), None, ALU.mult, None,
                           [PSN[gb]], [("QX", h, "q")])
            for gb, grp in fm_block(2):
                for g in range(2):
                    if grp == 0:
                        cp(KXs[0:64, g, t0:t0 + 512], ps[gb][g * 64:(g + 1) * 64, :], [PSN[gb]], [("KXs", g, j)])
                    else:
                        cp(KXw[0:64, g, slot * 512:(slot + 1) * 512], ps[gb][g * 64:(g + 1) * 64, :], [PSN[gb]],
                           [("KXw", g, slot, "k")])
            for kv in range(2):
                for g in range(2):
                    cp(KC2[0:64, kv, g, 0:16], KC2[0:64, kv, g, 512:528], [("KC2", kv, g)], [("KC2", kv, g)], eng="pool")
                    cp(KC2[64:128, kv, g, 0:15], KC2[64:128, kv, g, 512:527], [("KC2", kv, g)], [("KC2", kv, g)], eng="pool")
            for gb, grp in fm_block(3):
                kv = grp
                for g in range(2):
                    cp(KC2[0:64, kv, g, 16:528], ps[gb][g * 64:(g + 1) * 64, :], [PSN[gb], "KC2"], [("KC2", kv, g)])
                    cp(KC2[64:128, kv, g, 15:527], ps[gb][g * 64:(g + 1) * 64, :], [PSN[gb], "KC2"], [("KC2", kv, g)])
            for gb, grp in fm_block(4):
                for hh in range(2):
                    h = grp * 2 + hh
                    ts(QXm[0:64, h, :], ps[gb][hh * 64:(hh + 1) * 64, :], 0.125, None, ALU.mult, None, [PSN[gb]], [("QXm", h)])

            hb = gen_bank()
            for kv in range(2):
                for hf in range(2):
                    i = wload(W1_s[l, kv, hf], 2048, ("W1", l, kv, hf))
                    wv = wbuf[i][:, 0:2048].rearrange("p (c n) -> p c n", n=128)
                    for g in range(2):
                        col = ((kv * 2 + g) * 2 + hf) * 32
                        for pp in range(16):
                            rhs = KC2[:, kv, g, 2 * pp:2 * pp + 512].rearrange("p (r s) -> p r s", s=16)[:, :, 0]
                            mm(ps[hb][:, col:col + 32], wv[:, pp, :], rhs, pp == 0, pp == 15,
                               [("wbuf", i), ("KC2", kv, g)], [PSN[hb]])
            u_ = hidf[:, 0, :]
            v_ = hidf[:, 1, :]
            w_ = hidf[:, 2, :]
            tt(u_, ps[hb][:, 0:256], cbias[:], ALU.add, [PSN[hb], "cbias"], ["tmpA"])
            tt(v_, u_, u_, ALU.mult, ["tmpA"], ["tmpA"])
            ts(v_, v_, 0.044715, 1.0, ALU.mult, ALU.add, ["tmpA"], ["tmpA"])
            tt(v_, v_, u_, ALU.mult, ["tmpA"], ["tmpA"])
            actv(w_, v_, AF.Tanh, ["tmpA"], ["tmpA"], scale=0.7978845608028654)
            stt(hidb[:], w_, 1.0, u_, ALU.add, ALU.mult, ["tmpA"], ["hidb"])
            slot_lo = 32 * j
            for g in range(2):
                gb = gen_bank()
                for hf in range(2):
                    col = ((0 * 2 + g) * 2 + hf) * 32
                    mm(ps[gb][0:64, 0:32], w2b[:, 0, hf * 64:(hf + 1) * 64], hidb[:, col:col + 32], hf == 0, hf == 1,
                       ["w2b", "hidb"], [PSN[gb]])
                ts(KXc[0:64, g, slot_lo:slot_lo + 32], ps[gb][0:64, 0:32], 0.5, None, ALU.mult, None, [PSN[gb]], [("KXc", g, "k")])
                for hf in range(2):
                    col = ((1 * 2 + g) * 2 + hf) * 32
                    mm(ps[gb][0:32, 64:128], hidb[:, col:col + 32], w2b[:, 1, hf * 64:(hf + 1) * 64], hf == 0, hf == 1,
                       ["w2b", "hidb"], [PSN[gb]])
                ts(vcst[:, g, :], ps[gb][0:32, 64:128], 0.5, None, ALU.mult, None, [PSN[gb]], ["vcst"])
            pr0 = slot_lo % 128
            dma(VC[pr0:pr0 + 32, slot_lo // 128, :, 0:64], vcst[:], ["vcst", ("VC", "all")], ["VCd"])

            N = 32 * (j + 1)
            NB = 8 * (j + 1)
            nch = (NB - 1) // CH + 1
            mqv = 1 if j == 0 else 0
            for g in range(2):
                for s in range(4):
                    for r_ in range(4):
                        h = g * 4 + r_
                        gb = gen_bank()
                        mm(ps[gb][:, 0:N], QX[0:68, h, s * 128:(s + 1) * 128], KXc[0:68, g, 0:N], True, False,
                           [("QX", h, "q"), ("QX", h, "x"), ("KXc", g, "k"), ("KXc", g, "x")], [PSN[gb]])
                        mm(ps[gb][:, N - 32:N], identb[:], MQ[:, mqv, s, :], False, True, ["identb", "MQ"], [PSN[gb]])
                        eb = ebuf[r_ % 2]
                        ek = ("ebuf", r_ % 2)
                        actv(eb[:, 0:N], ps[gb][:, 0:N], AF.Exp, [PSN[gb]], [ek, "st4"], scale=SLOPES[h],
                             bias=-SLOPES[h] * 512.0 * j, accum=st[:, 4:5])
                        ts(st[:, 6:7], st[:, 4:5], 1e-30, None, ALU.max, None, ["st4"], ["st6"])
                        S.add("dve", lambda e: e.reciprocal(out=st[:, 5:6], in_=st[:, 6:7]), ["st6"], ["st5"])
                        if r_ == 0:
                            ts(imp[:, 0:N], eb[:, 0:N], st[:, 5:6], None, ALU.mult, None, [ek, "st5"], ["imp"])
                        else:
                            stt(imp[:, 0:N], eb[:, 0:N], st[:, 5:6], imp[:, 0:N], ALU.mult, ALU.add, [ek, "st5", "imp"], ["imp"])
                    mset(imp[:, N:N + 1], 0.0, ["imp"], eng="dve")
                    chb = ebuf[0]
                    tt(chb[:, 0:N], imp[:, 0:N], imp[:, 1:N + 1], ALU.add, ["imp"], [("ebuf", 0)])
                    S.add("dve", lambda e, chb=chb, NB=NB, N=N: e.tensor_reduce(
                        out=selv[:, 0:NB], in_=chb[:, 0:N].rearrange("p (n f) -> p n f", f=4), axis=AX.X, op=ALU.add),
                        [("ebuf", 0)], ["selv"])
                    lo = 8 * j + 2 * s
                    if lo + 2 < 128:
                        mset(selv[:, lo + 2:128], -1.0, ["selv"], eng="dve")
                    cp(selv[:, lo + 1:lo + 2], colab[:, 0:1], ["colab", "selv"], ["selv"])
                    mset(selv[:, lo:lo + 1], 1.0e4, ["selv"], eng="dve")
                    if lo - 1 >= 1:
                        ts(selv[:, lo - 1:lo], selv[:, lo - 1:lo], colab[:, 1:2], None, ALU.max, None, ["selv", "colab"], ["selv"])
                    mset(selv[:, 0:1], 1.0e4, ["selv"], eng="dve")
                    S.add("dve", lambda e: e.max(out=m8[:, 0:8], in_=selv[:]), ["selv"], ["m8"])
                    S.add("dve", lambda e: e.match_replace(out=selv2[:], in_to_replace=m8[:, 0:8], in_values=selv[:], imm_value=-2.0),
                          ["selv", "m8"], ["selv2"])
                    S.add("dve", lambda e: e.max(out=m8[:, 8:16], in_=selv2[:]), ["selv2"], ["m8b"])
                    for c in range(nch):
                        n0 = CH * c
                        n1 = min(128, n0 + CH)
                        ts(Zc[:, c, 68:68 + (n1 - n0)], selv[:, n0:n1], m8[:, 15:16], NEG, ALU.is_lt, ALU.mult,
                           ["selv", "m8b"], ["Zc"])
                    for c in range(nch):
                        trn(ps[7][:, c * 128:(c + 1) * 128], Zc[:, c, :], identf[:], ["Zc", "identf"], [PSN[7]])
                    cp(PX[64:128, g, 0:nch, s * 128:(s + 1) * 128],
                       ps[7][64:128, 0:nch * 128].rearrange("p (a b) -> p a b", b=128), [PSN[7], "PXinit"],
                       [("PX", g, c) for c in range(nch)])

            def tm_block(b, ncols):
                i = wload(WIN_s[l, b], 8 * BW, ("WIN", l, b))
                wv = wbuf[i][:, :].rearrange("p (c n) -> p c n", n=BW)
                for s in range(4):
                    gb = gen_bank()
                    for c in range(8):
                        mm(ps[gb][:, 0:ncols], hT[:, c, s * 128:(s + 1) * 128], wv[:, c, 0:ncols], c == 0, c == 7,
                           [("wbuf", i), "hT"], [PSN[gb]])
                    yield gb, s

            for gb, s in tm_block(9, 280):
                cp(Vs[:, 4 * j + s, :, 0:64], ps[gb][:, 0:128].rearrange("p (g d) -> p g d", d=64), [PSN[gb], ("Vs", "all")],
                   [("Vs", j)])
                cp(Vw[:, slot * 4 + s, :, 0:64], ps[gb][:, 128:256].rearrange("p (g d) -> p g d", d=64), [PSN[gb], ("Vw", "all")],
                   [("Vw", slot)])
                tt(GL2[:, s, :], ps[gb][:, 256:280], bgate_b[:, l * 24:(l + 1) * 24], ALU.add, [PSN[gb], "bgate"], ["GL2"])
            glf = GL2[:].rearrange("p a b -> p (a b)")
            actv(glf, glf, AF.Tanh, ["GL2"], ["GL2"], scale=0.5)
            ts(glf, glf, 1.0, None, ALU.add, None, ["GL2"], ["GL2"])
            for half in range(2):
                for gb, s in tm_block(10 + half, 256):
                    actv(thb[:, 0:256], ps[gb][:, 0:256], AF.Tanh, [PSN[gb]], ["tmpA"], scale=0.5)
                    stt(GN[:, s, half * 256:(half + 1) * 256], thb[:, 0:256], 1.0, ps[gb][:, 0:256], ALU.add, ALU.mult,
                        ["tmpA", PSN[gb]], ["GN"])
            for gb, s in tm_block(12, 256):
                actv(thb[:, 0:256], ps[gb][:, 0:256], AF.Tanh, [PSN[gb]], ["tmpA"], scale=0.5)
                stt(GM[:, s, :], thb[:, 0:256], 1.0, ps[gb][:, 0:256], ALU.add, ALU.mult, ["tmpA", PSN[gb]], ["GM"])

            cw = lambda cc, k: convw_t[:, l * 6 + cc * 3 + k:l * 6 + cc * 3 + k + 1]
            for gb, cc in fm_block(6):
                cp(cbuf[:, cc, :], ps[gb][:, :], [PSN[gb], "cb01"], [("cb", cc)])
            for gb, cc in fm_block(7):
                uk = "ubuf%d" % cc
                if j > 0:
                    cp(ubuf[:, cc, 0:2], ubuf[:, cc, 512:514], [uk], [uk])
                tt(ubuf[:, cc, 2:514], cbuf[:, cc, :], ps[gb][:, :], ALU.mult, [("cb", cc), PSN[gb]], [uk])
                ak = ("ca", cc)
                ts(cbuf[:, 2 + cc, :], ubuf[:, cc, 2:514], cw(cc, 2), convb_t[:, l * 2 + cc:l * 2 + cc + 1], ALU.mult, ALU.add,
                   [uk, "convw", "convb", "cb23"], [ak])
                stt(cbuf[:, 2 + cc, :], ubuf[:, cc, 1:513], cw(cc, 1), cbuf[:, 2 + cc, :], ALU.mult, ALU.add, [uk, ak, "convw"], [ak])
                stt(cbuf[:, 2 + cc, :], ubuf[:, cc, 0:512], cw(cc, 0), cbuf[:, 2 + cc, :], ALU.mult, ALU.add, [uk, ak, "convw"], [ak])
            for gb, cc in fm_block(5):
                ak = ("ca", cc)
                tt(cbuf[:, 2 + cc, :], cbuf[:, 2 + cc, :], ps[gb][:, :], ALU.mult, [ak, PSN[gb]], [ak])
            for gb, cc in fm_block(8):
                ak = ("ca", cc)
                actv(thb[:], ps[gb][:, :], AF.Tanh, [PSN[gb]], ["tmpA"], scale=0.5)
                stt(abuf[:], thb[:], 1.0, ps[gb][:, :], ALU.add, ALU.mult, ["tmpA", PSN[gb]], ["tmpA"])
                stt(yT[:, cc, :], cbuf[:, 2 + cc, :], 0.5, abuf[:], ALU.mult, ALU.mult, [ak, "tmpA"], [("yT", cc)])

            def fin_nsa(br, h, first_write):
                def f(u):
                    finalize(u, GL2[:, :, br * 8 + h], 0.25, acc[:, :, h * 64:(h + 1) * 64], ("acc", h), first_write)
                return f

            def fin_mem(h):
                def f(u):
                    finalize(u, None, 0.5, accm[:, :, h * 64:(h + 1) * 64], ("accm", h), True)
                return f

            units = []
            for h in range(8):
                g = h // 4
                ob = next_ob()
                tl = []
                if j >= 1:
                    for kr in range(4):
                        tl.append((pslot, kr, "ML"))
                for kr in range(4):
                    tl.append((slot, kr, "MC"))
                for ti, (sl_, kr, mk) in enumerate(tl):
                    mt_ = ML if mk == "ML" else MC
                    mm1 = [(KXw[0:68, g, sl_ * 512 + kr * 128:sl_ * 512 + (kr + 1) * 128], QX[0:68, h, :],
                            [("KXw", g, sl_, "k"), ("KXw", g, sl_, "x"), ("QX", h, "q"), ("QX", h, "x")]),
                           (identb[:], mt_[:, kr, :], ["identb", mk])]
                    units.append(mk_unit(mm1, 128, SLOPES[h], -SLOPES[h] * 512.0 * j, Vw[:, sl_ * 4 + kr, g, :], ("Vw", sl_),
                                         ob, ti == 0, ti == len(tl) - 1, fin_nsa(2, h, True)))
            for h in range(4):
                ob = next_ob()
                for kt in range(2):
                    mm1 = [(KM[0:64, h, kt * 128:(kt + 1) * 128], QXm[0:64, h, :], ["KM", ("QXm", h)])]
                    units.append(mk_unit(mm1, 128, 1.0, 0.0, VM[:, kt, h, :], "VM", ob, kt == 0, kt == 1, fin_mem(h)))
            nkc = (N - 1) // 128 + 1
            var = 4 if j == 0 else j % 4
            for h in range(8):
                g = h // 4
                ob = next_ob()
                for kt in range(nkc):
                    M = min(128, N - 128 * kt)
                    mm1 = [(KXc[0:68, g, kt * 128:kt * 128 + M], QX[0:68, h, :],
                            [("KXc", g, "k"), ("KXc", g, "x"), ("QX", h, "q"), ("QX", h, "x")])]
                    if kt == nkc - 1:
                        mm1.append((identb[0:M, 0:M], MCMP[0:M, var, :], ["identb", "MCMP"]))
                    units.append(mk_unit(mm1, M, SLOPES[h], -SLOPES[h] * 512.0 * j, VC[0:M, kt, g, :], "VCd",
                                         ob, kt == 0, kt == nkc - 1, fin_nsa(0, h, False)))
            run_units(units)

            units = []
            for h in range(8):
                g = h // 4
                ob = next_ob()
                nk = 4 * j + 4
                for kt in range(nk):
                    c = kt // 30
                    mm1 = [(KXs[0:68, g, kt * 128:(kt + 1) * 128], QX[0:68, h, :],
                            [("KXs", g, kt // 4), ("KXs", g, "x"), ("QX", h, "q"), ("QX", h, "x")]),
                           (KXs[64:128, g, kt * 128:(kt + 1) * 128], PX[64:128, g, c, :], [("KXs", g, "x"), ("PX", g, c), "PXinit"])]
                    if kt >= 4 * j:
                        mm1.append((identb[:], MC[:, kt - 4 * j, :], ["identb", "MC"]))
                    units.append(mk_unit(mm1, 128, SLOPES[h], -SLOPES[h] * 512.0 * j, Vs[:, kt, g, :], ("Vs", kt // 4),
                                         ob, kt == 0, kt == nk - 1, fin_nsa(1, h, False)))
            run_units(units)

            accf = acc[:].rearrange("p a b -> p (a b)")
            tt(accf, accf, GN[:].rearrange("p a b -> p (a b)"), ALU.mult, [("acc", h) for h in range(8)] + ["GN"], ["accg"])
            accmf = accm[:].rearrange("p a b -> p (a b)")
            tt(accmf, accmf, GM[:].rearrange("p a b -> p (a b)"), ALU.mult, [("accm", h) for h in range(4)] + ["GM"], ["accmg"])
            for s in range(4):
                for c4 in range(4):
                    trn(ps[7][:, c4 * 128:(c4 + 1) * 128], acc[:, s, c4 * 128:(c4 + 1) * 128], identf[:], ["accg", "identf"], [PSN[7]])
                cp(yT[:, 2:6, s * 128:(s + 1) * 128], ps[7][:].rearrange("p (a b) -> p a b", b=128), [PSN[7]], [("yT", 2 + s)])
            for s in range(4):
                for c2 in range(2):
                    trn(ps[7][:, c2 * 128:(c2 + 1) * 128], accm[:, s, c2 * 128:(c2 + 1) * 128], identf[:], ["accmg", "identf"], [PSN[7]])
                cp(yT[:, 6:8, s * 128:(s + 1) * 128], ps[7][:, 0:256].rearrange("p (a b) -> p a b", b=128), [PSN[7]], [("yT", 6 + s)])
            yT_all = [("yT", k) for k in range(10)]

            for nb in range(4):
                i = wload(WOUT_s[l, nb], 2048, ("WOUT", l, nb))
                wv = wbuf[i][:, 0:2048].rearrange("p (c n) -> p c n", n=256)
                for s in range(4):
                    bk = 2 * s + nb // 2
                    c0 = (nb % 2) * 256
                    for c in range(8):
                        mm(ps[bk][:, c0:c0 + 256], yT[:, c, s * 128:(s + 1) * 128], wv[:, c, :], c == 0, c == 7,
                           [("wbuf", i)] + yT_all, [PSN[bk]])
            junk = cbuf[:, 0:2, :].rearrange("p a b -> p (a b)")
            for s in range(4):
                b_ = s % 2
                cp(ycp[:, 0:512], ps[2 * s][:, :], [PSN[2 * s]], ["ycp"])
                cp(ycp[:, 512:1024], ps[2 * s + 1][:, :], [PSN[2 * s + 1]], ["ycp"])
                stt(junk, ycp[:], 1.0, ycp[:], ALU.mult, ALU.mult, ["ycp", ("cb", 0), ("cb", 1)], ["cb01", "st16"], accum=st[:, 16:17])
                ts(st[:, 17:18], st[:, 16:17], 1.0 / D, 1e-6, ALU.mult, ALU.add, ["st16"], ["st17"])
                tt(st[:, 18:19], st[:, 17:18], mhalf[:, 0:1], ALU.pow, ["st17", "mhalf"], ["st18"], eng="pool")
                stt(ycp[:], ycp[:], st[:, 18:19], gpost_b[:], ALU.mult, ALU.mult, ["ycp", "st18", "gpost"], ["ycp"])
                dma(xb[b_][:], src_d[t0 + s * 128:t0 + (s + 1) * 128, :], [xk(s)], [("xb", b_)])
                tt(ycp[:], ycp[:], xb[b_][:], ALU.add, ["ycp", ("xb", b_)], ["ycp"])
                dma(dst_d[t0 + s * 128:t0 + (s + 1) * 128, :], ycp[:], ["ycp"], [xk(s)] if dst_d is xs_d else [("orow", j * 4 + s)], q="pool")

    S.emit(ctx)
    ctx.close()
    return nc


_NC_CACHE = {}


def run(inp, T, L, n_cores=8):
    f = lambda a: np.ascontiguousarray(np.asarray(a, dtype=np.float32))
    x = f(inp["x"])
    B = x.shape[0]
    key = (T, L)
    if key not in _NC_CACHE:
        _NC_CACHE[key] = build(T, L)
    nc = _NC_CACHE[key]
    shared = host_consts(T)
    shared.update(host_weights(L, f(inp["w_in"]), f(inp["w_out"]), f(inp["cmp_w1_k"]), f(inp["cmp_w1_v"]),
                               f(inp["cmp_w2_k"]), f(inp["cmp_w2_v"]), f(inp["cmp_pos_k"]), f(inp["cmp_pos_v"]),
                               f(inp["w_mem_kv"]), f(inp["conv_w"]), f(inp["conv_b"])))
    shared["gpre"] = f(inp["pre_norm_g"])
    shared["gpost"] = f(inp["post_norm_g"])
    shared["gmem"] = f(inp["mem_norm_g"])
    shared["bgate"] = f(inp["b_gate"])
    mem = f(inp["mem"])
    in_maps = []
    for c in range(n_cores):
        b = c % B
        m = dict(shared)
        m["x"] = np.ascontiguousarray(x[b])
        m["mem"] = np.ascontiguousarray(mem[b])
        in_maps.append(m)
    res = run_bass_kernel_spmd(nc, in_maps, core_ids=list(range(n_cores)))
    out = np.stack([np.asarray(res.results[b]["out"], dtype=np.float32) for b in range(B)], axis=0)
    return out


def kernel(**inputs):
    return run(inputs, 8192, 4)
```

```python
import numpy as np
import ml_dtypes
from contextlib import ExitStack
import concourse.bass as bass
import concourse.mybir as mybir
from concourse.bass_utils import run_bass_kernel_spmd

F32 = mybir.dt.float32
BF16 = mybir.dt.bfloat16
AF = mybir.ActivationFunctionType
ALU = mybir.AluOpType
AX = mybir.AxisListType
NPBF = ml_dtypes.bfloat16

D = 1024
NEG = -1.0e6
SLOPES = [2.0 ** -(h + 1) for h in range(8)]
CH = 60


class Op:
    __slots__ = ("eng", "pos", "fn", "waits", "signal", "token", "is_dma", "lane", "lane_val", "snap")


class Sched:
    def __init__(self, nc, n_lanes=24, same_engine_sync=True):
        self.nc = nc
        self.eng = {"pe": nc.tensor, "act": nc.scalar, "dve": nc.vector, "pool": nc.gpsimd, "sp": nc.sync}
        self.ops = {e: [] for e in self.eng}
        self.lw = {}
        self.rd = {}
        self.known = {e: {} for e in self.eng}
        self.n_lanes = n_lanes
        self.lane_last = [None] * n_lanes
        self.lane_cnt = [0] * n_lanes
        self.next_lane = 0
        self.same_engine_sync = same_engine_sync
        self.nwaits = 0

    def _need(self, op, d):
        e = op.eng
        kn = self.known[e]
        if d.is_dma:
            key = ("L", d.lane)
            val = d.lane_val
        else:
            if d.eng == e:
                if e == "pe" or not self.same_engine_sync:
                    return
            key = d.eng
            val = d.pos
        if kn.get(key, -1) >= val:
            return
        kn[key] = val
        op.waits.append(d)
        d.signal = True
        self.nwaits += 1
        if d.snap is not None:
            for k, v in d.snap:
                if kn.get(k, -1) < v:
                    kn[k] = v

    def add(self, eng, fn, reads=(), writes=(), dma=False):
        op = Op()
        op.eng = eng
        op.fn = fn
        op.waits = []
        op.signal = False
        op.token = None
        op.is_dma = dma
        op.lane = None
        op.lane_val = None
        op.pos = len(self.ops[eng])
        deps = []
        for r in reads:
            w = self.lw.get(r)
            if w is not None:
                deps.append(w)
        for r in writes:
            w = self.lw.get(r)
            if w is not None:
                deps.append(w)
            rr = self.rd.get(r)
            if rr:
                deps.extend(rr.values())
        seen = set()
        for d in deps:
            if id(d) in seen:
                continue
            seen.add(id(d))
            self._need(op, d)
        if dma:
            lane = self.next_lane
            self.next_lane = (self.next_lane + 1) % self.n_lanes
            prev = self.lane_last[lane]
            if prev is not None:
                self._need(op, prev)
            self.lane_cnt[lane] += 1
            op.lane = lane
            op.lane_val = self.lane_cnt[lane]
            self.lane_last[lane] = op
        kn = self.known[eng]
        op.snap = tuple((k, v) for k, v in kn.items() if not isinstance(k, tuple))
        self.ops[eng].append(op)
        for r in reads:
            dd = self.rd.setdefault(r, {})
            dd[("D", id(op)) if dma else eng] = op
        for r in writes:
            self.lw[r] = op
            self.rd[r] = {}
        return op

    def emit(self, ctx):
        nc = self.nc
        esem = {e: ctx.enter_context(nc.semaphore("s_" + e)) for e in self.eng}
        lsem = [ctx.enter_context(nc.semaphore("l_%d" % i)) for i in range(self.n_lanes)]
        for e, lst in self.ops.items():
            c = 0
            for op in lst:
                if (not op.is_dma) and op.signal:
                    c += 1
                    op.token = c
        block = ctx.enter_context(nc.Block())
        reg = {"pe": block.tensor, "act": block.scalar, "dve": block.vector, "pool": block.gpsimd, "sp": block.sync}

        def make(e):
            def body(engh):
                for op in self.ops[e]:
                    for d in op.waits:
                        if d.is_dma:
                            engh.wait_ge(lsem[d.lane], 16 * d.lane_val)
                        else:
                            engh.wait_ge(esem[d.eng], d.token)
                    ins = op.fn(engh)
                    if op.is_dma:
                        ins.then_inc(lsem[op.lane], 16)
                    elif op.signal:
                        ins.then_inc(esem[e], 1)
                if e == "sp":
                    for i in range(self.n_lanes):
                        if self.lane_cnt[i]:
                            engh.wait_ge(lsem[i], 16 * self.lane_cnt[i])
            return body

        for e in self.eng:
            reg[e](make(e))


OFF = dict(cB=0, cC=256, ch=512, cg=768, q=1024, kc=1536, vc=1664, ks=1792, vs=1920, kw=2048, vw=2176,
           gl=2304, ng=2328, mq=2840, mg=3096)
NBLK = 13
BW = 288


def _block_cols():
    r = lambda a, n: list(range(a, a + n))
    blocks = [
        r(OFF["q"], 256), r(OFF["q"] + 256, 256),
        r(OFF["ks"], 128) + r(OFF["kw"], 128),
        r(OFF["kc"], 128) + r(OFF["vc"], 128),
        r(OFF["mq"], 256),
        r(OFF["cB"], 256), r(OFF["cC"], 256), r(OFF["ch"], 256), r(OFF["cg"], 256),
        r(OFF["vs"], 128) + r(OFF["vw"], 128) + r(OFF["gl"], 24),
        r(OFF["ng"], 256), r(OFF["ng"] + 256, 256),
        r(OFF["mg"], 256),
    ]
    return blocks


def host_consts(T):
    k = np.arange(128)[:, None]
    q = np.arange(512)[None, :]
    mc = np.zeros((128, 4, 512), np.float32)
    ml = np.zeros((128, 4, 512), np.float32)
    for kr in range(4):
        mc[:, kr, :] = np.where(128 * kr + k > q, NEG, 0.0)
        ml[:, kr, :] = np.where(128 * kr + k <= q, NEG, 0.0)
    mcmp = np.zeros((128, 5, 512), np.float32)
    for v in range(4):
        for rr in range(32):
            mcmp[32 * v + rr, v, :] = np.where(16 * rr + 15 > q[0], NEG, 0.0)
        mcmp[32 * v + 32:, v, :] = NEG
    mcmp[:, 4, :] = mcmp[:, 0, :]
    mcmp[0, 4, :] = NEG
    mq = np.zeros((128, 2, 4, 32), np.float32)
    p = np.arange(128)[:, None]
    rr = np.arange(32)[None, :]
    for s in range(4):
        mq[:, 0, s, :] = np.where(16 * rr + 15 > 128 * s + p, NEG, 0.0)
    mq[:, 1] = mq[:, 0]
    mq[:, 1, :, 0] = NEG
    pos = np.arange(T)
    kexts = np.zeros((64, T), np.float32)
    kexts[0] = pos // 128
    kexts[1] = pos % 128
    kexts[2] = 1.0
    kexts[3] = 1.0
    blk = (pos // 64) % CH
    for r_ in range(CH):
        kexts[4 + r_] = (blk == r_)
    sl = np.arange(512)
    pc = 16 * sl + 15
    kextc = np.stack([pc // 128, pc % 128, np.ones(512), np.ones(512)]).astype(np.float32)
    tq = np.arange(512)
    qext = np.stack([np.full(512, 128.0), np.ones(512), -128.0 * (tq // 128), -1.0 * (tq % 128)]).astype(np.float32)
    colab = np.zeros((128, 2), np.float32)
    colab[:, 0] = np.where(np.arange(128) >= 64, 1e4, -1.0)
    colab[:, 1] = np.where(np.arange(128) < 64, 1e4, -1.0)
    bf = lambda a: np.ascontiguousarray(a).astype(NPBF)
    return dict(mc=bf(mc.reshape(128, -1)), ml=bf(ml.reshape(128, -1)), mcmp=bf(mcmp.reshape(128, -1)),
                mq=bf(mq.reshape(128, -1)), kexts=bf(kexts), kextc=bf(kextc), qext=bf(qext), colab=colab)


def host_weights(L, w_in, w_out, w1k, w1v, w2k, w2v, posk, posv, wm, convw, convb):
    blocks = _block_cols()
    w_in_p = np.zeros((L, NBLK, 128, 8, BW), np.float32)
    for b, cols in enumerate(blocks):
        sub = w_in[:, :, cols]
        w_in_p[:, b, :, :, :len(cols)] = sub.reshape(L, 8, 128, len(cols)).transpose(0, 2, 1, 3)
    w_out_p = w_out.reshape(L, 8, 128, 4, 256).transpose(0, 3, 2, 1, 4)
    w1 = np.stack([w1k, w1v], axis=1)
    w1_p = w1.reshape(L, 2, 16, 128, 2, 128).transpose(0, 1, 4, 3, 2, 5)
    w2 = np.stack([w2k, w2v], axis=1)
    w2_p = w2.reshape(L, 2, 2, 128, 64).transpose(0, 1, 3, 2, 4)
    pos = np.stack([posk, posv], axis=1)
    pos_p = pos.reshape(L, 2, 16, 2, 64).transpose(0, 1, 3, 4, 2).reshape(L, 2, 128, 16)
    wm_p = wm.reshape(L, 8, 128, 2, 256).transpose(0, 3, 2, 1, 4)
    convw_t = convw.reshape(L, 3, 2, 128).transpose(3, 0, 2, 1)
    convb_t = convb.reshape(L, 2, 128).transpose(2, 0, 1)
    c = np.ascontiguousarray
    return dict(w_in_p=c(w_in_p.reshape(L, NBLK, 128, 8 * BW)), w_out_p=c(w_out_p.reshape(L, 4, 128, 2048)),
                w1_p=c(w1_p.reshape(L, 2, 2, 128, 2048)), w2_p=c(w2_p.reshape(L, 2, 128, 128)),
                pos_p=c(pos_p), wm_p=c(wm_p.reshape(L, 2, 128, 2048)),
                convw_t=c(convw_t.reshape(128, L * 6)), convb_t=c(convb_t.reshape(128, L * 2)))


def build(T=8192, L=4, same_engine_sync=True):
    NT = T // 512
    NKT = T // 128
    nc = bass.Bass("TRN2", target_bir_lowering=False)
    dram = lambda name, shape, dt_, kind: nc.dram_tensor(name, shape, dt_, kind=kind).ap()
    EI, EO, IN = "ExternalInput", "ExternalOutput", "Internal"
    x_d = dram("x", [T, D], F32, EI)
    mem_d = dram("mem", [256, D], F32, EI)
    win_d = dram("w_in_p", [L, NBLK, 128, 8 * BW], F32, EI)
    wout_d = dram("w_out_p", [L, 4, 128, 2048], F32, EI)
    w1_d = dram("w1_p", [L, 2, 2, 128, 2048], F32, EI)
    w2_d = dram("w2_p", [L, 2, 128, 128], F32, EI)
    pos_d = dram("pos_p", [L, 2, 128, 16], F32, EI)
    wm_d = dram("wm_p", [L, 2, 128, 2048], F32, EI)
    gpre_d = dram("gpre", [L, D], F32, EI)
    gpost_d = dram("gpost", [L, D], F32, EI)
    gmem_d = dram("gmem", [L, D], F32, EI)
    convw_d = dram("convw_t", [128, L * 6], F32, EI)
    convb_d = dram("convb_t", [128, L * 2], F32, EI)
    bgate_d = dram("bgate", [L, 24], F32, EI)
    mc_d = dram("mc", [128, 2048], BF16, EI)
    ml_d = dram("ml", [128, 2048], BF16, EI)
    mcmp_d = dram("mcmp", [128, 2560], BF16, EI)
    mq_d = dram("mq", [128, 256], BF16, EI)
    kexts_d = dram("kexts", [64, T], BF16, EI)
    kextc_d = dram("kextc", [4, 512], BF16, EI)
    qext_d = dram("qext", [4, 512], BF16, EI)
    colab_d = dram("colab", [128, 2], F32, EI)
    out_d = dram("out", [T, D], F32, EO)
    xs_d = dram("xs", [T, D], F32, IN)
    WIN_s = dram("WIN_s", [L, NBLK, 128, 8 * BW], BF16, IN)
    WOUT_s = dram("WOUT_s", [L, 4, 128, 2048], BF16, IN)
    W1_s = dram("W1_s", [L, 2, 2, 128, 2048], BF16, IN)
    WM_s = dram("WM_s", [L, 2, 128, 2048], BF16, IN)

    ctx = ExitStack()
    S = Sched(nc, same_engine_sync=same_engine_sync)
    sb = lambda name, shape, dt_=F32: nc.alloc_sbuf_tensor(name, shape, dt_)
    KXs = sb("KXs", [128, 2, T], BF16)
    Vs = sb("Vs", [128, NKT, 2, 65], BF16)
    KXw = sb("KXw", [128, 2, 1024], BF16)
    Vw = sb("Vw", [128, 8, 2, 65], BF16)
    KXc = sb("KXc", [128, 2, 512], BF16)
    VC = sb("VC", [128, 4, 2, 65], BF16)
    KC2 = sb("KC2", [128, 2, 2, 544], BF16)
    KM = sb("KM", [128, 4, 256], BF16)
    VM = sb("VM", [128, 2, 4, 65], BF16)
    QX = sb("QX", [128, 8, 512], BF16)
    QXm = sb("QXm", [128, 4, 512], BF16)
    PX = sb("PX", [128, 2, 3, 512], BF16)
    QXv = [sb("QXv%d" % i, [128, 512], BF16) for i in range(2)]
    MC = sb("MC", [128, 4, 512], BF16)
    ML = sb("ML", [128, 4, 512], BF16)
    MCMP = sb("MCMP", [128, 5, 512], BF16)
    MQ = sb("MQ", [128, 2, 4, 32], BF16)
    identb = sb("identb", [128, 128], BF16)
    identf = sb("identf", [128, 128], F32)
    gpre_b = sb("gpre_b", [128, D], F32)
    gpost_b = sb("gpost_b", [128, D], F32)
    convw_t = sb("convw_sb", [128, L * 6], F32)
    convb_t = sb("convb_sb", [128, L * 2], F32)
    bgate_b = sb("bgate_b", [128, L * 24], F32)
    colab = sb("colab_sb", [128, 2], F32)
    mhalf = sb("mhalf", [128, 4], F32)
    cbias = sb("cbias", [128, 256], F32)
    w2b = sb("w2b", [128, 2, 128], BF16)
    pos2b = sb("pos2b", [128, 2, 16], BF16)
    xb = [sb("xb%d" % i, [128, D], F32) for i in range(2)]
    hT = sb("hT", [128, 8, 512], BF16)
    NWB = 3
    wbuf = [sb("wbuf%d" % i, [128, 8 * BW], BF16) for i in range(NWB)]
    cbuf = sb("cbuf", [128, 4, 512], F32)
    ubuf = sb("ubuf", [128, 2, 514], F32)
    tmpA = sb("tmpA", [128, 1024], F32)
    abuf = tmpA[:, 0:512]
    thb = tmpA[:, 512:1024]
    hidf = tmpA[:, 0:768].rearrange("p (a b) -> p a b", b=256)
    ycp = sb("ycp", [128, D], F32)
    yT = sb("yT", [128, 8, 512], BF16)
    GN = sb("GN", [128, 4, 512], BF16)
    GM = sb("GM", [128, 4, 256], BF16)
    GL2 = sb("GL2", [128, 4, 24], F32)
    pbuf = [sb("pbuf%d" % i, [128, 512], BF16) for i in range(4)]
    OTs = [sb("OTs%d" % i, [128, 512], F32) for i in range(2)]
    acc = sb("acc", [128, 4, 512], F32)
    accm = sb("accm", [128, 4, 256], F32)
    tmpc = sb("tmpc", [128, 4, 64], F32)
    ebuf = [sb("ebuf%d" % i, [128, 512], F32) for i in range(2)]
    imp = sb("imp", [128, 516], F32)
    selv = sb("selv", [128, 128], F32)
    selv2 = sb("selv2", [128, 128], F32)
    Zc = sb("Zc", [128, 3, 128], F32)
    m8 = sb("m8", [128, 16], F32)
    st = sb("st", [128, 32], F32)
    hidb = sb("hidb", [128, 256], BF16)
    vcst = sb("vcst", [32, 2, 64], BF16)

    ps = [nc.alloc_psum_tensor("ps%d" % i, [128, 512], F32) for i in range(8)]
    PSN = ["ps%d" % i for i in range(8)]

    def dma(out, in_, r, w, q="sp"):
        S.add(q, lambda e: e.dma_start(out=out, in_=in_), r, w, dma=True)

    def mm(out, lhsT, rhs, start, stop, r, w):
        S.add("pe", lambda e: e.matmul(out, lhsT=lhsT, rhs=rhs, start=start, stop=stop), r, w)

    def trn(out, in_, ident, r, w):
        S.add("pe", lambda e: e.transpose(out=out, in_=in_, identity=ident), r, w)

    def actv(out, in_, func, r, w, scale=1.0, bias=0.0, accum=None):
        if accum is None:
            S.add("act", lambda e: e.activation(out=out, in_=in_, func=func, bias=bias, scale=scale), r, w)
        else:
            S.add("act", lambda e: e.activation(out=out, in_=in_, func=func, bias=bias, scale=scale, accum_out=accum), r, w)

    def cp(out, in_, r, w, eng="dve"):
        S.add(eng, lambda e: e.tensor_copy(out=out, in_=in_), r, w)

    def ts(out, in0, s1, s2, op0, op1, r, w, eng="dve"):
        if op1 is None:
            S.add(eng, lambda e: e.tensor_scalar(out=out, in0=in0, scalar1=s1, scalar2=None, op0=op0), r, w)
        else:
            S.add(eng, lambda e: e.tensor_scalar(out=out, in0=in0, scalar1=s1, scalar2=s2, op0=op0, op1=op1), r, w)

    def tt(out, in0, in1, op, r, w, eng="dve"):
        S.add(eng, lambda e: e.tensor_tensor(out=out, in0=in0, in1=in1, op=op), r, w)

    def stt(out, in0, scalar, in1, op0, op1, r, w, accum=None):
        if accum is None:
            S.add("dve", lambda e: e.scalar_tensor_tensor(out=out, in0=in0, scalar=scalar, in1=in1, op0=op0, op1=op1), r, w)
        else:
            S.add("dve", lambda e: e.scalar_tensor_tensor(out=out, in0=in0, scalar=scalar, in1=in1, op0=op0, op1=op1, accum_out=accum), r, w)

    def mset(ap, val, w, eng="pool"):
        S.add(eng, lambda e: e.memset(ap, val), (), w)

    dma(MC[:].rearrange("p a b -> p (a b)"), mc_d, [], ["MC"])
    dma(ML[:].rearrange("p a b -> p (a b)"), ml_d, [], ["ML"])
    dma(MCMP[:].rearrange("p a b -> p (a b)"), mcmp_d, [], ["MCMP"])
    dma(MQ[:].rearrange("p a b c -> p (a b c)"), mq_d, [], ["MQ"])
    dma(colab[:], colab_d, [], ["colab"])
    dma(convw_t[:], convw_d, [], ["convw"])
    dma(convb_t[:], convb_d, [], ["convb"])
    dma(bgate_b[:], bgate_d.rearrange("l n -> (l n)").partition_broadcast(128), [], ["bgate"])
    mset(QX[:].rearrange("p a b -> p (a b)"), 0.0, [("QX", h, "q") for h in range(8)] + [("QX", h, "x") for h in range(8)])
    mset(QXm[:].rearrange("p a b -> p (a b)"), 0.0, [("QXm", h) for h in range(4)])
    mset(KM[:].rearrange("p a b -> p (a b)"), 0.0, ["KM"])
    mset(KXw[:].rearrange("p a b -> p (a b)"), 0.0, [("KXw", g, sl, t) for g in range(2) for sl in range(2) for t in ("k", "x")])
    mset(KXc[:].rearrange("p a b -> p (a b)"), 0.0, [("KXc", g, t) for g in range(2) for t in ("k", "x")])
    for g in range(2):
        dma(KXs[64:128, g, :], kexts_d, [], [("KXs", g, "x")])
        dma(KXc[64:68, g, :], kextc_d, [], [("KXc", g, "x")])
    for h in range(8):
        dma(QX[64:68, h, :], qext_d, [], [("QX", h, "x")])
    mset(identf[:], 0.0, ["identf"])
    S.add("pool", lambda e: e.affine_select(out=identf[:], in_=identf[:], pattern=[[-1, 128]], compare_op=ALU.not_equal,
                                            fill=1.0, base=0, channel_multiplier=1), ["identf"], ["identf"])
    cp(identb[:], identf[:], ["identf"], ["identb"])
    mset(mhalf[:], -0.5, ["mhalf"])
    mset(Vs[:].rearrange("p a b c -> p (a b c)"), 1.0, [("Vs", "all")])
    mset(Vw[:].rearrange("p a b c -> p (a b c)"), 1.0, [("Vw", "all")])
    mset(VC[:].rearrange("p a b c -> p (a b c)"), 1.0, [("VC", "all")])
    mset(VM[:].rearrange("p a b c -> p (a b c)"), 1.0, [("VM", "all")])
    mset(KC2[:].rearrange("p a b c -> p (a b c)"), 0.0, ["KC2"])
    mset(PX[:].rearrange("p a b c -> p (a b c)"), 0.0, ["PXinit"])
    mset(Zc[:].rearrange("p a b -> p (a b)"), 0.0, ["Zc"])
    mset(imp[:], 0.0, ["imp"])

    wb_i = [0]

    def next_wb():
        i = wb_i[0]
        wb_i[0] = (i + 1) % NWB
        return i

    def prep(src, dst, n, last, dst_key):
        i = next_wb()
        wv = wbuf[i][:, 0:n].rearrange("p (c n) -> p c n", n=last)
        dma(wv, src.rearrange("p (c n) -> p c n", n=last), [], [("wbuf", i)], q="pool")
        dma(dst, wbuf[i][:, 0:n], [("wbuf", i)], [dst_key])

    for l in range(L):
        for b in range(NBLK):
            prep(win_d[l, b], WIN_s[l, b], 8 * BW, BW, ("WIN", l, b))
        for b in range(4):
            prep(wout_d[l, b], WOUT_s[l, b], 2048, 256, ("WOUT", l, b))
        for kv in range(2):
            for hf in range(2):
                prep(w1_d[l, kv, hf], W1_s[l, kv, hf], 2048, 128, ("W1", l, kv, hf))
        for b in range(2):
            prep(wm_d[l, b], WM_s[l, b], 2048, 256, ("WM", l, b))

    def wload(src, n, key):
        i = next_wb()
        dma(wbuf[i][:, 0:n], src, [key], [("wbuf", i)])
        return i

    gen_i = [0]

    def gen_bank():
        i = 5 + gen_i[0]
        gen_i[0] ^= 1
        return i

    def norm_transpose(xt, xkey, gtile, gkey, col0):
        stt(cbuf[:, 0:2, :].rearrange("p a b -> p (a b)"), xt[:], 1.0, xt[:], ALU.mult, ALU.mult,
            [xkey], ["cbuf", "st0"], accum=st[:, 0:1])
        ts(st[:, 1:2], st[:, 0:1], 1.0 / D, 1e-6, ALU.mult, ALU.add, ["st0"], ["st1"])
        tt(st[:, 2:3], st[:, 1:2], mhalf[:, 0:1], ALU.pow, ["st1", "mhalf"], ["st2"], eng="pool")
        stt(xt[:], xt[:], st[:, 2:3], gtile[:], ALU.mult, ALU.mult, [xkey, "st2", gkey], [xkey])
        for half in range(2):
            for c4 in range(4):
                c = half * 4 + c4
                trn(ps[7][:, c4 * 128:(c4 + 1) * 128], xt[:, c * 128:(c + 1) * 128], identf[:], [xkey, "identf"], [PSN[7]])
            cp(hT[:, half * 4:half * 4 + 4, col0:col0 + 128], ps[7][:].rearrange("p (a b) -> p a b", b=128),
               [PSN[7]], ["hT"])

    class Unit:
        pass

    def run_units(units):
        n = len(units)
        LA = 2
        pend = []
        for i in range(n + LA):
            if i < n:
                u = units[i]
                if u.pre is not None:
                    u.pre()
                sbk = i % 3
                nmm = len(u.mm1)
                for k, (lh, rh, rd_) in enumerate(u.mm1):
                    mm(ps[sbk][0:u.M, :], lh, rh, k == 0, k == nmm - 1, rd_, [PSN[sbk]])
                pb = i % 4
                actv(pbuf[pb][0:u.M, :], ps[sbk][0:u.M, :], AF.Exp, [PSN[sbk]], [("pbuf", pb)], scale=u.scale, bias=u.bias)
            k2 = i - LA
            if k2 >= 0:
                u = units[k2]
                pb = k2 % 4
                mm(ps[u.ob][0:65, :], u.v, pbuf[pb][0:u.M, :], u.first, u.last, [("pbuf", pb), u.vkey], [PSN[u.ob]])
                if u.last:
                    pend.append([k2 + 3, u])
            while pend and (pend[0][0] <= i or i == n + LA - 1):
                _, u = pend.pop(0)
                u.fin(u)

    ob_i = [0]
    qv_i = [0]

    def next_ob():
        i = 3 + ob_i[0]
        ob_i[0] ^= 1
        return i

    ot_i = [0]

    def finalize(u, gate_ap, const, dst, dkey, first_write):
        k = ot_i[0]
        ot_i[0] ^= 1
        cp(OTs[k][0:65, :], ps[u.ob][0:65, :], [PSN[u.ob]], [("OTs", k)])
        for s in range(4):
            trn(ps[7][:, s * 65:(s + 1) * 65], OTs[k][0:65, s * 128:(s + 1) * 128], identf[0:65, 0:65],
                [("OTs", k), "identf"], [PSN[7]])
        pv = ps[7][:, 0:260].rearrange("p (s c) -> p s c", c=65)
        ts(st[:, 20:24], pv[:, :, 64], 1e-30, None, ALU.max, None, [PSN[7]], ["st20"])
        S.add("dve", lambda e: e.reciprocal(out=st[:, 8:12], in_=st[:, 20:24]), ["st20"], ["st8"])
        if gate_ap is None:
            ts(st[:, 12:16], st[:, 8:12], const, None, ALU.mult, None, ["st8"], ["st12"])
        else:
            stt(st[:, 12:16], st[:, 8:12], const, gate_ap, ALU.mult, ALU.mult, ["st8", "GL2"], ["st12"])
        fb = st[:, 12:16].unsqueeze(2).to_broadcast([128, 4, 64])
        if first_write:
            tt(dst, pv[:, :, 0:64], fb, ALU.mult, [PSN[7], "st12"], [dkey])
        else:
            tt(tmpc[:], pv[:, :, 0:64], fb, ALU.mult, [PSN[7], "st12"], ["tmpc"])
            tt(dst, dst, tmpc[:], ALU.add, [dkey, "tmpc"], [dkey], eng="pool")

    def mk_unit(mm1, M, scale, bias, v, vkey, ob, first, last, fin, pre=None):
        u = Unit()
        u.pre = pre
        u.mm1, u.M, u.scale, u.bias, u.v, u.vkey, u.ob, u.first, u.last, u.fin = mm1, M, scale, bias, v, vkey, ob, first, last, fin
        return u

    for l in range(L):
        dma(gpre_b[:], gpre_d[l:l + 1, :].rearrange("a n -> (a n)").partition_broadcast(128), [], ["gpre"])
        dma(gpost_b[:], gpost_d[l:l + 1, :].rearrange("a n -> (a n)").partition_broadcast(128), [], ["gpost"])
        for kv in range(2):
            dma(w2b[:, kv, :], w2_d[l, kv], [], ["w2b"], q="pool")
            dma(pos2b[:, kv, :], pos_d[l, kv], [], ["pos2b"], q="pool")
        gb = gen_bank()
        for kv in range(2):
            for hf in range(2):
                i = wload(W1_s[l, kv, hf], 2048, ("W1", l, kv, hf))
                wv = wbuf[i][:, 0:2048].rearrange("p (c n) -> p c n", n=128)
                col = kv * 2 + hf
                for pp in range(16):
                    mm(ps[gb][:, col:col + 1], wv[:, pp, :], pos2b[:, kv, pp:pp + 1], pp == 0, pp == 15,
                       [("wbuf", i), "pos2b"], [PSN[gb]])
        cbv = cbias[:].rearrange("p (kv g hf r) -> p kv g hf r", kv=2, g=2, hf=2)
        for kv in range(2):
            for g in range(2):
                for hf in range(2):
                    col = kv * 2 + hf
                    cp(cbv[:, kv, g, hf, :], ps[gb][:, col:col + 1].to_broadcast([128, 32]), [PSN[gb]], ["cbias"])
        gmem_b = cbuf[:, 2:4, :].rearrange("p a b -> p (a b)")
        dma(gmem_b, gmem_d[l:l + 1, :].rearrange("a n -> (a n)").partition_broadcast(128), [], ["cb23"])
        for mt in range(2):
            dma(xb[mt][:], mem_d[mt * 128:(mt + 1) * 128, :], [], [("xb", mt)])
            norm_transpose(xb[mt], ("xb", mt), gmem_b, "cb23", mt * 128)
        ik = wload(WM_s[l, 0], 2048, ("WM", l, 0))
        wk = wbuf[ik][:, 0:2048].rearrange("p (c n) -> p c n", n=256)
        for pr in range(2):
            gb = gen_bank()
            for c in range(8):
                mm(ps[gb][:, 0:256], wk[:, c, pr * 128:(pr + 1) * 128], hT[:, c, 0:256], c == 0, c == 7,
                   [("wbuf", ik), "hT"], [PSN[gb]])
            cp(KM[0:64, 2 * pr, :], ps[gb][0:64, 0:256], [PSN[gb]], ["KM"])
            cp(KM[0:64, 2 * pr + 1, :], ps[gb][64:128, 0:256], [PSN[gb]], ["KM"])
        iv = wload(WM_s[l, 1], 2048, ("WM", l, 1))
        wv_ = wbuf[iv][:, 0:2048].rearrange("p (c n) -> p c n", n=256)
        for mt in range(2):
            gb = gen_bank()
            for c in range(8):
                mm(ps[gb][:, 0:256], hT[:, c, mt * 128:(mt + 1) * 128], wv_[:, c, :], c == 0, c == 7,
                   [("wbuf", iv), "hT"], [PSN[gb]])
            cp(VM[:, mt, :, 0:64], ps[gb][:, 0:256].rearrange("p (h d) -> p h d", d=64), [PSN[gb], ("VM", "all")], ["VM"])
        mset(ubuf[:, :, 0:2], 0.0, ["ubuf0", "ubuf1"])

        src_d = x_d if l == 0 else xs_d
        dst_d = out_d if l == L - 1 else xs_d

        for j in range(NT):
            t0 = 512 * j
            slot = j % 2
            pslot = 1 - slot
            xk = lambda s, j=j: ("xrow", j * 4 + s)
            for s in range(4):
                b_ = s % 2
                dma(xb[b_][:], src_d[t0 + s * 128:t0 + (s + 1) * 128, :], [xk(s)], [("xb", b_)])
                norm_transpose(xb[b_], ("xb", b_), gpre_b[:], "gpre", s * 128)
            for g in range(2):
                dma(KXw[64:68, g, slot * 512:(slot + 1) * 512], kexts_d[0:4, t0:t0 + 512], [], [("KXw", g, slot, "x")])

            def fm_block(b):
                i = wload(WIN_s[l, b], 8 * BW, ("WIN", l, b))
                wv = wbuf[i][:, :].rearrange("p (c n) -> p c n", n=BW)
                for grp in range(2):
                    gb = gen_bank()
                    for c in range(8):
                        mm(ps[gb][:, :], wv[:, c, grp * 128:(grp + 1) * 128], hT[:, c, :], c == 0, c == 7,
                           [("wbuf", i), "hT"], [PSN[gb]])
                    yield gb, grp

            for b in range(2):
                for gb, grp in fm_block(b):
                    for hh in range(2):
                        h = b * 4 + grp * 2 + hh
                        ts(QX[0:64, h, :], ps[gb][hh * 64:(hh + 1) * 64, :], 1.0 / (8.0 * SLOPES[h]), None, ALU.mult, None,
                           [PSN[gb]], [("QX", h, "q")])
            for gb, grp in fm_block(2):
                for g in range(2):
                    if grp == 0:
                        cp(KXs[0:64, g, t0:t0 + 512], ps[gb][g * 64:(g + 1) * 64, :], [PSN[gb]], [("KXs", g, j)])
                    else:
                        cp(KXw[0:64, g, slot * 512:(slot + 1) * 512], ps[gb][g * 64:(g + 1) * 64, :], [PSN[gb]],
                           [("KXw", g, slot, "k")])
            for kv in range(2):
                for g in range(2):
                    cp(KC2[0:64, kv, g, 0:16], KC2[0:64, kv, g, 512:528], [("KC2", kv, g)], [("KC2", kv, g)], eng="pool")
                    cp(KC2[64:128, kv, g, 0:15], KC2[64:128, kv, g, 512:527], [("KC2", kv, g)], [("KC2", kv, g)], eng="pool")
            for gb, grp in fm_block(3):
                kv = grp
                for g in range(2):
                    cp(KC2[0:64, kv, g, 16:528], ps[gb][g * 64:(g + 1) * 64, :], [PSN[gb], "KC2"], [("KC2", kv, g)])
                    cp(KC2[64:128, kv, g, 15:527], ps[gb][g * 64:(g + 1) * 64, :], [PSN[gb], "KC2"], [("KC2", kv, g)])
            for gb, grp in fm_block(4):
                for hh in range(2):
                    h = grp * 2 + hh
                    ts(QXm[0:64, h, :], ps[gb][hh * 64:(hh + 1) * 64, :], 0.125, None, ALU.mult, None, [PSN[gb]], [("QXm", h)])

            hb = gen_bank()
            for kv in range(2):
                for hf in range(2):
                    i = wload(W1_s[l, kv, hf], 2048, ("W1", l, kv, hf))
                    wv = wbuf[i][:, 0:2048].rearrange("p (c n) -> p c n", n=128)
                    for g in range(2):
                        col = ((kv * 2 + g) * 2 + hf) * 32
                        for pp in range(16):
                            rhs = KC2[:, kv, g, 2 * pp:2 * pp + 512].rearrange("p (r s) -> p r s", s=16)[:, :, 0]
                            mm(ps[hb][:, col:col + 32], wv[:, pp, :], rhs, pp == 0, pp == 15,
                               [("wbuf", i), ("KC2", kv, g)], [PSN[hb]])
            u_ = hidf[:, 0, :]
            v_ = hidf[:, 1, :]
            w_ = hidf[:, 2, :]
            tt(u_, ps[hb][:, 0:256], cbias[:], ALU.add, [PSN[hb], "cbias"], ["tmpA"])
            tt(v_, u_, u_, ALU.mult, ["tmpA"], ["tmpA"])
            ts(v_, v_, 0.044715, 1.0, ALU.mult, ALU.add, ["tmpA"], ["tmpA"])
            tt(v_, v_, u_, ALU.mult, ["tmpA"], ["tmpA"])
            actv(w_, v_, AF.Tanh, ["tmpA"], ["tmpA"], scale=0.7978845608028654)
            stt(hidb[:], w_, 1.0, u_, ALU.add, ALU.mult, ["tmpA"], ["hidb"])
            slot_lo = 32 * j
            for g in range(2):
                gb = gen_bank()
                for hf in range(2):
                    col = ((0 * 2 + g) * 2 + hf) * 32
                    mm(ps[gb][0:64, 0:32], w2b[:, 0, hf * 64:(hf + 1) * 64], hidb[:, col:col + 32], hf == 0, hf == 1,
                       ["w2b", "hidb"], [PSN[gb]])
                ts(KXc[0:64, g, slot_lo:slot_lo + 32], ps[gb][0:64, 0:32], 0.5, None, ALU.mult, None, [PSN[gb]], [("KXc", g, "k")])
                for hf in range(2):
                    col = ((1 * 2 + g) * 2 + hf) * 32
                    mm(ps[gb][0:32, 64:128], hidb[:, col:col + 32], w2b[:, 1, hf * 64:(hf + 1) * 64], hf == 0, hf == 1,
                       ["w2b", "hidb"], [PSN[gb]])
                ts(vcst[:, g, :], ps[gb][0:32, 64:128], 0.5, None, ALU.mult, None, [PSN[gb]], ["vcst"])
            pr0 = slot_lo % 128
            dma(VC[pr0:pr0 + 32, slot_lo // 128, :, 0:64], vcst[:], ["vcst", ("VC", "all")], ["VCd"])

            N = 32 * (j + 1)
            NB = 8 * (j + 1)
            nch = (NB - 1) // CH + 1
            mqv = 1 if j == 0 else 0
            for g in range(2):
                for s in range(4):
                    for r_ in range(4):
                        h = g * 4 + r_
                        gb = gen_bank()
                        mm(ps[gb][:, 0:N], QX[0:128, h, s * 128:(s + 1) * 128], KXc[0:128, g, 0:N], True, False,
                           [("QX", h, "q"), ("QX", h, "x"), ("KXc", g, "k"), ("KXc", g, "x")], [PSN[gb]])
                        mm(ps[gb][:, N - 32:N], identb[:], MQ[:, mqv, s, :], False, True, ["identb", "MQ"], [PSN[gb]])
                        eb = ebuf[r_ % 2]
                        ek = ("ebuf", r_ % 2)
                        actv(eb[:, 0:N], ps[gb][:, 0:N], AF.Exp, [PSN[gb]], [ek, "st4"], scale=SLOPES[h],
                             bias=-SLOPES[h] * 512.0 * j, accum=st[:, 4:5])
                        ts(st[:, 6:7], st[:, 4:5], 1e-30, None, ALU.max, None, ["st4"], ["st6"])
                        S.add("dve", lambda e: e.reciprocal(out=st[:, 5:6], in_=st[:, 6:7]), ["st6"], ["st5"])
                        if r_ == 0:
                            ts(imp[:, 0:N], eb[:, 0:N], st[:, 5:6], None, ALU.mult, None, [ek, "st5"], ["imp"])
                        else:
                            stt(imp[:, 0:N], eb[:, 0:N], st[:, 5:6], imp[:, 0:N], ALU.mult, ALU.add, [ek, "st5", "imp"], ["imp"])
                    mset(imp[:, N:N + 1], 0.0, ["imp"], eng="dve")
                    chb = ebuf[0]
                    tt(chb[:, 0:N], imp[:, 0:N], imp[:, 1:N + 1], ALU.add, ["imp"], [("ebuf", 0)])
                    S.add("dve", lambda e, chb=chb, NB=NB, N=N: e.tensor_reduce(
                        out=selv[:, 0:NB], in_=chb[:, 0:N].rearrange("p (n f) -> p n f", f=4), axis=AX.X, op=ALU.add),
                        [("ebuf", 0)], ["selv"])
                    lo = 8 * j + 2 * s
                    if lo + 2 < 128:
                        mset(selv[:, lo + 2:128], -1.0, ["selv"], eng="dve")
                    cp(selv[:, lo + 1:lo + 2], colab[:, 0:1], ["colab", "selv"], ["selv"])
                    mset(selv[:, lo:lo + 1], 1.0e4, ["selv"], eng="dve")
                    if lo - 1 >= 1:
                        ts(selv[:, lo - 1:lo], selv[:, lo - 1:lo], colab[:, 1:2], None, ALU.max, None, ["selv", "colab"], ["selv"])
                    mset(selv[:, 0:1], 1.0e4, ["selv"], eng="dve")
                    S.add("dve", lambda e: e.max(out=m8[:, 0:8], in_=selv[:]), ["selv"], ["m8"])
                    S.add("dve", lambda e: e.match_replace(out=selv2[:], in_to_replace=m8[:, 0:8], in_values=selv[:], imm_value=-2.0),
                          ["selv", "m8"], ["selv2"])
                    S.add("dve", lambda e: e.max(out=m8[:, 8:16], in_=selv2[:]), ["selv2"], ["m8b"])
                    for c in range(nch):
                        n0 = CH * c
                        n1 = min(128, n0 + CH)
                        ts(Zc[:, c, 68:68 + (n1 - n0)], selv[:, n0:n1], m8[:, 15:16], NEG, ALU.is_lt, ALU.mult,
                           ["selv", "m8b"], ["Zc"])
                    for c in range(nch):
                        trn(ps[7][:, c * 128:(c + 1) * 128], Zc[:, c, :], identf[:], ["Zc", "identf"], [PSN[7]])
                    cp(PX[64:128, g, 0:nch, s * 128:(s + 1) * 128],
                       ps[7][64:128, 0:nch * 128].rearrange("p (a b) -> p a b", b=128), [PSN[7], "PXinit"],
                       [("PX", g, c) for c in range(nch)])

            def tm_block(b, ncols):
                i = wload(WIN_s[l, b], 8 * BW, ("WIN", l, b))
                wv = wbuf[i][:, :].rearrange("p (c n) -> p c n", n=BW)
                for s in range(4):
                    gb = gen_bank()
                    for c in range(8):
                        mm(ps[gb][:, 0:ncols], hT[:, c, s * 128:(s + 1) * 128], wv[:, c, 0:ncols], c == 0, c == 7,
                           [("wbuf", i), "hT"], [PSN[gb]])
                    yield gb, s

            for gb, s in tm_block(9, 280):
                cp(Vs[:, 4 * j + s, :, 0:64], ps[gb][:, 0:128].rearrange("p (g d) -> p g d", d=64), [PSN[gb], ("Vs", "all")],
                   [("Vs", j)])
                cp(Vw[:, slot * 4 + s, :, 0:64], ps[gb][:, 128:256].rearrange("p (g d) -> p g d", d=64), [PSN[gb], ("Vw", "all")],
                   [("Vw", slot)])
                tt(GL2[:, s, :], ps[gb][:, 256:280], bgate_b[:, l * 24:(l + 1) * 24], ALU.add, [PSN[gb], "bgate"], ["GL2"])
            glf = GL2[:].rearrange("p a b -> p (a b)")
            actv(glf, glf, AF.Tanh, ["GL2"], ["GL2"], scale=0.5)
            ts(glf, glf, 1.0, None, ALU.add, None, ["GL2"], ["GL2"])
            for half in range(2):
                for gb, s in tm_block(10 + half, 256):
                    actv(thb[:, 0:256], ps[gb][:, 0:256], AF.Tanh, [PSN[gb]], ["tmpA"], scale=0.5)
                    stt(GN[:, s, half * 256:(half + 1) * 256], thb[:, 0:256], 1.0, ps[gb][:, 0:256], ALU.add, ALU.mult,
                        ["tmpA", PSN[gb]], ["GN"])
            for gb, s in tm_block(12, 256):
                actv(thb[:, 0:256], ps[gb][:, 0:256], AF.Tanh, [PSN[gb]], ["tmpA"], scale=0.5)
                stt(GM[:, s, :], thb[:, 0:256], 1.0, ps[gb][:, 0:256], ALU.add, ALU.mult, ["tmpA", PSN[gb]], ["GM"])

            cw = lambda cc, k: convw_t[:, l * 6 + cc * 3 + k:l * 6 + cc * 3 + k + 1]
            for gb, cc in fm_block(6):
                cp(cbuf[:, cc, :], ps[gb][:, :], [PSN[gb], "cb01"], [("cb", cc)])
            for gb, cc in fm_block(7):
                uk = "ubuf%d" % cc
                if j > 0:
                    cp(ubuf[:, cc, 0:2], ubuf[:, cc, 512:514], [uk], [uk])
                tt(ubuf[:, cc, 2:514], cbuf[:, cc, :], ps[gb][:, :], ALU.mult, [("cb", cc), PSN[gb]], [uk])
                ak = ("ca", cc)
                ts(cbuf[:, 2 + cc, :], ubuf[:, cc, 2:514], cw(cc, 2), convb_t[:, l * 2 + cc:l * 2 + cc + 1], ALU.mult, ALU.add,
                   [uk, "convw", "convb", "cb23"], [ak])
                stt(cbuf[:, 2 + cc, :], ubuf[:, cc, 1:513], cw(cc, 1), cbuf[:, 2 + cc, :], ALU.mult, ALU.add, [uk, ak, "convw"], [ak])
                stt(cbuf[:, 2 + cc, :], ubuf[:, cc, 0:512], cw(cc, 0), cbuf[:, 2 + cc, :], ALU.mult, ALU.add, [uk, ak, "convw"], [ak])
            for gb, cc in fm_block(5):
                ak = ("ca", cc)
                tt(cbuf[:, 2 + cc, :], cbuf[:, 2 + cc, :], ps[gb][:, :], ALU.mult, [ak, PSN[gb]], [ak])
            for gb, cc in fm_block(8):
                ak = ("ca", cc)
                actv(thb[:], ps[gb][:, :], AF.Tanh, [PSN[gb]], ["tmpA"], scale=0.5)
                stt(abuf[:], thb[:], 1.0, ps[gb][:, :], ALU.add, ALU.mult, ["tmpA", PSN[gb]], ["tmpA"])
                stt(yT[:, cc, :], cbuf[:, 2 + cc, :], 0.5, abuf[:], ALU.mult, ALU.mult, [ak, "tmpA"], [("yT", cc)])

            def fin_nsa(br, h, first_write):
                def f(u):
                    finalize(u, GL2[:, :, br * 8 + h], 0.25, acc[:, :, h * 64:(h + 1) * 64], ("acc", h), first_write)
                return f

            def fin_mem(h):
                def f(u):
                    finalize(u, None, 0.5, accm[:, :, h * 64:(h + 1) * 64], ("accm", h), True)
                return f

            units = []
            for h in range(8):
                g = h // 4
                ob = next_ob()
                tl = []
                if j >= 1:
                    for kr in range(4):
                        tl.append((pslot, kr, "ML"))
                for kr in range(4):
                    tl.append((slot, kr, "MC"))
                for ti, (sl_, kr, mk) in enumerate(tl):
                    mt_ = ML if mk == "ML" else MC
                    mm1 = [(KXw[0:128, g, sl_ * 512 + kr * 128:sl_ * 512 + (kr + 1) * 128], QX[0:128, h, :],
                            [("KXw", g, sl_, "k"), ("KXw", g, sl_, "x"), ("QX", h, "q"), ("QX", h, "x")]),
                           (identb[:], mt_[:, kr, :], ["identb", mk])]
                    units.append(mk_unit(mm1, 128, SLOPES[h], -SLOPES[h] * 512.0 * j, Vw[:, sl_ * 4 + kr, g, :], ("Vw", sl_),
                                         ob, ti == 0, ti == len(tl) - 1, fin_nsa(2, h, True)))
            for h in range(4):
                ob = next_ob()
                for kt in range(2):
                    mm1 = [(KM[0:128, h, kt * 128:(kt + 1) * 128], QXm[0:128, h, :], ["KM", ("QXm", h)])]
                    units.append(mk_unit(mm1, 128, 1.0, 0.0, VM[:, kt, h, :], "VM", ob, kt == 0, kt == 1, fin_mem(h)))
            nkc = (N - 1) // 128 + 1
            var = 4 if j == 0 else j % 4
            for h in range(8):
                g = h // 4
                ob = next_ob()
                for kt in range(nkc):
                    M = 128
                    mm1 = [(KXc[0:128, g, kt * 128:kt * 128 + M], QX[0:128, h, :],
                            [("KXc", g, "k"), ("KXc", g, "x"), ("QX", h, "q"), ("QX", h, "x")])]
                    if kt == nkc - 1:
                        mm1.append((identb[:], MCMP[:, var, :], ["identb", "MCMP"]))
                    units.append(mk_unit(mm1, M, SLOPES[h], -SLOPES[h] * 512.0 * j, VC[:, kt, g, :], "VCd",
                                         ob, kt == 0, kt == nkc - 1, fin_nsa(0, h, False)))
            run_units(units)

            units = []
            for h in range(8):
                g = h // 4
                ob = next_ob()
                nk = 4 * j + 4
                for kt in range(nk):
                    c = kt // 30
                    pre = None
                    if kt % 30 == 0:
                        vb = qv_i[0]
                        qv_i[0] ^= 1

                        def pre(vb=vb, g=g, c=c, h=h):
                            cp(QXv[vb][64:128, :], PX[64:128, g, c, :], [("PX", g, c), "PXinit"], [("QXv", vb)], eng="pool")
                            cp(QXv[vb][0:68, :], QX[0:68, h, :], [("QX", h, "q"), ("QX", h, "x")], [("QXv", vb)], eng="pool")
                    mm1 = [(KXs[0:128, g, kt * 128:(kt + 1) * 128], QXv[vb][0:128, :],
                            [("KXs", g, kt // 4), ("KXs", g, "x"), ("QXv", vb)])]
                    if kt >= 4 * j:
                        mm1.append((identb[:], MC[:, kt - 4 * j, :], ["identb", "MC"]))
                    units.append(mk_unit(mm1, 128, SLOPES[h], -SLOPES[h] * 512.0 * j, Vs[:, kt, g, :], ("Vs", kt // 4),
                                         ob, kt == 0, kt == nk - 1, fin_nsa(1, h, False), pre=pre))
            run_units(units)

            accf = acc[:].rearrange("p a b -> p (a b)")
            tt(accf, accf, GN[:].rearrange("p a b -> p (a b)"), ALU.mult, [("acc", h) for h in range(8)] + ["GN"], ["accg"])
            accmf = accm[:].rearrange("p a b -> p (a b)")
            tt(accmf, accmf, GM[:].rearrange("p a b -> p (a b)"), ALU.mult, [("accm", h) for h in range(4)] + ["GM"], ["accmg"])
            for s in range(4):
                for c4 in range(4):
                    trn(ps[7][:, c4 * 128:(c4 + 1) * 128], acc[:, s, c4 * 128:(c4 + 1) * 128], identf[:], ["accg", "identf"], [PSN[7]])
                cp(yT[:, 2:6, s * 128:(s + 1) * 128], ps[7][:].rearrange("p (a b) -> p a b", b=128), [PSN[7]], [("yT", 2 + s)])
            for s in range(4):
                for c2 in range(2):
                    trn(ps[7][:, c2 * 128:(c2 + 1) * 128], accm[:, s, c2 * 128:(c2 + 1) * 128], identf[:], ["accmg", "identf"], [PSN[7]])
                cp(yT[:, 6:8, s * 128:(s + 1) * 128], ps[7][:, 0:256].rearrange("p (a b) -> p a b", b=128), [PSN[7]], [("yT", 6 + s)])
            yT_all = [("yT", k) for k in range(10)]

            for nb in range(4):
                i = wload(WOUT_s[l, nb], 2048, ("WOUT", l, nb))
                wv = wbuf[i][:, 0:2048].rearrange("p (c n) -> p c n", n=256)
                for s in range(4):
                    bk = 2 * s + nb // 2
                    c0 = (nb % 2) * 256
                    for c in range(8):
                        mm(ps[bk][:, c0:c0 + 256], yT[:, c, s * 128:(s + 1) * 128], wv[:, c, :], c == 0, c == 7,
                           [("wbuf", i)] + yT_all, [PSN[bk]])
            junk = cbuf[:, 0:2, :].rearrange("p a b -> p (a b)")
            for s in range(4):
                b_ = s % 2
                cp(ycp[:, 0:512], ps[2 * s][:, :], [PSN[2 * s]], ["ycp"])
                cp(ycp[:, 512:1024], ps[2 * s + 1][:, :], [PSN[2 * s + 1]], ["ycp"])
                stt(junk, ycp[:], 1.0, ycp[:], ALU.mult, ALU.mult, ["ycp", ("cb", 0), ("cb", 1)], ["cb01", "st16"], accum=st[:, 16:17])
                ts(st[:, 17:18], st[:, 16:17], 1.0 / D, 1e-6, ALU.mult, ALU.add, ["st16"], ["st17"])
                tt(st[:, 18:19], st[:, 17:18], mhalf[:, 0:1], ALU.pow, ["st17", "mhalf"], ["st18"], eng="pool")
                stt(ycp[:], ycp[:], st[:, 18:19], gpost_b[:], ALU.mult, ALU.mult, ["ycp", "st18", "gpost"], ["ycp"])
                dma(xb[b_][:], src_d[t0 + s * 128:t0 + (s + 1) * 128, :], [xk(s)], [("xb", b_)])
                tt(ycp[:], ycp[:], xb[b_][:], ALU.add, ["ycp", ("xb", b_)], ["ycp"])
                dma(dst_d[t0 + s * 128:t0 + (s + 1) * 128, :], ycp[:], ["ycp"], [xk(s)] if dst_d is xs_d else [("orow", j * 4 + s)], q="pool")

    S.emit(ctx)
    ctx.close()
    return nc


_NC_CACHE = {}


def run(inp, T, L, n_cores=8):
    f = lambda a: np.ascontiguousarray(np.asarray(a, dtype=np.float32))
    x = f(inp["x"])
    B = x.shape[0]
    key = (T, L)
    if key not in _NC_CACHE:
        _NC_CACHE[key] = build(T, L)
    nc = _NC_CACHE[key]
    shared = host_consts(T)
    shared.update(host_weights(L, f(inp["w_in"]), f(inp["w_out"]), f(inp["cmp_w1_k"]), f(inp["cmp_w1_v"]),
                               f(inp["cmp_w2_k"]), f(inp["cmp_w2_v"]), f(inp["cmp_pos_k"]), f(inp["cmp_pos_v"]),
                               f(inp["w_mem_kv"]), f(inp["conv_w"]), f(inp["conv_b"])))
    shared["gpre"] = f(inp["pre_norm_g"])
    shared["gpost"] = f(inp["post_norm_g"])
    shared["gmem"] = f(inp["mem_norm_g"])
    shared["bgate"] = f(inp["b_gate"])
    mem = f(inp["mem"])
    in_maps = []
    for c in range(n_cores):
        b = c % B
        m = dict(shared)
        m["x"] = np.ascontiguousarray(x[b])
        m["mem"] = np.ascontiguousarray(mem[b])
        in_maps.append(m)
    res = run_bass_kernel_spmd(nc, in_maps, core_ids=list(range(n_cores)))
    out = np.stack([np.asarray(res.results[b]["out"], dtype=np.float32) for b in range(B)], axis=0)
    return out


def kernel(**inputs):
    return run(inputs, 8192, 4)
```

```python
import numpy as np
import ml_dtypes
from contextlib import ExitStack
import concourse.bass as bass
import concourse.mybir as mybir
from concourse.bass_utils import run_bass_kernel_spmd

F32 = mybir.dt.float32
BF16 = mybir.dt.bfloat16
AF = mybir.ActivationFunctionType
ALU = mybir.AluOpType
AX = mybir.AxisListType
NPBF = ml_dtypes.bfloat16

D = 1024
NEG = -1.0e6
SLOPES = [2.0 ** -(h + 1) for h in range(8)]
CH = 60


class Op:
    __slots__ = ("eng", "pos", "fn", "waits", "signal", "token", "is_dma", "lane", "lane_val", "snap")


class Sched:
    def __init__(self, nc, n_lanes=24, same_engine_sync=True):
        self.nc = nc
        self.eng = {"pe": nc.tensor, "act": nc.scalar, "dve": nc.vector, "pool": nc.gpsimd, "sp": nc.sync}
        self.ops = {e: [] for e in self.eng}
        self.lw = {}
        self.rd = {}
        self.known = {e: {} for e in self.eng}
        self.n_lanes = n_lanes
        self.lane_last = [None] * n_lanes
        self.lane_cnt = [0] * n_lanes
        self.next_lane = 0
        self.same_engine_sync = same_engine_sync
        self.nwaits = 0

    def _need(self, op, d, raw=True):
        e = op.eng
        kn = self.known[e]
        if d.is_dma:
            key = ("L", d.lane)
            val = d.lane_val
        else:
            if d.eng == e:
                if e == "pe" or not self.same_engine_sync or not raw:
                    return
            key = d.eng
            val = d.pos
        if kn.get(key, -1) >= val:
            return
        kn[key] = val
        op.waits.append(d)
        d.signal = True
        self.nwaits += 1
        if d.snap is not None:
            for k, v in d.snap:
                if kn.get(k, -1) < v:
                    kn[k] = v

    def add(self, eng, fn, reads=(), writes=(), dma=False):
        op = Op()
        op.eng = eng
        op.fn = fn
        op.waits = []
        op.signal = False
        op.token = None
        op.is_dma = dma
        op.lane = None
        op.lane_val = None
        op.pos = len(self.ops[eng])
        deps = []
        for r in reads:
            w = self.lw.get(r)
            if w is not None:
                deps.append((w, True))
        for r in writes:
            w = self.lw.get(r)
            if w is not None:
                deps.append((w, False))
            rr = self.rd.get(r)
            if rr:
                deps.extend((o, False) for o in rr.values())
        seen = set()
        for d, raw in deps:
            if raw and id(d) in seen:
                continue
            if raw:
                seen.add(id(d))
            self._need(op, d, raw)
        if dma:
            lane = self.next_lane
            self.next_lane = (self.next_lane + 1) % self.n_lanes
            prev = self.lane_last[lane]
            if prev is not None:
                self._need(op, prev)
            self.lane_cnt[lane] += 1
            op.lane = lane
            op.lane_val = self.lane_cnt[lane]
            self.lane_last[lane] = op
        kn = self.known[eng]
        op.snap = tuple((k, v) for k, v in kn.items() if not isinstance(k, tuple))
        self.ops[eng].append(op)
        for r in reads:
            dd = self.rd.setdefault(r, {})
            dd[("D", id(op)) if dma else eng] = op
        for r in writes:
            self.lw[r] = op
            self.rd[r] = {}
        return op

    def emit(self, ctx):
        nc = self.nc
        esem = {e: ctx.enter_context(nc.semaphore("s_" + e)) for e in self.eng}
        lsem = [ctx.enter_context(nc.semaphore("l_%d" % i)) for i in range(self.n_lanes)]
        for e, lst in self.ops.items():
            c = 0
            for op in lst:
                if (not op.is_dma) and op.signal:
                    c += 1
                    op.token = c
        block = ctx.enter_context(nc.Block())
        reg = {"pe": block.tensor, "act": block.scalar, "dve": block.vector, "pool": block.gpsimd, "sp": block.sync}

        def make(e):
            def body(engh):
                for op in self.ops[e]:
                    for d in op.waits:
                        if d.is_dma:
                            engh.wait_ge(lsem[d.lane], 16 * d.lane_val)
                        else:
                            engh.wait_ge(esem[d.eng], d.token)
                    ins = op.fn(engh)
                    if op.is_dma:
                        ins.then_inc(lsem[op.lane], 16)
                    elif op.signal:
                        ins.then_inc(esem[e], 1)
                if e == "sp":
                    for i in range(self.n_lanes):
                        if self.lane_cnt[i]:
                            engh.wait_ge(lsem[i], 16 * self.lane_cnt[i])
            return body

        for e in self.eng:
            reg[e](make(e))


OFF = dict(cB=0, cC=256, ch=512, cg=768, q=1024, kc=1536, vc=1664, ks=1792, vs=1920, kw=2048, vw=2176,
           gl=2304, ng=2328, mq=2840, mg=3096)
NBLK = 13
BW = 288


def _block_cols():
    r = lambda a, n: list(range(a, a + n))
    blocks = [
        r(OFF["q"], 256), r(OFF["q"] + 256, 256),
        r(OFF["ks"], 128) + r(OFF["kw"], 128),
        r(OFF["kc"], 128) + r(OFF["vc"], 128),
        r(OFF["mq"], 256),
        r(OFF["cB"], 256), r(OFF["cC"], 256), r(OFF["ch"], 256), r(OFF["cg"], 256),
        r(OFF["vs"], 128) + r(OFF["vw"], 128) + r(OFF["gl"], 24),
        r(OFF["ng"], 256), r(OFF["ng"] + 256, 256),
        r(OFF["mg"], 256),
    ]
    return blocks


def host_consts(T):
    k = np.arange(128)[:, None]
    q = np.arange(512)[None, :]
    mc = np.zeros((128, 4, 512), np.float32)
    ml = np.zeros((128, 4, 512), np.float32)
    for kr in range(4):
        mc[:, kr, :] = np.where(128 * kr + k > q, NEG, 0.0)
        ml[:, kr, :] = np.where(128 * kr + k <= q, NEG, 0.0)
    mcmp = np.zeros((128, 5, 512), np.float32)
    for v in range(4):
        for rr in range(32):
            mcmp[32 * v + rr, v, :] = np.where(16 * rr + 15 > q[0], NEG, 0.0)
        mcmp[32 * v + 32:, v, :] = NEG
    mcmp[:, 4, :] = mcmp[:, 0, :]
    mcmp[0, 4, :] = NEG
    mq = np.zeros((128, 2, 4, 32), np.float32)
    p = np.arange(128)[:, None]
    rr = np.arange(32)[None, :]
    for s in range(4):
        mq[:, 0, s, :] = np.where(16 * rr + 15 > 128 * s + p, NEG, 0.0)
    mq[:, 1] = mq[:, 0]
    mq[:, 1, :, 0] = NEG
    pos = np.arange(T)
    kexts = np.zeros((64, T), np.float32)
    kexts[0] = pos // 128
    kexts[1] = pos % 128
    kexts[2] = 1.0
    kexts[3] = 1.0
    blk = (pos // 64) % CH
    for r_ in range(CH):
        kexts[4 + r_] = (blk == r_)
    sl = np.arange(512)
    pc = 16 * sl + 15
    kextc = np.stack([pc // 128, pc % 128, np.ones(512), np.ones(512)]).astype(np.float32)
    tq = np.arange(512)
    qext = np.stack([np.full(512, 128.0), np.ones(512), -128.0 * (tq // 128), -1.0 * (tq % 128)]).astype(np.float32)
    colab = np.zeros((128, 2), np.float32)
    colab[:, 0] = np.where(np.arange(128) >= 64, 1e4, -1.0)
    colab[:, 1] = np.where(np.arange(128) < 64, 1e4, -1.0)
    bf = lambda a: np.ascontiguousarray(a).astype(NPBF)
    return dict(mc=bf(mc.reshape(128, -1)), ml=bf(ml.reshape(128, -1)), mcmp=bf(mcmp.reshape(128, -1)),
                mq=bf(mq.reshape(128, -1)), kexts=bf(kexts), kextc=bf(kextc), qext=bf(qext), colab=colab)


def host_weights(L, w_in, w_out, w1k, w1v, w2k, w2v, posk, posv, wm, convw, convb):
    blocks = _block_cols()
    w_in_p = np.zeros((L, NBLK, 128, 8, BW), np.float32)
    for b, cols in enumerate(blocks):
        sub = w_in[:, :, cols]
        w_in_p[:, b, :, :, :len(cols)] = sub.reshape(L, 8, 128, len(cols)).transpose(0, 2, 1, 3)
    w_out_p = w_out.reshape(L, 8, 128, 4, 256).transpose(0, 3, 2, 1, 4)
    w1 = np.stack([w1k, w1v], axis=1)
    w1_p = w1.reshape(L, 2, 16, 128, 2, 128).transpose(0, 1, 4, 3, 2, 5)
    w2 = np.stack([w2k, w2v], axis=1)
    w2_p = w2.reshape(L, 2, 2, 128, 64).transpose(0, 1, 3, 2, 4)
    pos = np.stack([posk, posv], axis=1)
    pos_p = pos.reshape(L, 2, 16, 2, 64).transpose(0, 1, 3, 4, 2).reshape(L, 2, 128, 16)
    wm_p = wm.reshape(L, 8, 128, 2, 256).transpose(0, 3, 2, 1, 4)
    convw_t = convw.reshape(L, 3, 2, 128).transpose(3, 0, 2, 1)
    convb_t = convb.reshape(L, 2, 128).transpose(2, 0, 1)
    c = np.ascontiguousarray
    return dict(w_in_p=c(w_in_p.reshape(L, NBLK, 128, 8 * BW)), w_out_p=c(w_out_p.reshape(L, 4, 128, 2048)),
                w1_p=c(w1_p.reshape(L, 2, 2, 128, 2048)), w2_p=c(w2_p.reshape(L, 2, 128, 128)),
                pos_p=c(pos_p), wm_p=c(wm_p.reshape(L, 2, 128, 2048)),
                convw_t=c(convw_t.reshape(128, L * 6)), convb_t=c(convb_t.reshape(128, L * 2)))


def build(T=8192, L=4, same_engine_sync=True):
    NT = T // 512
    NKT = T // 128
    nc = bass.Bass("TRN2", target_bir_lowering=False)
    dram = lambda name, shape, dt_, kind: nc.dram_tensor(name, shape, dt_, kind=kind).ap()
    EI, EO, IN = "ExternalInput", "ExternalOutput", "Internal"
    x_d = dram("x", [T, D], F32, EI)
    mem_d = dram("mem", [256, D], F32, EI)
    win_d = dram("w_in_p", [L, NBLK, 128, 8 * BW], F32, EI)
    wout_d = dram("w_out_p", [L, 4, 128, 2048], F32, EI)
    w1_d = dram("w1_p", [L, 2, 2, 128, 2048], F32, EI)
    w2_d = dram("w2_p", [L, 2, 128, 128], F32, EI)
    pos_d = dram("pos_p", [L, 2, 128, 16], F32, EI)
    wm_d = dram("wm_p", [L, 2, 128, 2048], F32, EI)
    gpre_d = dram("gpre", [L, D], F32, EI)
    gpost_d = dram("gpost", [L, D], F32, EI)
    gmem_d = dram("gmem", [L, D], F32, EI)
    convw_d = dram("convw_t", [128, L * 6], F32, EI)
    convb_d = dram("convb_t", [128, L * 2], F32, EI)
    bgate_d = dram("bgate", [L, 24], F32, EI)
    mc_d = dram("mc", [128, 2048], BF16, EI)
    ml_d = dram("ml", [128, 2048], BF16, EI)
    mcmp_d = dram("mcmp", [128, 2560], BF16, EI)
    mq_d = dram("mq", [128, 256], BF16, EI)
    kexts_d = dram("kexts", [64, T], BF16, EI)
    kextc_d = dram("kextc", [4, 512], BF16, EI)
    qext_d = dram("qext", [4, 512], BF16, EI)
    colab_d = dram("colab", [128, 2], F32, EI)
    out_d = dram("out", [T, D], F32, EO)
    xs_d = dram("xs", [T, D], F32, IN)
    WIN_s = dram("WIN_s", [L, NBLK, 128, 8 * BW], BF16, IN)
    WOUT_s = dram("WOUT_s", [L, 4, 128, 2048], BF16, IN)
    W1_s = dram("W1_s", [L, 2, 2, 128, 2048], BF16, IN)
    WM_s = dram("WM_s", [L, 2, 128, 2048], BF16, IN)

    ctx = ExitStack()
    S = Sched(nc, same_engine_sync=same_engine_sync)
    sb = lambda name, shape, dt_=F32: nc.alloc_sbuf_tensor(name, shape, dt_)
    KXs = sb("KXs", [128, 2, T], BF16)
    Vs = sb("Vs", [128, NKT, 2, 65], BF16)
    KXw = sb("KXw", [128, 2, 1024], BF16)
    Vw = sb("Vw", [128, 8, 2, 65], BF16)
    KXc = sb("KXc", [128, 2, 512], BF16)
    VC = sb("VC", [128, 4, 2, 65], BF16)
    KC2 = sb("KC2", [128, 2, 2, 544], BF16)
    KM = sb("KM", [128, 4, 256], BF16)
    VM = sb("VM", [128, 2, 4, 65], BF16)
    QX = sb("QX", [128, 8, 512], BF16)
    QXm = sb("QXm", [128, 4, 512], BF16)
    PX = sb("PX", [128, 2, 3, 512], BF16)
    QXv = [sb("QXv%d" % i, [128, 512], BF16) for i in range(2)]
    MC = sb("MC", [128, 4, 512], BF16)
    ML = sb("ML", [128, 4, 512], BF16)
    MCMP = sb("MCMP", [128, 5, 512], BF16)
    MQ = sb("MQ", [128, 2, 4, 32], BF16)
    identb = sb("identb", [128, 128], BF16)
    identf = sb("identf", [128, 128], F32)
    gpre_b = sb("gpre_b", [128, D], F32)
    gpost_b = sb("gpost_b", [128, D], F32)
    convw_t = sb("convw_sb", [128, L * 6], F32)
    convb_t = sb("convb_sb", [128, L * 2], F32)
    bgate_b = sb("bgate_b", [128, L * 24], F32)
    colab = sb("colab_sb", [128, 2], F32)
    mhalf = sb("mhalf", [128, 4], F32)
    cbias = sb("cbias", [128, 256], F32)
    w2b = sb("w2b", [128, 2, 128], BF16)
    pos2b = sb("pos2b", [128, 2, 16], BF16)
    xb = [sb("xb%d" % i, [128, D], F32) for i in range(2)]
    hT = sb("hT", [128, 8, 512], BF16)
    NWB = 3
    wbuf = [sb("wbuf%d" % i, [128, 8 * BW], BF16) for i in range(NWB)]
    cbuf = sb("cbuf", [128, 4, 512], F32)
    ubuf = sb("ubuf", [128, 2, 514], F32)
    tmpA = sb("tmpA", [128, 1024], F32)
    abuf = tmpA[:, 0:512]
    thb = tmpA[:, 512:1024]
    hidf = tmpA[:, 0:768].rearrange("p (a b) -> p a b", b=256)
    ycp = sb("ycp", [128, D], F32)
    yT = sb("yT", [128, 8, 512], BF16)
    GN = sb("GN", [128, 4, 512], BF16)
    GM = sb("GM", [128, 4, 256], BF16)
    GL2 = sb("GL2", [128, 4, 24], F32)
    pbuf = [sb("pbuf%d" % i, [128, 512], BF16) for i in range(4)]
    OTs = [sb("OTs%d" % i, [128, 512], F32) for i in range(2)]
    acc = sb("acc", [128, 4, 512], F32)
    accm = sb("accm", [128, 4, 256], F32)
    tmpc = sb("tmpc", [128, 4, 64], F32)
    ebuf = [sb("ebuf%d" % i, [128, 512], F32) for i in range(2)]
    imp = sb("imp", [128, 516], F32)
    selv = sb("selv", [128, 128], F32)
    selv2 = sb("selv2", [128, 128], F32)
    Zc = sb("Zc", [128, 3, 128], F32)
    m8 = sb("m8", [128, 16], F32)
    st = sb("st", [128, 40], F32)
    hidb = sb("hidb", [128, 256], BF16)
    vcst = sb("vcst", [32, 2, 64], BF16)

    ps = [nc.alloc_psum_tensor("ps%d" % i, [128, 512], F32) for i in range(8)]
    PSN = ["ps%d" % i for i in range(8)]

    def dma(out, in_, r, w, q="sp"):
        S.add(q, lambda e: e.dma_start(out=out, in_=in_), r, w, dma=True)

    def mm(out, lhsT, rhs, start, stop, r, w):
        S.add("pe", lambda e: e.matmul(out, lhsT=lhsT, rhs=rhs, start=start, stop=stop), r, w)

    def trn(out, in_, ident, r, w):
        S.add("pe", lambda e: e.transpose(out=out, in_=in_, identity=ident), r, w)

    def actv(out, in_, func, r, w, scale=1.0, bias=0.0, accum=None):
        if accum is None:
            S.add("act", lambda e: e.activation(out=out, in_=in_, func=func, bias=bias, scale=scale), r, w)
        else:
            S.add("act", lambda e: e.activation(out=out, in_=in_, func=func, bias=bias, scale=scale, accum_out=accum), r, w)

    def cp(out, in_, r, w, eng="dve"):
        S.add(eng, lambda e: e.tensor_copy(out=out, in_=in_), r, w)

    def ts(out, in0, s1, s2, op0, op1, r, w, eng="dve"):
        if op1 is None:
            S.add(eng, lambda e: e.tensor_scalar(out=out, in0=in0, scalar1=s1, scalar2=None, op0=op0), r, w)
        else:
            S.add(eng, lambda e: e.tensor_scalar(out=out, in0=in0, scalar1=s1, scalar2=s2, op0=op0, op1=op1), r, w)

    def tt(out, in0, in1, op, r, w, eng="dve"):
        S.add(eng, lambda e: e.tensor_tensor(out=out, in0=in0, in1=in1, op=op), r, w)

    def stt(out, in0, scalar, in1, op0, op1, r, w, accum=None):
        if accum is None:
            S.add("dve", lambda e: e.scalar_tensor_tensor(out=out, in0=in0, scalar=scalar, in1=in1, op0=op0, op1=op1), r, w)
        else:
            S.add("dve", lambda e: e.scalar_tensor_tensor(out=out, in0=in0, scalar=scalar, in1=in1, op0=op0, op1=op1, accum_out=accum), r, w)

    def mset(ap, val, w, eng="pool"):
        S.add(eng, lambda e: e.memset(ap, val), (), w)

    dma(MC[:].rearrange("p a b -> p (a b)"), mc_d, [], ["MC"])
    dma(ML[:].rearrange("p a b -> p (a b)"), ml_d, [], ["ML"])
    dma(MCMP[:].rearrange("p a b -> p (a b)"), mcmp_d, [], ["MCMP"])
    dma(MQ[:].rearrange("p a b c -> p (a b c)"), mq_d, [], ["MQ"])
    dma(colab[:], colab_d, [], ["colab"])
    dma(convw_t[:], convw_d, [], ["convw"])
    dma(convb_t[:], convb_d, [], ["convb"])
    dma(bgate_b[:], bgate_d.rearrange("l n -> (l n)").partition_broadcast(128), [], ["bgate"])
    mset(QX[:].rearrange("p a b -> p (a b)"), 0.0, [("QX", h, "q") for h in range(8)] + [("QX", h, "x") for h in range(8)])
    mset(QXm[:].rearrange("p a b -> p (a b)"), 0.0, [("QXm", h) for h in range(4)])
    mset(KM[:].rearrange("p a b -> p (a b)"), 0.0, ["KM"])
    mset(KXw[:].rearrange("p a b -> p (a b)"), 0.0, [("KXw", g, sl, t) for g in range(2) for sl in range(2) for t in ("k", "x")])
    mset(KXc[:].rearrange("p a b -> p (a b)"), 0.0, [("KXc", g, t) for g in range(2) for t in ("k", "x")])
    for g in range(2):
        dma(KXs[64:128, g, :], kexts_d, [], [("KXs", g, "x")])
        dma(KXc[64:68, g, :], kextc_d, [], [("KXc", g, "x")])
    for h in range(8):
        dma(QX[64:68, h, :], qext_d, [], [("QX", h, "x")])
    mset(identf[:], 0.0, ["identf"])
    S.add("pool", lambda e: e.affine_select(out=identf[:], in_=identf[:], pattern=[[-1, 128]], compare_op=ALU.not_equal,
                                            fill=1.0, base=0, channel_multiplier=1), ["identf"], ["identf"])
    cp(identb[:], identf[:], ["identf"], ["identb"])
    mset(mhalf[:], -0.5, ["mhalf"])
    mset(Vs[:].rearrange("p a b c -> p (a b c)"), 1.0, [("Vs", "all")])
    mset(Vw[:].rearrange("p a b c -> p (a b c)"), 1.0, [("Vw", "all")])
    mset(VC[:].rearrange("p a b c -> p (a b c)"), 1.0, [("VC", "all")])
    mset(VM[:].rearrange("p a b c -> p (a b c)"), 1.0, [("VM", "all")])
    mset(KC2[:].rearrange("p a b c -> p (a b c)"), 0.0, ["KC2"])
    mset(PX[:].rearrange("p a b c -> p (a b c)"), 0.0, ["PXinit"])
    mset(Zc[:].rearrange("p a b -> p (a b)"), 0.0, ["Zc"])
    mset(imp[:], 0.0, ["imp"])

    wb_i = [0]

    def next_wb():
        i = wb_i[0]
        wb_i[0] = (i + 1) % NWB
        return i

    def prep(src, dst, n, last, dst_key):
        i = next_wb()
        wv = wbuf[i][:, 0:n].rearrange("p (c n) -> p c n", n=last)
        dma(wv, src.rearrange("p (c n) -> p c n", n=last), [], [("wbuf", i)], q="pool")
        dma(dst, wbuf[i][:, 0:n], [("wbuf", i)], [dst_key])

    for l in range(L):
        for b in range(NBLK):
            prep(win_d[l, b], WIN_s[l, b], 8 * BW, BW, ("WIN", l, b))
        for b in range(4):
            prep(wout_d[l, b], WOUT_s[l, b], 2048, 256, ("WOUT", l, b))
        for kv in range(2):
            for hf in range(2):
                prep(w1_d[l, kv, hf], W1_s[l, kv, hf], 2048, 128, ("W1", l, kv, hf))
        for b in range(2):
            prep(wm_d[l, b], WM_s[l, b], 2048, 256, ("WM", l, b))

    def wload(src, n, key):
        i = next_wb()
        dma(wbuf[i][:, 0:n], src, [key], [("wbuf", i)])
        return i

    gen_i = [0]

    def gen_bank():
        i = 5 + gen_i[0]
        gen_i[0] ^= 1
        return i

    def norm_A(xt, xkey, gtile, gkey, k):
        o = 24 + 3 * k
        stt(cbuf[:, 0:2, :].rearrange("p a b -> p (a b)"), xt[:], 1.0, xt[:], ALU.mult, ALU.mult,
            [xkey], ["cb01", ("stn", k, 0)], accum=st[:, o:o + 1])
        ts(st[:, o + 1:o + 2], st[:, o:o + 1], 1.0 / D, 1e-6, ALU.mult, ALU.add, [("stn", k, 0)], [("stn", k, 1)])
        tt(st[:, o + 2:o + 3], st[:, o + 1:o + 2], mhalf[:, 0:1], ALU.pow, [("stn", k, 1), "mhalf"], [("stn", k, 2)], eng="pool")
        stt(xt[:], xt[:], st[:, o + 2:o + 3], gtile[:], ALU.mult, ALU.mult, [xkey, ("stn", k, 2), gkey], [xkey])

    def norm_B(xt, xkey, col0):
        for half in range(2):
            for c4 in range(4):
                c = half * 4 + c4
                trn(ps[7][:, c4 * 128:(c4 + 1) * 128], xt[:, c * 128:(c + 1) * 128], identf[:], [xkey, "identf"], [PSN[7]])
            S.add("act", lambda e, half=half: e.activation(out=hT[:, half * 4:half * 4 + 4, col0:col0 + 128],
                                                           in_=ps[7][:].rearrange("p (a b) -> p a b", b=128), func=AF.Copy),
                  [PSN[7]], ["hT"])

    def norm_transpose(xt, xkey, gtile, gkey, col0):
        norm_A(xt, xkey, gtile, gkey, 0)
        norm_B(xt, xkey, col0)

    def _unused(xt, xkey, col0):
        for half in range(2):
            for c4 in range(4):
                c = half * 4 + c4
                trn(ps[7][:, c4 * 128:(c4 + 1) * 128], xt[:, c * 128:(c + 1) * 128], identf[:], [xkey, "identf"], [PSN[7]])
            cp(hT[:, half * 4:half * 4 + 4, col0:col0 + 128], ps[7][:].rearrange("p (a b) -> p a b", b=128),
               [PSN[7]], ["hT"])

    class Unit:
        pass

    def run_units(units):
        n = len(units)
        LA = 2
        pend = []
        for i in range(n + LA):
            if i < n:
                u = units[i]
                if u.pre is not None:
                    u.pre()
                sbk = i % 3
                nmm = len(u.mm1)
                for k, (lh, rh, rd_) in enumerate(u.mm1):
                    mm(ps[sbk][0:u.M, :], lh, rh, k == 0, k == nmm - 1, rd_, [PSN[sbk]])
                pb = i % 4
                actv(pbuf[pb][0:u.M, :], ps[sbk][0:u.M, :], AF.Exp, [PSN[sbk]], [("pbuf", pb)], scale=u.scale, bias=u.bias)
            k2 = i - LA
            if k2 >= 0:
                u = units[k2]
                pb = k2 % 4
                mm(ps[u.ob][0:65, :], u.v, pbuf[pb][0:u.M, :], u.first, u.last, [("pbuf", pb), u.vkey], [PSN[u.ob]])
                if u.last:
                    pend.append([k2 + 3, u])
            while pend and (pend[0][0] <= i or i == n + LA - 1):
                _, u = pend.pop(0)
                u.fin(u)

    ob_i = [0]
    qv_i = [0]

    def next_ob():
        i = 3 + ob_i[0]
        ob_i[0] ^= 1
        return i

    ot_i = [0]

    def finalize(u, gate_ap, const, dst, dkey, first_write):
        k = ot_i[0]
        ot_i[0] ^= 1
        cp(OTs[k][0:65, :], ps[u.ob][0:65, :], [PSN[u.ob]], [("OTs", k)])
        for s in range(4):
            trn(ps[7][:, s * 65:(s + 1) * 65], OTs[k][0:65, s * 128:(s + 1) * 128], identf[0:65, 0:65],
                [("OTs", k), "identf"], [PSN[7]])
        pv = ps[7][:, 0:260].rearrange("p (s c) -> p s c", c=65)
        ts(st[:, 20:24], pv[:, :, 64], 1e-30, None, ALU.max, None, [PSN[7]], ["st20"])
        S.add("dve", lambda e: e.reciprocal(out=st[:, 8:12], in_=st[:, 20:24]), ["st20"], ["st8"])
        if gate_ap is None:
            ts(st[:, 12:16], st[:, 8:12], const, None, ALU.mult, None, ["st8"], ["st12"])
        else:
            stt(st[:, 12:16], st[:, 8:12], const, gate_ap, ALU.mult, ALU.mult, ["st8", "GL2"], ["st12"])
        fb = st[:, 12:16].unsqueeze(2).to_broadcast([128, 4, 64])
        if first_write:
            tt(dst, pv[:, :, 0:64], fb, ALU.mult, [PSN[7], "st12"], [dkey])
        else:
            tt(tmpc[:], pv[:, :, 0:64], fb, ALU.mult, [PSN[7], "st12"], ["tmpc"])
            tt(dst, dst, tmpc[:], ALU.add, [dkey, "tmpc"], [dkey], eng="pool")

    def mk_unit(mm1, M, scale, bias, v, vkey, ob, first, last, fin, pre=None):
        u = Unit()
        u.pre = pre
        u.mm1, u.M, u.scale, u.bias, u.v, u.vkey, u.ob, u.first, u.last, u.fin = mm1, M, scale, bias, v, vkey, ob, first, last, fin
        return u

    for l in range(L):
        dma(gpre_b[:], gpre_d[l:l + 1, :].rearrange("a n -> (a n)").partition_broadcast(128), [], ["gpre"])
        dma(gpost_b[:], gpost_d[l:l + 1, :].rearrange("a n -> (a n)").partition_broadcast(128), [], ["gpost"])
        for kv in range(2):
            dma(w2b[:, kv, :], w2_d[l, kv], [], ["w2b"], q="pool")
            dma(pos2b[:, kv, :], pos_d[l, kv], [], ["pos2b"], q="pool")
        gb = gen_bank()
        for kv in range(2):
            for hf in range(2):
                i = wload(W1_s[l, kv, hf], 2048, ("W1", l, kv, hf))
                wv = wbuf[i][:, 0:2048].rearrange("p (c n) -> p c n", n=128)
                col = kv * 2 + hf
                for pp in range(16):
                    mm(ps[gb][:, col:col + 1], wv[:, pp, :], pos2b[:, kv, pp:pp + 1], pp == 0, pp == 15,
                       [("wbuf", i), "pos2b"], [PSN[gb]])
        cbv = cbias[:].rearrange("p (kv g hf r) -> p kv g hf r", kv=2, g=2, hf=2)
        for kv in range(2):
            for g in range(2):
                for hf in range(2):
                    col = kv * 2 + hf
                    cp(cbv[:, kv, g, hf, :], ps[gb][:, col:col + 1].to_broadcast([128, 32]), [PSN[gb]], ["cbias"])
        gmem_b = cbuf[:, 2:4, :].rearrange("p a b -> p (a b)")
        dma(gmem_b, gmem_d[l:l + 1, :].rearrange("a n -> (a n)").partition_broadcast(128), [], ["cb23"])
        for mt in range(2):
            dma(xb[mt][:], mem_d[mt * 128:(mt + 1) * 128, :], [], [("xb", mt)])
            norm_transpose(xb[mt], ("xb", mt), gmem_b, "cb23", mt * 128)
        ik = wload(WM_s[l, 0], 2048, ("WM", l, 0))
        wk = wbuf[ik][:, 0:2048].rearrange("p (c n) -> p c n", n=256)
        for pr in range(2):
            gb = gen_bank()
            for c in range(8):
                mm(ps[gb][:, 0:256], wk[:, c, pr * 128:(pr + 1) * 128], hT[:, c, 0:256], c == 0, c == 7,
                   [("wbuf", ik), "hT"], [PSN[gb]])
            cp(KM[0:64, 2 * pr, :], ps[gb][0:64, 0:256], [PSN[gb]], ["KM"])
            cp(KM[0:64, 2 * pr + 1, :], ps[gb][64:128, 0:256], [PSN[gb]], ["KM"])
        iv = wload(WM_s[l, 1], 2048, ("WM", l, 1))
        wv_ = wbuf[iv][:, 0:2048].rearrange("p (c n) -> p c n", n=256)
        for mt in range(2):
            gb = gen_bank()
            for c in range(8):
                mm(ps[gb][:, 0:256], hT[:, c, mt * 128:(mt + 1) * 128], wv_[:, c, :], c == 0, c == 7,
                   [("wbuf", iv), "hT"], [PSN[gb]])
            cp(VM[:, mt, :, 0:64], ps[gb][:, 0:256].rearrange("p (h d) -> p h d", d=64), [PSN[gb], ("VM", "all")], ["VM"])
        mset(ubuf[:, :, 0:2], 0.0, ["ubuf0", "ubuf1"])

        src_d = x_d if l == 0 else xs_d
        dst_d = out_d if l == L - 1 else xs_d

        for j in range(NT):
            t0 = 512 * j
            slot = j % 2
            pslot = 1 - slot
            xk = lambda s, j=j: ("xrow", j * 4 + s)
            def stA(s):
                b_ = s % 2
                dma(xb[b_][:], src_d[t0 + s * 128:t0 + (s + 1) * 128, :], [xk(s)], [("xb", b_)])
                norm_A(xb[b_], ("xb", b_), gpre_b[:], "gpre", s % 2)

            def stB(s):
                norm_B(xb[s % 2], ("xb", s % 2), s * 128)
            stA(0); stA(1); stB(0); stA(2); stB(1); stA(3); stB(2); stB(3)
            for g in range(2):
                dma(KXw[64:68, g, slot * 512:(slot + 1) * 512], kexts_d[0:4, t0:t0 + 512], [], [("KXw", g, slot, "x")])

            def fm_block(b):
                i = wload(WIN_s[l, b], 8 * BW, ("WIN", l, b))
                wv = wbuf[i][:, :].rearrange("p (c n) -> p c n", n=BW)
                for grp in range(2):
                    gb = gen_bank()
                    for c in range(8):
                        mm(ps[gb][:, :], wv[:, c, grp * 128:(grp + 1) * 128], hT[:, c, :], c == 0, c == 7,
                           [("wbuf", i), "hT"], [PSN[gb]])
                    yield gb, grp

            for b in range(2):
                for gb, grp in fm_block(b):
                    for hh in range(2):
                        h = b * 4 + grp * 2 + hh
                        if hh == 0:
                            actv(QX[0:64, h, :], ps[gb][0:64, :], AF.Copy, [PSN[gb]], [("QX", h, "q")], scale=1.0 / (8.0 * SLOPES[h]))
                        else:
                            ts(QX[0:64, h, :], ps[gb][hh * 64:(hh + 1) * 64, :], 1.0 / (8.0 * SLOPES[h]), None, ALU.mult, None,
                               [PSN[gb]], [("QX", h, "q")])
            for gb, grp in fm_block(2):
                for g in range(2):
                    if grp == 0:
                        dst_, dk_ = KXs[0:64, g, t0:t0 + 512], ("KXs", g, j)
                    else:
                        dst_, dk_ = KXw[0:64, g, slot * 512:(slot + 1) * 512], ("KXw", g, slot, "k")
                    if g == 0:
                        actv(dst_, ps[gb][0:64, :], AF.Copy, [PSN[gb]], [dk_])
                    else:
                        cp(dst_, ps[gb][64:128, :], [PSN[gb]], [dk_])
            for kv in range(2):
                for g in range(2):
                    cp(KC2[0:64, kv, g, 0:16], KC2[0:64, kv, g, 512:528], [("KC2", kv, g)], [("KC2", kv, g)], eng="pool")
                    cp(KC2[64:128, kv, g, 0:15], KC2[64:128, kv, g, 512:527], [("KC2", kv, g)], [("KC2", kv, g)], eng="pool")
            for gb, grp in fm_block(3):
                kv = grp
                actv(KC2[0:64, kv, 0, 16:528], ps[gb][0:64, :], AF.Copy, [PSN[gb], "KC2"], [("KC2", kv, 0)])
                cp(KC2[64:128, kv, 0, 15:527], ps[gb][0:64, :], [PSN[gb], "KC2"], [("KC2", kv, 0)])
                cp(KC2[0:64, kv, 1, 16:528], ps[gb][64:128, :], [PSN[gb], "KC2"], [("KC2", kv, 1)])
                actv(KC2[64:128, kv, 1, 15:527], ps[gb][64:128, :], AF.Copy, [PSN[gb], "KC2"], [("KC2", kv, 1)])
            for gb, grp in fm_block(4):
                actv(QXm[0:64, grp * 2, :], ps[gb][0:64, :], AF.Copy, [PSN[gb]], [("QXm", grp * 2)], scale=0.125)
                ts(QXm[0:64, grp * 2 + 1, :], ps[gb][64:128, :], 0.125, None, ALU.mult, None, [PSN[gb]], [("QXm", grp * 2 + 1)])

            hb = gen_bank()
            for kv in range(2):
                for hf in range(2):
                    i = wload(W1_s[l, kv, hf], 2048, ("W1", l, kv, hf))
                    wv = wbuf[i][:, 0:2048].rearrange("p (c n) -> p c n", n=128)
                    for g in range(2):
                        col = ((kv * 2 + g) * 2 + hf) * 32
                        for pp in range(16):
                            rhs = KC2[:, kv, g, 2 * pp:2 * pp + 512].rearrange("p (r s) -> p r s", s=16)[:, :, 0]
                            mm(ps[hb][:, col:col + 32], wv[:, pp, :], rhs, pp == 0, pp == 15,
                               [("wbuf", i), ("KC2", kv, g)], [PSN[hb]])
            u_ = hidf[:, 0, :]
            v_ = hidf[:, 1, :]
            w_ = hidf[:, 2, :]
            tt(u_, ps[hb][:, 0:256], cbias[:], ALU.add, [PSN[hb], "cbias"], ["tmpA"])
            tt(v_, u_, u_, ALU.mult, ["tmpA"], ["tmpA"])
            ts(v_, v_, 0.044715, 1.0, ALU.mult, ALU.add, ["tmpA"], ["tmpA"])
            tt(v_, v_, u_, ALU.mult, ["tmpA"], ["tmpA"])
            actv(w_, v_, AF.Tanh, ["tmpA"], ["tmpA"], scale=0.7978845608028654)
            stt(hidb[:], w_, 1.0, u_, ALU.add, ALU.mult, ["tmpA"], ["hidb"])
            slot_lo = 32 * j
            for g in range(2):
                gb = gen_bank()
                for hf in range(2):
                    col = ((0 * 2 + g) * 2 + hf) * 32
                    mm(ps[gb][0:64, 0:32], w2b[:, 0, hf * 64:(hf + 1) * 64], hidb[:, col:col + 32], hf == 0, hf == 1,
                       ["w2b", "hidb"], [PSN[gb]])
                ts(KXc[0:64, g, slot_lo:slot_lo + 32], ps[gb][0:64, 0:32], 0.5, None, ALU.mult, None, [PSN[gb]], [("KXc", g, "k")])
                for hf in range(2):
                    col = ((1 * 2 + g) * 2 + hf) * 32
                    mm(ps[gb][0:32, 64:128], hidb[:, col:col + 32], w2b[:, 1, hf * 64:(hf + 1) * 64], hf == 0, hf == 1,
                       ["w2b", "hidb"], [PSN[gb]])
                ts(vcst[:, g, :], ps[gb][0:32, 64:128], 0.5, None, ALU.mult, None, [PSN[gb]], ["vcst"])
            pr0 = slot_lo % 128
            dma(VC[pr0:pr0 + 32, slot_lo // 128, :, 0:64], vcst[:], ["vcst", ("VC", "all")], ["VCd"])

            N = 32 * (j + 1)
            NB = 8 * (j + 1)
            nch = (NB - 1) // CH + 1
            mqv = 1 if j == 0 else 0
            for g in range(2):
                for s in range(4):
                    for r_ in range(4):
                        h = g * 4 + r_
                        gb = gen_bank()
                        mm(ps[gb][:, 0:N], QX[0:128, h, s * 128:(s + 1) * 128], KXc[0:128, g, 0:N], True, False,
                           [("QX", h, "q"), ("QX", h, "x"), ("KXc", g, "k"), ("KXc", g, "x")], [PSN[gb]])
                        mm(ps[gb][:, N - 32:N], identb[:], MQ[:, mqv, s, :], False, True, ["identb", "MQ"], [PSN[gb]])
                        eb = ebuf[r_ % 2]
                        ek = ("ebuf", r_ % 2)
                        actv(eb[:, 0:N], ps[gb][:, 0:N], AF.Exp, [PSN[gb]], [ek, "st4"], scale=SLOPES[h],
                             bias=-SLOPES[h] * 512.0 * j, accum=st[:, 4:5])
                        ts(st[:, 6:7], st[:, 4:5], 1e-30, None, ALU.max, None, ["st4"], ["st6"])
                        S.add("dve", lambda e: e.reciprocal(out=st[:, 5:6], in_=st[:, 6:7]), ["st6"], ["st5"])
                        if r_ == 0:
                            ts(imp[:, 0:N], eb[:, 0:N], st[:, 5:6], None, ALU.mult, None, [ek, "st5"], ["imp"])
                        else:
                            stt(imp[:, 0:N], eb[:, 0:N], st[:, 5:6], imp[:, 0:N], ALU.mult, ALU.add, [ek, "st5", "imp"], ["imp"])
                    mset(imp[:, N:N + 1], 0.0, ["imp"], eng="dve")
                    chb = ebuf[0]
                    tt(chb[:, 0:N], imp[:, 0:N], imp[:, 1:N + 1], ALU.add, ["imp"], [("ebuf", 0)])
                    S.add("dve", lambda e, chb=chb, NB=NB, N=N: e.tensor_reduce(
                        out=selv[:, 0:NB], in_=chb[:, 0:N].rearrange("p (n f) -> p n f", f=4), axis=AX.X, op=ALU.add),
                        [("ebuf", 0)], ["selv"])
                    lo = 8 * j + 2 * s
                    if lo + 2 < 128:
                        mset(selv[:, lo + 2:128], -1.0, ["selv"], eng="dve")
                    cp(selv[:, lo + 1:lo + 2], colab[:, 0:1], ["colab", "selv"], ["selv"])
                    mset(selv[:, lo:lo + 1], 1.0e4, ["selv"], eng="dve")
                    if lo - 1 >= 1:
                        ts(selv[:, lo - 1:lo], selv[:, lo - 1:lo], colab[:, 1:2], None, ALU.max, None, ["selv", "colab"], ["selv"])
                    mset(selv[:, 0:1], 1.0e4, ["selv"], eng="dve")
                    S.add("dve", lambda e: e.max(out=m8[:, 0:8], in_=selv[:]), ["selv"], ["m8"])
                    S.add("dve", lambda e: e.match_replace(out=selv2[:], in_to_replace=m8[:, 0:8], in_values=selv[:], imm_value=-2.0),
                          ["selv", "m8"], ["selv2"])
                    S.add("dve", lambda e: e.max(out=m8[:, 8:16], in_=selv2[:]), ["selv2"], ["m8b"])
                    for c in range(nch):
                        n0 = CH * c
                        n1 = min(128, n0 + CH)
                        ts(Zc[:, c, 68:68 + (n1 - n0)], selv[:, n0:n1], m8[:, 15:16], NEG, ALU.is_lt, ALU.mult,
                           ["selv", "m8b"], ["Zc"])
                    for c in range(nch):
                        trn(ps[7][:, c * 128:(c + 1) * 128], Zc[:, c, :], identf[:], ["Zc", "identf"], [PSN[7]])
                    cp(PX[64:128, g, 0:nch, s * 128:(s + 1) * 128],
                       ps[7][64:128, 0:nch * 128].rearrange("p (a b) -> p a b", b=128), [PSN[7], "PXinit"],
                       [("PX", g, c) for c in range(nch)])

            def tm_block(b, ncols):
                i = wload(WIN_s[l, b], 8 * BW, ("WIN", l, b))
                wv = wbuf[i][:, :].rearrange("p (c n) -> p c n", n=BW)
                for s in range(4):
                    gb = gen_bank()
                    for c in range(8):
                        mm(ps[gb][:, 0:ncols], hT[:, c, s * 128:(s + 1) * 128], wv[:, c, 0:ncols], c == 0, c == 7,
                           [("wbuf", i), "hT"], [PSN[gb]])
                    yield gb, s

            for gb, s in tm_block(9, 280):
                cp(Vs[:, 4 * j + s, :, 0:64], ps[gb][:, 0:128].rearrange("p (g d) -> p g d", d=64), [PSN[gb], ("Vs", "all")],
                   [("Vs", j)])
                cp(Vw[:, slot * 4 + s, :, 0:64], ps[gb][:, 128:256].rearrange("p (g d) -> p g d", d=64), [PSN[gb], ("Vw", "all")],
                   [("Vw", slot)])
                tt(GL2[:, s, :], ps[gb][:, 256:280], bgate_b[:, l * 24:(l + 1) * 24], ALU.add, [PSN[gb], "bgate"], ["GL2"])
            glf = GL2[:].rearrange("p a b -> p (a b)")
            actv(glf, glf, AF.Tanh, ["GL2"], ["GL2"], scale=0.5)
            ts(glf, glf, 1.0, None, ALU.add, None, ["GL2"], ["GL2"])
            for half in range(2):
                for gb, s in tm_block(10 + half, 256):
                    actv(thb[:, 0:256], ps[gb][:, 0:256], AF.Tanh, [PSN[gb]], ["tmpA"], scale=0.5)
                    stt(GN[:, s, half * 256:(half + 1) * 256], thb[:, 0:256], 1.0, ps[gb][:, 0:256], ALU.add, ALU.mult,
                        ["tmpA", PSN[gb]], ["GN"])
            for gb, s in tm_block(12, 256):
                actv(thb[:, 0:256], ps[gb][:, 0:256], AF.Tanh, [PSN[gb]], ["tmpA"], scale=0.5)
                stt(GM[:, s, :], thb[:, 0:256], 1.0, ps[gb][:, 0:256], ALU.add, ALU.mult, ["tmpA", PSN[gb]], ["GM"])

            cw = lambda cc, k: convw_t[:, l * 6 + cc * 3 + k:l * 6 + cc * 3 + k + 1]
            for gb, cc in fm_block(6):
                cp(cbuf[:, cc, :], ps[gb][:, :], [PSN[gb], "cb01"], [("cb", cc)])
            for gb, cc in fm_block(7):
                uk = "ubuf%d" % cc
                if j > 0:
                    cp(ubuf[:, cc, 0:2], ubuf[:, cc, 512:514], [uk], [uk])
                tt(ubuf[:, cc, 2:514], cbuf[:, cc, :], ps[gb][:, :], ALU.mult, [("cb", cc), PSN[gb]], [uk])
                ak = ("ca", cc)
                ts(cbuf[:, 2 + cc, :], ubuf[:, cc, 2:514], cw(cc, 2), convb_t[:, l * 2 + cc:l * 2 + cc + 1], ALU.mult, ALU.add,
                   [uk, "convw", "convb", "cb23"], [ak])
                stt(cbuf[:, 2 + cc, :], ubuf[:, cc, 1:513], cw(cc, 1), cbuf[:, 2 + cc, :], ALU.mult, ALU.add, [uk, ak, "convw"], [ak])
                stt(cbuf[:, 2 + cc, :], ubuf[:, cc, 0:512], cw(cc, 0), cbuf[:, 2 + cc, :], ALU.mult, ALU.add, [uk, ak, "convw"], [ak])
            for gb, cc in fm_block(5):
                ak = ("ca", cc)
                tt(cbuf[:, 2 + cc, :], cbuf[:, 2 + cc, :], ps[gb][:, :], ALU.mult, [ak, PSN[gb]], [ak])
            for gb, cc in fm_block(8):
                ak = ("ca", cc)
                actv(thb[:], ps[gb][:, :], AF.Tanh, [PSN[gb]], ["tmpA"], scale=0.5)
                stt(abuf[:], thb[:], 1.0, ps[gb][:, :], ALU.add, ALU.mult, ["tmpA", PSN[gb]], ["tmpA"])
                stt(yT[:, cc, :], cbuf[:, 2 + cc, :], 0.5, abuf[:], ALU.mult, ALU.mult, [ak, "tmpA"], [("yT", cc)])

            def fin_nsa(br, h, first_write):
                def f(u):
                    finalize(u, GL2[:, :, br * 8 + h], 0.25, acc[:, :, h * 64:(h + 1) * 64], ("acc", h), first_write)
                return f

            def fin_mem(h):
                def f(u):
                    finalize(u, None, 0.5, accm[:, :, h * 64:(h + 1) * 64], ("accm", h), True)
                return f

            units = []
            for h in range(8):
                g = h // 4
                ob = next_ob()
                tl = []
                if j >= 1:
                    for kr in range(4):
                        tl.append((pslot, kr, "ML"))
                for kr in range(4):
                    tl.append((slot, kr, "MC"))
                for ti, (sl_, kr, mk) in enumerate(tl):
                    mt_ = ML if mk == "ML" else MC
                    mm1 = [(KXw[0:128, g, sl_ * 512 + kr * 128:sl_ * 512 + (kr + 1) * 128], QX[0:128, h, :],
                            [("KXw", g, sl_, "k"), ("KXw", g, sl_, "x"), ("QX", h, "q"), ("QX", h, "x")]),
                           (identb[:], mt_[:, kr, :], ["identb", mk])]
                    units.append(mk_unit(mm1, 128, SLOPES[h], -SLOPES[h] * 512.0 * j, Vw[:, sl_ * 4 + kr, g, :], ("Vw", sl_),
                                         ob, ti == 0, ti == len(tl) - 1, fin_nsa(2, h, True)))
            for h in range(4):
                ob = next_ob()
                for kt in range(2):
                    mm1 = [(KM[0:128, h, kt * 128:(kt + 1) * 128], QXm[0:128, h, :], ["KM", ("QXm", h)])]
                    units.append(mk_unit(mm1, 128, 1.0, 0.0, VM[:, kt, h, :], "VM", ob, kt == 0, kt == 1, fin_mem(h)))
            nkc = (N - 1) // 128 + 1
            var = 4 if j == 0 else j % 4
            for h in range(8):
                g = h // 4
                ob = next_ob()
                for kt in range(nkc):
                    M = 128
                    mm1 = [(KXc[0:128, g, kt * 128:kt * 128 + M], QX[0:128, h, :],
                            [("KXc", g, "k"), ("KXc", g, "x"), ("QX", h, "q"), ("QX", h, "x")])]
                    if kt == nkc - 1:
                        mm1.append((identb[:], MCMP[:, var, :], ["identb", "MCMP"]))
                    units.append(mk_unit(mm1, M, SLOPES[h], -SLOPES[h] * 512.0 * j, VC[:, kt, g, :], "VCd",
                                         ob, kt == 0, kt == nkc - 1, fin_nsa(0, h, False)))
            run_units(units)

            units = []
            for h in range(8):
                g = h // 4
                ob = next_ob()
                nk = 4 * j + 4
                for kt in range(nk):
                    c = kt // 30
                    pre = None
                    if kt % 30 == 0:
                        vb = qv_i[0]
                        qv_i[0] ^= 1

                        def pre(vb=vb, g=g, c=c, h=h):
                            cp(QXv[vb][64:128, :], PX[64:128, g, c, :], [("PX", g, c), "PXinit"], [("QXv", vb)], eng="pool")
                            cp(QXv[vb][0:68, :], QX[0:68, h, :], [("QX", h, "q"), ("QX", h, "x")], [("QXv", vb)], eng="pool")
                    mm1 = [(KXs[0:128, g, kt * 128:(kt + 1) * 128], QXv[vb][0:128, :],
                            [("KXs", g, kt // 4), ("KXs", g, "x"), ("QXv", vb)])]
                    if kt >= 4 * j:
                        mm1.append((identb[:], MC[:, kt - 4 * j, :], ["identb", "MC"]))
                    units.append(mk_unit(mm1, 128, SLOPES[h], -SLOPES[h] * 512.0 * j, Vs[:, kt, g, :], ("Vs", kt // 4),
                                         ob, kt == 0, kt == nk - 1, fin_nsa(1, h, False), pre=pre))
            run_units(units)

            accf = acc[:].rearrange("p a b -> p (a b)")
            tt(accf, accf, GN[:].rearrange("p a b -> p (a b)"), ALU.mult, [("acc", h) for h in range(8)] + ["GN"], ["accg"])
            accmf = accm[:].rearrange("p a b -> p (a b)")
            tt(accmf, accmf, GM[:].rearrange("p a b -> p (a b)"), ALU.mult, [("accm", h) for h in range(4)] + ["GM"], ["accmg"])
            for s in range(4):
                for c4 in range(4):
                    trn(ps[7][:, c4 * 128:(c4 + 1) * 128], acc[:, s, c4 * 128:(c4 + 1) * 128], identf[:], ["accg", "identf"], [PSN[7]])
                cp(yT[:, 2:6, s * 128:(s + 1) * 128], ps[7][:].rearrange("p (a b) -> p a b", b=128), [PSN[7]], [("yT", 2 + s)])
            for s in range(4):
                for c2 in range(2):
                    trn(ps[7][:, c2 * 128:(c2 + 1) * 128], accm[:, s, c2 * 128:(c2 + 1) * 128], identf[:], ["accmg", "identf"], [PSN[7]])
                cp(yT[:, 6:8, s * 128:(s + 1) * 128], ps[7][:, 0:256].rearrange("p (a b) -> p a b", b=128), [PSN[7]], [("yT", 6 + s)])
            yT_all = [("yT", k) for k in range(10)]

            for nb in range(4):
                i = wload(WOUT_s[l, nb], 2048, ("WOUT", l, nb))
                wv = wbuf[i][:, 0:2048].rearrange("p (c n) -> p c n", n=256)
                for s in range(4):
                    bk = 2 * s + nb // 2
                    c0 = (nb % 2) * 256
                    for c in range(8):
                        mm(ps[bk][:, c0:c0 + 256], yT[:, c, s * 128:(s + 1) * 128], wv[:, c, :], c == 0, c == 7,
                           [("wbuf", i)] + yT_all, [PSN[bk]])
            junk = cbuf[:, 0:2, :].rearrange("p a b -> p (a b)")
            ycps = [(ycp[:], ["ycp"]), (cbuf[:, 2:4, :].rearrange("p a b -> p (a b)"), ["cb23", ("ca", 0), ("ca", 1)])]
            for s in range(2):
                dma(xb[s][:], src_d[t0 + s * 128:t0 + (s + 1) * 128, :], [xk(s)], [("xb", s)])
            for s in range(4):
                b_ = s % 2
                yc, yk = ycps[b_]
                o = 16 if b_ == 0 else 32
                S.add("act", lambda e, yc=yc, s=s: e.activation(out=yc[:, 0:512], in_=ps[2 * s][:, :], func=AF.Copy), [PSN[2 * s]], yk)
                cp(yc[:, 512:1024], ps[2 * s + 1][:, :], [PSN[2 * s + 1]], yk)
                stt(junk, yc, 1.0, yc, ALU.mult, ALU.mult, yk + [("cb", 0), ("cb", 1)], ["cb01", ("stp", b_, 0)], accum=st[:, o:o + 1])
                ts(st[:, o + 1:o + 2], st[:, o:o + 1], 1.0 / D, 1e-6, ALU.mult, ALU.add, [("stp", b_, 0)], [("stp", b_, 1)])
                tt(st[:, o + 2:o + 3], st[:, o + 1:o + 2], mhalf[:, 0:1], ALU.pow, [("stp", b_, 1), "mhalf"], [("stp", b_, 2)], eng="pool")
                stt(yc, yc, st[:, o + 2:o + 3], gpost_b[:], ALU.mult, ALU.mult, yk + [("stp", b_, 2), "gpost"], yk)
                tt(yc, yc, xb[b_][:], ALU.add, yk + [("xb", b_)], yk)
                dma(dst_d[t0 + s * 128:t0 + (s + 1) * 128, :], yc, yk, [xk(s)] if dst_d is xs_d else [("orow", j * 4 + s)], q="pool")
                if s + 2 < 4:
                    dma(xb[b_][:], src_d[t0 + (s + 2) * 128:t0 + (s + 3) * 128, :], [xk(s + 2)], [("xb", b_)])

    S.emit(ctx)
    ctx.close()
    return nc


_NC_CACHE = {}


def run(inp, T, L, n_cores=8):
    f = lambda a: np.ascontiguousarray(np.asarray(a, dtype=np.float32))
    x = f(inp["x"])
    B = x.shape[0]
    key = (T, L)
    if key not in _NC_CACHE:
        _NC_CACHE[key] = build(T, L)
    nc = _NC_CACHE[key]
    shared = host_consts(T)
    shared.update(host_weights(L, f(inp["w_in"]), f(inp["w_out"]), f(inp["cmp_w1_k"]), f(inp["cmp_w1_v"]),
                               f(inp["cmp_w2_k"]), f(inp["cmp_w2_v"]), f(inp["cmp_pos_k"]), f(inp["cmp_pos_v"]),
                               f(inp["w_mem_kv"]), f(inp["conv_w"]), f(inp["conv_b"])))
    shared["gpre"] = f(inp["pre_norm_g"])
    shared["gpost"] = f(inp["post_norm_g"])
    shared["gmem"] = f(inp["mem_norm_g"])
    shared["bgate"] = f(inp["b_gate"])
    mem = f(inp["mem"])
    in_maps = []
    for c in range(n_cores):
        b = c % B
        m = dict(shared)
        m["x"] = np.ascontiguousarray(x[b])
        m["mem"] = np.ascontiguousarray(mem[b])
        in_maps.append(m)
    res = run_bass_kernel_spmd(nc, in_maps, core_ids=list(range(n_cores)))
    out = np.stack([np.asarray(res.results[b]["out"], dtype=np.float32) for b in range(B)], axis=0)
    return out


def kernel(**inputs):
    return run(inputs, 8192, 4)
```

```python
import numpy as np
import ml_dtypes
from contextlib import ExitStack
import concourse.bass as bass
import concourse.mybir as mybir
from concourse.bass_utils import run_bass_kernel_spmd

F32 = mybir.dt.float32
BF16 = mybir.dt.bfloat16
AF = mybir.ActivationFunctionType
ALU = mybir.AluOpType
AX = mybir.AxisListType
NPBF = ml_dtypes.bfloat16

D = 1024
NEG = -1.0e6
SLOPES = [2.0 ** -(h + 1) for h in range(8)]
CH = 60


class Op:
    __slots__ = ("eng", "pos", "fn", "waits", "signal", "token", "is_dma", "lane", "lane_val", "snap")


class Sched:
    def __init__(self, nc, n_lanes=24, same_engine_sync=True):
        self.nc = nc
        self.eng = {"pe": nc.tensor, "act": nc.scalar, "dve": nc.vector, "pool": nc.gpsimd, "sp": nc.sync}
        self.ops = {e: [] for e in self.eng}
        self.lw = {}
        self.rd = {}
        self.known = {e: {} for e in self.eng}
        self.n_lanes = n_lanes
        self.lane_last = [None] * n_lanes
        self.lane_cnt = [0] * n_lanes
        self.next_lane = 0
        self.same_engine_sync = same_engine_sync
        self.nwaits = 0

    def _need(self, op, d, raw=True):
        e = op.eng
        kn = self.known[e]
        if d.is_dma:
            key = ("L", d.lane)
            val = d.lane_val
        else:
            if d.eng == e:
                if e == "pe" or not self.same_engine_sync or not raw:
                    return
            key = d.eng
            val = d.pos
        if kn.get(key, -1) >= val:
            return
        kn[key] = val
        op.waits.append(d)
        d.signal = True
        self.nwaits += 1
        if d.snap is not None:
            for k, v in d.snap:
                if kn.get(k, -1) < v:
                    kn[k] = v

    def add(self, eng, fn, reads=(), writes=(), dma=False):
        op = Op()
        op.eng = eng
        op.fn = fn
        op.waits = []
        op.signal = False
        op.token = None
        op.is_dma = dma
        op.lane = None
        op.lane_val = None
        op.pos = len(self.ops[eng])
        deps = []
        for r in reads:
            w = self.lw.get(r)
            if w is not None:
                deps.append((w, True))
        for r in writes:
            w = self.lw.get(r)
            if w is not None:
                deps.append((w, False))
            rr = self.rd.get(r)
            if rr:
                deps.extend((o, False) for o in rr.values())
        seen = set()
        for d, raw in deps:
            if raw and id(d) in seen:
                continue
            if raw:
                seen.add(id(d))
            self._need(op, d, raw)
        if dma:
            lane = self.next_lane
            self.next_lane = (self.next_lane + 1) % self.n_lanes
            prev = self.lane_last[lane]
            if prev is not None:
                self._need(op, prev)
            self.lane_cnt[lane] += 1
            op.lane = lane
            op.lane_val = self.lane_cnt[lane]
            self.lane_last[lane] = op
        kn = self.known[eng]
        op.snap = tuple((k, v) for k, v in kn.items() if not isinstance(k, tuple))
        self.ops[eng].append(op)
        for r in reads:
            dd = self.rd.setdefault(r, {})
            dd[("D", id(op)) if dma else eng] = op
        for r in writes:
            self.lw[r] = op
            self.rd[r] = {}
        return op

    def emit(self, ctx):
        nc = self.nc
        esem = {e: ctx.enter_context(nc.semaphore("s_" + e)) for e in self.eng}
        lsem = [ctx.enter_context(nc.semaphore("l_%d" % i)) for i in range(self.n_lanes)]
        for e, lst in self.ops.items():
            c = 0
            for op in lst:
                if (not op.is_dma) and op.signal:
                    c += 1
                    op.token = c
        block = ctx.enter_context(nc.Block())
        reg = {"pe": block.tensor, "act": block.scalar, "dve": block.vector, "pool": block.gpsimd, "sp": block.sync}

        def make(e):
            def body(engh):
                for op in self.ops[e]:
                    for d in op.waits:
                        if d.is_dma:
                            engh.wait_ge(lsem[d.lane], 16 * d.lane_val)
                        else:
                            engh.wait_ge(esem[d.eng], d.token)
                    ins = op.fn(engh)
                    if op.is_dma:
                        ins.then_inc(lsem[op.lane], 16)
                    elif op.signal:
                        ins.then_inc(esem[e], 1)
                if e == "sp":
                    for i in range(self.n_lanes):
                        if self.lane_cnt[i]:
                            engh.wait_ge(lsem[i], 16 * self.lane_cnt[i])
            return body

        for e in self.eng:
            reg[e](make(e))


OFF = dict(cB=0, cC=256, ch=512, cg=768, q=1024, kc=1536, vc=1664, ks=1792, vs=1920, kw=2048, vw=2176,
           gl=2304, ng=2328, mq=2840, mg=3096)
NBLK = 13
BW = 288


def _block_cols():
    r = lambda a, n: list(range(a, a + n))
    blocks = [
        r(OFF["q"], 256), r(OFF["q"] + 256, 256),
        r(OFF["ks"], 128) + r(OFF["kw"], 128),
        r(OFF["kc"], 128) + r(OFF["vc"], 128),
        r(OFF["mq"], 256),
        r(OFF["cB"], 256), r(OFF["cC"], 256), r(OFF["ch"], 256), r(OFF["cg"], 256),
        r(OFF["vs"], 128) + r(OFF["vw"], 128) + r(OFF["gl"], 24),
        r(OFF["ng"], 256), r(OFF["ng"] + 256, 256),
        r(OFF["mg"], 256),
    ]
    return blocks


def host_consts(T):
    k = np.arange(128)[:, None]
    q = np.arange(512)[None, :]
    xx = np.arange(896)[None, :] - 384
    mc = np.where(k > xx, NEG, 0.0).astype(np.float32)
    ml = np.where(k <= xx, NEG, 0.0).astype(np.float32)
    mcmp = np.zeros((128, 5, 512), np.float32)
    for v in range(4):
        for rr in range(32):
            mcmp[32 * v + rr, v, :] = np.where(16 * rr + 15 > q[0], NEG, 0.0)
        mcmp[32 * v + 32:, v, :] = NEG
    mcmp[:, 4, :] = mcmp[:, 0, :]
    mcmp[0, 4, :] = NEG
    mq = np.zeros((128, 2, 4, 32), np.float32)
    p = np.arange(128)[:, None]
    rr = np.arange(32)[None, :]
    for s in range(4):
        mq[:, 0, s, :] = np.where(16 * rr + 15 > 128 * s + p, NEG, 0.0)
    mq[:, 1] = mq[:, 0]
    mq[:, 1, :, 0] = NEG
    pos = np.arange(T)
    kexts = np.zeros((64, T), np.float32)
    kexts[0] = pos // 128
    kexts[1] = pos % 128
    kexts[2] = 1.0
    kexts[3] = 1.0
    blk = (pos // 64) % CH
    for r_ in range(CH):
        kexts[4 + r_] = (blk == r_)
    sl = np.arange(512)
    pc = 16 * sl + 15
    kextc = np.stack([pc // 128, pc % 128, np.ones(512), np.ones(512)]).astype(np.float32)
    tq = np.arange(512)
    qext = np.stack([np.full(512, 128.0), np.ones(512), -128.0 * (tq // 128), -1.0 * (tq % 128)]).astype(np.float32)
    colab = np.zeros((128, 2), np.float32)
    colab[:, 0] = np.where(np.arange(128) >= 64, 1e4, -1.0)
    colab[:, 1] = np.where(np.arange(128) < 64, 1e4, -1.0)
    bf = lambda a: np.ascontiguousarray(a).astype(NPBF)
    return dict(mc=bf(mc.reshape(128, -1)), ml=bf(ml.reshape(128, -1)), mcmp=bf(mcmp.reshape(128, -1)),
                mq=bf(mq.reshape(128, -1)), kexts=bf(kexts), kextc=bf(kextc), qext=bf(qext), colab=colab)


def host_weights(L, w_in, w_out, w1k, w1v, w2k, w2v, posk, posv, wm, convw, convb):
    blocks = _block_cols()
    w_in_p = np.zeros((L, NBLK, 128, 8, BW), np.float32)
    for b, cols in enumerate(blocks):
        sub = w_in[:, :, cols]
        w_in_p[:, b, :, :, :len(cols)] = sub.reshape(L, 8, 128, len(cols)).transpose(0, 2, 1, 3)
    w_out_p = w_out.reshape(L, 8, 128, 4, 256).transpose(0, 3, 2, 1, 4)
    w1 = np.stack([w1k, w1v], axis=1)
    w1_p = w1.reshape(L, 2, 16, 128, 2, 128).transpose(0, 1, 4, 3, 2, 5)
    w2 = np.stack([w2k, w2v], axis=1)
    w2_p = w2.reshape(L, 2, 2, 128, 64).transpose(0, 1, 3, 2, 4)
    pos = np.stack([posk, posv], axis=1)
    pos_p = pos.reshape(L, 2, 16, 2, 64).transpose(0, 1, 3, 4, 2).reshape(L, 2, 128, 16)
    wm_p = wm.reshape(L, 8, 128, 2, 256).transpose(0, 3, 2, 1, 4)
    convw_t = convw.reshape(L, 3, 2, 128).transpose(3, 0, 2, 1)
    convb_t = convb.reshape(L, 2, 128).transpose(2, 0, 1)
    c = np.ascontiguousarray
    return dict(w_in_p=c(w_in_p.reshape(L, NBLK, 128, 8 * BW)), w_out_p=c(w_out_p.reshape(L, 4, 128, 2048)),
                w1_p=c(w1_p.reshape(L, 2, 2, 128, 2048)), w2_p=c(w2_p.reshape(L, 2, 128, 128)),
                pos_p=c(pos_p), wm_p=c(wm_p.reshape(L, 2, 128, 2048)),
                convw_t=c(convw_t.reshape(128, L * 6)), convb_t=c(convb_t.reshape(128, L * 2)))


def build(T=8192, L=4, same_engine_sync=True):
    NT = T // 512
    NKT = T // 128
    nc = bass.Bass("TRN2", target_bir_lowering=False)
    dram = lambda name, shape, dt_, kind: nc.dram_tensor(name, shape, dt_, kind=kind).ap()
    EI, EO, IN = "ExternalInput", "ExternalOutput", "Internal"
    x_d = dram("x", [T, D], F32, EI)
    mem_d = dram("mem", [256, D], F32, EI)
    win_d = dram("w_in_p", [L, NBLK, 128, 8 * BW], F32, EI)
    wout_d = dram("w_out_p", [L, 4, 128, 2048], F32, EI)
    w1_d = dram("w1_p", [L, 2, 2, 128, 2048], F32, EI)
    w2_d = dram("w2_p", [L, 2, 128, 128], F32, EI)
    pos_d = dram("pos_p", [L, 2, 128, 16], F32, EI)
    wm_d = dram("wm_p", [L, 2, 128, 2048], F32, EI)
    gpre_d = dram("gpre", [L, D], F32, EI)
    gpost_d = dram("gpost", [L, D], F32, EI)
    gmem_d = dram("gmem", [L, D], F32, EI)
    convw_d = dram("convw_t", [128, L * 6], F32, EI)
    convb_d = dram("convb_t", [128, L * 2], F32, EI)
    bgate_d = dram("bgate", [L, 24], F32, EI)
    mc_d = dram("mc", [128, 896], BF16, EI)
    ml_d = dram("ml", [128, 896], BF16, EI)
    mcmp_d = dram("mcmp", [128, 2560], BF16, EI)
    mq_d = dram("mq", [128, 256], BF16, EI)
    kexts_d = dram("kexts", [64, T], BF16, EI)
    kextc_d = dram("kextc", [4, 512], BF16, EI)
    qext_d = dram("qext", [4, 512], BF16, EI)
    colab_d = dram("colab", [128, 2], F32, EI)
    out_d = dram("out", [T, D], F32, EO)
    xs_d = dram("xs", [T, D], F32, IN)
    WIN_s = dram("WIN_s", [L, NBLK, 128, 8 * BW], BF16, IN)
    WOUT_s = dram("WOUT_s", [L, 4, 128, 2048], BF16, IN)
    W1_s = dram("W1_s", [L, 2, 2, 128, 2048], BF16, IN)
    WM_s = dram("WM_s", [L, 2, 128, 2048], BF16, IN)

    ctx = ExitStack()
    S = Sched(nc, same_engine_sync=same_engine_sync)
    sb = lambda name, shape, dt_=F32: nc.alloc_sbuf_tensor(name, shape, dt_)
    KXs = sb("KXs", [128, 2, T], BF16)
    Vs = sb("Vs", [128, NKT, 2, 65], BF16)
    KXw = sb("KXw", [128, 2, 1024], BF16)
    Vw = sb("Vw", [128, 8, 2, 65], BF16)
    KXc = sb("KXc", [128, 2, 512], BF16)
    VC = sb("VC", [128, 4, 2, 65], BF16)
    KC2 = sb("KC2", [128, 2, 2, 544], BF16)
    KM = sb("KM", [128, 4, 256], BF16)
    VM = sb("VM", [128, 2, 4, 65], BF16)
    QXs = [sb("QX%d" % i, [128, 8, 512], BF16) for i in range(2)]
    QXm = sb("QXm", [128, 4, 512], BF16)
    PXs = [sb("PX%d" % i, [128, 2, 3, 512], BF16) for i in range(2)]
    QXv = [sb("QXv%d" % i, [128, 512], BF16) for i in range(2)]
    MCW = sb("MCW", [128, 896], BF16)
    MLW = sb("MLW", [128, 896], BF16)
    MCMP = sb("MCMP", [128, 5, 512], BF16)
    MQ = sb("MQ", [128, 2, 4, 32], BF16)
    identb = sb("identb", [128, 128], BF16)
    identf = sb("identf", [128, 128], F32)
    gpre_b = sb("gpre_b", [128, D], F32)
    gpost_b = sb("gpost_b", [128, D], F32)
    convw_t = sb("convw_sb", [128, L * 6], F32)
    convb_t = sb("convb_sb", [128, L * 2], F32)
    bgate_b = sb("bgate_b", [128, L * 24], F32)
    colab = sb("colab_sb", [128, 2], F32)
    mhalf = sb("mhalf", [128, 4], F32)
    cbias = sb("cbias", [128, 256], F32)
    w2b = sb("w2b", [128, 2, 128], BF16)
    pos2b = sb("pos2b", [128, 2, 16], BF16)
    xb = [sb("xb%d" % i, [128, D], F32) for i in range(2)]
    hT = sb("hT", [128, 8, 512], BF16)
    NWB = 2
    wbuf = [sb("wbuf%d" % i, [128, 8 * BW], BF16) for i in range(NWB)]
    cbuf = sb("cbuf", [128, 4, 512], F32)
    uh = sb("uh", [128, 2, 2], F32)
    tmpA = sb("tmpA", [128, 1024], F32)
    abuf = tmpA[:, 0:512]
    thb = tmpA[:, 512:1024]
    hidf = tmpA[:, 0:768].rearrange("p (a b) -> p a b", b=256)
    ycp = sb("ycp", [128, D], F32)
    yT = sb("yT", [128, 8, 512], BF16)
    GN = sb("GN", [128, 4, 512], BF16)
    GM = sb("GM", [128, 4, 256], BF16)
    GL2 = sb("GL2", [128, 4, 24], F32)
    pbuf = [sb("pbuf%d" % i, [128, 512], BF16) for i in range(4)]
    OTs = [sb("OTs%d" % i, [128, 512], F32) for i in range(2)]
    acc = sb("acc", [128, 4, 512], F32)
    accm = sb("accm", [128, 4, 256], F32)
    tmpc = sb("tmpc", [128, 4, 64], F32)
    ebuf = [sb("ebuf%d" % i, [128, 512], F32) for i in range(2)]
    imp = sb("imp", [128, 516], F32)
    selv = sb("selv", [128, 128], F32)
    selv2 = sb("selv2", [128, 128], F32)
    Zc = sb("Zc", [128, 3, 128], F32)
    m8 = sb("m8", [128, 16], F32)
    st = sb("st", [128, 40], F32)
    hidb = sb("hidb", [128, 256], BF16)
    vcst = sb("vcst", [32, 2, 64], BF16)

    ps = [nc.alloc_psum_tensor("ps%d" % i, [128, 512], F32) for i in range(8)]
    PSN = ["ps%d" % i for i in range(8)]

    def dma(out, in_, r, w, q="sp"):
        S.add(q, lambda e: e.dma_start(out=out, in_=in_), r, w, dma=True)

    def mm(out, lhsT, rhs, start, stop, r, w):
        S.add("pe", lambda e: e.matmul(out, lhsT=lhsT, rhs=rhs, start=start, stop=stop), r, w)

    def trn(out, in_, ident, r, w):
        S.add("pe", lambda e: e.transpose(out=out, in_=in_, identity=ident), r, w)

    def actv(out, in_, func, r, w, scale=1.0, bias=0.0, accum=None):
        if accum is None:
            S.add("act", lambda e: e.activation(out=out, in_=in_, func=func, bias=bias, scale=scale), r, w)
        else:
            S.add("act", lambda e: e.activation(out=out, in_=in_, func=func, bias=bias, scale=scale, accum_out=accum), r, w)

    def cp(out, in_, r, w, eng="dve"):
        S.add(eng, lambda e: e.tensor_copy(out=out, in_=in_), r, w)

    def ts(out, in0, s1, s2, op0, op1, r, w, eng="dve"):
        if op1 is None:
            S.add(eng, lambda e: e.tensor_scalar(out=out, in0=in0, scalar1=s1, scalar2=None, op0=op0), r, w)
        else:
            S.add(eng, lambda e: e.tensor_scalar(out=out, in0=in0, scalar1=s1, scalar2=s2, op0=op0, op1=op1), r, w)

    def tt(out, in0, in1, op, r, w, eng="dve"):
        S.add(eng, lambda e: e.tensor_tensor(out=out, in0=in0, in1=in1, op=op), r, w)

    def stt(out, in0, scalar, in1, op0, op1, r, w, accum=None):
        if accum is None:
            S.add("dve", lambda e: e.scalar_tensor_tensor(out=out, in0=in0, scalar=scalar, in1=in1, op0=op0, op1=op1), r, w)
        else:
            S.add("dve", lambda e: e.scalar_tensor_tensor(out=out, in0=in0, scalar=scalar, in1=in1, op0=op0, op1=op1, accum_out=accum), r, w)

    def mset(ap, val, w, eng="pool"):
        S.add(eng, lambda e: e.memset(ap, val), (), w)

    dma(MCW[:], mc_d, [], ["MC"])
    dma(MLW[:], ml_d, [], ["ML"])
    dma(MCMP[:].rearrange("p a b -> p (a b)"), mcmp_d, [], ["MCMP"])
    dma(MQ[:].rearrange("p a b c -> p (a b c)"), mq_d, [], ["MQ"])
    dma(colab[:], colab_d, [], ["colab"])
    dma(convw_t[:], convw_d, [], ["convw"])
    dma(convb_t[:], convb_d, [], ["convb"])
    dma(bgate_b[:], bgate_d.rearrange("l n -> (l n)").partition_broadcast(128), [], ["bgate"])
    for par in range(2):
        mset(QXs[par][:].rearrange("p a b -> p (a b)"), 0.0,
             [("QX", par, h, "q") for h in range(8)] + [("QX", par, h, "x") for h in range(8)])
    mset(QXm[:].rearrange("p a b -> p (a b)"), 0.0, [("QXm", h) for h in range(4)])
    mset(KM[:].rearrange("p a b -> p (a b)"), 0.0, ["KM"])
    mset(KXw[:].rearrange("p a b -> p (a b)"), 0.0, [("KXw", g, sl, t) for g in range(2) for sl in range(2) for t in ("k", "x")])
    mset(KXc[:].rearrange("p a b -> p (a b)"), 0.0, [("KXc", g, t) for g in range(2) for t in ("k", "x")])
    for g in range(2):
        dma(KXs[64:128, g, :], kexts_d, [], [("KXs", g, "x")])
        dma(KXc[64:68, g, :], kextc_d, [], [("KXc", g, "x")])
    for par in range(2):
        for h in range(8):
            dma(QXs[par][64:68, h, :], qext_d, [], [("QX", par, h, "x")])
    mset(identf[:], 0.0, ["identf"])
    S.add("pool", lambda e: e.affine_select(out=identf[:], in_=identf[:], pattern=[[-1, 128]], compare_op=ALU.not_equal,
                                            fill=1.0, base=0, channel_multiplier=1), ["identf"], ["identf"])
    cp(identb[:], identf[:], ["identf"], ["identb"])
    mset(mhalf[:], -0.5, ["mhalf"])
    mset(Vs[:].rearrange("p a b c -> p (a b c)"), 1.0, [("Vs", "all")])
    mset(Vw[:].rearrange("p a b c -> p (a b c)"), 1.0, [("Vw", "all")])
    mset(VC[:].rearrange("p a b c -> p (a b c)"), 1.0, [("VC", "all")])
    mset(VM[:].rearrange("p a b c -> p (a b c)"), 1.0, [("VM", "all")])
    mset(KC2[:].rearrange("p a b c -> p (a b c)"), 0.0, ["KC2"])
    for par in range(2):
        mset(PXs[par][:].rearrange("p a b c -> p (a b c)"), 0.0, ["PXinit"])
    mset(Zc[:].rearrange("p a b -> p (a b)"), 0.0, ["Zc"])
    mset(imp[:], 0.0, ["imp"])

    wb_i = [0]

    def next_wb():
        i = wb_i[0]
        wb_i[0] = (i + 1) % NWB
        return i

    def prep(src, dst, n, last, dst_key):
        i = next_wb()
        wv = wbuf[i][:, 0:n].rearrange("p (c n) -> p c n", n=last)
        dma(wv, src.rearrange("p (c n) -> p c n", n=last), [], [("wbuf", i)], q="pool")
        dma(dst, wbuf[i][:, 0:n], [("wbuf", i)], [dst_key])

    for l in range(L):
        for b in range(NBLK):
            prep(win_d[l, b], WIN_s[l, b], 8 * BW, BW, ("WIN", l, b))
        for b in range(4):
            prep(wout_d[l, b], WOUT_s[l, b], 2048, 256, ("WOUT", l, b))
        for kv in range(2):
            for hf in range(2):
                prep(w1_d[l, kv, hf], W1_s[l, kv, hf], 2048, 128, ("W1", l, kv, hf))
        for b in range(2):
            prep(wm_d[l, b], WM_s[l, b], 2048, 256, ("WM", l, b))

    def wload(src, n, key):
        i = next_wb()
        dma(wbuf[i][:, 0:n], src, [key], [("wbuf", i)])
        return i

    gen_i = [0]

    def gen_bank():
        i = 5 + gen_i[0]
        gen_i[0] ^= 1
        return i

    def norm_A(xt, xkey, gtile, gkey, k):
        o = 24 + 3 * k
        stt(cbuf[:, 0:2, :].rearrange("p a b -> p (a b)"), xt[:], 1.0, xt[:], ALU.mult, ALU.mult,
            [xkey], ["cb01", ("stn", k, 0)], accum=st[:, o:o + 1])
        ts(st[:, o + 1:o + 2], st[:, o:o + 1], 1.0 / D, 1e-6, ALU.mult, ALU.add, [("stn", k, 0)], [("stn", k, 1)])
        tt(st[:, o + 2:o + 3], st[:, o + 1:o + 2], mhalf[:, 0:1], ALU.pow, [("stn", k, 1), "mhalf"], [("stn", k, 2)], eng="pool")
        stt(xt[:], xt[:], st[:, o + 2:o + 3], gtile[:], ALU.mult, ALU.mult, [xkey, ("stn", k, 2), gkey], [xkey])

    def norm_B(xt, xkey, col0):
        for half in range(2):
            for c4 in range(4):
                c = half * 4 + c4
                trn(ps[7][:, c4 * 128:(c4 + 1) * 128], xt[:, c * 128:(c + 1) * 128], identf[:], [xkey, "identf"], [PSN[7]])
            S.add("act", lambda e, half=half: e.activation(out=hT[:, half * 4:half * 4 + 4, col0:col0 + 128],
                                                           in_=ps[7][:].rearrange("p (a b) -> p a b", b=128), func=AF.Copy),
                  [PSN[7]], ["hT"])

    def norm_transpose(xt, xkey, gtile, gkey, col0):
        norm_A(xt, xkey, gtile, gkey, 0)
        norm_B(xt, xkey, col0)

    def _unused(xt, xkey, col0):
        for half in range(2):
            for c4 in range(4):
                c = half * 4 + c4
                trn(ps[7][:, c4 * 128:(c4 + 1) * 128], xt[:, c * 128:(c + 1) * 128], identf[:], [xkey, "identf"], [PSN[7]])
            cp(hT[:, half * 4:half * 4 + 4, col0:col0 + 128], ps[7][:].rearrange("p (a b) -> p a b", b=128),
               [PSN[7]], ["hT"])

    class Unit:
        pass

    def run_units(units, hook=None):
        n = len(units)
        LA = 2
        pend = []
        for i in range(n + LA):
            if i < n:
                u = units[i]
                if hook is not None:
                    next(hook, None)
                if u.pre is not None:
                    u.pre()
                sbk = i % 3
                nmm = len(u.mm1)
                for k, (lh, rh, rd_) in enumerate(u.mm1):
                    mm(ps[sbk][0:u.M, :], lh, rh, k == 0, k == nmm - 1, rd_, [PSN[sbk]])
                pb = i % 4
                actv(pbuf[pb][0:u.M, :], ps[sbk][0:u.M, :], AF.Exp, [PSN[sbk]], [("pbuf", pb)], scale=u.scale, bias=u.bias)
            k2 = i - LA
            if k2 >= 0:
                u = units[k2]
                pb = k2 % 4
                mm(ps[u.ob][0:65, :], u.v, pbuf[pb][0:u.M, :], u.first, u.last, [("pbuf", pb), u.vkey], [PSN[u.ob]])
                if u.last:
                    pend.append([k2 + 3, u])
            while pend and (pend[0][0] <= i or i == n + LA - 1):
                _, u = pend.pop(0)
                u.fin(u)

    ob_i = [0]
    qv_i = [0]

    def next_ob():
        i = 3 + ob_i[0]
        ob_i[0] ^= 1
        return i

    ot_i = [0]

    def finalize(u, gate_ap, const, dst, dkey, first_write):
        k = ot_i[0]
        ot_i[0] ^= 1
        cp(OTs[k][0:65, :], ps[u.ob][0:65, :], [PSN[u.ob]], [("OTs", k)])
        for s in range(4):
            trn(ps[7][:, s * 65:(s + 1) * 65], OTs[k][0:65, s * 128:(s + 1) * 128], identf[0:65, 0:65],
                [("OTs", k), "identf"], [PSN[7]])
        pv = ps[7][:, 0:260].rearrange("p (s c) -> p s c", c=65)
        ts(st[:, 20:24], pv[:, :, 64], 1e-30, None, ALU.max, None, [PSN[7]], ["st20"])
        S.add("dve", lambda e: e.reciprocal(out=st[:, 8:12], in_=st[:, 20:24]), ["st20"], ["st8"])
        if gate_ap is None:
            ts(st[:, 12:16], st[:, 8:12], const, None, ALU.mult, None, ["st8"], ["st12"])
        else:
            stt(st[:, 12:16], st[:, 8:12], const, gate_ap, ALU.mult, ALU.mult, ["st8", "GL2"], ["st12"])
        fb = st[:, 12:16].unsqueeze(2).to_broadcast([128, 4, 64])
        if first_write:
            tt(dst, pv[:, :, 0:64], fb, ALU.mult, [PSN[7], "st12"], [dkey])
        else:
            tt(tmpc[:], pv[:, :, 0:64], fb, ALU.mult, [PSN[7], "st12"], ["tmpc"])
            tt(dst, dst, tmpc[:], ALU.add, [dkey, "tmpc"], [dkey], eng="pool")

    def mk_unit(mm1, M, scale, bias, v, vkey, ob, first, last, fin, pre=None):
        u = Unit()
        u.pre = pre
        u.mm1, u.M, u.scale, u.bias, u.v, u.vkey, u.ob, u.first, u.last, u.fin = mm1, M, scale, bias, v, vkey, ob, first, last, fin
        return u

    MCv = lambda kr: MCW[:, 384 - 128 * kr:896 - 128 * kr]
    MLv = lambda kr: MLW[:, 384 - 128 * kr:896 - 128 * kr]

    def layer_setup(l):
        dma(gpre_b[:], gpre_d[l:l + 1, :].rearrange("a n -> (a n)").partition_broadcast(128), [], ["gpre"])
        dma(gpost_b[:], gpost_d[l:l + 1, :].rearrange("a n -> (a n)").partition_broadcast(128), [], ["gpost"])
        for kv in range(2):
            dma(w2b[:, kv, :], w2_d[l, kv], [], ["w2b"], q="pool")
            dma(pos2b[:, kv, :], pos_d[l, kv], [], ["pos2b"], q="pool")
        gb = gen_bank()
        for kv in range(2):
            for hf in range(2):
                i = wload(W1_s[l, kv, hf], 2048, ("W1", l, kv, hf))
                wv = wbuf[i][:, 0:2048].rearrange("p (c n) -> p c n", n=128)
                col = kv * 2 + hf
                for pp in range(16):
                    mm(ps[gb][:, col:col + 1], wv[:, pp, :], pos2b[:, kv, pp:pp + 1], pp == 0, pp == 15,
                       [("wbuf", i), "pos2b"], [PSN[gb]])
        cbv = cbias[:].rearrange("p (kv g hf r) -> p kv g hf r", kv=2, g=2, hf=2)
        for kv in range(2):
            for g in range(2):
                for hf in range(2):
                    col = kv * 2 + hf
                    cp(cbv[:, kv, g, hf, :], ps[gb][:, col:col + 1].to_broadcast([128, 32]), [PSN[gb]], ["cbias"])
        gmem_b = cbuf[:, 2:4, :].rearrange("p a b -> p (a b)")
        dma(gmem_b, gmem_d[l:l + 1, :].rearrange("a n -> (a n)").partition_broadcast(128), [], ["cb23", ("ca", 0), ("ca", 1)])
        for mt in range(2):
            dma(xb[mt][:], mem_d[mt * 128:(mt + 1) * 128, :], [], [("xb", mt)])
            norm_transpose(xb[mt], ("xb", mt), gmem_b, "cb23", mt * 128)
        ik = wload(WM_s[l, 0], 2048, ("WM", l, 0))
        wk = wbuf[ik][:, 0:2048].rearrange("p (c n) -> p c n", n=256)
        for pr in range(2):
            gb = gen_bank()
            for c in range(8):
                mm(ps[gb][:, 0:256], wk[:, c, pr * 128:(pr + 1) * 128], hT[:, c, 0:256], c == 0, c == 7,
                   [("wbuf", ik), "hT"], [PSN[gb]])
            cp(KM[0:64, 2 * pr, :], ps[gb][0:64, 0:256], [PSN[gb]], ["KM"])
            cp(KM[0:64, 2 * pr + 1, :], ps[gb][64:128, 0:256], [PSN[gb]], ["KM"])
        iv = wload(WM_s[l, 1], 2048, ("WM", l, 1))
        wv_ = wbuf[iv][:, 0:2048].rearrange("p (c n) -> p c n", n=256)
        for mt in range(2):
            gb = gen_bank()
            for c in range(8):
                mm(ps[gb][:, 0:256], hT[:, c, mt * 128:(mt + 1) * 128], wv_[:, c, :], c == 0, c == 7,
                   [("wbuf", iv), "hT"], [PSN[gb]])
            cp(VM[:, mt, :, 0:64], ps[gb][:, 0:256].rearrange("p (h d) -> p h d", d=64), [PSN[gb], ("VM", "all")], ["VM"])
        mset(uh[:].rearrange("p a b -> p (a b)"), 0.0, ["uh"])

    def fm_block(l, b):
        i = wload(WIN_s[l, b], 8 * BW, ("WIN", l, b))
        wv = wbuf[i][:, :].rearrange("p (c n) -> p c n", n=BW)
        for grp in range(2):
            gb = gen_bank()
            for c in range(8):
                mm(ps[gb][:, :], wv[:, c, grp * 128:(grp + 1) * 128], hT[:, c, :], c == 0, c == 7,
                   [("wbuf", i), "hT"], [PSN[gb]])
            yield gb, grp

    def front_gen(l, j, par, src_d):
        t0 = 512 * j
        slot = j % 2
        QX = QXs[par]
        PX = PXs[par]
        xk = lambda s: ("xrow", j * 4 + s)

        def stA(s):
            b_ = s % 2
            dma(xb[b_][:], src_d[t0 + s * 128:t0 + (s + 1) * 128, :], [xk(s)], [("xb", b_)])
            norm_A(xb[b_], ("xb", b_), gpre_b[:], "gpre", s % 2)

        def stB(s):
            norm_B(xb[s % 2], ("xb", s % 2), s * 128)
        for f_, a_ in ((stA, 0), (stA, 1), (stB, 0), (stA, 2), (stB, 1), (stA, 3), (stB, 2), (stB, 3)):
            f_(a_)
            yield
            yield
        for g in range(2):
            dma(KXw[64:68, g, slot * 512:(slot + 1) * 512], kexts_d[0:4, t0:t0 + 512], [], [("KXw", g, slot, "x")])
        for b in range(2):
            for gb, grp in fm_block(l, b):
                yield
                for hh in range(2):
                    h = b * 4 + grp * 2 + hh
                    ts(QX[0:64, h, :], ps[gb][hh * 64:(hh + 1) * 64, :], 1.0 / (8.0 * SLOPES[h]), None, ALU.mult, None,
                       [PSN[gb]], [("QX", par, h, "q")])
                yield
        for gb, grp in fm_block(l, 2):
            yield
            for g in range(2):
                if grp == 0:
                    dst_, dk_ = KXs[0:64, g, t0:t0 + 512], ("KXs", g, j)
                else:
                    dst_, dk_ = KXw[0:64, g, slot * 512:(slot + 1) * 512], ("KXw", g, slot, "k")
                cp(dst_, ps[gb][g * 64:(g + 1) * 64, :], [PSN[gb]], [dk_])
            yield
        for kv in range(2):
            for g in range(2):
                cp(KC2[0:64, kv, g, 0:16], KC2[0:64, kv, g, 512:528], [("KC2", kv, g)], [("KC2", kv, g)], eng="pool")
                cp(KC2[64:128, kv, g, 0:15], KC2[64:128, kv, g, 512:527], [("KC2", kv, g)], [("KC2", kv, g)], eng="pool")
        for gb, grp in fm_block(l, 3):
            yield
            kv = grp
            for g in range(2):
                cp(KC2[0:64, kv, g, 16:528], ps[gb][g * 64:(g + 1) * 64, :], [PSN[gb], "KC2"], [("KC2", kv, g)])
                cp(KC2[64:128, kv, g, 15:527], ps[gb][g * 64:(g + 1) * 64, :], [PSN[gb], "KC2"], [("KC2", kv, g)])
            yield
        for gb, grp in fm_block(l, 4):
            yield
            for hh in range(2):
                h = grp * 2 + hh
                ts(QXm[0:64, h, :], ps[gb][hh * 64:(hh + 1) * 64, :], 0.125, None, ALU.mult, None, [PSN[gb]], [("QXm", h)])
            yield

        hb = gen_bank()
        for kv in range(2):
            for hf in range(2):
                i = wload(W1_s[l, kv, hf], 2048, ("W1", l, kv, hf))
                wv = wbuf[i][:, 0:2048].rearrange("p (c n) -> p c n", n=128)
                for g in range(2):
                    col = ((kv * 2 + g) * 2 + hf) * 32
                    for pp in range(16):
                        rhs = KC2[:, kv, g, 2 * pp:2 * pp + 512].rearrange("p (r s) -> p r s", s=16)[:, :, 0]
                        mm(ps[hb][:, col:col + 32], wv[:, pp, :], rhs, pp == 0, pp == 15,
                           [("wbuf", i), ("KC2", kv, g)], [PSN[hb]])
                    yield
        u_ = hidf[:, 0, :]
        v_ = hidf[:, 1, :]
        w_ = hidf[:, 2, :]
        tt(u_, ps[hb][:, 0:256], cbias[:], ALU.add, [PSN[hb], "cbias"], ["tmpA"])
        tt(v_, u_, u_, ALU.mult, ["tmpA"], ["tmpA"])
        ts(v_, v_, 0.044715, 1.0, ALU.mult, ALU.add, ["tmpA"], ["tmpA"])
        tt(v_, v_, u_, ALU.mult, ["tmpA"], ["tmpA"])
        yield
        yield
        actv(w_, v_, AF.Tanh, ["tmpA"], ["tmpA"], scale=0.7978845608028654)
        yield
        stt(hidb[:], w_, 1.0, u_, ALU.add, ALU.mult, ["tmpA"], ["hidb"])
        yield
        slot_lo = 32 * j
        for g in range(2):
            gb = gen_bank()
            for hf in range(2):
                col = ((0 * 2 + g) * 2 + hf) * 32
                mm(ps[gb][0:64, 0:32], w2b[:, 0, hf * 64:(hf + 1) * 64], hidb[:, col:col + 32], hf == 0, hf == 1,
                   ["w2b", "hidb"], [PSN[gb]])
            for hf in range(2):
                col = ((1 * 2 + g) * 2 + hf) * 32
                mm(ps[gb][0:32, 64:128], hidb[:, col:col + 32], w2b[:, 1, hf * 64:(hf + 1) * 64], hf == 0, hf == 1,
                   ["w2b", "hidb"], [PSN[gb]])
            yield
            ts(KXc[0:64, g, slot_lo:slot_lo + 32], ps[gb][0:64, 0:32], 0.5, None, ALU.mult, None, [PSN[gb]], [("KXc", g, "k")])
            ts(vcst[:, g, :], ps[gb][0:32, 64:128], 0.5, None, ALU.mult, None, [PSN[gb]], ["vcst"])
            yield
        pr0 = slot_lo % 128
        dma(VC[pr0:pr0 + 32, slot_lo // 128, :, 0:64], vcst[:], ["vcst", ("VC", "all")], ["VCd"])

        N = 32 * (j + 1)
        NB = 8 * (j + 1)
        nch = (NB - 1) // CH + 1
        mqv = 1 if j == 0 else 0
        for g in range(2):
            for s in range(4):
                for r_ in range(4):
                    h = g * 4 + r_
                    gb = gen_bank()
                    mm(ps[gb][:, 0:N], QX[0:128, h, s * 128:(s + 1) * 128], KXc[0:128, g, 0:N], True, False,
                       [("QX", par, h, "q"), ("QX", par, h, "x"), ("KXc", g, "k"), ("KXc", g, "x")], [PSN[gb]])
                    mm(ps[gb][:, N - 32:N], identb[:], MQ[:, mqv, s, :], False, True, ["identb", "MQ"], [PSN[gb]])
                    yield
                    eb = ebuf[r_ % 2]
                    ek = ("ebuf", r_ % 2)
                    actv(eb[:, 0:N], ps[gb][:, 0:N], AF.Exp, [PSN[gb]], [ek, "st4"], scale=SLOPES[h],
                         bias=-SLOPES[h] * 512.0 * j, accum=st[:, 4:5])
                    yield
                    ts(st[:, 6:7], st[:, 4:5], 1e-30, None, ALU.max, None, ["st4"], ["st6"])
                    S.add("dve", lambda e: e.reciprocal(out=st[:, 5:6], in_=st[:, 6:7]), ["st6"], ["st5"])
                    if r_ == 0:
                        ts(imp[:, 0:N], eb[:, 0:N], st[:, 5:6], None, ALU.mult, None, [ek, "st5"], ["imp"])
                    else:
                        stt(imp[:, 0:N], eb[:, 0:N], st[:, 5:6], imp[:, 0:N], ALU.mult, ALU.add, [ek, "st5", "imp"], ["imp"])
                mset(imp[:, N:N + 1], 0.0, ["imp"], eng="dve")
                chb = ebuf[0]
                tt(chb[:, 0:N], imp[:, 0:N], imp[:, 1:N + 1], ALU.add, ["imp"], [("ebuf", 0)])
                S.add("dve", lambda e, chb=chb, NB=NB, N=N: e.tensor_reduce(
                    out=selv[:, 0:NB], in_=chb[:, 0:N].rearrange("p (n f) -> p n f", f=4), axis=AX.X, op=ALU.add),
                    [("ebuf", 0)], ["selv"])
                lo = 8 * j + 2 * s
                if lo + 2 < 128:
                    mset(selv[:, lo + 2:128], -1.0, ["selv"], eng="dve")
                cp(selv[:, lo + 1:lo + 2], colab[:, 0:1], ["colab", "selv"], ["selv"])
                mset(selv[:, lo:lo + 1], 1.0e4, ["selv"], eng="dve")
                if lo - 1 >= 1:
                    ts(selv[:, lo - 1:lo], selv[:, lo - 1:lo], colab[:, 1:2], None, ALU.max, None, ["selv", "colab"], ["selv"])
                mset(selv[:, 0:1], 1.0e4, ["selv"], eng="dve")
                S.add("dve", lambda e: e.max(out=m8[:, 0:8], in_=selv[:]), ["selv"], ["m8"])
                S.add("dve", lambda e: e.match_replace(out=selv2[:], in_to_replace=m8[:, 0:8], in_values=selv[:], imm_value=-2.0),
                      ["selv", "m8"], ["selv2"])
                S.add("dve", lambda e: e.max(out=m8[:, 8:16], in_=selv2[:]), ["selv2"], ["m8b"])
                for c in range(nch):
                    n0 = CH * c
                    n1 = min(128, n0 + CH)
                    ts(Zc[:, c, 68:68 + (n1 - n0)], selv[:, n0:n1], m8[:, 15:16], NEG, ALU.is_lt, ALU.mult,
                       ["selv", "m8b"], ["Zc"])
                for _ in range(6):
                    yield
                for c in range(nch):
                    trn(ps[7][:, c * 128:(c + 1) * 128], Zc[:, c, :], identf[:], ["Zc", "identf"], [PSN[7]])
                cp(PX[64:128, g, 0:nch, s * 128:(s + 1) * 128],
                   ps[7][64:128, 0:nch * 128].rearrange("p (a b) -> p a b", b=128), [PSN[7], "PXinit"],
                   [("PX", par, g, c) for c in range(nch)])
                yield

    def front_rest(l, j):
        slot = j % 2

        def tm_block(b, ncols):
            i = wload(WIN_s[l, b], 8 * BW, ("WIN", l, b))
            wv = wbuf[i][:, :].rearrange("p (c n) -> p c n", n=BW)
            for s in range(4):
                gb = gen_bank()
                for c in range(8):
                    mm(ps[gb][:, 0:ncols], hT[:, c, s * 128:(s + 1) * 128], wv[:, c, 0:ncols], c == 0, c == 7,
                       [("wbuf", i), "hT"], [PSN[gb]])
                yield gb, s

        for gb, s in tm_block(9, 280):
            cp(Vs[:, 4 * j + s, :, 0:64], ps[gb][:, 0:128].rearrange("p (g d) -> p g d", d=64), [PSN[gb], ("Vs", "all")],
               [("Vs", j)])
            cp(Vw[:, slot * 4 + s, :, 0:64], ps[gb][:, 128:256].rearrange("p (g d) -> p g d", d=64), [PSN[gb], ("Vw", "all")],
               [("Vw", slot)])
            tt(GL2[:, s, :], ps[gb][:, 256:280], bgate_b[:, l * 24:(l + 1) * 24], ALU.add, [PSN[gb], "bgate"], ["GL2"])
        glf = GL2[:].rearrange("p a b -> p (a b)")
        actv(glf, glf, AF.Tanh, ["GL2"], ["GL2"], scale=0.5)
        ts(glf, glf, 1.0, None, ALU.add, None, ["GL2"], ["GL2"])
        for half in range(2):
            for gb, s in tm_block(10 + half, 256):
                actv(thb[:, 0:256], ps[gb][:, 0:256], AF.Tanh, [PSN[gb]], ["tmpA"], scale=0.5)
                stt(GN[:, s, half * 256:(half + 1) * 256], thb[:, 0:256], 1.0, ps[gb][:, 0:256], ALU.add, ALU.mult,
                    ["tmpA", PSN[gb]], ["GN"])
        for gb, s in tm_block(12, 256):
            actv(thb[:, 0:256], ps[gb][:, 0:256], AF.Tanh, [PSN[gb]], ["tmpA"], scale=0.5)
            stt(GM[:, s, :], thb[:, 0:256], 1.0, ps[gb][:, 0:256], ALU.add, ALU.mult, ["tmpA", PSN[gb]], ["GM"])

        cw = lambda cc, k: convw_t[:, l * 6 + cc * 3 + k:l * 6 + cc * 3 + k + 1]
        for gb, cc in fm_block(l, 6):
            cp(cbuf[:, cc, :], ps[gb][:, :], [PSN[gb], "cb01"], [("cb", cc)])
        uu = tmpA[:, 0:514]
        for gb, cc in fm_block(l, 7):
            cp(uu[:, 0:2], uh[:, cc, :], ["uh", "tmpA"], ["tmpA"])
            tt(uu[:, 2:514], cbuf[:, cc, :], ps[gb][:, :], ALU.mult, [("cb", cc), PSN[gb], "tmpA"], ["tmpA"])
            ak = ("ca", cc)
            ts(cbuf[:, 2 + cc, :], uu[:, 2:514], cw(cc, 2), convb_t[:, l * 2 + cc:l * 2 + cc + 1], ALU.mult, ALU.add,
               ["tmpA", "convw", "convb", "cb23"], [ak])
            stt(cbuf[:, 2 + cc, :], uu[:, 1:513], cw(cc, 1), cbuf[:, 2 + cc, :], ALU.mult, ALU.add, ["tmpA", ak, "convw"], [ak])
            stt(cbuf[:, 2 + cc, :], uu[:, 0:512], cw(cc, 0), cbuf[:, 2 + cc, :], ALU.mult, ALU.add, ["tmpA", ak, "convw"], [ak])
            cp(uh[:, cc, :], uu[:, 512:514], ["tmpA"], ["uh"])
        for gb, cc in fm_block(l, 5):
            ak = ("ca", cc)
            tt(cbuf[:, 2 + cc, :], cbuf[:, 2 + cc, :], ps[gb][:, :], ALU.mult, [ak, PSN[gb]], [ak])
        for gb, cc in fm_block(l, 8):
            ak = ("ca", cc)
            actv(thb[:], ps[gb][:, :], AF.Tanh, [PSN[gb]], ["tmpA"], scale=0.5)
            stt(abuf[:], thb[:], 1.0, ps[gb][:, :], ALU.add, ALU.mult, ["tmpA", PSN[gb]], ["tmpA"])
            stt(yT[:, cc, :], cbuf[:, 2 + cc, :], 0.5, abuf[:], ALU.mult, ALU.mult, [ak, "tmpA"], [("yT", cc)])

    def attention(l, j, par, hook):
        slot = j % 2
        pslot = 1 - slot
        QX = QXs[par]
        PX = PXs[par]
        N = 32 * (j + 1)
        qk = lambda h: [("QX", par, h, "q"), ("QX", par, h, "x")]

        def fin_nsa(br, h, first_write):
            def f(u):
                finalize(u, GL2[:, :, br * 8 + h], 0.25, acc[:, :, h * 64:(h + 1) * 64], ("acc", h), first_write)
            return f

        def fin_mem(h):
            def f(u):
                finalize(u, None, 0.5, accm[:, :, h * 64:(h + 1) * 64], ("accm", h), True)
            return f

        units = []
        for h in range(8):
            g = h // 4
            ob = next_ob()
            tl = []
            if j >= 1:
                for kr in range(4):
                    tl.append((pslot, kr, "ML"))
            for kr in range(4):
                tl.append((slot, kr, "MC"))
            for ti, (sl_, kr, mk) in enumerate(tl):
                mt_ = MLv(kr) if mk == "ML" else MCv(kr)
                mm1 = [(KXw[0:128, g, sl_ * 512 + kr * 128:sl_ * 512 + (kr + 1) * 128], QX[0:128, h, :],
                        [("KXw", g, sl_, "k"), ("KXw", g, sl_, "x")] + qk(h)),
                       (identb[:], mt_, ["identb", mk])]
                units.append(mk_unit(mm1, 128, SLOPES[h], -SLOPES[h] * 512.0 * j, Vw[:, sl_ * 4 + kr, g, :], ("Vw", sl_),
                                     ob, ti == 0, ti == len(tl) - 1, fin_nsa(2, h, True)))
        for h in range(4):
            ob = next_ob()
            for kt in range(2):
                mm1 = [(KM[0:128, h, kt * 128:(kt + 1) * 128], QXm[0:128, h, :], ["KM", ("QXm", h)])]
                units.append(mk_unit(mm1, 128, 1.0, 0.0, VM[:, kt, h, :], "VM", ob, kt == 0, kt == 1, fin_mem(h)))
        nkc = (N - 1) // 128 + 1
        var = 4 if j == 0 else j % 4
        for h in range(8):
            g = h // 4
            ob = next_ob()
            for kt in range(nkc):
                mm1 = [(KXc[0:128, g, kt * 128:kt * 128 + 128], QX[0:128, h, :],
                        [("KXc", g, "k"), ("KXc", g, "x")] + qk(h))]
                if kt == nkc - 1:
                    mm1.append((identb[:], MCMP[:, var, :], ["identb", "MCMP"]))
                units.append(mk_unit(mm1, 128, SLOPES[h], -SLOPES[h] * 512.0 * j, VC[:, kt, g, :], "VCd",
                                     ob, kt == 0, kt == nkc - 1, fin_nsa(0, h, False)))
        run_units(units, None)

        units = []
        for h in range(8):
            g = h // 4
            ob = next_ob()
            nk = 4 * j + 4
            for kt in range(nk):
                c = kt // 30
                pre = None
                if kt % 30 == 0:
                    vb = qv_i[0]
                    qv_i[0] ^= 1

                    def pre(vb=vb, g=g, c=c, h=h):
                        cp(QXv[vb][64:128, :], PX[64:128, g, c, :], [("PX", par, g, c), "PXinit"], [("QXv", vb)], eng="pool")
                        cp(QXv[vb][0:68, :], QX[0:68, h, :], qk(h), [("QXv", vb)], eng="pool")
                mm1 = [(KXs[0:128, g, kt * 128:(kt + 1) * 128], QXv[vb][0:128, :],
                        [("KXs", g, kt // 4), ("KXs", g, "x"), ("QXv", vb)])]
                if kt >= 4 * j:
                    mm1.append((identb[:], MCv(kt - 4 * j), ["identb", "MC"]))
                units.append(mk_unit(mm1, 128, SLOPES[h], -SLOPES[h] * 512.0 * j, Vs[:, kt, g, :], ("Vs", kt // 4),
                                     ob, kt == 0, kt == nk - 1, fin_nsa(1, h, False), pre=pre))
        run_units(units, hook)

    def back(l, j, src_d, dst_d):
        t0 = 512 * j
        xk = lambda s: ("xrow", j * 4 + s)
        accf = acc[:].rearrange("p a b -> p (a b)")
        tt(accf, accf, GN[:].rearrange("p a b -> p (a b)"), ALU.mult, [("acc", h) for h in range(8)] + ["GN"], ["accg"])
        accmf = accm[:].rearrange("p a b -> p (a b)")
        tt(accmf, accmf, GM[:].rearrange("p a b -> p (a b)"), ALU.mult, [("accm", h) for h in range(4)] + ["GM"], ["accmg"])
        for s in range(4):
            for c4 in range(4):
                trn(ps[7][:, c4 * 128:(c4 + 1) * 128], acc[:, s, c4 * 128:(c4 + 1) * 128], identf[:], ["accg", "identf"], [PSN[7]])
            cp(yT[:, 2:6, s * 128:(s + 1) * 128], ps[7][:].rearrange("p (a b) -> p a b", b=128), [PSN[7]], [("yT", 2 + s)])
        for s in range(4):
            for c2 in range(2):
                trn(ps[7][:, c2 * 128:(c2 + 1) * 128], accm[:, s, c2 * 128:(c2 + 1) * 128], identf[:], ["accmg", "identf"], [PSN[7]])
            cp(yT[:, 6:8, s * 128:(s + 1) * 128], ps[7][:, 0:256].rearrange("p (a b) -> p a b", b=128), [PSN[7]], [("yT", 6 + s)])
        yT_all = [("yT", k) for k in range(10)]
        for nb in range(4):
            i = wload(WOUT_s[l, nb], 2048, ("WOUT", l, nb))
            wv = wbuf[i][:, 0:2048].rearrange("p (c n) -> p c n", n=256)
            for s in range(4):
                bk = 2 * s + nb // 2
                c0 = (nb % 2) * 256
                for c in range(8):
                    mm(ps[bk][:, c0:c0 + 256], yT[:, c, s * 128:(s + 1) * 128], wv[:, c, :], c == 0, c == 7,
                       [("wbuf", i)] + yT_all, [PSN[bk]])
        junk = cbuf[:, 0:2, :].rearrange("p a b -> p (a b)")
        ycps = [(ycp[:], ["ycp"]), (cbuf[:, 2:4, :].rearrange("p a b -> p (a b)"), ["cb23", ("ca", 0), ("ca", 1)])]
        for s in range(2):
            dma(xb[s][:], src_d[t0 + s * 128:t0 + (s + 1) * 128, :], [xk(s)], [("xb", s)])
        for s in range(4):
            b_ = s % 2
            yc, yk = ycps[b_]
            o = 16 if b_ == 0 else 32
            S.add("act", lambda e, yc=yc, s=s: e.activation(out=yc[:, 0:512], in_=ps[2 * s][:, :], func=AF.Copy), [PSN[2 * s]], yk)
            cp(yc[:, 512:1024], ps[2 * s + 1][:, :], [PSN[2 * s + 1]], yk)
            stt(junk, yc, 1.0, yc, ALU.mult, ALU.mult, yk + [("cb", 0), ("cb", 1)], ["cb01", ("stp", b_, 0)], accum=st[:, o:o + 1])
            ts(st[:, o + 1:o + 2], st[:, o:o + 1], 1.0 / D, 1e-6, ALU.mult, ALU.add, [("stp", b_, 0)], [("stp", b_, 1)])
            tt(st[:, o + 2:o + 3], st[:, o + 1:o + 2], mhalf[:, 0:1], ALU.pow, [("stp", b_, 1), "mhalf"], [("stp", b_, 2)], eng="pool")
            stt(yc, yc, st[:, o + 2:o + 3], gpost_b[:], ALU.mult, ALU.mult, yk + [("stp", b_, 2), "gpost"], yk)
            tt(yc, yc, xb[b_][:], ALU.add, yk + [("xb", b_)], yk)
            dma(dst_d[t0 + s * 128:t0 + (s + 1) * 128, :], yc, yk, [xk(s)] if dst_d is xs_d else [("orow", j * 4 + s)], q="pool")
            if s + 2 < 4:
                dma(xb[b_][:], src_d[t0 + (s + 2) * 128:t0 + (s + 3) * 128, :], [xk(s + 2)], [("xb", b_)])

    def exhaust(gen):
        for _ in gen:
            pass

    for l in range(L):
        layer_setup(l)
        src_d = x_d if l == 0 else xs_d
        dst_d = out_d if l == L - 1 else xs_d
        exhaust(front_gen(l, 0, 0, src_d))
        front_rest(l, 0)
        for j in range(NT):
            par = j % 2
            nxt = front_gen(l, j + 1, 1 - par, src_d) if j + 1 < NT else None
            attention(l, j, par, nxt)
            if nxt is not None:
                exhaust(nxt)
            back(l, j, src_d, dst_d)
            if j + 1 < NT:
                front_rest(l, j + 1)

    print("sbuf bytes remaining/partition:", nc.sbuf_bytes_remaining() if callable(nc.sbuf_bytes_remaining) else nc.sbuf_bytes_remaining,
          " ops:", {e: len(v) for e, v in S.ops.items()}, " waits:", S.nwaits)
    S.emit(ctx)
    ctx.close()
    return nc


_NC_CACHE = {}


def run(inp, T, L, n_cores=8):
    f = lambda a: np.ascontiguousarray(np.asarray(a, dtype=np.float32))
    x = f(inp["x"])
    B = x.shape[0]
    key = (T, L)
    if key not in _NC_CACHE:
        _NC_CACHE[key] = build(T, L)
    nc = _NC_CACHE[key]
    shared = host_consts(T)
    shared.update(host_weights(L, f(inp["w_in"]), f(inp["w_out"]), f(inp["cmp_w1_k"]), f(inp["cmp_w1_v"]),
                               f(inp["cmp_w2_k"]), f(inp["cmp_w2_v"]), f(inp["cmp_pos_k"]), f(inp["cmp_pos_v"]),
                               f(inp["w_mem_kv"]), f(inp["conv_w"]), f(inp["conv_b"])))
    shared["gpre"] = f(inp["pre_norm_g"])
    shared["gpost"] = f(inp["post_norm_g"])
    shared["gmem"] = f(inp["mem_norm_g"])
    shared["bgate"] = f(inp["b_gate"])
    mem = f(inp["mem"])
    in_maps = []
    for c in range(n_cores):
        b = c % B
        m = dict(shared)
        m["x"] = np.ascontiguousarray(x[b])
        m["mem"] = np.ascontiguousarray(mem[b])
        in_maps.append(m)
    res = run_bass_kernel_spmd(nc, in_maps, core_ids=list(range(n_cores)))
    out = np.stack([np.asarray(res.results[b]["out"], dtype=np.float32) for b in range(B)], axis=0)
    return out


def kernel(**inputs):
    return run(inputs, 8192, 4)
```

```python
import numpy as np
import ml_dtypes
from contextlib import ExitStack
import concourse.bass as bass
import concourse.mybir as mybir
from concourse.bass_utils import run_bass_kernel_spmd

F32 = mybir.dt.float32
BF16 = mybir.dt.bfloat16
AF = mybir.ActivationFunctionType
ALU = mybir.AluOpType
AX = mybir.AxisListType
NPBF = ml_dtypes.bfloat16

D = 1024
NEG = -1.0e6
SLOPES = [2.0 ** -(h + 1) for h in range(8)]
CH = 60


class Op:
    __slots__ = ("eng", "pos", "fn", "waits", "signal", "token", "is_dma", "lane", "lane_val", "snap")


class Sched:
    def __init__(self, nc, n_lanes=24, same_engine_sync=True):
        self.nc = nc
        self.eng = {"pe": nc.tensor, "act": nc.scalar, "dve": nc.vector, "pool": nc.gpsimd, "sp": nc.sync}
        self.ops = {e: [] for e in self.eng}
        self.lw = {}
        self.rd = {}
        self.known = {e: {} for e in self.eng}
        self.n_lanes = n_lanes
        self.lane_last = [None] * n_lanes
        self.lane_cnt = [0] * n_lanes
        self.next_lane = 0
        self.same_engine_sync = same_engine_sync
        self.nwaits = 0

    def _need(self, op, d, raw=True):
        e = op.eng
        kn = self.known[e]
        if d.is_dma:
            key = ("L", d.lane)
            val = d.lane_val
        else:
            if d.eng == e:
                if e == "pe" or not self.same_engine_sync or not raw:
                    return
            key = d.eng
            val = d.pos
        if kn.get(key, -1) >= val:
            return
        kn[key] = val
        op.waits.append(d)
        d.signal = True
        self.nwaits += 1
        if d.snap is not None:
            for k, v in d.snap:
                if kn.get(k, -1) < v:
                    kn[k] = v

    def add(self, eng, fn, reads=(), writes=(), dma=False):
        op = Op()
        op.eng = eng
        op.fn = fn
        op.waits = []
        op.signal = False
        op.token = None
        op.is_dma = dma
        op.lane = None
        op.lane_val = None
        op.pos = len(self.ops[eng])
        deps = []
        for r in reads:
            w = self.lw.get(r)
            if w is not None:
                deps.append((w, True))
        for r in writes:
            w = self.lw.get(r)
            if w is not None:
                deps.append((w, False))
            rr = self.rd.get(r)
            if rr:
                deps.extend((o, False) for o in rr.values())
        seen = set()
        for d, raw in deps:
            if raw and id(d) in seen:
                continue
            if raw:
                seen.add(id(d))
            self._need(op, d, raw)
        if dma:
            lane = self.next_lane
            self.next_lane = (self.next_lane + 1) % self.n_lanes
            prev = self.lane_last[lane]
            if prev is not None:
                self._need(op, prev)
            self.lane_cnt[lane] += 1
            op.lane = lane
            op.lane_val = self.lane_cnt[lane]
            self.lane_last[lane] = op
        kn = self.known[eng]
        op.snap = tuple((k, v) for k, v in kn.items() if not isinstance(k, tuple))
        self.ops[eng].append(op)
        for r in reads:
            dd = self.rd.setdefault(r, {})
            dd[("D", id(op)) if dma else eng] = op
        for r in writes:
            self.lw[r] = op
            self.rd[r] = {}
        return op

    def emit(self, ctx):
        nc = self.nc
        esem = {e: ctx.enter_context(nc.semaphore("s_" + e)) for e in self.eng}
        lsem = [ctx.enter_context(nc.semaphore("l_%d" % i)) for i in range(self.n_lanes)]
        for e, lst in self.ops.items():
            c = 0
            for op in lst:
                if (not op.is_dma) and op.signal:
                    c += 1
                    op.token = c
        block = ctx.enter_context(nc.Block())
        reg = {"pe": block.tensor, "act": block.scalar, "dve": block.vector, "pool": block.gpsimd, "sp": block.sync}

        def make(e):
            def body(engh):
                for op in self.ops[e]:
                    for d in op.waits:
                        if d.is_dma:
                            engh.wait_ge(lsem[d.lane], 16 * d.lane_val)
                        else:
                            engh.wait_ge(esem[d.eng], d.token)
                    ins = op.fn(engh)
                    if op.is_dma:
                        ins.then_inc(lsem[op.lane], 16)
                    elif op.signal:
                        ins.then_inc(esem[e], 1)
                if e == "sp":
                    for i in range(self.n_lanes):
                        if self.lane_cnt[i]:
                            engh.wait_ge(lsem[i], 16 * self.lane_cnt[i])
            return body

        for e in self.eng:
            reg[e](make(e))


OFF = dict(cB=0, cC=256, ch=512, cg=768, q=1024, kc=1536, vc=1664, ks=1792, vs=1920, kw=2048, vw=2176,
           gl=2304, ng=2328, mq=2840, mg=3096)
NBLK = 13
BW = 288


def _block_cols():
    r = lambda a, n: list(range(a, a + n))
    blocks = [
        r(OFF["q"], 256), r(OFF["q"] + 256, 256),
        r(OFF["ks"], 128) + r(OFF["kw"], 128),
        r(OFF["kc"], 128) + r(OFF["vc"], 128),
        r(OFF["mq"], 256),
        r(OFF["cB"], 256), r(OFF["cC"], 256), r(OFF["ch"], 256), r(OFF["cg"], 256),
        r(OFF["vs"], 128) + r(OFF["vw"], 128) + r(OFF["gl"], 24),
        r(OFF["ng"], 256), r(OFF["ng"] + 256, 256),
        r(OFF["mg"], 256),
    ]
    return blocks


def host_consts(T):
    k = np.arange(128)[:, None]
    q = np.arange(512)[None, :]
    xx = np.arange(896)[None, :] - 384
    mc = np.where(k > xx, NEG, 0.0).astype(np.float32)
    ml = np.where(k <= xx, NEG, 0.0).astype(np.float32)
    mcmp = np.zeros((128, 5, 512), np.float32)
    for v in range(4):
        for rr in range(32):
            mcmp[32 * v + rr, v, :] = np.where(16 * rr + 15 > q[0], NEG, 0.0)
        mcmp[32 * v + 32:, v, :] = NEG
    mcmp[:, 4, :] = mcmp[:, 0, :]
    mcmp[0, 4, :] = NEG
    mq = np.zeros((128, 2, 4, 32), np.float32)
    p = np.arange(128)[:, None]
    rr = np.arange(32)[None, :]
    for s in range(4):
        mq[:, 0, s, :] = np.where(16 * rr + 15 > 128 * s + p, NEG, 0.0)
    mq[:, 1] = mq[:, 0]
    mq[:, 1, :, 0] = NEG
    pos = np.arange(T)
    kexts = np.zeros((64, T), np.float32)
    kexts[0] = pos // 128
    kexts[1] = pos % 128
    kexts[2] = 1.0
    kexts[3] = 1.0
    blk = (pos // 64) % CH
    for r_ in range(CH):
        kexts[4 + r_] = (blk == r_)
    sl = np.arange(512)
    pc = 16 * sl + 15
    kextc = np.stack([pc // 128, pc % 128, np.ones(512), np.ones(512)]).astype(np.float32)
    tq = np.arange(512)
    qext = np.stack([np.full(512, 128.0), np.ones(512), -128.0 * (tq // 128), -1.0 * (tq % 128)]).astype(np.float32)
    colab = np.zeros((128, 2), np.float32)
    colab[:, 0] = np.where(np.arange(128) >= 64, 1e4, -1.0)
    colab[:, 1] = np.where(np.arange(128) < 64, 1e4, -1.0)
    bf = lambda a: np.ascontiguousarray(a).astype(NPBF)
    return dict(mc=bf(mc.reshape(128, -1)), ml=bf(ml.reshape(128, -1)), mcmp=bf(mcmp.reshape(128, -1)),
                mq=bf(mq.reshape(128, -1)), kexts=bf(kexts), kextc=bf(kextc), qext=bf(qext), colab=colab)


def host_weights(L, w_in, w_out, w1k, w1v, w2k, w2v, posk, posv, wm, convw, convb):
    blocks = _block_cols()
    w_in_p = np.zeros((L, NBLK, 128, 8, BW), np.float32)
    for b, cols in enumerate(blocks):
        sub = w_in[:, :, cols]
        w_in_p[:, b, :, :, :len(cols)] = sub.reshape(L, 8, 128, len(cols)).transpose(0, 2, 1, 3)
    w_out_p = w_out.reshape(L, 8, 128, 4, 256).transpose(0, 3, 2, 1, 4)
    w1 = np.stack([w1k, w1v], axis=1)
    w1_p = w1.reshape(L, 2, 16, 128, 2, 128).transpose(0, 1, 4, 3, 2, 5)
    w2 = np.stack([w2k, w2v], axis=1)
    w2_p = w2.reshape(L, 2, 2, 128, 64).transpose(0, 1, 3, 2, 4)
    pos = np.stack([posk, posv], axis=1)
    pos_p = pos.reshape(L, 2, 16, 2, 64).transpose(0, 1, 3, 4, 2).reshape(L, 2, 128, 16)
    wm_p = wm.reshape(L, 8, 128, 2, 256).transpose(0, 3, 2, 1, 4)
    convw_t = convw.reshape(L, 3, 2, 128).transpose(3, 0, 2, 1)
    convb_t = convb.reshape(L, 2, 128).transpose(2, 0, 1)
    c = np.ascontiguousarray
    return dict(w_in_p=c(w_in_p.reshape(L, NBLK, 128, 8 * BW)), w_out_p=c(w_out_p.reshape(L, 4, 128, 2048)),
                w1_p=c(w1_p.reshape(L, 2, 2, 128, 2048)), w2_p=c(w2_p.reshape(L, 2, 128, 128)),
                pos_p=c(pos_p), wm_p=c(wm_p.reshape(L, 2, 128, 2048)),
                convw_t=c(convw_t.reshape(128, L * 6)), convb_t=c(convb_t.reshape(128, L * 2)))


def build(T=8192, L=4, same_engine_sync=True):
    NT = T // 512
    NKT = T // 128
    nc = bass.Bass("TRN2", target_bir_lowering=False)
    dram = lambda name, shape, dt_, kind: nc.dram_tensor(name, shape, dt_, kind=kind).ap()
    EI, EO, IN = "ExternalInput", "ExternalOutput", "Internal"
    x_d = dram("x", [T, D], F32, EI)
    mem_d = dram("mem", [256, D], F32, EI)
    win_d = dram("w_in_p", [L, NBLK, 128, 8 * BW], F32, EI)
    wout_d = dram("w_out_p", [L, 4, 128, 2048], F32, EI)
    w1_d = dram("w1_p", [L, 2, 2, 128, 2048], F32, EI)
    w2_d = dram("w2_p", [L, 2, 128, 128], F32, EI)
    pos_d = dram("pos_p", [L, 2, 128, 16], F32, EI)
    wm_d = dram("wm_p", [L, 2, 128, 2048], F32, EI)
    gpre_d = dram("gpre", [L, D], F32, EI)
    gpost_d = dram("gpost", [L, D], F32, EI)
    gmem_d = dram("gmem", [L, D], F32, EI)
    convw_d = dram("convw_t", [128, L * 6], F32, EI)
    convb_d = dram("convb_t", [128, L * 2], F32, EI)
    bgate_d = dram("bgate", [L, 24], F32, EI)
    mc_d = dram("mc", [128, 896], BF16, EI)
    ml_d = dram("ml", [128, 896], BF16, EI)
    mcmp_d = dram("mcmp", [128, 2560], BF16, EI)
    mq_d = dram("mq", [128, 256], BF16, EI)
    kexts_d = dram("kexts", [64, T], BF16, EI)
    kextc_d = dram("kextc", [4, 512], BF16, EI)
    qext_d = dram("qext", [4, 512], BF16, EI)
    colab_d = dram("colab", [128, 2], F32, EI)
    out_d = dram("out", [T, D], F32, EO)
    xs_d = dram("xs", [T, D], F32, IN)
    WIN_s = dram("WIN_s", [L, NBLK, 128, 8 * BW], BF16, IN)
    WOUT_s = dram("WOUT_s", [L, 4, 128, 2048], BF16, IN)
    W1_s = dram("W1_s", [L, 2, 2, 128, 2048], BF16, IN)
    WM_s = dram("WM_s", [L, 2, 128, 2048], BF16, IN)

    ctx = ExitStack()
    S = Sched(nc, same_engine_sync=same_engine_sync)
    sb = lambda name, shape, dt_=F32: nc.alloc_sbuf_tensor(name, shape, dt_)
    KXs = sb("KXs", [128, 2, T], BF16)
    Vs = sb("Vs", [128, NKT, 2, 65], BF16)
    KXw = sb("KXw", [128, 2, 1024], BF16)
    Vw = sb("Vw", [128, 8, 2, 65], BF16)
    KXc = sb("KXc", [128, 2, 512], BF16)
    VC = sb("VC", [128, 4, 2, 65], BF16)
    KC2 = sb("KC2", [128, 2, 2, 544], BF16)
    KM = sb("KM", [128, 4, 256], BF16)
    VM = sb("VM", [128, 2, 4, 65], BF16)
    QXs = [sb("QX%d" % i, [128, 8, 512], BF16) for i in range(2)]
    QXm = sb("QXm", [128, 4, 512], BF16)
    PXs = [sb("PX%d" % i, [128, 2, 3, 512], BF16) for i in range(2)]
    QXv = [sb("QXv%d" % i, [128, 512], BF16) for i in range(2)]
    MCW = sb("MCW", [128, 896], BF16)
    MLW = sb("MLW", [128, 896], BF16)
    MCMP = sb("MCMP", [128, 5, 512], BF16)
    MQ = sb("MQ", [128, 2, 4, 32], BF16)
    identb = sb("identb", [128, 128], BF16)
    identf = sb("identf", [128, 128], F32)
    gpre_b = sb("gpre_b", [128, D], F32)
    gpost_b = sb("gpost_b", [128, D], F32)
    convw_t = sb("convw_sb", [128, L * 6], F32)
    convb_t = sb("convb_sb", [128, L * 2], F32)
    bgate_b = sb("bgate_b", [128, L * 24], F32)
    colab = sb("colab_sb", [128, 2], F32)
    mhalf = sb("mhalf", [128, 4], F32)
    cbias = sb("cbias", [128, 256], F32)
    w2b = sb("w2b", [128, 2, 128], BF16)
    pos2b = sb("pos2b", [128, 2, 16], BF16)
    xb = [sb("xb%d" % i, [128, D], F32) for i in range(2)]
    hT = sb("hT", [128, 8, 512], BF16)
    NWB = 2
    wbuf = [sb("wbuf%d" % i, [128, 8 * BW], BF16) for i in range(NWB)]
    cbuf = sb("cbuf", [128, 4, 512], F32)
    uh = sb("uh", [128, 2, 2], F32)
    tmpA = sb("tmpA", [128, 1024], F32)
    abuf = tmpA[:, 0:512]
    thb = tmpA[:, 512:1024]
    hidf = tmpA[:, 0:768].rearrange("p (a b) -> p a b", b=256)
    ycp = sb("ycp", [128, D], F32)
    yT = sb("yT", [128, 8, 512], BF16)
    GN = sb("GN", [128, 4, 512], BF16)
    GM = sb("GM", [128, 4, 256], BF16)
    GL2 = sb("GL2", [128, 4, 24], F32)
    pbuf = [sb("pbuf%d" % i, [128, 512], BF16) for i in range(4)]
    OTs = [sb("OTs%d" % i, [128, 512], F32) for i in range(2)]
    acc = sb("acc", [128, 4, 512], F32)
    accm = sb("accm", [128, 4, 256], F32)
    tmpc = sb("tmpc", [128, 4, 64], F32)
    ebuf = [sb("ebuf%d" % i, [128, 512], F32) for i in range(2)]
    imp = sb("imp", [128, 516], F32)
    selv = sb("selv", [128, 128], F32)
    selv2 = sb("selv2", [128, 128], F32)
    Zc = sb("Zc", [128, 3, 128], F32)
    m8 = sb("m8", [128, 16], F32)
    st = sb("st", [128, 40], F32)
    hidb = sb("hidb", [128, 256], BF16)
    vcst = sb("vcst", [32, 2, 64], BF16)

    ps = [nc.alloc_psum_tensor("ps%d" % i, [128, 512], F32) for i in range(8)]
    PSN = ["ps%d" % i for i in range(8)]

    def dma(out, in_, r, w, q="sp"):
        S.add(q, lambda e: e.dma_start(out=out, in_=in_), r, w, dma=True)

    def mm(out, lhsT, rhs, start, stop, r, w):
        S.add("pe", lambda e: e.matmul(out, lhsT=lhsT, rhs=rhs, start=start, stop=stop), r, w)

    def trn(out, in_, ident, r, w):
        S.add("pe", lambda e: e.transpose(out=out, in_=in_, identity=ident), r, w)

    def actv(out, in_, func, r, w, scale=1.0, bias=0.0, accum=None):
        if accum is None:
            S.add("act", lambda e: e.activation(out=out, in_=in_, func=func, bias=bias, scale=scale), r, w)
        else:
            S.add("act", lambda e: e.activation(out=out, in_=in_, func=func, bias=bias, scale=scale, accum_out=accum), r, w)

    def cp(out, in_, r, w, eng="dve"):
        S.add(eng, lambda e: e.tensor_copy(out=out, in_=in_), r, w)

    def ts(out, in0, s1, s2, op0, op1, r, w, eng="dve"):
        if op1 is None:
            S.add(eng, lambda e: e.tensor_scalar(out=out, in0=in0, scalar1=s1, scalar2=None, op0=op0), r, w)
        else:
            S.add(eng, lambda e: e.tensor_scalar(out=out, in0=in0, scalar1=s1, scalar2=s2, op0=op0, op1=op1), r, w)

    def tt(out, in0, in1, op, r, w, eng="dve"):
        S.add(eng, lambda e: e.tensor_tensor(out=out, in0=in0, in1=in1, op=op), r, w)

    def stt(out, in0, scalar, in1, op0, op1, r, w, accum=None):
        if accum is None:
            S.add("dve", lambda e: e.scalar_tensor_tensor(out=out, in0=in0, scalar=scalar, in1=in1, op0=op0, op1=op1), r, w)
        else:
            S.add("dve", lambda e: e.scalar_tensor_tensor(out=out, in0=in0, scalar=scalar, in1=in1, op0=op0, op1=op1, accum_out=accum), r, w)

    def mset(ap, val, w, eng="pool"):
        S.add(eng, lambda e: e.memset(ap, val), (), w)

    dma(MCW[:], mc_d, [], ["MC"])
    dma(MLW[:], ml_d, [], ["ML"])
    dma(MCMP[:].rearrange("p a b -> p (a b)"), mcmp_d, [], ["MCMP"])
    dma(MQ[:].rearrange("p a b c -> p (a b c)"), mq_d, [], ["MQ"])
    dma(colab[:], colab_d, [], ["colab"])
    dma(convw_t[:], convw_d, [], ["convw"])
    dma(convb_t[:], convb_d, [], ["convb"])
    dma(bgate_b[:], bgate_d.rearrange("l n -> (l n)").partition_broadcast(128), [], ["bgate"])
    for par in range(2):
        mset(QXs[par][:].rearrange("p a b -> p (a b)"), 0.0,
             [("QX", par, h, "q") for h in range(8)] + [("QX", par, h, "x") for h in range(8)])
    mset(QXm[:].rearrange("p a b -> p (a b)"), 0.0, [("QXm", h) for h in range(4)])
    mset(KM[:].rearrange("p a b -> p (a b)"), 0.0, ["KM"])
    mset(KXw[:].rearrange("p a b -> p (a b)"), 0.0, [("KXw", g, sl, t) for g in range(2) for sl in range(2) for t in ("k", "x")])
    mset(KXc[:].rearrange("p a b -> p (a b)"), 0.0, [("KXc", g, t) for g in range(2) for t in ("k", "x")])
    for g in range(2):
        dma(KXs[64:128, g, :], kexts_d, [], [("KXs", g, "x")])
        dma(KXc[64:68, g, :], kextc_d, [], [("KXc", g, "x")])
    for par in range(2):
        for h in range(8):
            dma(QXs[par][64:68, h, :], qext_d, [], [("QX", par, h, "x")])
    mset(identf[:], 0.0, ["identf"])
    S.add("pool", lambda e: e.affine_select(out=identf[:], in_=identf[:], pattern=[[-1, 128]], compare_op=ALU.not_equal,
                                            fill=1.0, base=0, channel_multiplier=1), ["identf"], ["identf"])
    cp(identb[:], identf[:], ["identf"], ["identb"])
    mset(mhalf[:], -0.5, ["mhalf"])
    mset(Vs[:].rearrange("p a b c -> p (a b c)"), 1.0, [("Vs", "all")])
    mset(Vw[:].rearrange("p a b c -> p (a b c)"), 1.0, [("Vw", "all")])
    mset(VC[:].rearrange("p a b c -> p (a b c)"), 1.0, [("VC", "all")])
    mset(VM[:].rearrange("p a b c -> p (a b c)"), 1.0, [("VM", "all")])
    mset(KC2[:].rearrange("p a b c -> p (a b c)"), 0.0, ["KC2"])
    for par in range(2):
        mset(PXs[par][:].rearrange("p a b c -> p (a b c)"), 0.0, ["PXinit"])
    mset(Zc[:].rearrange("p a b -> p (a b)"), 0.0, ["Zc"])
    mset(imp[:], 0.0, ["imp"])

    wb_i = [0]

    def next_wb():
        i = wb_i[0]
        wb_i[0] = (i + 1) % NWB
        return i

    def prep(src, dst, n, last, dst_key):
        i = next_wb()
        wv = wbuf[i][:, 0:n].rearrange("p (c n) -> p c n", n=last)
        dma(wv, src.rearrange("p (c n) -> p c n", n=last), [], [("wbuf", i)], q="pool")
        dma(dst, wbuf[i][:, 0:n], [("wbuf", i)], [dst_key])

    for l in range(L):
        for b in range(NBLK):
            prep(win_d[l, b], WIN_s[l, b], 8 * BW, BW, ("WIN", l, b))
        for b in range(4):
            prep(wout_d[l, b], WOUT_s[l, b], 2048, 256, ("WOUT", l, b))
        for kv in range(2):
            for hf in range(2):
                prep(w1_d[l, kv, hf], W1_s[l, kv, hf], 2048, 128, ("W1", l, kv, hf))
        for b in range(2):
            prep(wm_d[l, b], WM_s[l, b], 2048, 256, ("WM", l, b))

    def wload(src, n, key):
        i = next_wb()
        dma(wbuf[i][:, 0:n], src, [key], [("wbuf", i)])
        return i

    gen_i = [0]

    def gen_bank():
        i = 5 + gen_i[0]
        gen_i[0] ^= 1
        return i

    def norm_A(xt, xkey, gtile, gkey, k):
        o = 24 + 3 * k
        stt(cbuf[:, 0:2, :].rearrange("p a b -> p (a b)"), xt[:], 1.0, xt[:], ALU.mult, ALU.mult,
            [xkey], ["cb01", ("stn", k, 0)], accum=st[:, o:o + 1])
        ts(st[:, o + 1:o + 2], st[:, o:o + 1], 1.0 / D, 1e-6, ALU.mult, ALU.add, [("stn", k, 0)], [("stn", k, 1)])
        tt(st[:, o + 2:o + 3], st[:, o + 1:o + 2], mhalf[:, 0:1], ALU.pow, [("stn", k, 1), "mhalf"], [("stn", k, 2)], eng="pool")
        stt(xt[:], xt[:], st[:, o + 2:o + 3], gtile[:], ALU.mult, ALU.mult, [xkey, ("stn", k, 2), gkey], [xkey])

    def norm_B(xt, xkey, col0):
        for half in range(2):
            for c4 in range(4):
                c = half * 4 + c4
                trn(ps[7][:, c4 * 128:(c4 + 1) * 128], xt[:, c * 128:(c + 1) * 128], identf[:], [xkey, "identf"], [PSN[7]])
            cp(hT[:, half * 4:half * 4 + 4, col0:col0 + 128], ps[7][:].rearrange("p (a b) -> p a b", b=128), [PSN[7]], ["hT"])

    def norm_transpose(xt, xkey, gtile, gkey, col0):
        norm_A(xt, xkey, gtile, gkey, 0)
        norm_B(xt, xkey, col0)

    def _unused(xt, xkey, col0):
        for half in range(2):
            for c4 in range(4):
                c = half * 4 + c4
                trn(ps[7][:, c4 * 128:(c4 + 1) * 128], xt[:, c * 128:(c + 1) * 128], identf[:], [xkey, "identf"], [PSN[7]])
            cp(hT[:, half * 4:half * 4 + 4, col0:col0 + 128], ps[7][:].rearrange("p (a b) -> p a b", b=128),
               [PSN[7]], ["hT"])

    class Unit:
        pass

    def run_units(units, hook=None):
        n = len(units)
        LA = 2
        pend = []
        for i in range(n + LA):
            if i < n:
                u = units[i]
                if hook is not None:
                    next(hook, None)
                if u.pre is not None:
                    u.pre()
                sbk = i % 3
                nmm = len(u.mm1)
                for k, (lh, rh, rd_) in enumerate(u.mm1):
                    mm(ps[sbk][0:u.M, :], lh, rh, k == 0, k == nmm - 1, rd_, [PSN[sbk]])
                pb = i % 4
                actv(pbuf[pb][0:u.M, :], ps[sbk][0:u.M, :], AF.Exp, [PSN[sbk]], [("pbuf", pb)], scale=u.scale, bias=u.bias)
            k2 = i - LA
            if k2 >= 0:
                u = units[k2]
                pb = k2 % 4
                mm(ps[u.ob][0:65, :], u.v, pbuf[pb][0:u.M, :], u.first, u.last, [("pbuf", pb), u.vkey], [PSN[u.ob]])
                if u.last:
                    pend.append([k2 + 3, u])
            while pend and (pend[0][0] <= i or i == n + LA - 1):
                _, u = pend.pop(0)
                u.fin(u)

    ob_i = [0]
    qv_i = [0]

    def next_ob():
        i = 3 + ob_i[0]
        ob_i[0] ^= 1
        return i

    ot_i = [0]

    def finalize(u, gate_ap, const, dst, dkey, first_write):
        k = ot_i[0]
        ot_i[0] ^= 1
        cp(OTs[k][0:65, :], ps[u.ob][0:65, :], [PSN[u.ob]], [("OTs", k)])
        for s in range(4):
            trn(ps[7][:, s * 65:(s + 1) * 65], OTs[k][0:65, s * 128:(s + 1) * 128], identf[0:65, 0:65],
                [("OTs", k), "identf"], [PSN[7]])
        pv = ps[7][:, 0:260].rearrange("p (s c) -> p s c", c=65)
        ts(st[:, 20:24], pv[:, :, 64], 1e-30, None, ALU.max, None, [PSN[7]], ["st20"])
        S.add("dve", lambda e: e.reciprocal(out=st[:, 8:12], in_=st[:, 20:24]), ["st20"], ["st8"])
        if gate_ap is None:
            ts(st[:, 12:16], st[:, 8:12], const, None, ALU.mult, None, ["st8"], ["st12"])
        else:
            stt(st[:, 12:16], st[:, 8:12], const, gate_ap, ALU.mult, ALU.mult, ["st8", "GL2"], ["st12"])
        fb = st[:, 12:16].unsqueeze(2).to_broadcast([128, 4, 64])
        if first_write:
            tt(dst, pv[:, :, 0:64], fb, ALU.mult, [PSN[7], "st12"], [dkey])
        else:
            tt(tmpc[:], pv[:, :, 0:64], fb, ALU.mult, [PSN[7], "st12"], ["tmpc"])
            tt(dst, dst, tmpc[:], ALU.add, [dkey, "tmpc"], [dkey], eng="pool")

    def mk_unit(mm1, M, scale, bias, v, vkey, ob, first, last, fin, pre=None):
        u = Unit()
        u.pre = pre
        u.mm1, u.M, u.scale, u.bias, u.v, u.vkey, u.ob, u.first, u.last, u.fin = mm1, M, scale, bias, v, vkey, ob, first, last, fin
        return u

    MCv = lambda kr: MCW[:, 384 - 128 * kr:896 - 128 * kr]
    MLv = lambda kr: MLW[:, 384 - 128 * kr:896 - 128 * kr]

    def layer_setup(l):
        dma(gpre_b[:], gpre_d[l:l + 1, :].rearrange("a n -> (a n)").partition_broadcast(128), [], ["gpre"])
        dma(gpost_b[:], gpost_d[l:l + 1, :].rearrange("a n -> (a n)").partition_broadcast(128), [], ["gpost"])
        for kv in range(2):
            dma(w2b[:, kv, :], w2_d[l, kv], [], ["w2b"], q="pool")
            dma(pos2b[:, kv, :], pos_d[l, kv], [], ["pos2b"], q="pool")
        gb = gen_bank()
        for kv in range(2):
            for hf in range(2):
                i = wload(W1_s[l, kv, hf], 2048, ("W1", l, kv, hf))
                wv = wbuf[i][:, 0:2048].rearrange("p (c n) -> p c n", n=128)
                col = kv * 2 + hf
                for pp in range(16):
                    mm(ps[gb][:, col:col + 1], wv[:, pp, :], pos2b[:, kv, pp:pp + 1], pp == 0, pp == 15,
                       [("wbuf", i), "pos2b"], [PSN[gb]])
        cbv = cbias[:].rearrange("p (kv g hf r) -> p kv g hf r", kv=2, g=2, hf=2)
        for kv in range(2):
            for g in range(2):
                for hf in range(2):
                    col = kv * 2 + hf
                    cp(cbv[:, kv, g, hf, :], ps[gb][:, col:col + 1].to_broadcast([128, 32]), [PSN[gb]], ["cbias"])
        gmem_b = cbuf[:, 2:4, :].rearrange("p a b -> p (a b)")
        dma(gmem_b, gmem_d[l:l + 1, :].rearrange("a n -> (a n)").partition_broadcast(128), [], ["cb23", ("ca", 0), ("ca", 1)])
        for mt in range(2):
            dma(xb[mt][:], mem_d[mt * 128:(mt + 1) * 128, :], [], [("xb", mt)])
            norm_transpose(xb[mt], ("xb", mt), gmem_b, "cb23", mt * 128)
        ik = wload(WM_s[l, 0], 2048, ("WM", l, 0))
        wk = wbuf[ik][:, 0:2048].rearrange("p (c n) -> p c n", n=256)
        for pr in range(2):
            gb = gen_bank()
            for c in range(8):
                mm(ps[gb][:, 0:256], wk[:, c, pr * 128:(pr + 1) * 128], hT[:, c, 0:256], c == 0, c == 7,
                   [("wbuf", ik), "hT"], [PSN[gb]])
            cp(KM[0:64, 2 * pr, :], ps[gb][0:64, 0:256], [PSN[gb]], ["KM"])
            cp(KM[0:64, 2 * pr + 1, :], ps[gb][64:128, 0:256], [PSN[gb]], ["KM"])
        iv = wload(WM_s[l, 1], 2048, ("WM", l, 1))
        wv_ = wbuf[iv][:, 0:2048].rearrange("p (c n) -> p c n", n=256)
        for mt in range(2):
            gb = gen_bank()
            for c in range(8):
                mm(ps[gb][:, 0:256], hT[:, c, mt * 128:(mt + 1) * 128], wv_[:, c, :], c == 0, c == 7,
                   [("wbuf", iv), "hT"], [PSN[gb]])
            cp(VM[:, mt, :, 0:64], ps[gb][:, 0:256].rearrange("p (h d) -> p h d", d=64), [PSN[gb], ("VM", "all")], ["VM"])
        mset(uh[:].rearrange("p a b -> p (a b)"), 0.0, ["uh"])

    def fm_block(l, b):
        i = wload(WIN_s[l, b], 8 * BW, ("WIN", l, b))
        wv = wbuf[i][:, :].rearrange("p (c n) -> p c n", n=BW)
        for grp in range(2):
            gb = gen_bank()
            for c in range(8):
                mm(ps[gb][:, :], wv[:, c, grp * 128:(grp + 1) * 128], hT[:, c, :], c == 0, c == 7,
                   [("wbuf", i), "hT"], [PSN[gb]])
            yield gb, grp

    def front_gen(l, j, par, src_d):
        t0 = 512 * j
        slot = j % 2
        QX = QXs[par]
        PX = PXs[par]
        xk = lambda s: ("xrow", j * 4 + s)

        def stA(s):
            b_ = s % 2
            dma(xb[b_][:], src_d[t0 + s * 128:t0 + (s + 1) * 128, :], [xk(s)], [("xb", b_)])
            norm_A(xb[b_], ("xb", b_), gpre_b[:], "gpre", s % 2)

        def stB(s):
            norm_B(xb[s % 2], ("xb", s % 2), s * 128)
        for f_, a_ in ((stA, 0), (stA, 1), (stB, 0), (stA, 2), (stB, 1), (stA, 3), (stB, 2), (stB, 3)):
            f_(a_)
            yield
            yield
        for g in range(2):
            dma(KXw[64:68, g, slot * 512:(slot + 1) * 512], kexts_d[0:4, t0:t0 + 512], [], [("KXw", g, slot, "x")])
        for b in range(2):
            for gb, grp in fm_block(l, b):
                yield
                for hh in range(2):
                    h = b * 4 + grp * 2 + hh
                    ts(QX[0:64, h, :], ps[gb][hh * 64:(hh + 1) * 64, :], 1.0 / (8.0 * SLOPES[h]), None, ALU.mult, None,
                       [PSN[gb]], [("QX", par, h, "q")])
                yield
        for gb, grp in fm_block(l, 2):
            yield
            for g in range(2):
                if grp == 0:
                    dst_, dk_ = KXs[0:64, g, t0:t0 + 512], ("KXs", g, j)
                else:
                    dst_, dk_ = KXw[0:64, g, slot * 512:(slot + 1) * 512], ("KXw", g, slot, "k")
                cp(dst_, ps[gb][g * 64:(g + 1) * 64, :], [PSN[gb]], [dk_])
            yield
        for kv in range(2):
            for g in range(2):
                cp(KC2[0:64, kv, g, 0:16], KC2[0:64, kv, g, 512:528], [("KC2", kv, g)], [("KC2", kv, g)], eng="pool")
                cp(KC2[64:128, kv, g, 0:15], KC2[64:128, kv, g, 512:527], [("KC2", kv, g)], [("KC2", kv, g)], eng="pool")
        for gb, grp in fm_block(l, 3):
            yield
            kv = grp
            for g in range(2):
                cp(KC2[0:64, kv, g, 16:528], ps[gb][g * 64:(g + 1) * 64, :], [PSN[gb], "KC2"], [("KC2", kv, g)])
                cp(KC2[64:128, kv, g, 15:527], ps[gb][g * 64:(g + 1) * 64, :], [PSN[gb], "KC2"], [("KC2", kv, g)])
            yield
        for gb, grp in fm_block(l, 4):
            yield
            for hh in range(2):
                h = grp * 2 + hh
                ts(QXm[0:64, h, :], ps[gb][hh * 64:(hh + 1) * 64, :], 0.125, None, ALU.mult, None, [PSN[gb]], [("QXm", h)])
            yield

        hb = gen_bank()
        for kv in range(2):
            for hf in range(2):
                i = wload(W1_s[l, kv, hf], 2048, ("W1", l, kv, hf))
                wv = wbuf[i][:, 0:2048].rearrange("p (c n) -> p c n", n=128)
                for g in range(2):
                    col = ((kv * 2 + g) * 2 + hf) * 32
                    for pp in range(16):
                        rhs = KC2[:, kv, g, 2 * pp:2 * pp + 512].rearrange("p (r s) -> p r s", s=16)[:, :, 0]
                        mm(ps[hb][:, col:col + 32], wv[:, pp, :], rhs, pp == 0, pp == 15,
                           [("wbuf", i), ("KC2", kv, g)], [PSN[hb]])
                    yield
        u_ = hidf[:, 0, :]
        v_ = hidf[:, 1, :]
        w_ = hidf[:, 2, :]
        tt(u_, ps[hb][:, 0:256], cbias[:], ALU.add, [PSN[hb], "cbias"], ["tmpA"])
        tt(v_, u_, u_, ALU.mult, ["tmpA"], ["tmpA"])
        ts(v_, v_, 0.044715, 1.0, ALU.mult, ALU.add, ["tmpA"], ["tmpA"])
        tt(v_, v_, u_, ALU.mult, ["tmpA"], ["tmpA"])
        yield
        yield
        actv(w_, v_, AF.Tanh, ["tmpA"], ["tmpA"], scale=0.7978845608028654)
        yield
        stt(hidb[:], w_, 1.0, u_, ALU.add, ALU.mult, ["tmpA"], ["hidb"])
        yield
        slot_lo = 32 * j
        for g in range(2):
            gb = gen_bank()
            for hf in range(2):
                col = ((0 * 2 + g) * 2 + hf) * 32
                mm(ps[gb][0:64, 0:32], w2b[:, 0, hf * 64:(hf + 1) * 64], hidb[:, col:col + 32], hf == 0, hf == 1,
                   ["w2b", "hidb"], [PSN[gb]])
            for hf in range(2):
                col = ((1 * 2 + g) * 2 + hf) * 32
                mm(ps[gb][0:32, 64:128], hidb[:, col:col + 32], w2b[:, 1, hf * 64:(hf + 1) * 64], hf == 0, hf == 1,
                   ["w2b", "hidb"], [PSN[gb]])
            yield
            ts(KXc[0:64, g, slot_lo:slot_lo + 32], ps[gb][0:64, 0:32], 0.5, None, ALU.mult, None, [PSN[gb]], [("KXc", g, "k")])
            ts(vcst[:, g, :], ps[gb][0:32, 64:128], 0.5, None, ALU.mult, None, [PSN[gb]], ["vcst"])
            yield
        pr0 = slot_lo % 128
        dma(VC[pr0:pr0 + 32, slot_lo // 128, :, 0:64], vcst[:], ["vcst", ("VC", "all")], ["VCd"])

        N = 32 * (j + 1)
        NB = 8 * (j + 1)
        nch = (NB - 1) // CH + 1
        mqv = 1 if j == 0 else 0
        for g in range(2):
            for s in range(4):
                for r_ in range(4):
                    h = g * 4 + r_
                    gb = gen_bank()
                    mm(ps[gb][:, 0:N], QX[0:128, h, s * 128:(s + 1) * 128], KXc[0:128, g, 0:N], True, False,
                       [("QX", par, h, "q"), ("QX", par, h, "x"), ("KXc", g, "k"), ("KXc", g, "x")], [PSN[gb]])
                    mm(ps[gb][:, N - 32:N], identb[:], MQ[:, mqv, s, :], False, True, ["identb", "MQ"], [PSN[gb]])
                    yield
                    eb = ebuf[r_ % 2]
                    ek = ("ebuf", r_ % 2)
                    actv(eb[:, 0:N], ps[gb][:, 0:N], AF.Exp, [PSN[gb]], [ek, "st4"], scale=SLOPES[h],
                         bias=-SLOPES[h] * 512.0 * j, accum=st[:, 4:5])
                    yield
                    ts(st[:, 6:7], st[:, 4:5], 1e-30, None, ALU.max, None, ["st4"], ["st6"])
                    S.add("dve", lambda e: e.reciprocal(out=st[:, 5:6], in_=st[:, 6:7]), ["st6"], ["st5"])
                    if r_ == 0:
                        ts(imp[:, 0:N], eb[:, 0:N], st[:, 5:6], None, ALU.mult, None, [ek, "st5"], ["imp"])
                    else:
                        stt(imp[:, 0:N], eb[:, 0:N], st[:, 5:6], imp[:, 0:N], ALU.mult, ALU.add, [ek, "st5", "imp"], ["imp"])
                mset(imp[:, N:N + 1], 0.0, ["imp"], eng="dve")
                chb = ebuf[0]
                tt(chb[:, 0:N], imp[:, 0:N], imp[:, 1:N + 1], ALU.add, ["imp"], [("ebuf", 0)])
                S.add("dve", lambda e, chb=chb, NB=NB, N=N: e.tensor_reduce(
                    out=selv[:, 0:NB], in_=chb[:, 0:N].rearrange("p (n f) -> p n f", f=4), axis=AX.X, op=ALU.add),
                    [("ebuf", 0)], ["selv"])
                lo = 8 * j + 2 * s
                if lo + 2 < 128:
                    mset(selv[:, lo + 2:128], -1.0, ["selv"], eng="dve")
                cp(selv[:, lo + 1:lo + 2], colab[:, 0:1], ["colab", "selv"], ["selv"])
                mset(selv[:, lo:lo + 1], 1.0e4, ["selv"], eng="dve")
                if lo - 1 >= 1:
                    ts(selv[:, lo - 1:lo], selv[:, lo - 1:lo], colab[:, 1:2], None, ALU.max, None, ["selv", "colab"], ["selv"])
                mset(selv[:, 0:1], 1.0e4, ["selv"], eng="dve")
                S.add("dve", lambda e: e.max(out=m8[:, 0:8], in_=selv[:]), ["selv"], ["m8"])
                S.add("dve", lambda e: e.match_replace(out=selv2[:], in_to_replace=m8[:, 0:8], in_values=selv[:], imm_value=-2.0),
                      ["selv", "m8"], ["selv2"])
                S.add("dve", lambda e: e.max(out=m8[:, 8:16], in_=selv2[:]), ["selv2"], ["m8b"])
                for c in range(nch):
                    n0 = CH * c
                    n1 = min(128, n0 + CH)
                    ts(Zc[:, c, 68:68 + (n1 - n0)], selv[:, n0:n1], m8[:, 15:16], NEG, ALU.is_lt, ALU.mult,
                       ["selv", "m8b"], ["Zc"])
                for _ in range(12):
                    yield
                for c in range(nch):
                    trn(ps[7][:, c * 128:(c + 1) * 128], Zc[:, c, :], identf[:], ["Zc", "identf"], [PSN[7]])
                cp(PX[64:128, g, 0:nch, s * 128:(s + 1) * 128],
                   ps[7][64:128, 0:nch * 128].rearrange("p (a b) -> p a b", b=128), [PSN[7], "PXinit"],
                   [("PX", par, g, c) for c in range(nch)])
                yield

    def front_rest(l, j):
        slot = j % 2

        def tm_block(b, ncols):
            i = wload(WIN_s[l, b], 8 * BW, ("WIN", l, b))
            wv = wbuf[i][:, :].rearrange("p (c n) -> p c n", n=BW)
            for s in range(4):
                gb = gen_bank()
                for c in range(8):
                    mm(ps[gb][:, 0:ncols], hT[:, c, s * 128:(s + 1) * 128], wv[:, c, 0:ncols], c == 0, c == 7,
                       [("wbuf", i), "hT"], [PSN[gb]])
                yield gb, s

        for gb, s in tm_block(9, 280):
            cp(Vs[:, 4 * j + s, :, 0:64], ps[gb][:, 0:128].rearrange("p (g d) -> p g d", d=64), [PSN[gb], ("Vs", "all")],
               [("Vs", j)])
            cp(Vw[:, slot * 4 + s, :, 0:64], ps[gb][:, 128:256].rearrange("p (g d) -> p g d", d=64), [PSN[gb], ("Vw", "all")],
               [("Vw", slot)])
            tt(GL2[:, s, :], ps[gb][:, 256:280], bgate_b[:, l * 24:(l + 1) * 24], ALU.add, [PSN[gb], "bgate"], ["GL2"])
        glf = GL2[:].rearrange("p a b -> p (a b)")
        actv(glf, glf, AF.Tanh, ["GL2"], ["GL2"], scale=0.5)
        ts(glf, glf, 1.0, None, ALU.add, None, ["GL2"], ["GL2"])
        for half in range(2):
            for gb, s in tm_block(10 + half, 256):
                actv(thb[:, 0:256], ps[gb][:, 0:256], AF.Tanh, [PSN[gb]], ["tmpA"], scale=0.5)
                stt(GN[:, s, half * 256:(half + 1) * 256], thb[:, 0:256], 1.0, ps[gb][:, 0:256], ALU.add, ALU.mult,
                    ["tmpA", PSN[gb]], ["GN"])
        for gb, s in tm_block(12, 256):
            actv(thb[:, 0:256], ps[gb][:, 0:256], AF.Tanh, [PSN[gb]], ["tmpA"], scale=0.5)
            stt(GM[:, s, :], thb[:, 0:256], 1.0, ps[gb][:, 0:256], ALU.add, ALU.mult, ["tmpA", PSN[gb]], ["GM"])

        cw = lambda cc, k: convw_t[:, l * 6 + cc * 3 + k:l * 6 + cc * 3 + k + 1]
        for gb, cc in fm_block(l, 6):
            cp(cbuf[:, cc, :], ps[gb][:, :], [PSN[gb], "cb01"], [("cb", cc)])
        uu = tmpA[:, 0:514]
        for gb, cc in fm_block(l, 7):
            cp(uu[:, 0:2], uh[:, cc, :], ["uh", "tmpA"], ["tmpA"])
            tt(uu[:, 2:514], cbuf[:, cc, :], ps[gb][:, :], ALU.mult, [("cb", cc), PSN[gb], "tmpA"], ["tmpA"])
            ak = ("ca", cc)
            ts(cbuf[:, 2 + cc, :], uu[:, 2:514], cw(cc, 2), convb_t[:, l * 2 + cc:l * 2 + cc + 1], ALU.mult, ALU.add,
               ["tmpA", "convw", "convb", "cb23"], [ak])
            stt(cbuf[:, 2 + cc, :], uu[:, 1:513], cw(cc, 1), cbuf[:, 2 + cc, :], ALU.mult, ALU.add, ["tmpA", ak, "convw"], [ak])
            stt(cbuf[:, 2 + cc, :], uu[:, 0:512], cw(cc, 0), cbuf[:, 2 + cc, :], ALU.mult, ALU.add, ["tmpA", ak, "convw"], [ak])
            cp(uh[:, cc, :], uu[:, 512:514], ["tmpA"], ["uh"])
        for gb, cc in fm_block(l, 5):
            ak = ("ca", cc)
            tt(cbuf[:, 2 + cc, :], cbuf[:, 2 + cc, :], ps[gb][:, :], ALU.mult, [ak, PSN[gb]], [ak])
        for gb, cc in fm_block(l, 8):
            ak = ("ca", cc)
            actv(thb[:], ps[gb][:, :], AF.Tanh, [PSN[gb]], ["tmpA"], scale=0.5)
            stt(abuf[:], thb[:], 1.0, ps[gb][:, :], ALU.add, ALU.mult, ["tmpA", PSN[gb]], ["tmpA"])
            stt(yT[:, cc, :], cbuf[:, 2 + cc, :], 0.5, abuf[:], ALU.mult, ALU.mult, [ak, "tmpA"], [("yT", cc)])

    def attention(l, j, par, hook):
        slot = j % 2
        pslot = 1 - slot
        QX = QXs[par]
        PX = PXs[par]
        N = 32 * (j + 1)
        qk = lambda h: [("QX", par, h, "q"), ("QX", par, h, "x")]

        def fin_nsa(br, h, first_write):
            def f(u):
                finalize(u, GL2[:, :, br * 8 + h], 0.25, acc[:, :, h * 64:(h + 1) * 64], ("acc", h), first_write)
            return f

        def fin_mem(h):
            def f(u):
                finalize(u, None, 0.5, accm[:, :, h * 64:(h + 1) * 64], ("accm", h), True)
            return f

        units = []
        for h in range(8):
            g = h // 4
            ob = next_ob()
            tl = []
            if j >= 1:
                for kr in range(4):
                    tl.append((pslot, kr, "ML"))
            for kr in range(4):
                tl.append((slot, kr, "MC"))
            for ti, (sl_, kr, mk) in enumerate(tl):
                mt_ = MLv(kr) if mk == "ML" else MCv(kr)
                mm1 = [(KXw[0:128, g, sl_ * 512 + kr * 128:sl_ * 512 + (kr + 1) * 128], QX[0:128, h, :],
                        [("KXw", g, sl_, "k"), ("KXw", g, sl_, "x")] + qk(h)),
                       (identb[:], mt_, ["identb", mk])]
                units.append(mk_unit(mm1, 128, SLOPES[h], -SLOPES[h] * 512.0 * j, Vw[:, sl_ * 4 + kr, g, :], ("Vw", sl_),
                                     ob, ti == 0, ti == len(tl) - 1, fin_nsa(2, h, True)))
        for h in range(4):
            ob = next_ob()
            for kt in range(2):
                mm1 = [(KM[0:128, h, kt * 128:(kt + 1) * 128], QXm[0:128, h, :], ["KM", ("QXm", h)])]
                units.append(mk_unit(mm1, 128, 1.0, 0.0, VM[:, kt, h, :], "VM", ob, kt == 0, kt == 1, fin_mem(h)))
        nkc = (N - 1) // 128 + 1
        var = 4 if j == 0 else j % 4
        for h in range(8):
            g = h // 4
            ob = next_ob()
            for kt in range(nkc):
                mm1 = [(KXc[0:128, g, kt * 128:kt * 128 + 128], QX[0:128, h, :],
                        [("KXc", g, "k"), ("KXc", g, "x")] + qk(h))]
                if kt == nkc - 1:
                    mm1.append((identb[:], MCMP[:, var, :], ["identb", "MCMP"]))
                units.append(mk_unit(mm1, 128, SLOPES[h], -SLOPES[h] * 512.0 * j, VC[:, kt, g, :], "VCd",
                                     ob, kt == 0, kt == nkc - 1, fin_nsa(0, h, False)))
        run_units(units, None)

        units = []
        for h in range(8):
            g = h // 4
            ob = next_ob()
            nk = 4 * j + 4
            for kt in range(nk):
                c = kt // 30
                pre = None
                if kt % 30 == 0:
                    vb = qv_i[0]
                    qv_i[0] ^= 1

                    def pre(vb=vb, g=g, c=c, h=h):
                        cp(QXv[vb][64:128, :], PX[64:128, g, c, :], [("PX", par, g, c), "PXinit"], [("QXv", vb)], eng="pool")
                        cp(QXv[vb][0:68, :], QX[0:68, h, :], qk(h), [("QXv", vb)], eng="pool")
                mm1 = [(KXs[0:128, g, kt * 128:(kt + 1) * 128], QXv[vb][0:128, :],
                        [("KXs", g, kt // 4), ("KXs", g, "x"), ("QXv", vb)])]
                if kt >= 4 * j:
                    mm1.append((identb[:], MCv(kt - 4 * j), ["identb", "MC"]))
                units.append(mk_unit(mm1, 128, SLOPES[h], -SLOPES[h] * 512.0 * j, Vs[:, kt, g, :], ("Vs", kt // 4),
                                     ob, kt == 0, kt == nk - 1, fin_nsa(1, h, False), pre=pre))
        run_units(units, hook)

    def back(l, j, src_d, dst_d):
        t0 = 512 * j
        xk = lambda s: ("xrow", j * 4 + s)
        accf = acc[:].rearrange("p a b -> p (a b)")
        tt(accf, accf, GN[:].rearrange("p a b -> p (a b)"), ALU.mult, [("acc", h) for h in range(8)] + ["GN"], ["accg"])
        accmf = accm[:].rearrange("p a b -> p (a b)")
        tt(accmf, accmf, GM[:].rearrange("p a b -> p (a b)"), ALU.mult, [("accm", h) for h in range(4)] + ["GM"], ["accmg"])
        for s in range(4):
            for c4 in range(4):
                trn(ps[7][:, c4 * 128:(c4 + 1) * 128], acc[:, s, c4 * 128:(c4 + 1) * 128], identf[:], ["accg", "identf"], [PSN[7]])
            S.add("act", lambda e, s=s: e.activation(out=yT[:, 2:6, s * 128:(s + 1) * 128],
                                                     in_=ps[7][:].rearrange("p (a b) -> p a b", b=128), func=AF.Copy),
                  [PSN[7]], [("yT", 2 + s)])
        for s in range(4):
            for c2 in range(2):
                trn(ps[7][:, c2 * 128:(c2 + 1) * 128], accm[:, s, c2 * 128:(c2 + 1) * 128], identf[:], ["accmg", "identf"], [PSN[7]])
            cp(yT[:, 6:8, s * 128:(s + 1) * 128], ps[7][:, 0:256].rearrange("p (a b) -> p a b", b=128), [PSN[7]], [("yT", 6 + s)])
        yT_all = [("yT", k) for k in range(10)]
        for nb in range(4):
            i = wload(WOUT_s[l, nb], 2048, ("WOUT", l, nb))
            wv = wbuf[i][:, 0:2048].rearrange("p (c n) -> p c n", n=256)
            for s in range(4):
                bk = 2 * s + nb // 2
                c0 = (nb % 2) * 256
                for c in range(8):
                    mm(ps[bk][:, c0:c0 + 256], yT[:, c, s * 128:(s + 1) * 128], wv[:, c, :], c == 0, c == 7,
                       [("wbuf", i)] + yT_all, [PSN[bk]])
        junk = cbuf[:, 0:2, :].rearrange("p a b -> p (a b)")
        ycps = [(ycp[:], ["ycp"]), (cbuf[:, 2:4, :].rearrange("p a b -> p (a b)"), ["cb23", ("ca", 0), ("ca", 1)])]
        for s in range(2):
            dma(xb[s][:], src_d[t0 + s * 128:t0 + (s + 1) * 128, :], [xk(s)], [("xb", s)])
        for s in range(4):
            b_ = s % 2
            yc, yk = ycps[b_]
            o = 16 if b_ == 0 else 32
            S.add("act", lambda e, yc=yc, s=s: e.activation(out=yc[:, 0:512], in_=ps[2 * s][:, :], func=AF.Copy), [PSN[2 * s]], yk)
            cp(yc[:, 512:1024], ps[2 * s + 1][:, :], [PSN[2 * s + 1]], yk)
            stt(junk, yc, 1.0, yc, ALU.mult, ALU.mult, yk + [("cb", 0), ("cb", 1)], ["cb01", ("stp", b_, 0)], accum=st[:, o:o + 1])
            ts(st[:, o + 1:o + 2], st[:, o:o + 1], 1.0 / D, 1e-6, ALU.mult, ALU.add, [("stp", b_, 0)], [("stp", b_, 1)])
            tt(st[:, o + 2:o + 3], st[:, o + 1:o + 2], mhalf[:, 0:1], ALU.pow, [("stp", b_, 1), "mhalf"], [("stp", b_, 2)], eng="pool")
            stt(yc, yc, st[:, o + 2:o + 3], gpost_b[:], ALU.mult, ALU.mult, yk + [("stp", b_, 2), "gpost"], yk)
            tt(yc, yc, xb[b_][:], ALU.add, yk + [("xb", b_)], yk)
            dma(dst_d[t0 + s * 128:t0 + (s + 1) * 128, :], yc, yk, [xk(s)] if dst_d is xs_d else [("orow", j * 4 + s)], q="pool")
            if s + 2 < 4:
                dma(xb[b_][:], src_d[t0 + (s + 2) * 128:t0 + (s + 3) * 128, :], [xk(s + 2)], [("xb", b_)])

    def exhaust(gen):
        for _ in gen:
            pass

    for l in range(L):
        layer_setup(l)
        src_d = x_d if l == 0 else xs_d
        dst_d = out_d if l == L - 1 else xs_d
        exhaust(front_gen(l, 0, 0, src_d))
        front_rest(l, 0)
        for j in range(NT):
            par = j % 2
            nxt = front_gen(l, j + 1, 1 - par, src_d) if j + 1 < NT else None
            attention(l, j, par, nxt)
            if nxt is not None:
                exhaust(nxt)
            back(l, j, src_d, dst_d)
            if j + 1 < NT:
                front_rest(l, j + 1)

    print("sbuf bytes remaining/partition:", nc.sbuf_bytes_remaining() if callable(nc.sbuf_bytes_remaining) else nc.sbuf_bytes_remaining,
          " ops:", {e: len(v) for e, v in S.ops.items()}, " waits:", S.nwaits)
    S.emit(ctx)
    ctx.close()
    return nc


_NC_CACHE = {}


def run(inp, T, L, n_cores=8):
    f = lambda a: np.ascontiguousarray(np.asarray(a, dtype=np.float32))
    x = f(inp["x"])
    B = x.shape[0]
    key = (T, L)
    if key not in _NC_CACHE:
        _NC_CACHE[key] = build(T, L)
    nc = _NC_CACHE[key]
    shared = host_consts(T)
    shared.update(host_weights(L, f(inp["w_in"]), f(inp["w_out"]), f(inp["cmp_w1_k"]), f(inp["cmp_w1_v"]),
                               f(inp["cmp_w2_k"]), f(inp["cmp_w2_v"]), f(inp["cmp_pos_k"]), f(inp["cmp_pos_v"]),
                               f(inp["w_mem_kv"]), f(inp["conv_w"]), f(inp["conv_b"])))
    shared["gpre"] = f(inp["pre_norm_g"])
    shared["gpost"] = f(inp["post_norm_g"])
    shared["gmem"] = f(inp["mem_norm_g"])
    shared["bgate"] = f(inp["b_gate"])
    mem = f(inp["mem"])
    in_maps = []
    for c in range(n_cores):
        b = c % B
        m = dict(shared)
        m["x"] = np.ascontiguousarray(x[b])
        m["mem"] = np.ascontiguousarray(mem[b])
        in_maps.append(m)
    res = run_bass_kernel_spmd(nc, in_maps, core_ids=list(range(n_cores)))
    out = np.stack([np.asarray(res.results[b]["out"], dtype=np.float32) for b in range(B)], axis=0)
    return out


def kernel(**inputs):
    return run(inputs, 8192, 4)
```

```python
import numpy as np
import ml_dtypes
from contextlib import ExitStack
import concourse.bass as bass
import concourse.mybir as mybir
from concourse.bass_utils import run_bass_kernel_spmd

F32 = mybir.dt.float32
BF16 = mybir.dt.bfloat16
AF = mybir.ActivationFunctionType
ALU = mybir.AluOpType
AX = mybir.AxisListType
NPBF = ml_dtypes.bfloat16

D = 1024
NEG = -1.0e6
SLOPES = [2.0 ** -(h + 1) for h in range(8)]
CH = 60


class Op:
    __slots__ = ("eng", "pos", "fn", "waits", "signal", "token", "is_dma", "lane", "lane_val", "snap")


class Sched:
    def __init__(self, nc, n_lanes=24, same_engine_sync=True):
        self.nc = nc
        self.eng = {"pe": nc.tensor, "act": nc.scalar, "dve": nc.vector, "pool": nc.gpsimd, "sp": nc.sync}
        self.ops = {e: [] for e in self.eng}
        self.lw = {}
        self.rd = {}
        self.known = {e: {} for e in self.eng}
        self.n_lanes = n_lanes
        self.lane_last = [None] * n_lanes
        self.lane_cnt = [0] * n_lanes
        self.next_lane = 0
        self.same_engine_sync = same_engine_sync
        self.nwaits = 0

    def _need(self, op, d, raw=True):
        e = op.eng
        kn = self.known[e]
        if d.is_dma:
            key = ("L", d.lane)
            val = d.lane_val
        else:
            if d.eng == e:
                if e == "pe" or not self.same_engine_sync or not raw:
                    return
            key = d.eng
            val = d.pos
        if kn.get(key, -1) >= val:
            return
        kn[key] = val
        op.waits.append(d)
        d.signal = True
        self.nwaits += 1
        if d.snap is not None:
            for k, v in d.snap:
                if kn.get(k, -1) < v:
                    kn[k] = v

    def add(self, eng, fn, reads=(), writes=(), dma=False):
        op = Op()
        op.eng = eng
        op.fn = fn
        op.waits = []
        op.signal = False
        op.token = None
        op.is_dma = dma
        op.lane = None
        op.lane_val = None
        op.pos = len(self.ops[eng])
        deps = []
        for r in reads:
            w = self.lw.get(r)
            if w is not None:
                deps.append((w, True))
        for r in writes:
            w = self.lw.get(r)
            if w is not None:
                deps.append((w, False))
            rr = self.rd.get(r)
            if rr:
                deps.extend((o, False) for o in rr.values())
        seen = set()
        for d, raw in deps:
            if raw and id(d) in seen:
                continue
            if raw:
                seen.add(id(d))
            self._need(op, d, raw)
        if dma:
            lane = self.next_lane
            self.next_lane = (self.next_lane + 1) % self.n_lanes
            prev = self.lane_last[lane]
            if prev is not None:
                self._need(op, prev)
            self.lane_cnt[lane] += 1
            op.lane = lane
            op.lane_val = self.lane_cnt[lane]
            self.lane_last[lane] = op
        kn = self.known[eng]
        op.snap = tuple((k, v) for k, v in kn.items() if not isinstance(k, tuple))
        self.ops[eng].append(op)
        for r in reads:
            dd = self.rd.setdefault(r, {})
            dd[("D", id(op)) if dma else eng] = op
        for r in writes:
            self.lw[r] = op
            self.rd[r] = {}
        return op

    def emit(self, ctx):
        nc = self.nc
        esem = {e: ctx.enter_context(nc.semaphore("s_" + e)) for e in self.eng}
        lsem = [ctx.enter_context(nc.semaphore("l_%d" % i)) for i in range(self.n_lanes)]
        for e, lst in self.ops.items():
            c = 0
            for op in lst:
                if (not op.is_dma) and op.signal:
                    c += 1
                    op.token = c
        block = ctx.enter_context(nc.Block())
        reg = {"pe": block.tensor, "act": block.scalar, "dve": block.vector, "pool": block.gpsimd, "sp": block.sync}

        def make(e):
            def body(engh):
                for op in self.ops[e]:
                    for d in op.waits:
                        if d.is_dma:
                            engh.wait_ge(lsem[d.lane], 16 * d.lane_val)
                        else:
                            engh.wait_ge(esem[d.eng], d.token)
                    ins = op.fn(engh)
                    if op.is_dma:
                        ins.then_inc(lsem[op.lane], 16)
                    elif op.signal:
                        ins.then_inc(esem[e], 1)
                if e == "sp":
                    for i in range(self.n_lanes):
                        if self.lane_cnt[i]:
                            engh.wait_ge(lsem[i], 16 * self.lane_cnt[i])
            return body

        for e in self.eng:
            reg[e](make(e))


OFF = dict(cB=0, cC=256, ch=512, cg=768, q=1024, kc=1536, vc=1664, ks=1792, vs=1920, kw=2048, vw=2176,
           gl=2304, ng=2328, mq=2840, mg=3096)
NBLK = 13
BW = 288


def _block_cols():
    r = lambda a, n: list(range(a, a + n))
    blocks = [
        r(OFF["q"], 256), r(OFF["q"] + 256, 256),
        r(OFF["ks"], 128) + r(OFF["kw"], 128),
        r(OFF["kc"], 128) + r(OFF["vc"], 128),
        r(OFF["mq"], 256),
        r(OFF["cB"], 256), r(OFF["cC"], 256), r(OFF["ch"], 256), r(OFF["cg"], 256),
        r(OFF["vs"], 128) + r(OFF["vw"], 128) + r(OFF["gl"], 24),
        r(OFF["ng"], 256), r(OFF["ng"] + 256, 256),
        r(OFF["mg"], 256),
    ]
    return blocks


def host_consts(T):
    k = np.arange(128)[:, None]
    q = np.arange(512)[None, :]
    xx = np.arange(896)[None, :] - 384
    mc = np.where(k > xx, NEG, 0.0).astype(np.float32)
    ml = np.where(k <= xx, NEG, 0.0).astype(np.float32)
    mcmp = np.zeros((128, 5, 512), np.float32)
    for v in range(4):
        for rr in range(32):
            mcmp[32 * v + rr, v, :] = np.where(16 * rr + 15 > q[0], NEG, 0.0)
        mcmp[32 * v + 32:, v, :] = NEG
    mcmp[:, 4, :] = mcmp[:, 0, :]
    mcmp[0, 4, :] = NEG
    mq = np.zeros((128, 2, 4, 32), np.float32)
    p = np.arange(128)[:, None]
    rr = np.arange(32)[None, :]
    for s in range(4):
        mq[:, 0, s, :] = np.where(16 * rr + 15 > 128 * s + p, NEG, 0.0)
    mq[:, 1] = mq[:, 0]
    mq[:, 1, :, 0] = NEG
    pos = np.arange(T)
    kexts = np.zeros((64, T), np.float32)
    kexts[0] = pos // 128
    kexts[1] = pos % 128
    kexts[2] = 1.0
    kexts[3] = 1.0
    blk = (pos // 64) % CH
    for r_ in range(CH):
        kexts[4 + r_] = (blk == r_)
    sl = np.arange(512)
    pc = 16 * sl + 15
    kextc = np.stack([pc // 128, pc % 128, np.ones(512), np.ones(512)]).astype(np.float32)
    tq = np.arange(512)
    qext = np.stack([np.full(512, 128.0), np.ones(512), -128.0 * (tq // 128), -1.0 * (tq % 128)]).astype(np.float32)
    colab = np.zeros((128, 2), np.float32)
    colab[:, 0] = np.where(np.arange(128) >= 64, 1e4, -1.0)
    colab[:, 1] = np.where(np.arange(128) < 64, 1e4, -1.0)
    bf = lambda a: np.ascontiguousarray(a).astype(NPBF)
    return dict(mc=bf(mc.reshape(128, -1)), ml=bf(ml.reshape(128, -1)), mcmp=bf(mcmp.reshape(128, -1)),
                mq=bf(mq.reshape(128, -1)), kexts=bf(kexts), kextc=bf(kextc), qext=bf(qext), colab=colab)


def host_weights(L, w_in, w_out, w1k, w1v, w2k, w2v, posk, posv, wm, convw, convb):
    blocks = _block_cols()
    w_in_p = np.zeros((L, NBLK, 128, 8, BW), np.float32)
    for b, cols in enumerate(blocks):
        sub = w_in[:, :, cols]
        w_in_p[:, b, :, :, :len(cols)] = sub.reshape(L, 8, 128, len(cols)).transpose(0, 2, 1, 3)
    w_out_p = w_out.reshape(L, 8, 128, 4, 256).transpose(0, 3, 2, 1, 4)
    w1 = np.stack([w1k, w1v], axis=1)
    w1_p = w1.reshape(L, 2, 16, 128, 2, 128).transpose(0, 1, 4, 3, 2, 5)
    w2 = np.stack([w2k, w2v], axis=1)
    w2_p = w2.reshape(L, 2, 2, 128, 64).transpose(0, 1, 3, 2, 4)
    pos = np.stack([posk, posv], axis=1)
    pos_p = pos.reshape(L, 2, 16, 2, 64).transpose(0, 1, 3, 4, 2).reshape(L, 2, 128, 16)
    wm_p = wm.reshape(L, 8, 128, 2, 256).transpose(0, 3, 2, 1, 4)
    convw_t = convw.reshape(L, 3, 2, 128).transpose(3, 0, 2, 1)
    convb_t = convb.reshape(L, 2, 128).transpose(2, 0, 1)
    c = np.ascontiguousarray
    return dict(w_in_p=c(w_in_p.reshape(L, NBLK, 128, 8 * BW)), w_out_p=c(w_out_p.reshape(L, 4, 128, 2048)),
                w1_p=c(w1_p.reshape(L, 2, 2, 128, 2048)), w2_p=c(w2_p.reshape(L, 2, 128, 128)),
                pos_p=c(pos_p), wm_p=c(wm_p.reshape(L, 2, 128, 2048)),
                convw_t=c(convw_t.reshape(128, L * 6)), convb_t=c(convb_t.reshape(128, L * 2)))


def build(T=8192, L=4, same_engine_sync=True):
    NT = T // 512
    NKT = T // 128
    nc = bass.Bass("TRN2", target_bir_lowering=False)
    dram = lambda name, shape, dt_, kind: nc.dram_tensor(name, shape, dt_, kind=kind).ap()
    EI, EO, IN = "ExternalInput", "ExternalOutput", "Internal"
    x_d = dram("x", [T, D], F32, EI)
    mem_d = dram("mem", [256, D], F32, EI)
    win_d = dram("w_in_p", [L, NBLK, 128, 8 * BW], F32, EI)
    wout_d = dram("w_out_p", [L, 4, 128, 2048], F32, EI)
    w1_d = dram("w1_p", [L, 2, 2, 128, 2048], F32, EI)
    w2_d = dram("w2_p", [L, 2, 128, 128], F32, EI)
    pos_d = dram("pos_p", [L, 2, 128, 16], F32, EI)
    wm_d = dram("wm_p", [L, 2, 128, 2048], F32, EI)
    gpre_d = dram("gpre", [L, D], F32, EI)
    gpost_d = dram("gpost", [L, D], F32, EI)
    gmem_d = dram("gmem", [L, D], F32, EI)
    convw_d = dram("convw_t", [128, L * 6], F32, EI)
    convb_d = dram("convb_t", [128, L * 2], F32, EI)
    bgate_d = dram("bgate", [L, 24], F32, EI)
    mc_d = dram("mc", [128, 896], BF16, EI)
    ml_d = dram("ml", [128, 896], BF16, EI)
    mcmp_d = dram("mcmp", [128, 2560], BF16, EI)
    mq_d = dram("mq", [128, 256], BF16, EI)
    kexts_d = dram("kexts", [64, T], BF16, EI)
    kextc_d = dram("kextc", [4, 512], BF16, EI)
    qext_d = dram("qext", [4, 512], BF16, EI)
    colab_d = dram("colab", [128, 2], F32, EI)
    out_d = dram("out", [T, D], F32, EO)
    xs_d = dram("xs", [T, D], F32, IN)
    WIN_s = dram("WIN_s", [L, NBLK, 128, 8 * BW], BF16, IN)
    WOUT_s = dram("WOUT_s", [L, 4, 128, 2048], BF16, IN)
    W1_s = dram("W1_s", [L, 2, 2, 128, 2048], BF16, IN)
    WM_s = dram("WM_s", [L, 2, 128, 2048], BF16, IN)

    ctx = ExitStack()
    S = Sched(nc, same_engine_sync=same_engine_sync)
    sb = lambda name, shape, dt_=F32: nc.alloc_sbuf_tensor(name, shape, dt_)
    KXs = sb("KXs", [128, 2, T], BF16)
    Vs = sb("Vs", [128, NKT, 2, 65], BF16)
    KXw = sb("KXw", [128, 2, 1024], BF16)
    Vw = sb("Vw", [128, 8, 2, 65], BF16)
    KXc = sb("KXc", [128, 2, 512], BF16)
    VC = sb("VC", [128, 4, 2, 65], BF16)
    KC2 = sb("KC2", [128, 2, 2, 544], BF16)
    KM = sb("KM", [128, 4, 256], BF16)
    VM = sb("VM", [128, 2, 4, 65], BF16)
    QXs = [sb("QX%d" % i, [128, 8, 512], BF16) for i in range(2)]
    QXm = sb("QXm", [128, 4, 512], BF16)
    PXs = [sb("PX%d" % i, [128, 2, 3, 512], BF16) for i in range(2)]
    QXv = [sb("QXv%d" % i, [128, 512], BF16) for i in range(2)]
    MCW = sb("MCW", [128, 896], BF16)
    MLW = sb("MLW", [128, 896], BF16)
    MCMP = sb("MCMP", [128, 5, 512], BF16)
    MQ = sb("MQ", [128, 2, 4, 32], BF16)
    identb = sb("identb", [128, 128], BF16)
    identf = sb("identf", [128, 128], F32)
    gpre_b = sb("gpre_b", [128, D], F32)
    gpost_b = sb("gpost_b", [128, D], F32)
    convw_t = sb("convw_sb", [128, L * 6], F32)
    convb_t = sb("convb_sb", [128, L * 2], F32)
    bgate_b = sb("bgate_b", [128, L * 24], F32)
    colab = sb("colab_sb", [128, 2], F32)
    mhalf = sb("mhalf", [128, 4], F32)
    cbias = sb("cbias", [128, 256], F32)
    w2b = sb("w2b", [128, 2, 128], BF16)
    pos2b = sb("pos2b", [128, 2, 16], BF16)
    xb = [sb("xb%d" % i, [128, D], F32) for i in range(2)]
    hT = sb("hT", [128, 8, 512], BF16)
    NWB = 2
    wbuf = [sb("wbuf%d" % i, [128, 8 * BW], BF16) for i in range(NWB)]
    cbuf = sb("cbuf", [128, 4, 512], F32)
    uh = sb("uh", [128, 2, 2], F32)
    tmpA = sb("tmpA", [128, 1024], F32)
    abuf = tmpA[:, 0:512]
    thb = tmpA[:, 512:1024]
    hidf = tmpA[:, 0:768].rearrange("p (a b) -> p a b", b=256)
    ycp = sb("ycp", [128, D], F32)
    yT = sb("yT", [128, 8, 512], BF16)
    GN = sb("GN", [128, 4, 512], BF16)
    GM = sb("GM", [128, 4, 256], BF16)
    GL2 = sb("GL2", [128, 4, 24], F32)
    pbuf = [sb("pbuf%d" % i, [128, 512], BF16) for i in range(4)]
    OTs = [sb("OTs%d" % i, [128, 512], F32) for i in range(2)]
    acc = sb("acc", [128, 4, 512], F32)
    accm = sb("accm", [128, 4, 256], F32)
    tmpc = sb("tmpc", [128, 4, 64], F32)
    ebuf = [sb("ebuf%d" % i, [128, 512], F32) for i in range(2)]
    imp = sb("imp", [128, 516], F32)
    selv = sb("selv", [128, 128], F32)
    selv2 = sb("selv2", [128, 128], F32)
    Zc = sb("Zc", [128, 3, 128], F32)
    m8 = sb("m8", [128, 16], F32)
    st = sb("st", [128, 40], F32)
    hidb = sb("hidb", [128, 256], BF16)
    vcst = sb("vcst", [32, 2, 64], BF16)

    ps = [nc.alloc_psum_tensor("ps%d" % i, [128, 512], F32) for i in range(8)]
    PSN = ["ps%d" % i for i in range(8)]

    def dma(out, in_, r, w, q="sp"):
        S.add(q, lambda e: e.dma_start(out=out, in_=in_), r, w, dma=True)

    def mm(out, lhsT, rhs, start, stop, r, w):
        S.add("pe", lambda e: e.matmul(out, lhsT=lhsT, rhs=rhs, start=start, stop=stop), r, w)

    def trn(out, in_, ident, r, w):
        S.add("pe", lambda e: e.transpose(out=out, in_=in_, identity=ident), r, w)

    def actv(out, in_, func, r, w, scale=1.0, bias=0.0, accum=None):
        if accum is None:
            S.add("act", lambda e: e.activation(out=out, in_=in_, func=func, bias=bias, scale=scale), r, w)
        else:
            S.add("act", lambda e: e.activation(out=out, in_=in_, func=func, bias=bias, scale=scale, accum_out=accum), r, w)

    def cp(out, in_, r, w, eng="dve"):
        S.add(eng, lambda e: e.tensor_copy(out=out, in_=in_), r, w)

    def ts(out, in0, s1, s2, op0, op1, r, w, eng="dve"):
        if op1 is None:
            S.add(eng, lambda e: e.tensor_scalar(out=out, in0=in0, scalar1=s1, scalar2=None, op0=op0), r, w)
        else:
            S.add(eng, lambda e: e.tensor_scalar(out=out, in0=in0, scalar1=s1, scalar2=s2, op0=op0, op1=op1), r, w)

    def tt(out, in0, in1, op, r, w, eng="dve"):
        S.add(eng, lambda e: e.tensor_tensor(out=out, in0=in0, in1=in1, op=op), r, w)

    def stt(out, in0, scalar, in1, op0, op1, r, w, accum=None):
        if accum is None:
            S.add("dve", lambda e: e.scalar_tensor_tensor(out=out, in0=in0, scalar=scalar, in1=in1, op0=op0, op1=op1), r, w)
        else:
            S.add("dve", lambda e: e.scalar_tensor_tensor(out=out, in0=in0, scalar=scalar, in1=in1, op0=op0, op1=op1, accum_out=accum), r, w)

    def mset(ap, val, w, eng="pool"):
        S.add(eng, lambda e: e.memset(ap, val), (), w)

    dma(MCW[:], mc_d, [], ["MC"])
    dma(MLW[:], ml_d, [], ["ML"])
    dma(MCMP[:].rearrange("p a b -> p (a b)"), mcmp_d, [], ["MCMP"])
    dma(MQ[:].rearrange("p a b c -> p (a b c)"), mq_d, [], ["MQ"])
    dma(colab[:], colab_d, [], ["colab"])
    dma(convw_t[:], convw_d, [], ["convw"])
    dma(convb_t[:], convb_d, [], ["convb"])
    dma(bgate_b[:], bgate_d.rearrange("l n -> (l n)").partition_broadcast(128), [], ["bgate"])
    for par in range(2):
        mset(QXs[par][:].rearrange("p a b -> p (a b)"), 0.0,
             [("QX", par, h, "q") for h in range(8)] + [("QX", par, h, "x") for h in range(8)])
    mset(QXm[:].rearrange("p a b -> p (a b)"), 0.0, [("QXm", h) for h in range(4)])
    mset(KM[:].rearrange("p a b -> p (a b)"), 0.0, ["KM"])
    mset(KXw[:].rearrange("p a b -> p (a b)"), 0.0, [("KXw", g, sl, t) for g in range(2) for sl in range(2) for t in ("k", "x")])
    mset(KXc[:].rearrange("p a b -> p (a b)"), 0.0, [("KXc", g, t) for g in range(2) for t in ("k", "x")])
    for g in range(2):
        dma(KXs[64:128, g, :], kexts_d, [], [("KXs", g, "x")])
        dma(KXc[64:68, g, :], kextc_d, [], [("KXc", g, "x")])
    for par in range(2):
        for h in range(8):
            dma(QXs[par][64:68, h, :], qext_d, [], [("QX", par, h, "x")])
    mset(identf[:], 0.0, ["identf"])
    S.add("pool", lambda e: e.affine_select(out=identf[:], in_=identf[:], pattern=[[-1, 128]], compare_op=ALU.not_equal,
                                            fill=1.0, base=0, channel_multiplier=1), ["identf"], ["identf"])
    cp(identb[:], identf[:], ["identf"], ["identb"])
    mset(mhalf[:], -0.5, ["mhalf"])
    mset(Vs[:].rearrange("p a b c -> p (a b c)"), 1.0, [("Vs", "all")])
    mset(Vw[:].rearrange("p a b c -> p (a b c)"), 1.0, [("Vw", "all")])
    mset(VC[:].rearrange("p a b c -> p (a b c)"), 1.0, [("VC", "all")])
    mset(VM[:].rearrange("p a b c -> p (a b c)"), 1.0, [("VM", "all")])
    mset(KC2[:].rearrange("p a b c -> p (a b c)"), 0.0, ["KC2"])
    for par in range(2):
        mset(PXs[par][:].rearrange("p a b c -> p (a b c)"), 0.0, ["PXinit"])
    mset(Zc[:].rearrange("p a b -> p (a b)"), 0.0, ["Zc"])
    mset(imp[:], 0.0, ["imp"])

    wb_i = [0]

    def next_wb():
        i = wb_i[0]
        wb_i[0] = (i + 1) % NWB
        return i

    def prep(src, dst, n, last, dst_key):
        dma(dst.rearrange("p (c n) -> p c n", n=last), src.rearrange("p (c n) -> p c n", n=last), [], [dst_key], q="pool")

    prep_list = []
    for l in range(L):
        pl = []
        for kv in range(2):
            for hf in range(2):
                pl.append((w1_d[l, kv, hf], W1_s[l, kv, hf], 2048, 128, ("W1", l, kv, hf)))
        for b in range(2):
            pl.append((wm_d[l, b], WM_s[l, b], 2048, 256, ("WM", l, b)))
        for b in range(NBLK):
            pl.append((win_d[l, b], WIN_s[l, b], 8 * BW, BW, ("WIN", l, b)))
        for b in range(4):
            pl.append((wout_d[l, b], WOUT_s[l, b], 2048, 256, ("WOUT", l, b)))
        prep_list.append(pl)
    for a_ in prep_list[0]:
        prep(*a_)

    def wload(src, n, key):
        i = next_wb()
        dma(wbuf[i][:, 0:n], src, [key], [("wbuf", i)])
        return i

    gen_i = [0]

    def gen_bank():
        i = 5 + gen_i[0]
        gen_i[0] ^= 1
        return i

    def norm_A(xt, xkey, gtile, gkey, k):
        o = 24 + 3 * k
        stt(cbuf[:, 0:2, :].rearrange("p a b -> p (a b)"), xt[:], 1.0, xt[:], ALU.mult, ALU.mult,
            [xkey], ["cb01", ("stn", k, 0)], accum=st[:, o:o + 1])
        ts(st[:, o + 1:o + 2], st[:, o:o + 1], 1.0 / D, 1e-6, ALU.mult, ALU.add, [("stn", k, 0)], [("stn", k, 1)])
        tt(st[:, o + 2:o + 3], st[:, o + 1:o + 2], mhalf[:, 0:1], ALU.pow, [("stn", k, 1), "mhalf"], [("stn", k, 2)], eng="pool")
        stt(xt[:], xt[:], st[:, o + 2:o + 3], gtile[:], ALU.mult, ALU.mult, [xkey, ("stn", k, 2), gkey], [xkey])

    def norm_B(xt, xkey, col0):
        for half in range(2):
            for c4 in range(4):
                c = half * 4 + c4
                trn(ps[7][:, c4 * 128:(c4 + 1) * 128], xt[:, c * 128:(c + 1) * 128], identf[:], [xkey, "identf"], [PSN[7]])
            cp(hT[:, half * 4:half * 4 + 4, col0:col0 + 128], ps[7][:].rearrange("p (a b) -> p a b", b=128), [PSN[7]], ["hT"])

    def norm_transpose(xt, xkey, gtile, gkey, col0):
        norm_A(xt, xkey, gtile, gkey, 0)
        norm_B(xt, xkey, col0)

    def _unused(xt, xkey, col0):
        for half in range(2):
            for c4 in range(4):
                c = half * 4 + c4
                trn(ps[7][:, c4 * 128:(c4 + 1) * 128], xt[:, c * 128:(c + 1) * 128], identf[:], [xkey, "identf"], [PSN[7]])
            cp(hT[:, half * 4:half * 4 + 4, col0:col0 + 128], ps[7][:].rearrange("p (a b) -> p a b", b=128),
               [PSN[7]], ["hT"])

    class Unit:
        pass

    def run_units(units, hook=None):
        n = len(units)
        LA = 2
        pend = []
        for i in range(n + LA):
            if i < n:
                u = units[i]
                if hook is not None:
                    next(hook, None)
                if u.pre is not None:
                    u.pre()
                sbk = i % 3
                nmm = len(u.mm1)
                for k, (lh, rh, rd_) in enumerate(u.mm1):
                    mm(ps[sbk][0:u.M, :], lh, rh, k == 0, k == nmm - 1, rd_, [PSN[sbk]])
                pb = i % 4
                actv(pbuf[pb][0:u.M, :], ps[sbk][0:u.M, :], AF.Exp, [PSN[sbk]], [("pbuf", pb)], scale=u.scale, bias=u.bias)
            k2 = i - LA
            if k2 >= 0:
                u = units[k2]
                pb = k2 % 4
                mm(ps[u.ob][0:65, :], u.v, pbuf[pb][0:u.M, :], u.first, u.last, [("pbuf", pb), u.vkey], [PSN[u.ob]])
                if u.last:
                    pend.append([k2 + 3, u])
            while pend and (pend[0][0] <= i or i == n + LA - 1):
                _, u = pend.pop(0)
                u.fin(u)

    ob_i = [0]
    qv_i = [0]

    def next_ob():
        i = 3 + ob_i[0]
        ob_i[0] ^= 1
        return i

    ot_i = [0]

    def finalize(u, gate_ap, const, dst, dkey, first_write):
        k = ot_i[0]
        ot_i[0] ^= 1
        cp(OTs[k][0:65, :], ps[u.ob][0:65, :], [PSN[u.ob]], [("OTs", k)])
        for s in range(4):
            trn(ps[7][:, s * 65:(s + 1) * 65], OTs[k][0:65, s * 128:(s + 1) * 128], identf[0:65, 0:65],
                [("OTs", k), "identf"], [PSN[7]])
        pv = ps[7][:, 0:260].rearrange("p (s c) -> p s c", c=65)
        ts(st[:, 20:24], pv[:, :, 64], 1e-30, None, ALU.max, None, [PSN[7]], ["st20"])
        S.add("dve", lambda e: e.reciprocal(out=st[:, 8:12], in_=st[:, 20:24]), ["st20"], ["st8"])
        if gate_ap is None:
            ts(st[:, 12:16], st[:, 8:12], const, None, ALU.mult, None, ["st8"], ["st12"])
        else:
            stt(st[:, 12:16], st[:, 8:12], const, gate_ap, ALU.mult, ALU.mult, ["st8", "GL2"], ["st12"])
        fb = st[:, 12:16].unsqueeze(2).to_broadcast([128, 4, 64])
        if first_write:
            tt(dst, pv[:, :, 0:64], fb, ALU.mult, [PSN[7], "st12"], [dkey])
        else:
            tt(tmpc[:], pv[:, :, 0:64], fb, ALU.mult, [PSN[7], "st12"], ["tmpc"])
            tt(dst, dst, tmpc[:], ALU.add, [dkey, "tmpc"], [dkey], eng="pool")

    def mk_unit(mm1, M, scale, bias, v, vkey, ob, first, last, fin, pre=None):
        u = Unit()
        u.pre = pre
        u.mm1, u.M, u.scale, u.bias, u.v, u.vkey, u.ob, u.first, u.last, u.fin = mm1, M, scale, bias, v, vkey, ob, first, last, fin
        return u

    MCv = lambda kr: MCW[:, 384 - 128 * kr:896 - 128 * kr]
    MLv = lambda kr: MLW[:, 384 - 128 * kr:896 - 128 * kr]

    def layer_setup(l):
        dma(gpre_b[:], gpre_d[l:l + 1, :].rearrange("a n -> (a n)").partition_broadcast(128), [], ["gpre"])
        dma(gpost_b[:], gpost_d[l:l + 1, :].rearrange("a n -> (a n)").partition_broadcast(128), [], ["gpost"])
        for kv in range(2):
            dma(w2b[:, kv, :], w2_d[l, kv], [], ["w2b"], q="pool")
            dma(pos2b[:, kv, :], pos_d[l, kv], [], ["pos2b"], q="pool")
        gb = gen_bank()
        for kv in range(2):
            for hf in range(2):
                i = wload(W1_s[l, kv, hf], 2048, ("W1", l, kv, hf))
                wv = wbuf[i][:, 0:2048].rearrange("p (c n) -> p c n", n=128)
                col = kv * 2 + hf
                for pp in range(16):
                    mm(ps[gb][:, col:col + 1], wv[:, pp, :], pos2b[:, kv, pp:pp + 1], pp == 0, pp == 15,
                       [("wbuf", i), "pos2b"], [PSN[gb]])
        cbv = cbias[:].rearrange("p (kv g hf r) -> p kv g hf r", kv=2, g=2, hf=2)
        for kv in range(2):
            for g in range(2):
                for hf in range(2):
                    col = kv * 2 + hf
                    cp(cbv[:, kv, g, hf, :], ps[gb][:, col:col + 1].to_broadcast([128, 32]), [PSN[gb]], ["cbias"])
        gmem_b = cbuf[:, 2:4, :].rearrange("p a b -> p (a b)")
        dma(gmem_b, gmem_d[l:l + 1, :].rearrange("a n -> (a n)").partition_broadcast(128), [], ["cb23", ("ca", 0), ("ca", 1)])
        for mt in range(2):
            dma(xb[mt][:], mem_d[mt * 128:(mt + 1) * 128, :], [], [("xb", mt)])
            norm_transpose(xb[mt], ("xb", mt), gmem_b, "cb23", mt * 128)
        ik = wload(WM_s[l, 0], 2048, ("WM", l, 0))
        wk = wbuf[ik][:, 0:2048].rearrange("p (c n) -> p c n", n=256)
        for pr in range(2):
            gb = gen_bank()
            for c in range(8):
                mm(ps[gb][:, 0:256], wk[:, c, pr * 128:(pr + 1) * 128], hT[:, c, 0:256], c == 0, c == 7,
                   [("wbuf", ik), "hT"], [PSN[gb]])
            cp(KM[0:64, 2 * pr, :], ps[gb][0:64, 0:256], [PSN[gb]], ["KM"])
            cp(KM[0:64, 2 * pr + 1, :], ps[gb][64:128, 0:256], [PSN[gb]], ["KM"])
        iv = wload(WM_s[l, 1], 2048, ("WM", l, 1))
        wv_ = wbuf[iv][:, 0:2048].rearrange("p (c n) -> p c n", n=256)
        for mt in range(2):
            gb = gen_bank()
            for c in range(8):
                mm(ps[gb][:, 0:256], hT[:, c, mt * 128:(mt + 1) * 128], wv_[:, c, :], c == 0, c == 7,
                   [("wbuf", iv), "hT"], [PSN[gb]])
            cp(VM[:, mt, :, 0:64], ps[gb][:, 0:256].rearrange("p (h d) -> p h d", d=64), [PSN[gb], ("VM", "all")], ["VM"])
        mset(uh[:].rearrange("p a b -> p (a b)"), 0.0, ["uh"])

    def fm_block(l, b):
        i = wload(WIN_s[l, b], 8 * BW, ("WIN", l, b))
        wv = wbuf[i][:, :].rearrange("p (c n) -> p c n", n=BW)
        for grp in range(2):
            gb = gen_bank()
            for c in range(8):
                mm(ps[gb][:, :], wv[:, c, grp * 128:(grp + 1) * 128], hT[:, c, :], c == 0, c == 7,
                   [("wbuf", i), "hT"], [PSN[gb]])
            yield gb, grp

    def front_gen(l, j, par, src_d):
        t0 = 512 * j
        slot = j % 2
        QX = QXs[par]
        PX = PXs[par]
        xk = lambda s: ("xrow", j * 4 + s)

        def stA(s):
            b_ = s % 2
            dma(xb[b_][:], src_d[t0 + s * 128:t0 + (s + 1) * 128, :], [xk(s)], [("xb", b_)])
            norm_A(xb[b_], ("xb", b_), gpre_b[:], "gpre", s % 2)

        def stB(s):
            norm_B(xb[s % 2], ("xb", s % 2), s * 128)
        for f_, a_ in ((stA, 0), (stA, 1), (stB, 0), (stA, 2), (stB, 1), (stA, 3), (stB, 2), (stB, 3)):
            f_(a_)
            yield
            yield
        for g in range(2):
            dma(KXw[64:68, g, slot * 512:(slot + 1) * 512], kexts_d[0:4, t0:t0 + 512], [], [("KXw", g, slot, "x")])
        for b in range(2):
            for gb, grp in fm_block(l, b):
                yield
                for hh in range(2):
                    h = b * 4 + grp * 2 + hh
                    ts(QX[0:64, h, :], ps[gb][hh * 64:(hh + 1) * 64, :], 1.0 / (8.0 * SLOPES[h]), None, ALU.mult, None,
                       [PSN[gb]], [("QX", par, h, "q")])
                yield
        for gb, grp in fm_block(l, 2):
            yield
            for g in range(2):
                if grp == 0:
                    dst_, dk_ = KXs[0:64, g, t0:t0 + 512], ("KXs", g, j)
                else:
                    dst_, dk_ = KXw[0:64, g, slot * 512:(slot + 1) * 512], ("KXw", g, slot, "k")
                cp(dst_, ps[gb][g * 64:(g + 1) * 64, :], [PSN[gb]], [dk_])
            yield
        for kv in range(2):
            for g in range(2):
                cp(KC2[0:64, kv, g, 0:16], KC2[0:64, kv, g, 512:528], [("KC2", kv, g)], [("KC2", kv, g)], eng="pool")
                cp(KC2[64:128, kv, g, 0:15], KC2[64:128, kv, g, 512:527], [("KC2", kv, g)], [("KC2", kv, g)], eng="pool")
        for gb, grp in fm_block(l, 3):
            yield
            kv = grp
            for g in range(2):
                cp(KC2[0:64, kv, g, 16:528], ps[gb][g * 64:(g + 1) * 64, :], [PSN[gb], "KC2"], [("KC2", kv, g)])
                cp(KC2[64:128, kv, g, 15:527], ps[gb][g * 64:(g + 1) * 64, :], [PSN[gb], "KC2"], [("KC2", kv, g)])
            yield
        for gb, grp in fm_block(l, 4):
            yield
            for hh in range(2):
                h = grp * 2 + hh
                ts(QXm[0:64, h, :], ps[gb][hh * 64:(hh + 1) * 64, :], 0.125, None, ALU.mult, None, [PSN[gb]], [("QXm", h)])
            yield

        hb = gen_bank()
        for kv in range(2):
            for hf in range(2):
                i = wload(W1_s[l, kv, hf], 2048, ("W1", l, kv, hf))
                wv = wbuf[i][:, 0:2048].rearrange("p (c n) -> p c n", n=128)
                for g in range(2):
                    col = ((kv * 2 + g) * 2 + hf) * 32
                    for pp in range(16):
                        rhs = KC2[:, kv, g, 2 * pp:2 * pp + 512].rearrange("p (r s) -> p r s", s=16)[:, :, 0]
                        mm(ps[hb][:, col:col + 32], wv[:, pp, :], rhs, pp == 0, pp == 15,
                           [("wbuf", i), ("KC2", kv, g)], [PSN[hb]])
                    yield
        u_ = hidf[:, 0, :]
        v_ = hidf[:, 1, :]
        w_ = hidf[:, 2, :]
        tt(u_, ps[hb][:, 0:256], cbias[:], ALU.add, [PSN[hb], "cbias"], ["tmpA"])
        tt(v_, u_, u_, ALU.mult, ["tmpA"], ["tmpA"])
        ts(v_, v_, 0.044715, 1.0, ALU.mult, ALU.add, ["tmpA"], ["tmpA"])
        tt(v_, v_, u_, ALU.mult, ["tmpA"], ["tmpA"])
        yield
        yield
        actv(w_, v_, AF.Tanh, ["tmpA"], ["tmpA"], scale=0.7978845608028654)
        yield
        stt(hidb[:], w_, 1.0, u_, ALU.add, ALU.mult, ["tmpA"], ["hidb"])
        yield
        slot_lo = 32 * j
        for g in range(2):
            gb = gen_bank()
            for hf in range(2):
                col = ((0 * 2 + g) * 2 + hf) * 32
                mm(ps[gb][0:64, 0:32], w2b[:, 0, hf * 64:(hf + 1) * 64], hidb[:, col:col + 32], hf == 0, hf == 1,
                   ["w2b", "hidb"], [PSN[gb]])
            for hf in range(2):
                col = ((1 * 2 + g) * 2 + hf) * 32
                mm(ps[gb][0:32, 64:128], hidb[:, col:col + 32], w2b[:, 1, hf * 64:(hf + 1) * 64], hf == 0, hf == 1,
                   ["w2b", "hidb"], [PSN[gb]])
            yield
            ts(KXc[0:64, g, slot_lo:slot_lo + 32], ps[gb][0:64, 0:32], 0.5, None, ALU.mult, None, [PSN[gb]], [("KXc", g, "k")])
            ts(vcst[:, g, :], ps[gb][0:32, 64:128], 0.5, None, ALU.mult, None, [PSN[gb]], ["vcst"])
            yield
        pr0 = slot_lo % 128
        dma(VC[pr0:pr0 + 32, slot_lo // 128, :, 0:64], vcst[:], ["vcst", ("VC", "all")], ["VCd"])

        N = 32 * (j + 1)
        NB = 8 * (j + 1)
        nch = (NB - 1) // CH + 1
        mqv = 1 if j == 0 else 0
        for g in range(2):
            for s in range(4):
                for r_ in range(4):
                    h = g * 4 + r_
                    gb = gen_bank()
                    mm(ps[gb][:, 0:N], QX[0:128, h, s * 128:(s + 1) * 128], KXc[0:128, g, 0:N], True, False,
                       [("QX", par, h, "q"), ("QX", par, h, "x"), ("KXc", g, "k"), ("KXc", g, "x")], [PSN[gb]])
                    mm(ps[gb][:, N - 32:N], identb[:], MQ[:, mqv, s, :], False, True, ["identb", "MQ"], [PSN[gb]])
                    yield
                    eb = ebuf[r_ % 2]
                    ek = ("ebuf", r_ % 2)
                    actv(eb[:, 0:N], ps[gb][:, 0:N], AF.Exp, [PSN[gb]], [ek, "st4"], scale=SLOPES[h],
                         bias=-SLOPES[h] * 512.0 * j, accum=st[:, 4:5])
                    yield
                    ts(st[:, 6:7], st[:, 4:5], 1e-30, None, ALU.max, None, ["st4"], ["st6"])
                    S.add("dve", lambda e: e.reciprocal(out=st[:, 5:6], in_=st[:, 6:7]), ["st6"], ["st5"])
                    if r_ == 0:
                        ts(imp[:, 0:N], eb[:, 0:N], st[:, 5:6], None, ALU.mult, None, [ek, "st5"], ["imp"])
                    else:
                        stt(imp[:, 0:N], eb[:, 0:N], st[:, 5:6], imp[:, 0:N], ALU.mult, ALU.add, [ek, "st5", "imp"], ["imp"])
                mset(imp[:, N:N + 1], 0.0, ["imp"], eng="dve")
                chb = ebuf[0]
                tt(chb[:, 0:N], imp[:, 0:N], imp[:, 1:N + 1], ALU.add, ["imp"], [("ebuf", 0)])
                S.add("dve", lambda e, chb=chb, NB=NB, N=N: e.tensor_reduce(
                    out=selv[:, 0:NB], in_=chb[:, 0:N].rearrange("p (n f) -> p n f", f=4), axis=AX.X, op=ALU.add),
                    [("ebuf", 0)], ["selv"])
                lo = 8 * j + 2 * s
                if lo + 2 < 128:
                    mset(selv[:, lo + 2:128], -1.0, ["selv"], eng="dve")
                cp(selv[:, lo + 1:lo + 2], colab[:, 0:1], ["colab", "selv"], ["selv"])
                mset(selv[:, lo:lo + 1], 1.0e4, ["selv"], eng="dve")
                if lo - 1 >= 1:
                    ts(selv[:, lo - 1:lo], selv[:, lo - 1:lo], colab[:, 1:2], None, ALU.max, None, ["selv", "colab"], ["selv"])
                mset(selv[:, 0:1], 1.0e4, ["selv"], eng="dve")
                S.add("dve", lambda e: e.max(out=m8[:, 0:8], in_=selv[:]), ["selv"], ["m8"])
                S.add("dve", lambda e: e.match_replace(out=selv2[:], in_to_replace=m8[:, 0:8], in_values=selv[:], imm_value=-2.0),
                      ["selv", "m8"], ["selv2"])
                S.add("dve", lambda e: e.max(out=m8[:, 8:16], in_=selv2[:]), ["selv2"], ["m8b"])
                for c in range(nch):
                    n0 = CH * c
                    n1 = min(128, n0 + CH)
                    ts(Zc[:, c, 68:68 + (n1 - n0)], selv[:, n0:n1], m8[:, 15:16], NEG, ALU.is_lt, ALU.mult,
                       ["selv", "m8b"], ["Zc"])
                for _ in range(12):
                    yield
                for c in range(nch):
                    trn(ps[7][:, c * 128:(c + 1) * 128], Zc[:, c, :], identf[:], ["Zc", "identf"], [PSN[7]])
                cp(PX[64:128, g, 0:nch, s * 128:(s + 1) * 128],
                   ps[7][64:128, 0:nch * 128].rearrange("p (a b) -> p a b", b=128), [PSN[7], "PXinit"],
                   [("PX", par, g, c) for c in range(nch)])
                yield

    def front_rest(l, j):
        slot = j % 2

        def tm_block(b, ncols):
            i = wload(WIN_s[l, b], 8 * BW, ("WIN", l, b))
            wv = wbuf[i][:, :].rearrange("p (c n) -> p c n", n=BW)
            for s in range(4):
                gb = gen_bank()
                for c in range(8):
                    mm(ps[gb][:, 0:ncols], hT[:, c, s * 128:(s + 1) * 128], wv[:, c, 0:ncols], c == 0, c == 7,
                       [("wbuf", i), "hT"], [PSN[gb]])
                yield gb, s

        for gb, s in tm_block(9, 280):
            cp(Vs[:, 4 * j + s, :, 0:64], ps[gb][:, 0:128].rearrange("p (g d) -> p g d", d=64), [PSN[gb], ("Vs", "all")],
               [("Vs", j)])
            cp(Vw[:, slot * 4 + s, :, 0:64], ps[gb][:, 128:256].rearrange("p (g d) -> p g d", d=64), [PSN[gb], ("Vw", "all")],
               [("Vw", slot)])
            tt(GL2[:, s, :], ps[gb][:, 256:280], bgate_b[:, l * 24:(l + 1) * 24], ALU.add, [PSN[gb], "bgate"], ["GL2"])
        glf = GL2[:].rearrange("p a b -> p (a b)")
        actv(glf, glf, AF.Tanh, ["GL2"], ["GL2"], scale=0.5)
        ts(glf, glf, 1.0, None, ALU.add, None, ["GL2"], ["GL2"])
        for half in range(2):
            for gb, s in tm_block(10 + half, 256):
                actv(thb[:, 0:256], ps[gb][:, 0:256], AF.Tanh, [PSN[gb]], ["tmpA"], scale=0.5)
                stt(GN[:, s, half * 256:(half + 1) * 256], thb[:, 0:256], 1.0, ps[gb][:, 0:256], ALU.add, ALU.mult,
                    ["tmpA", PSN[gb]], ["GN"])
        for gb, s in tm_block(12, 256):
            actv(thb[:, 0:256], ps[gb][:, 0:256], AF.Tanh, [PSN[gb]], ["tmpA"], scale=0.5)
            stt(GM[:, s, :], thb[:, 0:256], 1.0, ps[gb][:, 0:256], ALU.add, ALU.mult, ["tmpA", PSN[gb]], ["GM"])

        cw = lambda cc, k: convw_t[:, l * 6 + cc * 3 + k:l * 6 + cc * 3 + k + 1]
        for gb, cc in fm_block(l, 6):
            cp(cbuf[:, cc, :], ps[gb][:, :], [PSN[gb], "cb01"], [("cb", cc)])
        uu = tmpA[:, 0:514]
        for gb, cc in fm_block(l, 7):
            cp(uu[:, 0:2], uh[:, cc, :], ["uh", "tmpA"], ["tmpA"])
            tt(uu[:, 2:514], cbuf[:, cc, :], ps[gb][:, :], ALU.mult, [("cb", cc), PSN[gb], "tmpA"], ["tmpA"])
            ak = ("ca", cc)
            ts(cbuf[:, 2 + cc, :], uu[:, 2:514], cw(cc, 2), convb_t[:, l * 2 + cc:l * 2 + cc + 1], ALU.mult, ALU.add,
               ["tmpA", "convw", "convb", "cb23"], [ak])
            stt(cbuf[:, 2 + cc, :], uu[:, 1:513], cw(cc, 1), cbuf[:, 2 + cc, :], ALU.mult, ALU.add, ["tmpA", ak, "convw"], [ak])
            stt(cbuf[:, 2 + cc, :], uu[:, 0:512], cw(cc, 0), cbuf[:, 2 + cc, :], ALU.mult, ALU.add, ["tmpA", ak, "convw"], [ak])
            cp(uh[:, cc, :], uu[:, 512:514], ["tmpA"], ["uh"])
        for gb, cc in fm_block(l, 5):
            ak = ("ca", cc)
            tt(cbuf[:, 2 + cc, :], cbuf[:, 2 + cc, :], ps[gb][:, :], ALU.mult, [ak, PSN[gb]], [ak])
        for gb, cc in fm_block(l, 8):
            ak = ("ca", cc)
            actv(thb[:], ps[gb][:, :], AF.Tanh, [PSN[gb]], ["tmpA"], scale=0.5)
            stt(abuf[:], thb[:], 1.0, ps[gb][:, :], ALU.add, ALU.mult, ["tmpA", PSN[gb]], ["tmpA"])
            stt(yT[:, cc, :], cbuf[:, 2 + cc, :], 0.5, abuf[:], ALU.mult, ALU.mult, [ak, "tmpA"], [("yT", cc)])

    def attention(l, j, par, hook):
        slot = j % 2
        pslot = 1 - slot
        QX = QXs[par]
        PX = PXs[par]
        N = 32 * (j + 1)
        qk = lambda h: [("QX", par, h, "q"), ("QX", par, h, "x")]

        def fin_nsa(br, h, first_write):
            def f(u):
                finalize(u, GL2[:, :, br * 8 + h], 0.25, acc[:, :, h * 64:(h + 1) * 64], ("acc", h), first_write)
            return f

        def fin_mem(h):
            def f(u):
                finalize(u, None, 0.5, accm[:, :, h * 64:(h + 1) * 64], ("accm", h), True)
            return f

        units = []
        for h in range(8):
            g = h // 4
            ob = next_ob()
            tl = []
            if j >= 1:
                for kr in range(4):
                    tl.append((pslot, kr, "ML"))
            for kr in range(4):
                tl.append((slot, kr, "MC"))
            for ti, (sl_, kr, mk) in enumerate(tl):
                mt_ = MLv(kr) if mk == "ML" else MCv(kr)
                mm1 = [(KXw[0:128, g, sl_ * 512 + kr * 128:sl_ * 512 + (kr + 1) * 128], QX[0:128, h, :],
                        [("KXw", g, sl_, "k"), ("KXw", g, sl_, "x")] + qk(h)),
                       (identb[:], mt_, ["identb", mk])]
                units.append(mk_unit(mm1, 128, SLOPES[h], -SLOPES[h] * 512.0 * j, Vw[:, sl_ * 4 + kr, g, :], ("Vw", sl_),
                                     ob, ti == 0, ti == len(tl) - 1, fin_nsa(2, h, True)))
        for h in range(4):
            ob = next_ob()
            for kt in range(2):
                mm1 = [(KM[0:128, h, kt * 128:(kt + 1) * 128], QXm[0:128, h, :], ["KM", ("QXm", h)])]
                units.append(mk_unit(mm1, 128, 1.0, 0.0, VM[:, kt, h, :], "VM", ob, kt == 0, kt == 1, fin_mem(h)))
        nkc = (N - 1) // 128 + 1
        var = 4 if j == 0 else j % 4
        for h in range(8):
            g = h // 4
            ob = next_ob()
            for kt in range(nkc):
                mm1 = [(KXc[0:128, g, kt * 128:kt * 128 + 128], QX[0:128, h, :],
                        [("KXc", g, "k"), ("KXc", g, "x")] + qk(h))]
                if kt == nkc - 1:
                    mm1.append((identb[:], MCMP[:, var, :], ["identb", "MCMP"]))
                units.append(mk_unit(mm1, 128, SLOPES[h], -SLOPES[h] * 512.0 * j, VC[:, kt, g, :], "VCd",
                                     ob, kt == 0, kt == nkc - 1, fin_nsa(0, h, False)))
        run_units(units, None)

        units = []
        for h in range(8):
            g = h // 4
            ob = next_ob()
            nk = 4 * j + 4
            for kt in range(nk):
                c = kt // 30
                pre = None
                if kt % 30 == 0:
                    vb = qv_i[0]
                    qv_i[0] ^= 1

                    def pre(vb=vb, g=g, c=c, h=h):
                        cp(QXv[vb][64:128, :], PX[64:128, g, c, :], [("PX", par, g, c), "PXinit"], [("QXv", vb)], eng="pool")
                        cp(QXv[vb][0:68, :], QX[0:68, h, :], qk(h), [("QXv", vb)], eng="pool")
                mm1 = [(KXs[0:128, g, kt * 128:(kt + 1) * 128], QXv[vb][0:128, :],
                        [("KXs", g, kt // 4), ("KXs", g, "x"), ("QXv", vb)])]
                if kt >= 4 * j:
                    mm1.append((identb[:], MCv(kt - 4 * j), ["identb", "MC"]))
                units.append(mk_unit(mm1, 128, SLOPES[h], -SLOPES[h] * 512.0 * j, Vs[:, kt, g, :], ("Vs", kt // 4),
                                     ob, kt == 0, kt == nk - 1, fin_nsa(1, h, False), pre=pre))
        run_units(units, hook)

    def back(l, j, src_d, dst_d):
        t0 = 512 * j
        xk = lambda s: ("xrow", j * 4 + s)
        accf = acc[:].rearrange("p a b -> p (a b)")
        tt(accf, accf, GN[:].rearrange("p a b -> p (a b)"), ALU.mult, [("acc", h) for h in range(8)] + ["GN"], ["accg"])
        accmf = accm[:].rearrange("p a b -> p (a b)")
        tt(accmf, accmf, GM[:].rearrange("p a b -> p (a b)"), ALU.mult, [("accm", h) for h in range(4)] + ["GM"], ["accmg"])
        for s in range(4):
            for c4 in range(4):
                trn(ps[7][:, c4 * 128:(c4 + 1) * 128], acc[:, s, c4 * 128:(c4 + 1) * 128], identf[:], ["accg", "identf"], [PSN[7]])
            S.add("act", lambda e, s=s: e.activation(out=yT[:, 2:6, s * 128:(s + 1) * 128],
                                                     in_=ps[7][:].rearrange("p (a b) -> p a b", b=128), func=AF.Copy),
                  [PSN[7]], [("yT", 2 + s)])
        for s in range(4):
            for c2 in range(2):
                trn(ps[7][:, c2 * 128:(c2 + 1) * 128], accm[:, s, c2 * 128:(c2 + 1) * 128], identf[:], ["accmg", "identf"], [PSN[7]])
            cp(yT[:, 6:8, s * 128:(s + 1) * 128], ps[7][:, 0:256].rearrange("p (a b) -> p a b", b=128), [PSN[7]], [("yT", 6 + s)])
        yT_all = [("yT", k) for k in range(10)]
        for nb in range(4):
            i = wload(WOUT_s[l, nb], 2048, ("WOUT", l, nb))
            wv = wbuf[i][:, 0:2048].rearrange("p (c n) -> p c n", n=256)
            for s in range(4):
                bk = 2 * s + nb // 2
                c0 = (nb % 2) * 256
                for c in range(8):
                    mm(ps[bk][:, c0:c0 + 256], yT[:, c, s * 128:(s + 1) * 128], wv[:, c, :], c == 0, c == 7,
                       [("wbuf", i)] + yT_all, [PSN[bk]])
        junk = cbuf[:, 0:2, :].rearrange("p a b -> p (a b)")
        ycps = [(ycp[:], ["ycp"]), (cbuf[:, 2:4, :].rearrange("p a b -> p (a b)"), ["cb23", ("ca", 0), ("ca", 1)])]
        for s in range(2):
            dma(xb[s][:], src_d[t0 + s * 128:t0 + (s + 1) * 128, :], [xk(s)], [("xb", s)])
        for s in range(4):
            b_ = s % 2
            yc, yk = ycps[b_]
            o = 16 if b_ == 0 else 32
            S.add("act", lambda e, yc=yc, s=s: e.activation(out=yc[:, 0:512], in_=ps[2 * s][:, :], func=AF.Copy), [PSN[2 * s]], yk)
            cp(yc[:, 512:1024], ps[2 * s + 1][:, :], [PSN[2 * s + 1]], yk)
            stt(junk, yc, 1.0, yc, ALU.mult, ALU.mult, yk + [("cb", 0), ("cb", 1)], ["cb01", ("stp", b_, 0)], accum=st[:, o:o + 1])
            ts(st[:, o + 1:o + 2], st[:, o:o + 1], 1.0 / D, 1e-6, ALU.mult, ALU.add, [("stp", b_, 0)], [("stp", b_, 1)])
            tt(st[:, o + 2:o + 3], st[:, o + 1:o + 2], mhalf[:, 0:1], ALU.pow, [("stp", b_, 1), "mhalf"], [("stp", b_, 2)], eng="pool")
            stt(yc, yc, st[:, o + 2:o + 3], gpost_b[:], ALU.mult, ALU.mult, yk + [("stp", b_, 2), "gpost"], yk)
            tt(yc, yc, xb[b_][:], ALU.add, yk + [("xb", b_)], yk)
            dma(dst_d[t0 + s * 128:t0 + (s + 1) * 128, :], yc, yk, [xk(s)] if dst_d is xs_d else [("orow", j * 4 + s)], q="pool")
            if s + 2 < 4:
                dma(xb[b_][:], src_d[t0 + (s + 2) * 128:t0 + (s + 3) * 128, :], [xk(s + 2)], [("xb", b_)])

    def exhaust(gen):
        for _ in gen:
            pass

    for l in range(L):
        layer_setup(l)
        src_d = x_d if l == 0 else xs_d
        dst_d = out_d if l == L - 1 else xs_d
        nprep = iter(prep_list[l + 1]) if l + 1 < L else iter(())
        exhaust(front_gen(l, 0, 0, src_d))
        front_rest(l, 0)
        for j in range(NT):
            par = j % 2
            nxt = front_gen(l, j + 1, 1 - par, src_d) if j + 1 < NT else None
            attention(l, j, par, nxt)
            if nxt is not None:
                exhaust(nxt)
            for _ in range(2):
                a_ = next(nprep, None)
                if a_ is not None:
                    prep(*a_)
            back(l, j, src_d, dst_d)
            if j + 1 < NT:
                front_rest(l, j + 1)
        for a_ in nprep:
            prep(*a_)

    print("sbuf bytes remaining/partition:", nc.sbuf_bytes_remaining() if callable(nc.sbuf_bytes_remaining) else nc.sbuf_bytes_remaining,
          " ops:", {e: len(v) for e, v in S.ops.items()}, " waits:", S.nwaits)
    S.emit(ctx)
    ctx.close()
    return nc


_NC_CACHE = {}


def run(inp, T, L, n_cores=8):
    f = lambda a: np.ascontiguousarray(np.asarray(a, dtype=np.float32))
    x = f(inp["x"])
    B = x.shape[0]
    key = (T, L)
    if key not in _NC_CACHE:
        _NC_CACHE[key] = build(T, L)
    nc = _NC_CACHE[key]
    shared = host_consts(T)
    shared.update(host_weights(L, f(inp["w_in"]), f(inp["w_out"]), f(inp["cmp_w1_k"]), f(inp["cmp_w1_v"]),
                               f(inp["cmp_w2_k"]), f(inp["cmp_w2_v"]), f(inp["cmp_pos_k"]), f(inp["cmp_pos_v"]),
                               f(inp["w_mem_kv"]), f(inp["conv_w"]), f(inp["conv_b"])))
    shared["gpre"] = f(inp["pre_norm_g"])
    shared["gpost"] = f(inp["post_norm_g"])
    shared["gmem"] = f(inp["mem_norm_g"])
    shared["bgate"] = f(inp["b_gate"])
    mem = f(inp["mem"])
    in_maps = []
    for c in range(n_cores):
        b = c % B
        m = dict(shared)
        m["x"] = np.ascontiguousarray(x[b])
        m["mem"] = np.ascontiguousarray(mem[b])
        in_maps.append(m)
    res = run_bass_kernel_spmd(nc, in_maps, core_ids=list(range(n_cores)))
    out = np.stack([np.asarray(res.results[b]["out"], dtype=np.float32) for b in range(B)], axis=0)
    return out


def kernel(**inputs):
    return run(inputs, 8192, 4)
```

```python
import numpy as np
import ml_dtypes
from contextlib import ExitStack
import concourse.bass as bass
import concourse.mybir as mybir
from concourse.bass_utils import run_bass_kernel_spmd

F32 = mybir.dt.float32
BF16 = mybir.dt.bfloat16
AF = mybir.ActivationFunctionType
ALU = mybir.AluOpType
AX = mybir.AxisListType
NPBF = ml_dtypes.bfloat16

D = 1024
NEG = -1.0e6
SLOPES = [2.0 ** -(h + 1) for h in range(8)]
CH = 60


class Op:
    __slots__ = ("eng", "pos", "fn", "waits", "signal", "token", "is_dma", "lane", "lane_val", "snap")


class Sched:
    def __init__(self, nc, n_lanes=24, same_engine_sync=True):
        self.nc = nc
        self.eng = {"pe": nc.tensor, "act": nc.scalar, "dve": nc.vector, "pool": nc.gpsimd, "sp": nc.sync}
        self.ops = {e: [] for e in self.eng}
        self.lw = {}
        self.rd = {}
        self.known = {e: {} for e in self.eng}
        self.n_lanes = n_lanes
        self.lane_last = [None] * n_lanes
        self.lane_cnt = [0] * n_lanes
        self.next_lane = 0
        self.same_engine_sync = same_engine_sync
        self.nwaits = 0

    def _need(self, op, d, raw=True):
        e = op.eng
        kn = self.known[e]
        if d.is_dma:
            key = ("L", d.lane)
            val = d.lane_val
        else:
            if d.eng == e:
                if e == "pe" or not self.same_engine_sync or not raw:
                    return
            key = d.eng
            val = d.pos
        if kn.get(key, -1) >= val:
            return
        kn[key] = val
        op.waits.append(d)
        d.signal = True
        self.nwaits += 1
        if d.snap is not None:
            for k, v in d.snap:
                if kn.get(k, -1) < v:
                    kn[k] = v

    def add(self, eng, fn, reads=(), writes=(), dma=False):
        op = Op()
        op.eng = eng
        op.fn = fn
        op.waits = []
        op.signal = False
        op.token = None
        op.is_dma = dma
        op.lane = None
        op.lane_val = None
        op.pos = len(self.ops[eng])
        deps = []
        for r in reads:
            w = self.lw.get(r)
            if w is not None:
                deps.append((w, True))
        for r in writes:
            w = self.lw.get(r)
            if w is not None:
                deps.append((w, False))
            rr = self.rd.get(r)
            if rr:
                deps.extend((o, False) for o in rr.values())
        seen = set()
        for d, raw in deps:
            if raw and id(d) in seen:
                continue
            if raw:
                seen.add(id(d))
            self._need(op, d, raw)
        if dma:
            lane = self.next_lane
            self.next_lane = (self.next_lane + 1) % self.n_lanes
            prev = self.lane_last[lane]
            if prev is not None:
                self._need(op, prev)
            self.lane_cnt[lane] += 1
            op.lane = lane
            op.lane_val = self.lane_cnt[lane]
            self.lane_last[lane] = op
        kn = self.known[eng]
        op.snap = tuple((k, v) for k, v in kn.items() if not isinstance(k, tuple))
        self.ops[eng].append(op)
        for r in reads:
            dd = self.rd.setdefault(r, {})
            dd[("D", id(op)) if dma else eng] = op
        for r in writes:
            self.lw[r] = op
            self.rd[r] = {}
        return op

    def emit(self, ctx):
        nc = self.nc
        esem = {e: ctx.enter_context(nc.semaphore("s_" + e)) for e in self.eng}
        lsem = [ctx.enter_context(nc.semaphore("l_%d" % i)) for i in range(self.n_lanes)]
        for e, lst in self.ops.items():
            c = 0
            for op in lst:
                if (not op.is_dma) and op.signal:
                    c += 1
                    op.token = c
        block = ctx.enter_context(nc.Block())
        reg = {"pe": block.tensor, "act": block.scalar, "dve": block.vector, "pool": block.gpsimd, "sp": block.sync}

        def make(e):
            def body(engh):
                for op in self.ops[e]:
                    for d in op.waits:
                        if d.is_dma:
                            engh.wait_ge(lsem[d.lane], 16 * d.lane_val)
                        else:
                            engh.wait_ge(esem[d.eng], d.token)
                    ins = op.fn(engh)
                    if op.is_dma:
                        ins.then_inc(lsem[op.lane], 16)
                    elif op.signal:
                        ins.then_inc(esem[e], 1)
                if e == "sp":
                    for i in range(self.n_lanes):
                        if self.lane_cnt[i]:
                            engh.wait_ge(lsem[i], 16 * self.lane_cnt[i])
            return body

        for e in self.eng:
            reg[e](make(e))


OFF = dict(cB=0, cC=256, ch=512, cg=768, q=1024, kc=1536, vc=1664, ks=1792, vs=1920, kw=2048, vw=2176,
           gl=2304, ng=2328, mq=2840, mg=3096)
NBLK = 13
BW = 288


def _block_cols():
    r = lambda a, n: list(range(a, a + n))
    blocks = [
        r(OFF["q"], 256), r(OFF["q"] + 256, 256),
        r(OFF["ks"], 128) + r(OFF["kw"], 128),
        r(OFF["kc"], 128) + r(OFF["vc"], 128),
        r(OFF["mq"], 256),
        r(OFF["cB"], 256), r(OFF["cC"], 256), r(OFF["ch"], 256), r(OFF["cg"], 256),
        r(OFF["vs"], 128) + r(OFF["vw"], 128) + r(OFF["gl"], 24),
        r(OFF["ng"], 256), r(OFF["ng"] + 256, 256),
        r(OFF["mg"], 256),
    ]
    return blocks


def host_consts(T):
    k = np.arange(128)[:, None]
    q = np.arange(512)[None, :]
    xx = np.arange(896)[None, :] - 384
    mc = np.where(k > xx, NEG, 0.0).astype(np.float32)
    ml = np.where(k <= xx, NEG, 0.0).astype(np.float32)
    mcmp = np.zeros((128, 5, 512), np.float32)
    for v in range(4):
        for rr in range(32):
            mcmp[32 * v + rr, v, :] = np.where(16 * rr + 15 > q[0], NEG, 0.0)
        mcmp[32 * v + 32:, v, :] = NEG
    mcmp[:, 4, :] = mcmp[:, 0, :]
    mcmp[0, 4, :] = NEG
    mq = np.zeros((128, 2, 4, 32), np.float32)
    p = np.arange(128)[:, None]
    rr = np.arange(32)[None, :]
    for s in range(4):
        mq[:, 0, s, :] = np.where(16 * rr + 15 > 128 * s + p, NEG, 0.0)
    mq[:, 1] = mq[:, 0]
    mq[:, 1, :, 0] = NEG
    pos = np.arange(T)
    kexts = np.zeros((64, T), np.float32)
    kexts[0] = pos // 128
    kexts[1] = pos % 128
    kexts[2] = 1.0
    kexts[3] = 1.0
    blk = (pos // 64) % CH
    for r_ in range(CH):
        kexts[4 + r_] = (blk == r_)
    sl = np.arange(512)
    pc = 16 * sl + 15
    kextc = np.stack([pc // 128, pc % 128, np.ones(512), np.ones(512)]).astype(np.float32)
    tq = np.arange(512)
    qext = np.stack([np.full(512, 128.0), np.ones(512), -128.0 * (tq // 128), -1.0 * (tq % 128)]).astype(np.float32)
    colab = np.zeros((128, 2), np.float32)
    colab[:, 0] = np.where(np.arange(128) >= 64, 1e4, -1.0)
    colab[:, 1] = np.where(np.arange(128) < 64, 1e4, -1.0)
    bf = lambda a: np.ascontiguousarray(a).astype(NPBF)
    return dict(mc=bf(mc.reshape(128, -1)), ml=bf(ml.reshape(128, -1)), mcmp=bf(mcmp.reshape(128, -1)),
                mq=bf(mq.reshape(128, -1)), kexts=bf(kexts), kextc=bf(kextc), qext=bf(qext), colab=colab)


def host_weights(L, w_in, w_out, w1k, w1v, w2k, w2v, posk, posv, wm, convw, convb):
    blocks = _block_cols()
    w_in_p = np.zeros((L, NBLK, 128, 8, BW), np.float32)
    for b, cols in enumerate(blocks):
        sub = w_in[:, :, cols]
        w_in_p[:, b, :, :, :len(cols)] = sub.reshape(L, 8, 128, len(cols)).transpose(0, 2, 1, 3)
    w_out_p = w_out.reshape(L, 8, 128, 4, 256).transpose(0, 3, 2, 1, 4)
    w1 = np.stack([w1k, w1v], axis=1)
    w1_p = w1.reshape(L, 2, 16, 128, 2, 128).transpose(0, 1, 4, 3, 2, 5)
    w2 = np.stack([w2k, w2v], axis=1)
    w2_p = w2.reshape(L, 2, 2, 128, 64).transpose(0, 1, 3, 2, 4)
    pos = np.stack([posk, posv], axis=1)
    pos_p = pos.reshape(L, 2, 16, 2, 64).transpose(0, 1, 3, 4, 2).reshape(L, 2, 128, 16)
    wm_p = wm.reshape(L, 8, 128, 2, 256).transpose(0, 3, 2, 1, 4)
    convw_t = convw.reshape(L, 3, 2, 128).transpose(3, 0, 2, 1)
    convb_t = convb.reshape(L, 2, 128).transpose(2, 0, 1)
    c = np.ascontiguousarray
    return dict(w_in_p=c(w_in_p.reshape(L, NBLK, 128, 8 * BW)), w_out_p=c(w_out_p.reshape(L, 4, 128, 2048)),
                w1_p=c(w1_p.reshape(L, 2, 2, 128, 2048)), w2_p=c(w2_p.reshape(L, 2, 128, 128)),
                pos_p=c(pos_p), wm_p=c(wm_p.reshape(L, 2, 128, 2048)),
                convw_t=c(convw_t.reshape(128, L * 6)), convb_t=c(convb_t.reshape(128, L * 2)))


def build(T=8192, L=4, same_engine_sync=True):
    NT = T // 512
    NKT = T // 128
    nc = bass.Bass("TRN2", target_bir_lowering=False)
    dram = lambda name, shape, dt_, kind: nc.dram_tensor(name, shape, dt_, kind=kind).ap()
    EI, EO, IN = "ExternalInput", "ExternalOutput", "Internal"
    x_d = dram("x", [T, D], F32, EI)
    mem_d = dram("mem", [256, D], F32, EI)
    win_d = dram("w_in_p", [L, NBLK, 128, 8 * BW], F32, EI)
    wout_d = dram("w_out_p", [L, 4, 128, 2048], F32, EI)
    w1_d = dram("w1_p", [L, 2, 2, 128, 2048], F32, EI)
    w2_d = dram("w2_p", [L, 2, 128, 128], F32, EI)
    pos_d = dram("pos_p", [L, 2, 128, 16], F32, EI)
    wm_d = dram("wm_p", [L, 2, 128, 2048], F32, EI)
    gpre_d = dram("gpre", [L, D], F32, EI)
    gpost_d = dram("gpost", [L, D], F32, EI)
    gmem_d = dram("gmem", [L, D], F32, EI)
    convw_d = dram("convw_t", [128, L * 6], F32, EI)
    convb_d = dram("convb_t", [128, L * 2], F32, EI)
    bgate_d = dram("bgate", [L, 24], F32, EI)
    mc_d = dram("mc", [128, 896], BF16, EI)
    ml_d = dram("ml", [128, 896], BF16, EI)
    mcmp_d = dram("mcmp", [128, 2560], BF16, EI)
    mq_d = dram("mq", [128, 256], BF16, EI)
    kexts_d = dram("kexts", [64, T], BF16, EI)
    kextc_d = dram("kextc", [4, 512], BF16, EI)
    qext_d = dram("qext", [4, 512], BF16, EI)
    colab_d = dram("colab", [128, 2], F32, EI)
    out_d = dram("out", [T, D], F32, EO)
    xs_d = dram("xs", [T, D], F32, IN)
    WIN_s = dram("WIN_s", [L, NBLK, 128, 8 * BW], BF16, IN)
    WOUT_s = dram("WOUT_s", [L, 4, 128, 2048], BF16, IN)
    W1_s = dram("W1_s", [L, 2, 2, 128, 2048], BF16, IN)
    WM_s = dram("WM_s", [L, 2, 128, 2048], BF16, IN)

    ctx = ExitStack()
    S = Sched(nc, same_engine_sync=same_engine_sync)
    sb = lambda name, shape, dt_=F32: nc.alloc_sbuf_tensor(name, shape, dt_)
    KXs = sb("KXs", [128, 2, T], BF16)
    Vs = sb("Vs", [128, NKT, 2, 65], BF16)
    KXw = sb("KXw", [128, 2, 1024], BF16)
    Vw = sb("Vw", [128, 8, 2, 65], BF16)
    KXc = sb("KXc", [128, 2, 512], BF16)
    VC = sb("VC", [128, 4, 2, 65], BF16)
    KC2 = sb("KC2", [128, 2, 2, 544], BF16)
    KM = sb("KM", [128, 4, 256], BF16)
    VM = sb("VM", [128, 2, 4, 65], BF16)
    QXs = [sb("QX%d" % i, [128, 8, 512], BF16) for i in range(2)]
    QXm = sb("QXm", [128, 4, 512], BF16)
    PXs = [sb("PX%d" % i, [128, 2, 3, 512], BF16) for i in range(2)]
    QXv = [sb("QXv%d" % i, [128, 512], BF16) for i in range(2)]
    MCW = sb("MCW", [128, 896], BF16)
    MLW = sb("MLW", [128, 896], BF16)
    MCMP = sb("MCMP", [128, 5, 512], BF16)
    MQ = sb("MQ", [128, 2, 4, 32], BF16)
    identb = sb("identb", [128, 128], BF16)
    identf = sb("identf", [128, 128], F32)
    gpre_b = sb("gpre_b", [128, D], F32)
    gpost_b = sb("gpost_b", [128, D], F32)
    convw_t = sb("convw_sb", [128, L * 6], F32)
    convb_t = sb("convb_sb", [128, L * 2], F32)
    bgate_b = sb("bgate_b", [128, L * 24], F32)
    colab = sb("colab_sb", [128, 2], F32)
    mhalf = sb("mhalf", [128, 4], F32)
    cbias = sb("cbias", [128, 256], F32)
    w2b = sb("w2b", [128, 2, 128], BF16)
    pos2b = sb("pos2b", [128, 2, 16], BF16)
    xb = [sb("xb%d" % i, [128, D], F32) for i in range(2)]
    hT = sb("hT", [128, 8, 512], BF16)
    NWB = 2
    wbuf = [sb("wbuf%d" % i, [128, 8 * BW], BF16) for i in range(NWB)]
    cbuf = sb("cbuf", [128, 4, 512], F32)
    uh = sb("uh", [128, 2, 2], F32)
    tmpA = sb("tmpA", [128, 1024], F32)
    abuf = tmpA[:, 0:512]
    thb = tmpA[:, 512:1024]
    hidf = tmpA[:, 0:768].rearrange("p (a b) -> p a b", b=256)
    ycp = sb("ycp", [128, D], F32)
    yT = sb("yT", [128, 8, 512], BF16)
    GN = sb("GN", [128, 4, 512], BF16)
    GM = sb("GM", [128, 4, 256], BF16)
    GL2s = [sb("GL2_%d" % i, [128, 4, 24], F32) for i in range(2)]
    pbuf = [sb("pbuf%d" % i, [128, 512], BF16) for i in range(4)]
    OTs = [sb("OTs%d" % i, [128, 512], F32) for i in range(2)]
    acc = sb("acc", [128, 4, 512], F32)
    accm = sb("accm", [128, 4, 256], F32)
    tmpc = sb("tmpc", [128, 4, 64], F32)
    ebuf = [sb("ebuf%d" % i, [128, 512], F32) for i in range(2)]
    imp = sb("imp", [128, 516], F32)
    selv = sb("selv", [128, 128], F32)
    selv2 = sb("selv2", [128, 128], F32)
    Zc = sb("Zc", [128, 3, 128], F32)
    m8 = sb("m8", [128, 16], F32)
    st = sb("st", [128, 40], F32)
    hidb = sb("hidb", [128, 256], BF16)
    vcst = sb("vcst", [32, 2, 64], BF16)

    ps = [nc.alloc_psum_tensor("ps%d" % i, [128, 512], F32) for i in range(8)]
    PSN = ["ps%d" % i for i in range(8)]

    def dma(out, in_, r, w, q="sp"):
        S.add(q, lambda e: e.dma_start(out=out, in_=in_), r, w, dma=True)

    def mm(out, lhsT, rhs, start, stop, r, w):
        S.add("pe", lambda e: e.matmul(out, lhsT=lhsT, rhs=rhs, start=start, stop=stop), r, w)

    def trn(out, in_, ident, r, w):
        S.add("pe", lambda e: e.transpose(out=out, in_=in_, identity=ident), r, w)

    def actv(out, in_, func, r, w, scale=1.0, bias=0.0, accum=None):
        if accum is None:
            S.add("act", lambda e: e.activation(out=out, in_=in_, func=func, bias=bias, scale=scale), r, w)
        else:
            S.add("act", lambda e: e.activation(out=out, in_=in_, func=func, bias=bias, scale=scale, accum_out=accum), r, w)

    def cp(out, in_, r, w, eng="dve"):
        S.add(eng, lambda e: e.tensor_copy(out=out, in_=in_), r, w)

    def ts(out, in0, s1, s2, op0, op1, r, w, eng="dve"):
        if op1 is None:
            S.add(eng, lambda e: e.tensor_scalar(out=out, in0=in0, scalar1=s1, scalar2=None, op0=op0), r, w)
        else:
            S.add(eng, lambda e: e.tensor_scalar(out=out, in0=in0, scalar1=s1, scalar2=s2, op0=op0, op1=op1), r, w)

    def tt(out, in0, in1, op, r, w, eng="dve"):
        S.add(eng, lambda e: e.tensor_tensor(out=out, in0=in0, in1=in1, op=op), r, w)

    def stt(out, in0, scalar, in1, op0, op1, r, w, accum=None):
        if accum is None:
            S.add("dve", lambda e: e.scalar_tensor_tensor(out=out, in0=in0, scalar=scalar, in1=in1, op0=op0, op1=op1), r, w)
        else:
            S.add("dve", lambda e: e.scalar_tensor_tensor(out=out, in0=in0, scalar=scalar, in1=in1, op0=op0, op1=op1, accum_out=accum), r, w)

    def mset(ap, val, w, eng="pool"):
        S.add(eng, lambda e: e.memset(ap, val), (), w)

    dma(MCW[:], mc_d, [], ["MC"])
    dma(MLW[:], ml_d, [], ["ML"])
    dma(MCMP[:].rearrange("p a b -> p (a b)"), mcmp_d, [], ["MCMP"])
    dma(MQ[:].rearrange("p a b c -> p (a b c)"), mq_d, [], ["MQ"])
    dma(colab[:], colab_d, [], ["colab"])
    dma(convw_t[:], convw_d, [], ["convw"])
    dma(convb_t[:], convb_d, [], ["convb"])
    dma(bgate_b[:], bgate_d.rearrange("l n -> (l n)").partition_broadcast(128), [], ["bgate"])
    for par in range(2):
        mset(QXs[par][:].rearrange("p a b -> p (a b)"), 0.0,
             [("QX", par, h, "q") for h in range(8)] + [("QX", par, h, "x") for h in range(8)])
    mset(QXm[:].rearrange("p a b -> p (a b)"), 0.0, [("QXm", h) for h in range(4)])
    mset(KM[:].rearrange("p a b -> p (a b)"), 0.0, ["KM"])
    mset(KXw[:].rearrange("p a b -> p (a b)"), 0.0, [("KXw", g, sl, t) for g in range(2) for sl in range(2) for t in ("k", "x")])
    mset(KXc[:].rearrange("p a b -> p (a b)"), 0.0, [("KXc", g, t) for g in range(2) for t in ("k", "x")])
    for g in range(2):
        dma(KXs[64:128, g, :], kexts_d, [], [("KXs", g, "x")])
        dma(KXc[64:68, g, :], kextc_d, [], [("KXc", g, "x")])
    for par in range(2):
        for h in range(8):
            dma(QXs[par][64:68, h, :], qext_d, [], [("QX", par, h, "x")])
    mset(identf[:], 0.0, ["identf"])
    S.add("pool", lambda e: e.affine_select(out=identf[:], in_=identf[:], pattern=[[-1, 128]], compare_op=ALU.not_equal,
                                            fill=1.0, base=0, channel_multiplier=1), ["identf"], ["identf"])
    cp(identb[:], identf[:], ["identf"], ["identb"])
    mset(mhalf[:], -0.5, ["mhalf"])
    mset(Vs[:].rearrange("p a b c -> p (a b c)"), 1.0, [("Vs", "all")])
    mset(Vw[:].rearrange("p a b c -> p (a b c)"), 1.0, [("Vw", "all")])
    mset(VC[:].rearrange("p a b c -> p (a b c)"), 1.0, [("VC", "all")])
    mset(VM[:].rearrange("p a b c -> p (a b c)"), 1.0, [("VM", "all")])
    mset(KC2[:].rearrange("p a b c -> p (a b c)"), 0.0, ["KC2"])
    for par in range(2):
        mset(PXs[par][:].rearrange("p a b c -> p (a b c)"), 0.0, ["PXinit"])
    mset(Zc[:].rearrange("p a b -> p (a b)"), 0.0, ["Zc"])
    mset(imp[:], 0.0, ["imp"])

    wb_i = [0]

    def next_wb():
        i = wb_i[0]
        wb_i[0] = (i + 1) % NWB
        return i

    def prep(src, dst, n, last, dst_key):
        dma(dst.rearrange("p (c n) -> p c n", n=last), src.rearrange("p (c n) -> p c n", n=last), [], [dst_key], q="pool")

    prep_list = []
    for l in range(L):
        pl = []
        for kv in range(2):
            for hf in range(2):
                pl.append((w1_d[l, kv, hf], W1_s[l, kv, hf], 2048, 128, ("W1", l, kv, hf)))
        for b in range(2):
            pl.append((wm_d[l, b], WM_s[l, b], 2048, 256, ("WM", l, b)))
        for b in range(NBLK):
            pl.append((win_d[l, b], WIN_s[l, b], 8 * BW, BW, ("WIN", l, b)))
        for b in range(4):
            pl.append((wout_d[l, b], WOUT_s[l, b], 2048, 256, ("WOUT", l, b)))
        prep_list.append(pl)
    for a_ in prep_list[0]:
        prep(*a_)

    def wload(src, n, key):
        i = next_wb()
        dma(wbuf[i][:, 0:n], src, [key], [("wbuf", i)])
        return i

    gen_i = [0]

    def gen_bank():
        i = 5 + gen_i[0]
        gen_i[0] ^= 1
        return i

    def norm_A(xt, xkey, gtile, gkey, k):
        o = 24 + 3 * k
        stt(cbuf[:, 0:2, :].rearrange("p a b -> p (a b)"), xt[:], 1.0, xt[:], ALU.mult, ALU.mult,
            [xkey], ["cb01", ("stn", k, 0)], accum=st[:, o:o + 1])
        ts(st[:, o + 1:o + 2], st[:, o:o + 1], 1.0 / D, 1e-6, ALU.mult, ALU.add, [("stn", k, 0)], [("stn", k, 1)])
        tt(st[:, o + 2:o + 3], st[:, o + 1:o + 2], mhalf[:, 0:1], ALU.pow, [("stn", k, 1), "mhalf"], [("stn", k, 2)], eng="pool")
        stt(xt[:], xt[:], st[:, o + 2:o + 3], gtile[:], ALU.mult, ALU.mult, [xkey, ("stn", k, 2), gkey], [xkey])

    def norm_B(xt, xkey, col0):
        for half in range(2):
            for c4 in range(4):
                c = half * 4 + c4
                trn(ps[7][:, c4 * 128:(c4 + 1) * 128], xt[:, c * 128:(c + 1) * 128], identf[:], [xkey, "identf"], [PSN[7]])
            cp(hT[:, half * 4:half * 4 + 4, col0:col0 + 128], ps[7][:].rearrange("p (a b) -> p a b", b=128), [PSN[7]], ["hT"])

    def norm_transpose(xt, xkey, gtile, gkey, col0):
        norm_A(xt, xkey, gtile, gkey, 0)
        norm_B(xt, xkey, col0)

    def _unused(xt, xkey, col0):
        for half in range(2):
            for c4 in range(4):
                c = half * 4 + c4
                trn(ps[7][:, c4 * 128:(c4 + 1) * 128], xt[:, c * 128:(c + 1) * 128], identf[:], [xkey, "identf"], [PSN[7]])
            cp(hT[:, half * 4:half * 4 + 4, col0:col0 + 128], ps[7][:].rearrange("p (a b) -> p a b", b=128),
               [PSN[7]], ["hT"])

    class Unit:
        pass

    def run_units(units, hook=None):
        n = len(units)
        LA = 2
        pend = []
        for i in range(n + LA):
            if i < n:
                u = units[i]
                if hook is not None:
                    next(hook, None)
                if u.pre is not None:
                    u.pre()
                sbk = i % 3
                nmm = len(u.mm1)
                for k, (lh, rh, rd_) in enumerate(u.mm1):
                    mm(ps[sbk][0:u.M, :], lh, rh, k == 0, k == nmm - 1, rd_, [PSN[sbk]])
                pb = i % 4
                actv(pbuf[pb][0:u.M, :], ps[sbk][0:u.M, :], AF.Exp, [PSN[sbk]], [("pbuf", pb)], scale=u.scale, bias=u.bias)
            k2 = i - LA
            if k2 >= 0:
                u = units[k2]
                pb = k2 % 4
                mm(ps[u.ob][0:65, :], u.v, pbuf[pb][0:u.M, :], u.first, u.last, [("pbuf", pb), u.vkey], [PSN[u.ob]])
                if u.last:
                    pend.append([k2 + 3, u])
            while pend and (pend[0][0] <= i or i == n + LA - 1):
                _, u = pend.pop(0)
                u.fin(u)

    ob_i = [0]
    qv_i = [0]

    def next_ob():
        i = 3 + ob_i[0]
        ob_i[0] ^= 1
        return i

    ot_i = [0]

    def finalize(u, gate_ap, const, dst, dkey, first_write, gkey="GL2_0"):
        k = ot_i[0]
        ot_i[0] ^= 1
        cp(OTs[k][0:65, :], ps[u.ob][0:65, :], [PSN[u.ob]], [("OTs", k)])
        for s in range(4):
            trn(ps[7][:, s * 65:(s + 1) * 65], OTs[k][0:65, s * 128:(s + 1) * 128], identf[0:65, 0:65],
                [("OTs", k), "identf"], [PSN[7]])
        pv = ps[7][:, 0:260].rearrange("p (s c) -> p s c", c=65)
        ts(st[:, 20:24], pv[:, :, 64], 1e-30, None, ALU.max, None, [PSN[7]], ["st20"])
        S.add("dve", lambda e: e.reciprocal(out=st[:, 8:12], in_=st[:, 20:24]), ["st20"], ["st8"])
        if gate_ap is None:
            ts(st[:, 12:16], st[:, 8:12], const, None, ALU.mult, None, ["st8"], ["st12"])
        else:
            stt(st[:, 12:16], st[:, 8:12], const, gate_ap, ALU.mult, ALU.mult, ["st8", gkey], ["st12"])
        fb = st[:, 12:16].unsqueeze(2).to_broadcast([128, 4, 64])
        if first_write:
            tt(dst, pv[:, :, 0:64], fb, ALU.mult, [PSN[7], "st12"], [dkey])
        else:
            tt(tmpc[:], pv[:, :, 0:64], fb, ALU.mult, [PSN[7], "st12"], ["tmpc"])
            tt(dst, dst, tmpc[:], ALU.add, [dkey, "tmpc"], [dkey], eng="pool")

    def mk_unit(mm1, M, scale, bias, v, vkey, ob, first, last, fin, pre=None):
        u = Unit()
        u.pre = pre
        u.mm1, u.M, u.scale, u.bias, u.v, u.vkey, u.ob, u.first, u.last, u.fin = mm1, M, scale, bias, v, vkey, ob, first, last, fin
        return u

    MCv = lambda kr: MCW[:, 384 - 128 * kr:896 - 128 * kr]
    MLv = lambda kr: MLW[:, 384 - 128 * kr:896 - 128 * kr]

    def layer_setup(l):
        dma(gpre_b[:], gpre_d[l:l + 1, :].rearrange("a n -> (a n)").partition_broadcast(128), [], ["gpre"])
        dma(gpost_b[:], gpost_d[l:l + 1, :].rearrange("a n -> (a n)").partition_broadcast(128), [], ["gpost"])
        for kv in range(2):
            dma(w2b[:, kv, :], w2_d[l, kv], [], ["w2b"], q="pool")
            dma(pos2b[:, kv, :], pos_d[l, kv], [], ["pos2b"], q="pool")
        gb = gen_bank()
        for kv in range(2):
            for hf in range(2):
                i = wload(W1_s[l, kv, hf], 2048, ("W1", l, kv, hf))
                wv = wbuf[i][:, 0:2048].rearrange("p (c n) -> p c n", n=128)
                col = kv * 2 + hf
                for pp in range(16):
                    mm(ps[gb][:, col:col + 1], wv[:, pp, :], pos2b[:, kv, pp:pp + 1], pp == 0, pp == 15,
                       [("wbuf", i), "pos2b"], [PSN[gb]])
        cbv = cbias[:].rearrange("p (kv g hf r) -> p kv g hf r", kv=2, g=2, hf=2)
        for kv in range(2):
            for g in range(2):
                for hf in range(2):
                    col = kv * 2 + hf
                    cp(cbv[:, kv, g, hf, :], ps[gb][:, col:col + 1].to_broadcast([128, 32]), [PSN[gb]], ["cbias"])
        gmem_b = cbuf[:, 2:4, :].rearrange("p a b -> p (a b)")
        dma(gmem_b, gmem_d[l:l + 1, :].rearrange("a n -> (a n)").partition_broadcast(128), [], ["cb23", ("ca", 0), ("ca", 1)])
        for mt in range(2):
            dma(xb[mt][:], mem_d[mt * 128:(mt + 1) * 128, :], [], [("xb", mt)])
            norm_transpose(xb[mt], ("xb", mt), gmem_b, "cb23", mt * 128)
        ik = wload(WM_s[l, 0], 2048, ("WM", l, 0))
        wk = wbuf[ik][:, 0:2048].rearrange("p (c n) -> p c n", n=256)
        for pr in range(2):
            gb = gen_bank()
            for c in range(8):
                mm(ps[gb][:, 0:256], wk[:, c, pr * 128:(pr + 1) * 128], hT[:, c, 0:256], c == 0, c == 7,
                   [("wbuf", ik), "hT"], [PSN[gb]])
            cp(KM[0:64, 2 * pr, :], ps[gb][0:64, 0:256], [PSN[gb]], ["KM"])
            cp(KM[0:64, 2 * pr + 1, :], ps[gb][64:128, 0:256], [PSN[gb]], ["KM"])
        iv = wload(WM_s[l, 1], 2048, ("WM", l, 1))
        wv_ = wbuf[iv][:, 0:2048].rearrange("p (c n) -> p c n", n=256)
        for mt in range(2):
            gb = gen_bank()
            for c in range(8):
                mm(ps[gb][:, 0:256], hT[:, c, mt * 128:(mt + 1) * 128], wv_[:, c, :], c == 0, c == 7,
                   [("wbuf", iv), "hT"], [PSN[gb]])
            cp(VM[:, mt, :, 0:64], ps[gb][:, 0:256].rearrange("p (h d) -> p h d", d=64), [PSN[gb], ("VM", "all")], ["VM"])
        mset(uh[:].rearrange("p a b -> p (a b)"), 0.0, ["uh"])

    def fm_block(l, b):
        i = wload(WIN_s[l, b], 8 * BW, ("WIN", l, b))
        wv = wbuf[i][:, :].rearrange("p (c n) -> p c n", n=BW)
        for grp in range(2):
            gb = gen_bank()
            for c in range(8):
                mm(ps[gb][:, :], wv[:, c, grp * 128:(grp + 1) * 128], hT[:, c, :], c == 0, c == 7,
                   [("wbuf", i), "hT"], [PSN[gb]])
            yield gb, grp

    def tm_block(l, b, ncols):
        i = wload(WIN_s[l, b], 8 * BW, ("WIN", l, b))
        wv = wbuf[i][:, :].rearrange("p (c n) -> p c n", n=BW)
        for s in range(4):
            gb = gen_bank()
            for c in range(8):
                mm(ps[gb][:, 0:ncols], hT[:, c, s * 128:(s + 1) * 128], wv[:, c, 0:ncols], c == 0, c == 7,
                   [("wbuf", i), "hT"], [PSN[gb]])
            yield gb, s

    def front_gen(l, j, par, src_d):
        t0 = 512 * j
        slot = j % 2
        QX = QXs[par]
        PX = PXs[par]
        xk = lambda s: ("xrow", j * 4 + s)

        def stA(s):
            b_ = s % 2
            dma(xb[b_][:], src_d[t0 + s * 128:t0 + (s + 1) * 128, :], [xk(s)], [("xb", b_)])
            norm_A(xb[b_], ("xb", b_), gpre_b[:], "gpre", s % 2)

        def stB(s):
            norm_B(xb[s % 2], ("xb", s % 2), s * 128)
        for f_, a_ in ((stA, 0), (stA, 1), (stB, 0), (stA, 2), (stB, 1), (stA, 3), (stB, 2), (stB, 3)):
            f_(a_)
            yield
            yield
        for g in range(2):
            dma(KXw[64:68, g, slot * 512:(slot + 1) * 512], kexts_d[0:4, t0:t0 + 512], [], [("KXw", g, slot, "x")])
        for b in range(2):
            for gb, grp in fm_block(l, b):
                yield
                for hh in range(2):
                    h = b * 4 + grp * 2 + hh
                    ts(QX[0:64, h, :], ps[gb][hh * 64:(hh + 1) * 64, :], 1.0 / (8.0 * SLOPES[h]), None, ALU.mult, None,
                       [PSN[gb]], [("QX", par, h, "q")])
                yield
        for gb, grp in fm_block(l, 2):
            yield
            for g in range(2):
                if grp == 0:
                    dst_, dk_ = KXs[0:64, g, t0:t0 + 512], ("KXs", g, j)
                else:
                    dst_, dk_ = KXw[0:64, g, slot * 512:(slot + 1) * 512], ("KXw", g, slot, "k")
                cp(dst_, ps[gb][g * 64:(g + 1) * 64, :], [PSN[gb]], [dk_])
            yield
        for kv in range(2):
            for g in range(2):
                cp(KC2[0:64, kv, g, 0:16], KC2[0:64, kv, g, 512:528], [("KC2", kv, g)], [("KC2", kv, g)], eng="pool")
                cp(KC2[64:128, kv, g, 0:15], KC2[64:128, kv, g, 512:527], [("KC2", kv, g)], [("KC2", kv, g)], eng="pool")
        for gb, grp in fm_block(l, 3):
            yield
            kv = grp
            for g in range(2):
                cp(KC2[0:64, kv, g, 16:528], ps[gb][g * 64:(g + 1) * 64, :], [PSN[gb], "KC2"], [("KC2", kv, g)])
                cp(KC2[64:128, kv, g, 15:527], ps[gb][g * 64:(g + 1) * 64, :], [PSN[gb], "KC2"], [("KC2", kv, g)])
            yield
        for gb, grp in fm_block(l, 4):
            yield
            for hh in range(2):
                h = grp * 2 + hh
                ts(QXm[0:64, h, :], ps[gb][hh * 64:(hh + 1) * 64, :], 0.125, None, ALU.mult, None, [PSN[gb]], [("QXm", h)])
            yield
        GL2 = GL2s[par]
        gk = "GL2_%d" % par
        for gb, s in tm_block(l, 9, 280):
            yield
            cp(Vs[:, 4 * j + s, :, 0:64], ps[gb][:, 0:128].rearrange("p (g d) -> p g d", d=64), [PSN[gb], ("Vs", "all")],
               [("Vs", j)])
            cp(Vw[:, slot * 4 + s, :, 0:64], ps[gb][:, 128:256].rearrange("p (g d) -> p g d", d=64), [PSN[gb], ("Vw", "all")],
               [("Vw", slot)])
            tt(GL2[:, s, :], ps[gb][:, 256:280], bgate_b[:, l * 24:(l + 1) * 24], ALU.add, [PSN[gb], "bgate"], [gk])
            yield
        glf = GL2[:].rearrange("p a b -> p (a b)")
        actv(glf, glf, AF.Tanh, [gk], [gk], scale=0.5)
        yield
        ts(glf, glf, 1.0, None, ALU.add, None, [gk], [gk])
        yield

        hb = gen_bank()
        for kv in range(2):
            for hf in range(2):
                i = wload(W1_s[l, kv, hf], 2048, ("W1", l, kv, hf))
                wv = wbuf[i][:, 0:2048].rearrange("p (c n) -> p c n", n=128)
                for g in range(2):
                    col = ((kv * 2 + g) * 2 + hf) * 32
                    for pp in range(16):
                        rhs = KC2[:, kv, g, 2 * pp:2 * pp + 512].rearrange("p (r s) -> p r s", s=16)[:, :, 0]
                        mm(ps[hb][:, col:col + 32], wv[:, pp, :], rhs, pp == 0, pp == 15,
                           [("wbuf", i), ("KC2", kv, g)], [PSN[hb]])
                    yield
        u_ = hidf[:, 0, :]
        v_ = hidf[:, 1, :]
        w_ = hidf[:, 2, :]
        tt(u_, ps[hb][:, 0:256], cbias[:], ALU.add, [PSN[hb], "cbias"], ["tmpA"])
        tt(v_, u_, u_, ALU.mult, ["tmpA"], ["tmpA"])
        ts(v_, v_, 0.044715, 1.0, ALU.mult, ALU.add, ["tmpA"], ["tmpA"])
        tt(v_, v_, u_, ALU.mult, ["tmpA"], ["tmpA"])
        yield
        yield
        actv(w_, v_, AF.Tanh, ["tmpA"], ["tmpA"], scale=0.7978845608028654)
        yield
        stt(hidb[:], w_, 1.0, u_, ALU.add, ALU.mult, ["tmpA"], ["hidb"])
        yield
        slot_lo = 32 * j
        for g in range(2):
            gb = gen_bank()
            for hf in range(2):
                col = ((0 * 2 + g) * 2 + hf) * 32
                mm(ps[gb][0:64, 0:32], w2b[:, 0, hf * 64:(hf + 1) * 64], hidb[:, col:col + 32], hf == 0, hf == 1,
                   ["w2b", "hidb"], [PSN[gb]])
            for hf in range(2):
                col = ((1 * 2 + g) * 2 + hf) * 32
                mm(ps[gb][0:32, 64:128], hidb[:, col:col + 32], w2b[:, 1, hf * 64:(hf + 1) * 64], hf == 0, hf == 1,
                   ["w2b", "hidb"], [PSN[gb]])
            yield
            ts(KXc[0:64, g, slot_lo:slot_lo + 32], ps[gb][0:64, 0:32], 0.5, None, ALU.mult, None, [PSN[gb]], [("KXc", g, "k")])
            ts(vcst[:, g, :], ps[gb][0:32, 64:128], 0.5, None, ALU.mult, None, [PSN[gb]], ["vcst"])
            yield
        pr0 = slot_lo % 128
        dma(VC[pr0:pr0 + 32, slot_lo // 128, :, 0:64], vcst[:], ["vcst", ("VC", "all")], ["VCd"])

        N = 32 * (j + 1)
        NB = 8 * (j + 1)
        nch = (NB - 1) // CH + 1
        mqv = 1 if j == 0 else 0
        for g in range(2):
            for s in range(4):
                for r_ in range(4):
                    h = g * 4 + r_
                    gb = gen_bank()
                    mm(ps[gb][:, 0:N], QX[0:128, h, s * 128:(s + 1) * 128], KXc[0:128, g, 0:N], True, False,
                       [("QX", par, h, "q"), ("QX", par, h, "x"), ("KXc", g, "k"), ("KXc", g, "x")], [PSN[gb]])
                    mm(ps[gb][:, N - 32:N], identb[:], MQ[:, mqv, s, :], False, True, ["identb", "MQ"], [PSN[gb]])
                    yield
                    eb = ebuf[r_ % 2]
                    ek = ("ebuf", r_ % 2)
                    actv(eb[:, 0:N], ps[gb][:, 0:N], AF.Exp, [PSN[gb]], [ek, "st4"], scale=SLOPES[h],
                         bias=-SLOPES[h] * 512.0 * j, accum=st[:, 4:5])
                    yield
                    ts(st[:, 6:7], st[:, 4:5], 1e-30, None, ALU.max, None, ["st4"], ["st6"])
                    S.add("dve", lambda e: e.reciprocal(out=st[:, 5:6], in_=st[:, 6:7]), ["st6"], ["st5"])
                    if r_ == 0:
                        ts(imp[:, 0:N], eb[:, 0:N], st[:, 5:6], None, ALU.mult, None, [ek, "st5"], ["imp"])
                    else:
                        stt(imp[:, 0:N], eb[:, 0:N], st[:, 5:6], imp[:, 0:N], ALU.mult, ALU.add, [ek, "st5", "imp"], ["imp"])
                mset(imp[:, N:N + 1], 0.0, ["imp"], eng="dve")
                chb = ebuf[0]
                tt(chb[:, 0:N], imp[:, 0:N], imp[:, 1:N + 1], ALU.add, ["imp"], [("ebuf", 0)])
                S.add("dve", lambda e, chb=chb, NB=NB, N=N: e.tensor_reduce(
                    out=selv[:, 0:NB], in_=chb[:, 0:N].rearrange("p (n f) -> p n f", f=4), axis=AX.X, op=ALU.add),
                    [("ebuf", 0)], ["selv"])
                lo = 8 * j + 2 * s
                if lo + 2 < 128:
                    mset(selv[:, lo + 2:128], -1.0, ["selv"], eng="dve")
                cp(selv[:, lo + 1:lo + 2], colab[:, 0:1], ["colab", "selv"], ["selv"])
                mset(selv[:, lo:lo + 1], 1.0e4, ["selv"], eng="dve")
                if lo - 1 >= 1:
                    ts(selv[:, lo - 1:lo], selv[:, lo - 1:lo], colab[:, 1:2], None, ALU.max, None, ["selv", "colab"], ["selv"])
                mset(selv[:, 0:1], 1.0e4, ["selv"], eng="dve")
                S.add("dve", lambda e: e.max(out=m8[:, 0:8], in_=selv[:]), ["selv"], ["m8"])
                S.add("dve", lambda e: e.match_replace(out=selv2[:], in_to_replace=m8[:, 0:8], in_values=selv[:], imm_value=-2.0),
                      ["selv", "m8"], ["selv2"])
                S.add("dve", lambda e: e.max(out=m8[:, 8:16], in_=selv2[:]), ["selv2"], ["m8b"])
                for c in range(nch):
                    n0 = CH * c
                    n1 = min(128, n0 + CH)
                    ts(Zc[:, c, 68:68 + (n1 - n0)], selv[:, n0:n1], m8[:, 15:16], NEG, ALU.is_lt, ALU.mult,
                       ["selv", "m8b"], ["Zc"])
                for _ in range(12):
                    yield
                for c in range(nch):
                    trn(ps[7][:, c * 128:(c + 1) * 128], Zc[:, c, :], identf[:], ["Zc", "identf"], [PSN[7]])
                cp(PX[64:128, g, 0:nch, s * 128:(s + 1) * 128],
                   ps[7][64:128, 0:nch * 128].rearrange("p (a b) -> p a b", b=128), [PSN[7], "PXinit"],
                   [("PX", par, g, c) for c in range(nch)])
                yield

    def rest_gen(l, j):
        for half in range(2):
            for gb, s in tm_block(l, 10 + half, 256):
                yield
                actv(thb[:, 0:256], ps[gb][:, 0:256], AF.Tanh, [PSN[gb]], ["tmpA"], scale=0.5)
                yield
                stt(GN[:, s, half * 256:(half + 1) * 256], thb[:, 0:256], 1.0, ps[gb][:, 0:256], ALU.add, ALU.mult,
                    ["tmpA", PSN[gb]], ["GN"])
        for gb, s in tm_block(l, 12, 256):
            yield
            actv(thb[:, 0:256], ps[gb][:, 0:256], AF.Tanh, [PSN[gb]], ["tmpA"], scale=0.5)
            yield
            stt(GM[:, s, :], thb[:, 0:256], 1.0, ps[gb][:, 0:256], ALU.add, ALU.mult, ["tmpA", PSN[gb]], ["GM"])
        cw = lambda cc, k: convw_t[:, l * 6 + cc * 3 + k:l * 6 + cc * 3 + k + 1]
        for gb, cc in fm_block(l, 6):
            yield
            cp(cbuf[:, cc, :], ps[gb][:, :], [PSN[gb], "cb01"], [("cb", cc)])
        uu = tmpA[:, 0:514]
        for gb, cc in fm_block(l, 7):
            yield
            cp(uu[:, 0:2], uh[:, cc, :], ["uh", "tmpA"], ["tmpA"])
            tt(uu[:, 2:514], cbuf[:, cc, :], ps[gb][:, :], ALU.mult, [("cb", cc), PSN[gb], "tmpA"], ["tmpA"])
            ak = ("ca", cc)
            ts(cbuf[:, 2 + cc, :], uu[:, 2:514], cw(cc, 2), convb_t[:, l * 2 + cc:l * 2 + cc + 1], ALU.mult, ALU.add,
               ["tmpA", "convw", "convb", "cb23"], [ak])
            stt(cbuf[:, 2 + cc, :], uu[:, 1:513], cw(cc, 1), cbuf[:, 2 + cc, :], ALU.mult, ALU.add, ["tmpA", ak, "convw"], [ak])
            stt(cbuf[:, 2 + cc, :], uu[:, 0:512], cw(cc, 0), cbuf[:, 2 + cc, :], ALU.mult, ALU.add, ["tmpA", ak, "convw"], [ak])
            cp(uh[:, cc, :], uu[:, 512:514], ["tmpA"], ["uh"])
            yield
        for gb, cc in fm_block(l, 5):
            yield
            ak = ("ca", cc)
            tt(cbuf[:, 2 + cc, :], cbuf[:, 2 + cc, :], ps[gb][:, :], ALU.mult, [ak, PSN[gb]], [ak])
        for gb, cc in fm_block(l, 8):
            yield
            ak = ("ca", cc)
            actv(thb[:], ps[gb][:, :], AF.Tanh, [PSN[gb]], ["tmpA"], scale=0.5)
            yield
            stt(abuf[:], thb[:], 1.0, ps[gb][:, :], ALU.add, ALU.mult, ["tmpA", PSN[gb]], ["tmpA"])
            stt(yT[:, cc, :], cbuf[:, 2 + cc, :], 0.5, abuf[:], ALU.mult, ALU.mult, [ak, "tmpA"], [("yT", cc)])
            yield

    def attention(l, j, par, hook1, hook):
        slot = j % 2
        pslot = 1 - slot
        QX = QXs[par]
        PX = PXs[par]
        N = 32 * (j + 1)
        qk = lambda h: [("QX", par, h, "q"), ("QX", par, h, "x")]

        def fin_nsa(br, h, first_write):
            def f(u):
                finalize(u, GL2s[par][:, :, br * 8 + h], 0.25, acc[:, :, h * 64:(h + 1) * 64], ("acc", h), first_write,
                         gkey="GL2_%d" % par)
            return f

        def fin_mem(h):
            def f(u):
                finalize(u, None, 0.5, accm[:, :, h * 64:(h + 1) * 64], ("accm", h), True)
            return f

        units = []
        for h in range(8):
            g = h // 4
            ob = next_ob()
            tl = []
            if j >= 1:
                for kr in range(4):
                    tl.append((pslot, kr, "ML"))
            for kr in range(4):
                tl.append((slot, kr, "MC"))
            for ti, (sl_, kr, mk) in enumerate(tl):
                mt_ = MLv(kr) if mk == "ML" else MCv(kr)
                mm1 = [(KXw[0:128, g, sl_ * 512 + kr * 128:sl_ * 512 + (kr + 1) * 128], QX[0:128, h, :],
                        [("KXw", g, sl_, "k"), ("KXw", g, sl_, "x")] + qk(h)),
                       (identb[:], mt_, ["identb", mk])]
                units.append(mk_unit(mm1, 128, SLOPES[h], -SLOPES[h] * 512.0 * j, Vw[:, sl_ * 4 + kr, g, :], ("Vw", sl_),
                                     ob, ti == 0, ti == len(tl) - 1, fin_nsa(2, h, True)))
        for h in range(4):
            ob = next_ob()
            for kt in range(2):
                mm1 = [(KM[0:128, h, kt * 128:(kt + 1) * 128], QXm[0:128, h, :], ["KM", ("QXm", h)])]
                units.append(mk_unit(mm1, 128, 1.0, 0.0, VM[:, kt, h, :], "VM", ob, kt == 0, kt == 1, fin_mem(h)))
        nkc = (N - 1) // 128 + 1
        var = 4 if j == 0 else j % 4
        for h in range(8):
            g = h // 4
            ob = next_ob()
            for kt in range(nkc):
                mm1 = [(KXc[0:128, g, kt * 128:kt * 128 + 128], QX[0:128, h, :],
                        [("KXc", g, "k"), ("KXc", g, "x")] + qk(h))]
                if kt == nkc - 1:
                    mm1.append((identb[:], MCMP[:, var, :], ["identb", "MCMP"]))
                units.append(mk_unit(mm1, 128, SLOPES[h], -SLOPES[h] * 512.0 * j, VC[:, kt, g, :], "VCd",
                                     ob, kt == 0, kt == nkc - 1, fin_nsa(0, h, False)))
        run_units(units, hook1)

        units = []
        for h in range(8):
            g = h // 4
            ob = next_ob()
            nk = 4 * j + 4
            for kt in range(nk):
                c = kt // 30
                pre = None
                if kt % 30 == 0:
                    vb = qv_i[0]
                    qv_i[0] ^= 1

                    def pre(vb=vb, g=g, c=c, h=h):
                        cp(QXv[vb][64:128, :], PX[64:128, g, c, :], [("PX", par, g, c), "PXinit"], [("QXv", vb)], eng="pool")
                        cp(QXv[vb][0:68, :], QX[0:68, h, :], qk(h), [("QXv", vb)], eng="pool")
                mm1 = [(KXs[0:128, g, kt * 128:(kt + 1) * 128], QXv[vb][0:128, :],
                        [("KXs", g, kt // 4), ("KXs", g, "x"), ("QXv", vb)])]
                if kt >= 4 * j:
                    mm1.append((identb[:], MCv(kt - 4 * j), ["identb", "MC"]))
                units.append(mk_unit(mm1, 128, SLOPES[h], -SLOPES[h] * 512.0 * j, Vs[:, kt, g, :], ("Vs", kt // 4),
                                     ob, kt == 0, kt == nk - 1, fin_nsa(1, h, False), pre=pre))
        run_units(units, hook)

    def stage_H(l, j):
        accf = acc[:].rearrange("p a b -> p (a b)")
        tt(accf, accf, GN[:].rearrange("p a b -> p (a b)"), ALU.mult, [("acc", h) for h in range(8)] + ["GN"],
           ["accg"] + [("acc", h) for h in range(8)])
        accmf = accm[:].rearrange("p a b -> p (a b)")
        tt(accmf, accmf, GM[:].rearrange("p a b -> p (a b)"), ALU.mult, [("accm", h) for h in range(4)] + ["GM"],
           ["accmg"] + [("accm", h) for h in range(4)])
        for s in range(4):
            for c4 in range(4):
                trn(ps[7][:, c4 * 128:(c4 + 1) * 128], acc[:, s, c4 * 128:(c4 + 1) * 128], identf[:],
                    ["accg", "identf"] + [("acc", h) for h in range(8)], [PSN[7]])
            cp(yT[:, 2:6, s * 128:(s + 1) * 128], ps[7][:].rearrange("p (a b) -> p a b", b=128), [PSN[7]], [("yT", 2 + s)])
        for s in range(4):
            for c2 in range(2):
                trn(ps[7][:, c2 * 128:(c2 + 1) * 128], accm[:, s, c2 * 128:(c2 + 1) * 128], identf[:],
                    ["accmg", "identf"] + [("accm", h) for h in range(4)], [PSN[7]])
            cp(yT[:, 6:8, s * 128:(s + 1) * 128], ps[7][:, 0:256].rearrange("p (a b) -> p a b", b=128), [PSN[7]], [("yT", 6 + s)])

    def back_gen(l, j, src_d, dst_d):
        t0 = 512 * j
        xk = lambda s: ("xrow", j * 4 + s)
        yT_all = [("yT", k) for k in range(10)]
        junk = cbuf[:, 0:2, :].rearrange("p a b -> p (a b)")
        ycps = [(ycp[:], ["ycp"]), (cbuf[:, 2:4, :].rearrange("p a b -> p (a b)"), ["cb23", ("ca", 0), ("ca", 1)])]
        for sp_ in range(2):
            subs = (2 * sp_, 2 * sp_ + 1)
            for s in subs:
                dma(xb[s % 2][:], src_d[t0 + s * 128:t0 + (s + 1) * 128, :], [xk(s)], [("xb", s % 2)])
            for half in range(2):
                wvs = []
                for nb2 in range(2):
                    i = wload(WOUT_s[l, 2 * half + nb2], 2048, ("WOUT", l, 2 * half + nb2))
                    wvs.append((i, wbuf[i][:, 0:2048].rearrange("p (c n) -> p c n", n=256)))
                for s in subs:
                    yc, yk = ycps[s % 2]
                    gb = gen_bank()
                    for nb2 in range(2):
                        i, wv = wvs[nb2]
                        for c in range(8):
                            mm(ps[gb][:, nb2 * 256:(nb2 + 1) * 256], yT[:, c, s * 128:(s + 1) * 128], wv[:, c, :], c == 0, c == 7,
                               [("wbuf", i)] + yT_all, [PSN[gb]])
                        yield
                    cp(yc[:, half * 512:(half + 1) * 512], ps[gb][:, :], [PSN[gb]], yk)
                    yield
            for s in subs:
                b_ = s % 2
                yc, yk = ycps[b_]
                o = 16 if b_ == 0 else 32
                stt(junk, yc, 1.0, yc, ALU.mult, ALU.mult, yk + [("cb", 0), ("cb", 1)], ["cb01", ("stp", b_, 0)], accum=st[:, o:o + 1])
                ts(st[:, o + 1:o + 2], st[:, o:o + 1], 1.0 / D, 1e-6, ALU.mult, ALU.add, [("stp", b_, 0)], [("stp", b_, 1)])
                tt(st[:, o + 2:o + 3], st[:, o + 1:o + 2], mhalf[:, 0:1], ALU.pow, [("stp", b_, 1), "mhalf"], [("stp", b_, 2)], eng="pool")
                yield
                stt(yc, yc, st[:, o + 2:o + 3], gpost_b[:], ALU.mult, ALU.mult, yk + [("stp", b_, 2), "gpost"], yk)
                tt(yc, yc, xb[b_][:], ALU.add, yk + [("xb", b_)], yk)
                dma(dst_d[t0 + s * 128:t0 + (s + 1) * 128, :], yc, yk, [xk(s)] if dst_d is xs_d else [("orow", j * 4 + s)], q="pool")
                yield

    def exhaust(gen):
        for _ in gen:
            pass

    import itertools
    for l in range(L):
        layer_setup(l)
        src_d = x_d if l == 0 else xs_d
        dst_d = out_d if l == L - 1 else xs_d
        nprep = iter(prep_list[l + 1]) if l + 1 < L else iter(())
        exhaust(front_gen(l, 0, 0, src_d))
        exhaust(rest_gen(l, 0))
        pending = iter(())
        for j in range(NT):
            par = j % 2
            nxt = front_gen(l, j + 1, 1 - par, src_d) if j + 1 < NT else iter(())
            stream = itertools.chain(pending, nxt)
            attention(l, j, par, pending, stream)
            exhaust(stream)
            for _ in range(2):
                a_ = next(nprep, None)
                if a_ is not None:
                    prep(*a_)
            stage_H(l, j)
            pending = itertools.chain(back_gen(l, j, src_d, dst_d), rest_gen(l, j + 1) if j + 1 < NT else iter(()))
        exhaust(pending)
        for a_ in nprep:
            prep(*a_)

    print("sbuf bytes remaining/partition:", nc.sbuf_bytes_remaining() if callable(nc.sbuf_bytes_remaining) else nc.sbuf_bytes_remaining,
          " ops:", {e: len(v) for e, v in S.ops.items()}, " waits:", S.nwaits)
    S.emit(ctx)
    ctx.close()
    return nc


_NC_CACHE = {}


def run(inp, T, L, n_cores=8):
    f = lambda a: np.ascontiguousarray(np.asarray(a, dtype=np.float32))
    x = f(inp["x"])
    B = x.shape[0]
    key = (T, L)
    if key not in _NC_CACHE:
        _NC_CACHE[key] = build(T, L)
    nc = _NC_CACHE[key]
    shared = host_consts(T)
    shared.update(host_weights(L, f(inp["w_in"]), f(inp["w_out"]), f(inp["cmp_w1_k"]), f(inp["cmp_w1_v"]),
                               f(inp["cmp_w2_k"]), f(inp["cmp_w2_v"]), f(inp["cmp_pos_k"]), f(inp["cmp_pos_v"]),
                               f(inp["w_mem_kv"]), f(inp["conv_w"]), f(inp["conv_b"])))
    shared["gpre"] = f(inp["pre_norm_g"])
    shared["gpost"] = f(inp["post_norm_g"])
    shared["gmem"] = f(inp["mem_norm_g"])
    shared["bgate"] = f(inp["b_gate"])
    mem = f(inp["mem"])
    in_maps = []
    for c in range(n_cores):
        b = c % B
        m = dict(shared)
        m["x"] = np.ascontiguousarray(x[b])
        m["mem"] = np.ascontiguousarray(mem[b])
        in_maps.append(m)
    res = run_bass_kernel_spmd(nc, in_maps, core_ids=list(range(n_cores)))
    out = np.stack([np.asarray(res.results[b]["out"], dtype=np.float32) for b in range(B)], axis=0)
    return out


def kernel(**inputs):
    return run(inputs, 8192, 4)
```

```python
import numpy as np
import ml_dtypes
from contextlib import ExitStack
import concourse.bass as bass
import concourse.mybir as mybir
from concourse.bass_utils import run_bass_kernel_spmd

F32 = mybir.dt.float32
BF16 = mybir.dt.bfloat16
AF = mybir.ActivationFunctionType
ALU = mybir.AluOpType
AX = mybir.AxisListType
NPBF = ml_dtypes.bfloat16

D = 1024
NEG = -1.0e6
SLOPES = [2.0 ** -(h + 1) for h in range(8)]
CH = 60


class Op:
    __slots__ = ("eng", "pos", "fn", "waits", "signal", "token", "is_dma", "lane", "lane_val", "snap")


class Sched:
    def __init__(self, nc, n_lanes=24, same_engine_sync=True):
        self.nc = nc
        self.eng = {"pe": nc.tensor, "act": nc.scalar, "dve": nc.vector, "pool": nc.gpsimd, "sp": nc.sync}
        self.ops = {e: [] for e in self.eng}
        self.lw = {}
        self.rd = {}
        self.known = {e: {} for e in self.eng}
        self.n_lanes = n_lanes
        self.lane_last = [None] * n_lanes
        self.lane_cnt = [0] * n_lanes
        self.next_lane = 0
        self.same_engine_sync = same_engine_sync
        self.nwaits = 0

    def _need(self, op, d, raw=True):
        e = op.eng
        kn = self.known[e]
        if d.is_dma:
            key = ("L", d.lane)
            val = d.lane_val
        else:
            if d.eng == e:
                if e == "pe" or not self.same_engine_sync or not raw:
                    return
            key = d.eng
            val = d.pos
        if kn.get(key, -1) >= val:
            return
        kn[key] = val
        op.waits.append(d)
        d.signal = True
        self.nwaits += 1
        if d.snap is not None:
            for k, v in d.snap:
                if kn.get(k, -1) < v:
                    kn[k] = v

    def add(self, eng, fn, reads=(), writes=(), dma=False):
        op = Op()
        op.eng = eng
        op.fn = fn
        op.waits = []
        op.signal = False
        op.token = None
        op.is_dma = dma
        op.lane = None
        op.lane_val = None
        op.pos = len(self.ops[eng])
        deps = []
        for r in reads:
            w = self.lw.get(r)
            if w is not None:
                deps.append((w, True))
        for r in writes:
            w = self.lw.get(r)
            if w is not None:
                deps.append((w, False))
            rr = self.rd.get(r)
            if rr:
                deps.extend((o, False) for o in rr.values())
        seen = set()
        for d, raw in deps:
            if raw and id(d) in seen:
                continue
            if raw:
                seen.add(id(d))
            self._need(op, d, raw)
        if dma:
            lane = self.next_lane
            self.next_lane = (self.next_lane + 1) % self.n_lanes
            prev = self.lane_last[lane]
            if prev is not None:
                self._need(op, prev)
            self.lane_cnt[lane] += 1
            op.lane = lane
            op.lane_val = self.lane_cnt[lane]
            self.lane_last[lane] = op
        kn = self.known[eng]
        op.snap = tuple((k, v) for k, v in kn.items() if not isinstance(k, tuple))
        self.ops[eng].append(op)
        for r in reads:
            dd = self.rd.setdefault(r, {})
            dd[("D", id(op)) if dma else eng] = op
        for r in writes:
            self.lw[r] = op
            self.rd[r] = {}
        return op

    def emit(self, ctx):
        nc = self.nc
        esem = {e: ctx.enter_context(nc.semaphore("s_" + e)) for e in self.eng}
        lsem = [ctx.enter_context(nc.semaphore("l_%d" % i)) for i in range(self.n_lanes)]
        for e, lst in self.ops.items():
            c = 0
            for op in lst:
                if (not op.is_dma) and op.signal:
                    c += 1
                    op.token = c
        block = ctx.enter_context(nc.Block())
        reg = {"pe": block.tensor, "act": block.scalar, "dve": block.vector, "pool": block.gpsimd, "sp": block.sync}

        def make(e):
            def body(engh):
                for op in self.ops[e]:
                    for d in op.waits:
                        if d.is_dma:
                            engh.wait_ge(lsem[d.lane], 16 * d.lane_val)
                        else:
                            engh.wait_ge(esem[d.eng], d.token)
                    ins = op.fn(engh)
                    if op.is_dma:
                        ins.then_inc(lsem[op.lane], 16)
                    elif op.signal:
                        ins.then_inc(esem[e], 1)
                if e == "sp":
                    for i in range(self.n_lanes):
                        if self.lane_cnt[i]:
                            engh.wait_ge(lsem[i], 16 * self.lane_cnt[i])
            return body

        for e in self.eng:
            reg[e](make(e))


OFF = dict(cB=0, cC=256, ch=512, cg=768, q=1024, kc=1536, vc=1664, ks=1792, vs=1920, kw=2048, vw=2176,
           gl=2304, ng=2328, mq=2840, mg=3096)
NBLK = 13
BW = 288


def _block_cols():
    r = lambda a, n: list(range(a, a + n))
    blocks = [
        r(OFF["q"], 256), r(OFF["q"] + 256, 256),
        r(OFF["ks"], 128) + r(OFF["kw"], 128),
        r(OFF["kc"], 128) + r(OFF["vc"], 128),
        r(OFF["mq"], 256),
        r(OFF["cB"], 256), r(OFF["cC"], 256), r(OFF["ch"], 256), r(OFF["cg"], 256),
        r(OFF["vs"], 128) + r(OFF["vw"], 128) + r(OFF["gl"], 24),
        r(OFF["ng"], 256), r(OFF["ng"] + 256, 256),
        r(OFF["mg"], 256),
    ]
    return blocks


def host_consts(T):
    k = np.arange(128)[:, None]
    q = np.arange(512)[None, :]
    xx = np.arange(896)[None, :] - 384
    mc = np.where(k > xx, NEG, 0.0).astype(np.float32)
    ml = np.where(k <= xx, NEG, 0.0).astype(np.float32)
    mcmp = np.zeros((128, 5, 512), np.float32)
    for v in range(4):
        for rr in range(32):
            mcmp[32 * v + rr, v, :] = np.where(16 * rr + 15 > q[0], NEG, 0.0)
        mcmp[32 * v + 32:, v, :] = NEG
    mcmp[:, 4, :] = mcmp[:, 0, :]
    mcmp[0, 4, :] = NEG
    mq = np.zeros((128, 2, 4, 32), np.float32)
    p = np.arange(128)[:, None]
    rr = np.arange(32)[None, :]
    for s in range(4):
        mq[:, 0, s, :] = np.where(16 * rr + 15 > 128 * s + p, NEG, 0.0)
    mq[:, 1] = mq[:, 0]
    mq[:, 1, :, 0] = NEG
    pos = np.arange(T)
    kexts = np.zeros((64, T), np.float32)
    kexts[0] = pos // 128
    kexts[1] = pos % 128
    kexts[2] = 1.0
    kexts[3] = 1.0
    blk = (pos // 64) % CH
    for r_ in range(CH):
        kexts[4 + r_] = (blk == r_)
    sl = np.arange(512)
    pc = 16 * sl + 15
    kextc = np.stack([pc // 128, pc % 128, np.ones(512), np.ones(512)]).astype(np.float32)
    tq = np.arange(512)
    qext = np.stack([np.full(512, 128.0), np.ones(512), -128.0 * (tq // 128), -1.0 * (tq % 128)]).astype(np.float32)
    colab = np.zeros((128, 2), np.float32)
    colab[:, 0] = np.where(np.arange(128) >= 64, 1e4, -1.0)
    colab[:, 1] = np.where(np.arange(128) < 64, 1e4, -1.0)
    bf = lambda a: np.ascontiguousarray(a).astype(NPBF)
    return dict(mc=bf(mc.reshape(128, -1)), ml=bf(ml.reshape(128, -1)), mcmp=bf(mcmp.reshape(128, -1)),
                mq=bf(mq.reshape(128, -1)), kexts=bf(kexts), kextc=bf(kextc), qext=bf(qext), colab=colab)


def host_weights(L, w_in, w_out, w1k, w1v, w2k, w2v, posk, posv, wm, convw, convb):
    blocks = _block_cols()
    w_in_p = np.zeros((L, NBLK, 128, 8, BW), np.float32)
    for b, cols in enumerate(blocks):
        sub = w_in[:, :, cols]
        w_in_p[:, b, :, :, :len(cols)] = sub.reshape(L, 8, 128, len(cols)).transpose(0, 2, 1, 3)
    w_out_p = w_out.reshape(L, 8, 128, 4, 256).transpose(0, 3, 2, 1, 4)
    w1 = np.stack([w1k, w1v], axis=1)
    w1_p = w1.reshape(L, 2, 16, 128, 2, 128).transpose(0, 1, 4, 3, 2, 5)
    w2 = np.stack([w2k, w2v], axis=1)
    w2_p = w2.reshape(L, 2, 2, 128, 64).transpose(0, 1, 3, 2, 4)
    pos = np.stack([posk, posv], axis=1)
    pos_p = pos.reshape(L, 2, 16, 2, 64).transpose(0, 1, 3, 4, 2).reshape(L, 2, 128, 16)
    wm_p = wm.reshape(L, 8, 128, 2, 256).transpose(0, 3, 2, 1, 4)
    convw_t = convw.reshape(L, 3, 2, 128).transpose(3, 0, 2, 1)
    convb_t = convb.reshape(L, 2, 128).transpose(2, 0, 1)
    c = np.ascontiguousarray
    return dict(w_in_p=c(w_in_p.reshape(L, NBLK, 128, 8 * BW)), w_out_p=c(w_out_p.reshape(L, 4, 128, 2048)),
                w1_p=c(w1_p.reshape(L, 2, 2, 128, 2048)), w2_p=c(w2_p.reshape(L, 2, 128, 128)),
                pos_p=c(pos_p), wm_p=c(wm_p.reshape(L, 2, 128, 2048)),
                convw_t=c(convw_t.reshape(128, L * 6)), convb_t=c(convb_t.reshape(128, L * 2)))


def build(T=8192, L=4, same_engine_sync=True):
    NT = T // 512
    NKT = T // 128
    nc = bass.Bass("TRN2", target_bir_lowering=False)
    dram = lambda name, shape, dt_, kind: nc.dram_tensor(name, shape, dt_, kind=kind).ap()
    EI, EO, IN = "ExternalInput", "ExternalOutput", "Internal"
    x_d = dram("x", [T, D], F32, EI)
    mem_d = dram("mem", [256, D], F32, EI)
    win_d = dram("w_in_p", [L, NBLK, 128, 8 * BW], F32, EI)
    wout_d = dram("w_out_p", [L, 4, 128, 2048], F32, EI)
    w1_d = dram("w1_p", [L, 2, 2, 128, 2048], F32, EI)
    w2_d = dram("w2_p", [L, 2, 128, 128], F32, EI)
    pos_d = dram("pos_p", [L, 2, 128, 16], F32, EI)
    wm_d = dram("wm_p", [L, 2, 128, 2048], F32, EI)
    gpre_d = dram("gpre", [L, D], F32, EI)
    gpost_d = dram("gpost", [L, D], F32, EI)
    gmem_d = dram("gmem", [L, D], F32, EI)
    convw_d = dram("convw_t", [128, L * 6], F32, EI)
    convb_d = dram("convb_t", [128, L * 2], F32, EI)
    bgate_d = dram("bgate", [L, 24], F32, EI)
    mc_d = dram("mc", [128, 896], BF16, EI)
    ml_d = dram("ml", [128, 896], BF16, EI)
    mcmp_d = dram("mcmp", [128, 2560], BF16, EI)
    mq_d = dram("mq", [128, 256], BF16, EI)
    kexts_d = dram("kexts", [64, T], BF16, EI)
    kextc_d = dram("kextc", [4, 512], BF16, EI)
    qext_d = dram("qext", [4, 512], BF16, EI)
    colab_d = dram("colab", [128, 2], F32, EI)
    out_d = dram("out", [T, D], F32, EO)
    xs_d = dram("xs", [T, D], F32, IN)
    WIN_s = dram("WIN_s", [L, NBLK, 128, 8 * BW], BF16, IN)
    WOUT_s = dram("WOUT_s", [L, 4, 128, 2048], BF16, IN)
    W1_s = dram("W1_s", [L, 2, 2, 128, 2048], BF16, IN)
    WM_s = dram("WM_s", [L, 2, 128, 2048], BF16, IN)

    ctx = ExitStack()
    S = Sched(nc, same_engine_sync=same_engine_sync)
    sb = lambda name, shape, dt_=F32: nc.alloc_sbuf_tensor(name, shape, dt_)
    KXs = sb("KXs", [128, 2, T], BF16)
    Vs = sb("Vs", [128, NKT, 2, 65], BF16)
    KXw = sb("KXw", [128, 2, 1024], BF16)
    Vw = sb("Vw", [128, 8, 2, 65], BF16)
    KXc = sb("KXc", [128, 2, 512], BF16)
    VC = sb("VC", [128, 4, 2, 65], BF16)
    KC2 = sb("KC2", [128, 2, 2, 544], BF16)
    KM = sb("KM", [128, 4, 256], BF16)
    VM = sb("VM", [128, 2, 4, 65], BF16)
    QXs = [sb("QX%d" % i, [128, 8, 512], BF16) for i in range(2)]
    QXm = sb("QXm", [128, 4, 512], BF16)
    PXs = [sb("PX%d" % i, [128, 2, 3, 512], BF16) for i in range(2)]
    QXv = [sb("QXv%d" % i, [128, 512], BF16) for i in range(2)]
    MCW = sb("MCW", [128, 896], BF16)
    MLW = sb("MLW", [128, 896], BF16)
    MCMP = sb("MCMP", [128, 5, 512], BF16)
    MQ = sb("MQ", [128, 2, 4, 32], BF16)
    identb = sb("identb", [128, 128], BF16)
    identf = sb("identf", [128, 128], F32)
    gpre_b = sb("gpre_b", [128, D], F32)
    gpost_b = sb("gpost_b", [128, D], F32)
    convw_t = sb("convw_sb", [128, L * 6], F32)
    convb_t = sb("convb_sb", [128, L * 2], F32)
    bgate_b = sb("bgate_b", [128, L * 24], F32)
    colab = sb("colab_sb", [128, 2], F32)
    mhalf = sb("mhalf", [128, 4], F32)
    cbias = sb("cbias", [128, 256], F32)
    w2b = sb("w2b", [128, 2, 128], BF16)
    pos2b = sb("pos2b", [128, 2, 16], BF16)
    xb = [sb("xb%d" % i, [128, D], F32) for i in range(2)]
    hT = sb("hT", [128, 8, 512], BF16)
    NWB = 2
    wbuf = [sb("wbuf%d" % i, [128, 8 * BW], BF16) for i in range(NWB)]
    cbuf = sb("cbuf", [128, 4, 512], F32)
    uh = sb("uh", [128, 2, 2], F32)
    tmpA = sb("tmpA", [128, 1024], F32)
    abuf = tmpA[:, 0:512]
    thb = tmpA[:, 512:1024]
    hidf = tmpA[:, 0:768].rearrange("p (a b) -> p a b", b=256)
    ycp = sb("ycp", [128, D], F32)
    yT = sb("yT", [128, 8, 512], BF16)
    GN = sb("GN", [128, 4, 512], BF16)
    GM = sb("GM", [128, 4, 256], BF16)
    GL2s = [sb("GL2_%d" % i, [128, 4, 24], F32) for i in range(2)]
    pbuf = [sb("pbuf%d" % i, [128, 512], BF16) for i in range(4)]
    OTs = [sb("OTs%d" % i, [128, 512], F32) for i in range(2)]
    acc = sb("acc", [128, 4, 512], F32)
    accm = sb("accm", [128, 4, 256], F32)
    tmpc = sb("tmpc", [128, 4, 64], F32)
    ebuf = [sb("ebuf%d" % i, [128, 512], F32) for i in range(2)]
    imp = sb("imp", [128, 516], F32)
    selv = sb("selv", [128, 128], F32)
    selv2 = sb("selv2", [128, 128], F32)
    Zc = sb("Zc", [128, 3, 128], F32)
    m8 = sb("m8", [128, 16], F32)
    st = sb("st", [128, 40], F32)
    hidb = sb("hidb", [128, 256], BF16)
    vcst = sb("vcst", [32, 2, 64], BF16)

    ps = [nc.alloc_psum_tensor("ps%d" % i, [128, 512], F32) for i in range(8)]
    PSN = ["ps%d" % i for i in range(8)]

    def dma(out, in_, r, w, q="sp"):
        S.add(q, lambda e: e.dma_start(out=out, in_=in_), r, w, dma=True)

    def mm(out, lhsT, rhs, start, stop, r, w):
        S.add("pe", lambda e: e.matmul(out, lhsT=lhsT, rhs=rhs, start=start, stop=stop), r, w)

    def trn(out, in_, ident, r, w):
        S.add("pe", lambda e: e.transpose(out=out, in_=in_, identity=ident), r, w)

    def actv(out, in_, func, r, w, scale=1.0, bias=0.0, accum=None):
        if accum is None:
            S.add("act", lambda e: e.activation(out=out, in_=in_, func=func, bias=bias, scale=scale), r, w)
        else:
            S.add("act", lambda e: e.activation(out=out, in_=in_, func=func, bias=bias, scale=scale, accum_out=accum), r, w)

    def cp(out, in_, r, w, eng="dve"):
        S.add(eng, lambda e: e.tensor_copy(out=out, in_=in_), r, w)

    def ts(out, in0, s1, s2, op0, op1, r, w, eng="dve"):
        if op1 is None:
            S.add(eng, lambda e: e.tensor_scalar(out=out, in0=in0, scalar1=s1, scalar2=None, op0=op0), r, w)
        else:
            S.add(eng, lambda e: e.tensor_scalar(out=out, in0=in0, scalar1=s1, scalar2=s2, op0=op0, op1=op1), r, w)

    def tt(out, in0, in1, op, r, w, eng="dve"):
        S.add(eng, lambda e: e.tensor_tensor(out=out, in0=in0, in1=in1, op=op), r, w)

    def stt(out, in0, scalar, in1, op0, op1, r, w, accum=None):
        if accum is None:
            S.add("dve", lambda e: e.scalar_tensor_tensor(out=out, in0=in0, scalar=scalar, in1=in1, op0=op0, op1=op1), r, w)
        else:
            S.add("dve", lambda e: e.scalar_tensor_tensor(out=out, in0=in0, scalar=scalar, in1=in1, op0=op0, op1=op1, accum_out=accum), r, w)

    def mset(ap, val, w, eng="pool"):
        S.add(eng, lambda e: e.memset(ap, val), (), w)

    dma(MCW[:], mc_d, [], ["MC"])
    dma(MLW[:], ml_d, [], ["ML"])
    dma(MCMP[:].rearrange("p a b -> p (a b)"), mcmp_d, [], ["MCMP"])
    dma(MQ[:].rearrange("p a b c -> p (a b c)"), mq_d, [], ["MQ"])
    dma(colab[:], colab_d, [], ["colab"])
    dma(convw_t[:], convw_d, [], ["convw"])
    dma(convb_t[:], convb_d, [], ["convb"])
    dma(bgate_b[:], bgate_d.rearrange("l n -> (l n)").partition_broadcast(128), [], ["bgate"])
    for par in range(2):
        mset(QXs[par][:].rearrange("p a b -> p (a b)"), 0.0,
             [("QX", par, h, "q") for h in range(8)] + [("QX", par, h, "x") for h in range(8)])
    mset(QXm[:].rearrange("p a b -> p (a b)"), 0.0, [("QXm", h) for h in range(4)])
    mset(KM[:].rearrange("p a b -> p (a b)"), 0.0, ["KM"])
    mset(KXw[:].rearrange("p a b -> p (a b)"), 0.0, [("KXw", g, sl, t) for g in range(2) for sl in range(2) for t in ("k", "x")])
    mset(KXc[:].rearrange("p a b -> p (a b)"), 0.0, [("KXc", g, t) for g in range(2) for t in ("k", "x")])
    for g in range(2):
        dma(KXs[64:128, g, :], kexts_d, [], [("KXs", g, "x")])
        dma(KXc[64:68, g, :], kextc_d, [], [("KXc", g, "x")])
    for par in range(2):
        for h in range(8):
            dma(QXs[par][64:68, h, :], qext_d, [], [("QX", par, h, "x")])
    mset(identf[:], 0.0, ["identf"])
    S.add("pool", lambda e: e.affine_select(out=identf[:], in_=identf[:], pattern=[[-1, 128]], compare_op=ALU.not_equal,
                                            fill=1.0, base=0, channel_multiplier=1), ["identf"], ["identf"])
    cp(identb[:], identf[:], ["identf"], ["identb"])
    mset(mhalf[:], -0.5, ["mhalf"])
    mset(Vs[:].rearrange("p a b c -> p (a b c)"), 1.0, [("Vs", "all")])
    mset(Vw[:].rearrange("p a b c -> p (a b c)"), 1.0, [("Vw", "all")])
    mset(VC[:].rearrange("p a b c -> p (a b c)"), 1.0, [("VC", "all")])
    mset(VM[:].rearrange("p a b c -> p (a b c)"), 1.0, [("VM", "all")])
    mset(KC2[:].rearrange("p a b c -> p (a b c)"), 0.0, ["KC2"])
    for par in range(2):
        mset(PXs[par][:].rearrange("p a b c -> p (a b c)"), 0.0, ["PXinit"])
    mset(Zc[:].rearrange("p a b -> p (a b)"), 0.0, ["Zc"])
    mset(imp[:], 0.0, ["imp"])

    wb_i = [0]

    def next_wb():
        i = wb_i[0]
        wb_i[0] = (i + 1) % NWB
        return i

    def prep(src, dst, n, last, dst_key):
        dma(dst.rearrange("p (c n) -> p c n", n=last), src.rearrange("p (c n) -> p c n", n=last), [], [dst_key], q="pool")

    prep_list = []
    for l in range(L):
        pl = []
        for kv in range(2):
            for hf in range(2):
                pl.append((w1_d[l, kv, hf], W1_s[l, kv, hf], 2048, 128, ("W1", l, kv, hf)))
        for b in range(2):
            pl.append((wm_d[l, b], WM_s[l, b], 2048, 256, ("WM", l, b)))
        for b in range(NBLK):
            pl.append((win_d[l, b], WIN_s[l, b], 8 * BW, BW, ("WIN", l, b)))
        for b in range(4):
            pl.append((wout_d[l, b], WOUT_s[l, b], 2048, 256, ("WOUT", l, b)))
        prep_list.append(pl)
    for a_ in prep_list[0]:
        prep(*a_)

    wpre = {}

    def wprefetch(src, n, key):
        if key in wpre:
            return
        i = next_wb()
        dma(wbuf[i][:, 0:n], src, [key], [("wbuf", i)])
        wpre[key] = i

    def wload(src, n, key):
        if key in wpre:
            return wpre.pop(key)
        i = next_wb()
        dma(wbuf[i][:, 0:n], src, [key], [("wbuf", i)])
        return i

    def WINb(l, b):
        return (WIN_s[l, b], 8 * BW, ("WIN", l, b))

    def W1b(l, kv, hf):
        return (W1_s[l, kv, hf], 2048, ("W1", l, kv, hf))

    def WOb(l, nb):
        return (WOUT_s[l, nb], 2048, ("WOUT", l, nb))

    gen_i = [0]

    def gen_bank():
        i = 5 + gen_i[0]
        gen_i[0] ^= 1
        return i

    def norm_A(xt, xkey, gtile, gkey, k):
        o = 24 + 3 * k
        stt(cbuf[:, 0:2, :].rearrange("p a b -> p (a b)"), xt[:], 1.0, xt[:], ALU.mult, ALU.mult,
            [xkey], ["cb01", ("stn", k, 0)], accum=st[:, o:o + 1])
        ts(st[:, o + 1:o + 2], st[:, o:o + 1], 1.0 / D, 1e-6, ALU.mult, ALU.add, [("stn", k, 0)], [("stn", k, 1)])
        tt(st[:, o + 2:o + 3], st[:, o + 1:o + 2], mhalf[:, 0:1], ALU.pow, [("stn", k, 1), "mhalf"], [("stn", k, 2)], eng="pool")
        stt(xt[:], xt[:], st[:, o + 2:o + 3], gtile[:], ALU.mult, ALU.mult, [xkey, ("stn", k, 2), gkey], [xkey])

    def norm_B(xt, xkey, col0):
        for half in range(2):
            for c4 in range(4):
                c = half * 4 + c4
                trn(ps[7][:, c4 * 128:(c4 + 1) * 128], xt[:, c * 128:(c + 1) * 128], identf[:], [xkey, "identf"], [PSN[7]])
            cp(hT[:, half * 4:half * 4 + 4, col0:col0 + 128], ps[7][:].rearrange("p (a b) -> p a b", b=128), [PSN[7]], ["hT"])

    def norm_transpose(xt, xkey, gtile, gkey, col0):
        norm_A(xt, xkey, gtile, gkey, 0)
        norm_B(xt, xkey, col0)

    def _unused(xt, xkey, col0):
        for half in range(2):
            for c4 in range(4):
                c = half * 4 + c4
                trn(ps[7][:, c4 * 128:(c4 + 1) * 128], xt[:, c * 128:(c + 1) * 128], identf[:], [xkey, "identf"], [PSN[7]])
            cp(hT[:, half * 4:half * 4 + 4, col0:col0 + 128], ps[7][:].rearrange("p (a b) -> p a b", b=128),
               [PSN[7]], ["hT"])

    class Unit:
        pass

    def run_units(units, hook=None):
        n = len(units)
        LA = 2
        pend = []
        for i in range(n + LA):
            if i < n:
                u = units[i]
                if hook is not None:
                    next(hook, None)
                if u.pre is not None:
                    u.pre()
                sbk = i % 3
                nmm = len(u.mm1)
                for k, (lh, rh, rd_) in enumerate(u.mm1):
                    mm(ps[sbk][0:u.M, :], lh, rh, k == 0, k == nmm - 1, rd_, [PSN[sbk]])
                pb = i % 4
                actv(pbuf[pb][0:u.M, :], ps[sbk][0:u.M, :], AF.Exp, [PSN[sbk]], [("pbuf", pb)], scale=u.scale, bias=u.bias)
            k2 = i - LA
            if k2 >= 0:
                u = units[k2]
                pb = k2 % 4
                mm(ps[u.ob][0:65, :], u.v, pbuf[pb][0:u.M, :], u.first, u.last, [("pbuf", pb), u.vkey], [PSN[u.ob]])
                if u.last:
                    pend.append([k2 + 3, u])
            while pend and (pend[0][0] <= i or i == n + LA - 1):
                _, u = pend.pop(0)
                u.fin(u)

    ob_i = [0]
    qv_i = [0]

    def next_ob():
        i = 3 + ob_i[0]
        ob_i[0] ^= 1
        return i

    ot_i = [0]

    def finalize(u, gate_ap, const, dst, dkey, first_write, gkey="GL2_0"):
        k = ot_i[0]
        ot_i[0] ^= 1
        cp(OTs[k][0:65, :], ps[u.ob][0:65, :], [PSN[u.ob]], [("OTs", k)])
        for s in range(4):
            trn(ps[7][:, s * 65:(s + 1) * 65], OTs[k][0:65, s * 128:(s + 1) * 128], identf[0:65, 0:65],
                [("OTs", k), "identf"], [PSN[7]])
        pv = ps[7][:, 0:260].rearrange("p (s c) -> p s c", c=65)
        ts(st[:, 20:24], pv[:, :, 64], 1e-30, None, ALU.max, None, [PSN[7]], ["st20"])
        S.add("dve", lambda e: e.reciprocal(out=st[:, 8:12], in_=st[:, 20:24]), ["st20"], ["st8"])
        if gate_ap is None:
            ts(st[:, 12:16], st[:, 8:12], const, None, ALU.mult, None, ["st8"], ["st12"])
        else:
            stt(st[:, 12:16], st[:, 8:12], const, gate_ap, ALU.mult, ALU.mult, ["st8", gkey], ["st12"])
        fb = st[:, 12:16].unsqueeze(2).to_broadcast([128, 4, 64])
        if first_write:
            tt(dst, pv[:, :, 0:64], fb, ALU.mult, [PSN[7], "st12"], [dkey])
        else:
            tt(tmpc[:], pv[:, :, 0:64], fb, ALU.mult, [PSN[7], "st12"], ["tmpc"])
            tt(dst, dst, tmpc[:], ALU.add, [dkey, "tmpc"], [dkey], eng="pool")

    def mk_unit(mm1, M, scale, bias, v, vkey, ob, first, last, fin, pre=None):
        u = Unit()
        u.pre = pre
        u.mm1, u.M, u.scale, u.bias, u.v, u.vkey, u.ob, u.first, u.last, u.fin = mm1, M, scale, bias, v, vkey, ob, first, last, fin
        return u

    MCv = lambda kr: MCW[:, 384 - 128 * kr:896 - 128 * kr]
    MLv = lambda kr: MLW[:, 384 - 128 * kr:896 - 128 * kr]

    def layer_setup(l):
        dma(gpre_b[:], gpre_d[l:l + 1, :].rearrange("a n -> (a n)").partition_broadcast(128), [], ["gpre"])
        dma(gpost_b[:], gpost_d[l:l + 1, :].rearrange("a n -> (a n)").partition_broadcast(128), [], ["gpost"])
        for kv in range(2):
            dma(w2b[:, kv, :], w2_d[l, kv], [], ["w2b"], q="pool")
            dma(pos2b[:, kv, :], pos_d[l, kv], [], ["pos2b"], q="pool")
        gb = gen_bank()
        for kv in range(2):
            for hf in range(2):
                i = wload(W1_s[l, kv, hf], 2048, ("W1", l, kv, hf))
                wv = wbuf[i][:, 0:2048].rearrange("p (c n) -> p c n", n=128)
                col = kv * 2 + hf
                for pp in range(16):
                    mm(ps[gb][:, col:col + 1], wv[:, pp, :], pos2b[:, kv, pp:pp + 1], pp == 0, pp == 15,
                       [("wbuf", i), "pos2b"], [PSN[gb]])
        cbv = cbias[:].rearrange("p (kv g hf r) -> p kv g hf r", kv=2, g=2, hf=2)
        for kv in range(2):
            for g in range(2):
                for hf in range(2):
                    col = kv * 2 + hf
                    cp(cbv[:, kv, g, hf, :], ps[gb][:, col:col + 1].to_broadcast([128, 32]), [PSN[gb]], ["cbias"])
        gmem_b = cbuf[:, 2:4, :].rearrange("p a b -> p (a b)")
        dma(gmem_b, gmem_d[l:l + 1, :].rearrange("a n -> (a n)").partition_broadcast(128), [], ["cb23", ("ca", 0), ("ca", 1)])
        for mt in range(2):
            dma(xb[mt][:], mem_d[mt * 128:(mt + 1) * 128, :], [], [("xb", mt)])
            norm_transpose(xb[mt], ("xb", mt), gmem_b, "cb23", mt * 128)
        ik = wload(WM_s[l, 0], 2048, ("WM", l, 0))
        wk = wbuf[ik][:, 0:2048].rearrange("p (c n) -> p c n", n=256)
        for pr in range(2):
            gb = gen_bank()
            for c in range(8):
                mm(ps[gb][:, 0:256], wk[:, c, pr * 128:(pr + 1) * 128], hT[:, c, 0:256], c == 0, c == 7,
                   [("wbuf", ik), "hT"], [PSN[gb]])
            cp(KM[0:64, 2 * pr, :], ps[gb][0:64, 0:256], [PSN[gb]], ["KM"])
            cp(KM[0:64, 2 * pr + 1, :], ps[gb][64:128, 0:256], [PSN[gb]], ["KM"])
        iv = wload(WM_s[l, 1], 2048, ("WM", l, 1))
        wv_ = wbuf[iv][:, 0:2048].rearrange("p (c n) -> p c n", n=256)
        for mt in range(2):
            gb = gen_bank()
            for c in range(8):
                mm(ps[gb][:, 0:256], hT[:, c, mt * 128:(mt + 1) * 128], wv_[:, c, :], c == 0, c == 7,
                   [("wbuf", iv), "hT"], [PSN[gb]])
            cp(VM[:, mt, :, 0:64], ps[gb][:, 0:256].rearrange("p (h d) -> p h d", d=64), [PSN[gb], ("VM", "all")], ["VM"])
        mset(uh[:].rearrange("p a b -> p (a b)"), 0.0, ["uh"])

    def fm_block(l, b, nxt=None):
        i = wload(WIN_s[l, b], 8 * BW, ("WIN", l, b))
        if nxt is not None:
            wprefetch(*nxt)
        wv = wbuf[i][:, :].rearrange("p (c n) -> p c n", n=BW)
        for grp in range(2):
            gb = gen_bank()
            for c in range(8):
                mm(ps[gb][:, :], wv[:, c, grp * 128:(grp + 1) * 128], hT[:, c, :], c == 0, c == 7,
                   [("wbuf", i), "hT"], [PSN[gb]])
            yield gb, grp

    def tm_block(l, b, ncols, nxt=None):
        i = wload(WIN_s[l, b], 8 * BW, ("WIN", l, b))
        if nxt is not None:
            wprefetch(*nxt)
        wv = wbuf[i][:, :].rearrange("p (c n) -> p c n", n=BW)
        for s in range(4):
            gb = gen_bank()
            for c in range(8):
                mm(ps[gb][:, 0:ncols], hT[:, c, s * 128:(s + 1) * 128], wv[:, c, 0:ncols], c == 0, c == 7,
                   [("wbuf", i), "hT"], [PSN[gb]])
            yield gb, s

    def front_gen(l, j, par, src_d):
        t0 = 512 * j
        slot = j % 2
        QX = QXs[par]
        PX = PXs[par]
        xk = lambda s: ("xrow", j * 4 + s)

        def stA(s):
            b_ = s % 2
            dma(xb[b_][:], src_d[t0 + s * 128:t0 + (s + 1) * 128, :], [xk(s)], [("xb", b_)])
            norm_A(xb[b_], ("xb", b_), gpre_b[:], "gpre", s % 2)

        def stB(s):
            norm_B(xb[s % 2], ("xb", s % 2), s * 128)
        for f_, a_ in ((stA, 0), (stA, 1), (stB, 0), (stA, 2), (stB, 1), (stA, 3), (stB, 2), (stB, 3)):
            f_(a_)
            yield
            yield
        for g in range(2):
            dma(KXw[64:68, g, slot * 512:(slot + 1) * 512], kexts_d[0:4, t0:t0 + 512], [], [("KXw", g, slot, "x")])
        for b in range(2):
            for gb, grp in fm_block(l, b, WINb(l, b + 1)):
                yield
                for hh in range(2):
                    h = b * 4 + grp * 2 + hh
                    ts(QX[0:64, h, :], ps[gb][hh * 64:(hh + 1) * 64, :], 1.0 / (8.0 * SLOPES[h]), None, ALU.mult, None,
                       [PSN[gb]], [("QX", par, h, "q")])
                yield
        for gb, grp in fm_block(l, 2, WINb(l, 3)):
            yield
            for g in range(2):
                if grp == 0:
                    dst_, dk_ = KXs[0:64, g, t0:t0 + 512], ("KXs", g, j)
                else:
                    dst_, dk_ = KXw[0:64, g, slot * 512:(slot + 1) * 512], ("KXw", g, slot, "k")
                cp(dst_, ps[gb][g * 64:(g + 1) * 64, :], [PSN[gb]], [dk_])
            yield
        for kv in range(2):
            for g in range(2):
                cp(KC2[0:64, kv, g, 0:16], KC2[0:64, kv, g, 512:528], [("KC2", kv, g)], [("KC2", kv, g)], eng="pool")
                cp(KC2[64:128, kv, g, 0:15], KC2[64:128, kv, g, 512:527], [("KC2", kv, g)], [("KC2", kv, g)], eng="pool")
        for gb, grp in fm_block(l, 3, WINb(l, 4)):
            yield
            kv = grp
            for g in range(2):
                cp(KC2[0:64, kv, g, 16:528], ps[gb][g * 64:(g + 1) * 64, :], [PSN[gb], "KC2"], [("KC2", kv, g)])
                cp(KC2[64:128, kv, g, 15:527], ps[gb][g * 64:(g + 1) * 64, :], [PSN[gb], "KC2"], [("KC2", kv, g)])
            yield
        for gb, grp in fm_block(l, 4, WINb(l, 9)):
            yield
            for hh in range(2):
                h = grp * 2 + hh
                ts(QXm[0:64, h, :], ps[gb][hh * 64:(hh + 1) * 64, :], 0.125, None, ALU.mult, None, [PSN[gb]], [("QXm", h)])
            yield
        GL2 = GL2s[par]
        gk = "GL2_%d" % par
        for gb, s in tm_block(l, 9, 280, W1b(l, 0, 0)):
            yield
            cp(Vs[:, 4 * j + s, :, 0:64], ps[gb][:, 0:128].rearrange("p (g d) -> p g d", d=64), [PSN[gb], ("Vs", "all")],
               [("Vs", j)])
            cp(Vw[:, slot * 4 + s, :, 0:64], ps[gb][:, 128:256].rearrange("p (g d) -> p g d", d=64), [PSN[gb], ("Vw", "all")],
               [("Vw", slot)])
            tt(GL2[:, s, :], ps[gb][:, 256:280], bgate_b[:, l * 24:(l + 1) * 24], ALU.add, [PSN[gb], "bgate"], [gk])
            yield
        glf = GL2[:].rearrange("p a b -> p (a b)")
        actv(glf, glf, AF.Tanh, [gk], [gk], scale=0.5)
        yield
        ts(glf, glf, 1.0, None, ALU.add, None, [gk], [gk])
        yield

        hb = gen_bank()
        w1seq = [(0, 0), (0, 1), (1, 0), (1, 1)]
        for wi, (kv, hf) in enumerate(w1seq):
            if True:
                i = wload(W1_s[l, kv, hf], 2048, ("W1", l, kv, hf))
                if wi + 1 < 4:
                    wprefetch(*W1b(l, *w1seq[wi + 1]))
                wv = wbuf[i][:, 0:2048].rearrange("p (c n) -> p c n", n=128)
                for g in range(2):
                    col = ((kv * 2 + g) * 2 + hf) * 32
                    for pp in range(16):
                        rhs = KC2[:, kv, g, 2 * pp:2 * pp + 512].rearrange("p (r s) -> p r s", s=16)[:, :, 0]
                        mm(ps[hb][:, col:col + 32], wv[:, pp, :], rhs, pp == 0, pp == 15,
                           [("wbuf", i), ("KC2", kv, g)], [PSN[hb]])
                    yield
        u_ = hidf[:, 0, :]
        v_ = hidf[:, 1, :]
        w_ = hidf[:, 2, :]
        tt(u_, ps[hb][:, 0:256], cbias[:], ALU.add, [PSN[hb], "cbias"], ["tmpA"])
        tt(v_, u_, u_, ALU.mult, ["tmpA"], ["tmpA"])
        ts(v_, v_, 0.044715, 1.0, ALU.mult, ALU.add, ["tmpA"], ["tmpA"])
        tt(v_, v_, u_, ALU.mult, ["tmpA"], ["tmpA"])
        yield
        yield
        actv(w_, v_, AF.Tanh, ["tmpA"], ["tmpA"], scale=0.7978845608028654)
        yield
        stt(hidb[:], w_, 1.0, u_, ALU.add, ALU.mult, ["tmpA"], ["hidb"])
        yield
        slot_lo = 32 * j
        for g in range(2):
            gb = gen_bank()
            for hf in range(2):
                col = ((0 * 2 + g) * 2 + hf) * 32
                mm(ps[gb][0:64, 0:32], w2b[:, 0, hf * 64:(hf + 1) * 64], hidb[:, col:col + 32], hf == 0, hf == 1,
                   ["w2b", "hidb"], [PSN[gb]])
            for hf in range(2):
                col = ((1 * 2 + g) * 2 + hf) * 32
                mm(ps[gb][0:32, 64:128], hidb[:, col:col + 32], w2b[:, 1, hf * 64:(hf + 1) * 64], hf == 0, hf == 1,
                   ["w2b", "hidb"], [PSN[gb]])
            yield
            ts(KXc[0:64, g, slot_lo:slot_lo + 32], ps[gb][0:64, 0:32], 0.5, None, ALU.mult, None, [PSN[gb]], [("KXc", g, "k")])
            ts(vcst[:, g, :], ps[gb][0:32, 64:128], 0.5, None, ALU.mult, None, [PSN[gb]], ["vcst"])
            yield
        pr0 = slot_lo % 128
        dma(VC[pr0:pr0 + 32, slot_lo // 128, :, 0:64], vcst[:], ["vcst", ("VC", "all")], ["VCd"])

        N = 32 * (j + 1)
        NB = 8 * (j + 1)
        nch = (NB - 1) // CH + 1
        mqv = 1 if j == 0 else 0
        for g in range(2):
            for s in range(4):
                for r_ in range(4):
                    h = g * 4 + r_
                    gb = gen_bank()
                    mm(ps[gb][:, 0:N], QX[0:128, h, s * 128:(s + 1) * 128], KXc[0:128, g, 0:N], True, False,
                       [("QX", par, h, "q"), ("QX", par, h, "x"), ("KXc", g, "k"), ("KXc", g, "x")], [PSN[gb]])
                    mm(ps[gb][:, N - 32:N], identb[:], MQ[:, mqv, s, :], False, True, ["identb", "MQ"], [PSN[gb]])
                    yield
                    eb = ebuf[r_ % 2]
                    ek = ("ebuf", r_ % 2)
                    actv(eb[:, 0:N], ps[gb][:, 0:N], AF.Exp, [PSN[gb]], [ek, "st4"], scale=SLOPES[h],
                         bias=-SLOPES[h] * 512.0 * j, accum=st[:, 4:5])
                    yield
                    ts(st[:, 6:7], st[:, 4:5], 1e-30, None, ALU.max, None, ["st4"], ["st6"])
                    S.add("dve", lambda e: e.reciprocal(out=st[:, 5:6], in_=st[:, 6:7]), ["st6"], ["st5"])
                    if r_ == 0:
                        ts(imp[:, 0:N], eb[:, 0:N], st[:, 5:6], None, ALU.mult, None, [ek, "st5"], ["imp"])
                    else:
                        stt(imp[:, 0:N], eb[:, 0:N], st[:, 5:6], imp[:, 0:N], ALU.mult, ALU.add, [ek, "st5", "imp"], ["imp"])
                mset(imp[:, N:N + 1], 0.0, ["imp"], eng="dve")
                chb = ebuf[0]
                tt(chb[:, 0:N], imp[:, 0:N], imp[:, 1:N + 1], ALU.add, ["imp"], [("ebuf", 0)])
                S.add("dve", lambda e, chb=chb, NB=NB, N=N: e.tensor_reduce(
                    out=selv[:, 0:NB], in_=chb[:, 0:N].rearrange("p (n f) -> p n f", f=4), axis=AX.X, op=ALU.add),
                    [("ebuf", 0)], ["selv"])
                lo = 8 * j + 2 * s
                if lo + 2 < 128:
                    mset(selv[:, lo + 2:128], -1.0, ["selv"], eng="dve")
                cp(selv[:, lo + 1:lo + 2], colab[:, 0:1], ["colab", "selv"], ["selv"])
                mset(selv[:, lo:lo + 1], 1.0e4, ["selv"], eng="dve")
                if lo - 1 >= 1:
                    ts(selv[:, lo - 1:lo], selv[:, lo - 1:lo], colab[:, 1:2], None, ALU.max, None, ["selv", "colab"], ["selv"])
                mset(selv[:, 0:1], 1.0e4, ["selv"], eng="dve")
                S.add("dve", lambda e: e.max(out=m8[:, 0:8], in_=selv[:]), ["selv"], ["m8"])
                S.add("dve", lambda e: e.match_replace(out=selv2[:], in_to_replace=m8[:, 0:8], in_values=selv[:], imm_value=-2.0),
                      ["selv", "m8"], ["selv2"])
                S.add("dve", lambda e: e.max(out=m8[:, 8:16], in_=selv2[:]), ["selv2"], ["m8b"])
                for c in range(nch):
                    n0 = CH * c
                    n1 = min(128, n0 + CH)
                    ts(Zc[:, c, 68:68 + (n1 - n0)], selv[:, n0:n1], m8[:, 15:16], NEG, ALU.is_lt, ALU.mult,
                       ["selv", "m8b"], ["Zc"])
                for _ in range(12):
                    yield
                for c in range(nch):
                    trn(ps[7][:, c * 128:(c + 1) * 128], Zc[:, c, :], identf[:], ["Zc", "identf"], [PSN[7]])
                cp(PX[64:128, g, 0:nch, s * 128:(s + 1) * 128],
                   ps[7][64:128, 0:nch * 128].rearrange("p (a b) -> p a b", b=128), [PSN[7], "PXinit"],
                   [("PX", par, g, c) for c in range(nch)])
                yield

    def rest_gen(l, j):
        for half in range(2):
            for gb, s in tm_block(l, 10 + half, 256, WINb(l, 11 + half)):
                yield
                actv(thb[:, 0:256], ps[gb][:, 0:256], AF.Tanh, [PSN[gb]], ["tmpA"], scale=0.5)
                yield
                stt(GN[:, s, half * 256:(half + 1) * 256], thb[:, 0:256], 1.0, ps[gb][:, 0:256], ALU.add, ALU.mult,
                    ["tmpA", PSN[gb]], ["GN"])
        for gb, s in tm_block(l, 12, 256, WINb(l, 6)):
            yield
            actv(thb[:, 0:256], ps[gb][:, 0:256], AF.Tanh, [PSN[gb]], ["tmpA"], scale=0.5)
            yield
            stt(GM[:, s, :], thb[:, 0:256], 1.0, ps[gb][:, 0:256], ALU.add, ALU.mult, ["tmpA", PSN[gb]], ["GM"])
        cw = lambda cc, k: convw_t[:, l * 6 + cc * 3 + k:l * 6 + cc * 3 + k + 1]
        for gb, cc in fm_block(l, 6, WINb(l, 7)):
            yield
            cp(cbuf[:, cc, :], ps[gb][:, :], [PSN[gb], "cb01"], [("cb", cc)])
        uu = tmpA[:, 0:514]
        for gb, cc in fm_block(l, 7, WINb(l, 5)):
            yield
            cp(uu[:, 0:2], uh[:, cc, :], ["uh", "tmpA"], ["tmpA"])
            tt(uu[:, 2:514], cbuf[:, cc, :], ps[gb][:, :], ALU.mult, [("cb", cc), PSN[gb], "tmpA"], ["tmpA"])
            ak = ("ca", cc)
            ts(cbuf[:, 2 + cc, :], uu[:, 2:514], cw(cc, 2), convb_t[:, l * 2 + cc:l * 2 + cc + 1], ALU.mult, ALU.add,
               ["tmpA", "convw", "convb", "cb23"], [ak])
            stt(cbuf[:, 2 + cc, :], uu[:, 1:513], cw(cc, 1), cbuf[:, 2 + cc, :], ALU.mult, ALU.add, ["tmpA", ak, "convw"], [ak])
            stt(cbuf[:, 2 + cc, :], uu[:, 0:512], cw(cc, 0), cbuf[:, 2 + cc, :], ALU.mult, ALU.add, ["tmpA", ak, "convw"], [ak])
            cp(uh[:, cc, :], uu[:, 512:514], ["tmpA"], ["uh"])
            yield
        for gb, cc in fm_block(l, 5, WINb(l, 8)):
            yield
            ak = ("ca", cc)
            tt(cbuf[:, 2 + cc, :], cbuf[:, 2 + cc, :], ps[gb][:, :], ALU.mult, [ak, PSN[gb]], [ak])
        for gb, cc in fm_block(l, 8):
            yield
            ak = ("ca", cc)
            actv(thb[:], ps[gb][:, :], AF.Tanh, [PSN[gb]], ["tmpA"], scale=0.5)
            yield
            stt(abuf[:], thb[:], 1.0, ps[gb][:, :], ALU.add, ALU.mult, ["tmpA", PSN[gb]], ["tmpA"])
            stt(yT[:, cc, :], cbuf[:, 2 + cc, :], 0.5, abuf[:], ALU.mult, ALU.mult, [ak, "tmpA"], [("yT", cc)])
            yield

    def attention(l, j, par, hook1, hook):
        slot = j % 2
        pslot = 1 - slot
        QX = QXs[par]
        PX = PXs[par]
        N = 32 * (j + 1)
        qk = lambda h: [("QX", par, h, "q"), ("QX", par, h, "x")]

        def fin_nsa(br, h, first_write):
            def f(u):
                finalize(u, GL2s[par][:, :, br * 8 + h], 0.25, acc[:, :, h * 64:(h + 1) * 64], ("acc", h), first_write,
                         gkey="GL2_%d" % par)
            return f

        def fin_mem(h):
            def f(u):
                finalize(u, None, 0.5, accm[:, :, h * 64:(h + 1) * 64], ("accm", h), True)
            return f

        units = []
        for h in range(8):
            g = h // 4
            ob = next_ob()
            tl = []
            if j >= 1:
                for kr in range(4):
                    tl.append((pslot, kr, "ML"))
            for kr in range(4):
                tl.append((slot, kr, "MC"))
            for ti, (sl_, kr, mk) in enumerate(tl):
                mt_ = MLv(kr) if mk == "ML" else MCv(kr)
                mm1 = [(KXw[0:128, g, sl_ * 512 + kr * 128:sl_ * 512 + (kr + 1) * 128], QX[0:128, h, :],
                        [("KXw", g, sl_, "k"), ("KXw", g, sl_, "x")] + qk(h)),
                       (identb[:], mt_, ["identb", mk])]
                units.append(mk_unit(mm1, 128, SLOPES[h], -SLOPES[h] * 512.0 * j, Vw[:, sl_ * 4 + kr, g, :], ("Vw", sl_),
                                     ob, ti == 0, ti == len(tl) - 1, fin_nsa(2, h, True)))
        for h in range(4):
            ob = next_ob()
            for kt in range(2):
                mm1 = [(KM[0:128, h, kt * 128:(kt + 1) * 128], QXm[0:128, h, :], ["KM", ("QXm", h)])]
                units.append(mk_unit(mm1, 128, 1.0, 0.0, VM[:, kt, h, :], "VM", ob, kt == 0, kt == 1, fin_mem(h)))
        nkc = (N - 1) // 128 + 1
        var = 4 if j == 0 else j % 4
        for h in range(8):
            g = h // 4
            ob = next_ob()
            for kt in range(nkc):
                mm1 = [(KXc[0:128, g, kt * 128:kt * 128 + 128], QX[0:128, h, :],
                        [("KXc", g, "k"), ("KXc", g, "x")] + qk(h))]
                if kt == nkc - 1:
                    mm1.append((identb[:], MCMP[:, var, :], ["identb", "MCMP"]))
                units.append(mk_unit(mm1, 128, SLOPES[h], -SLOPES[h] * 512.0 * j, VC[:, kt, g, :], "VCd",
                                     ob, kt == 0, kt == nkc - 1, fin_nsa(0, h, False)))
        run_units(units, hook1)

        units = []
        for h in range(8):
            g = h // 4
            ob = next_ob()
            nk = 4 * j + 4
            for kt in range(nk):
                c = kt // 30
                pre = None
                if kt % 30 == 0:
                    vb = qv_i[0]
                    qv_i[0] ^= 1

                    def pre(vb=vb, g=g, c=c, h=h):
                        cp(QXv[vb][64:128, :], PX[64:128, g, c, :], [("PX", par, g, c), "PXinit"], [("QXv", vb)], eng="pool")
                        cp(QXv[vb][0:68, :], QX[0:68, h, :], qk(h), [("QXv", vb)], eng="pool")
                mm1 = [(KXs[0:128, g, kt * 128:(kt + 1) * 128], QXv[vb][0:128, :],
                        [("KXs", g, kt // 4), ("KXs", g, "x"), ("QXv", vb)])]
                if kt >= 4 * j:
                    mm1.append((identb[:], MCv(kt - 4 * j), ["identb", "MC"]))
                units.append(mk_unit(mm1, 128, SLOPES[h], -SLOPES[h] * 512.0 * j, Vs[:, kt, g, :], ("Vs", kt // 4),
                                     ob, kt == 0, kt == nk - 1, fin_nsa(1, h, False), pre=pre))
        run_units(units, hook)

    def stage_H(l, j):
        accf = acc[:].rearrange("p a b -> p (a b)")
        tt(accf, accf, GN[:].rearrange("p a b -> p (a b)"), ALU.mult, [("acc", h) for h in range(8)] + ["GN"],
           ["accg"] + [("acc", h) for h in range(8)])
        accmf = accm[:].rearrange("p a b -> p (a b)")
        tt(accmf, accmf, GM[:].rearrange("p a b -> p (a b)"), ALU.mult, [("accm", h) for h in range(4)] + ["GM"],
           ["accmg"] + [("accm", h) for h in range(4)])
        for s in range(4):
            for c4 in range(4):
                trn(ps[7][:, c4 * 128:(c4 + 1) * 128], acc[:, s, c4 * 128:(c4 + 1) * 128], identf[:],
                    ["accg", "identf"] + [("acc", h) for h in range(8)], [PSN[7]])
            cp(yT[:, 2:6, s * 128:(s + 1) * 128], ps[7][:].rearrange("p (a b) -> p a b", b=128), [PSN[7]], [("yT", 2 + s)])
        for s in range(4):
            for c2 in range(2):
                trn(ps[7][:, c2 * 128:(c2 + 1) * 128], accm[:, s, c2 * 128:(c2 + 1) * 128], identf[:],
                    ["accmg", "identf"] + [("accm", h) for h in range(4)], [PSN[7]])
            cp(yT[:, 6:8, s * 128:(s + 1) * 128], ps[7][:, 0:256].rearrange("p (a b) -> p a b", b=128), [PSN[7]], [("yT", 6 + s)])

    def back_gen(l, j, src_d, dst_d):
        t0 = 512 * j
        xk = lambda s: ("xrow", j * 4 + s)
        yT_all = [("yT", k) for k in range(10)]
        junk = cbuf[:, 0:2, :].rearrange("p a b -> p (a b)")
        ycps = [(ycp[:], ["ycp"]), (cbuf[:, 2:4, :].rearrange("p a b -> p (a b)"), ["cb23", ("ca", 0), ("ca", 1)])]
        for sp_ in range(2):
            subs = (2 * sp_, 2 * sp_ + 1)
            for s in subs:
                dma(xb[s % 2][:], src_d[t0 + s * 128:t0 + (s + 1) * 128, :], [xk(s)], [("xb", s % 2)])
            for nb in range(4):
                i = wload(*WOb(l, nb))
                if nb + 1 < 4 or sp_ == 0:
                    wprefetch(*WOb(l, (nb + 1) % 4))
                wv = wbuf[i][:, 0:2048].rearrange("p (c n) -> p c n", n=256)
                for s in subs:
                    yc, yk = ycps[s % 2]
                    gb = gen_bank()
                    for c in range(8):
                        mm(ps[gb][:, 0:256], yT[:, c, s * 128:(s + 1) * 128], wv[:, c, :], c == 0, c == 7,
                           [("wbuf", i)] + yT_all, [PSN[gb]])
                    yield
                    cp(yc[:, nb * 256:(nb + 1) * 256], ps[gb][:, 0:256], [PSN[gb]], yk)
                    yield
            for s in subs:
                b_ = s % 2
                yc, yk = ycps[b_]
                o = 16 if b_ == 0 else 32
                stt(junk, yc, 1.0, yc, ALU.mult, ALU.mult, yk + [("cb", 0), ("cb", 1)], ["cb01", ("stp", b_, 0)], accum=st[:, o:o + 1])
                ts(st[:, o + 1:o + 2], st[:, o:o + 1], 1.0 / D, 1e-6, ALU.mult, ALU.add, [("stp", b_, 0)], [("stp", b_, 1)])
                tt(st[:, o + 2:o + 3], st[:, o + 1:o + 2], mhalf[:, 0:1], ALU.pow, [("stp", b_, 1), "mhalf"], [("stp", b_, 2)], eng="pool")
                yield
                stt(yc, yc, st[:, o + 2:o + 3], gpost_b[:], ALU.mult, ALU.mult, yk + [("stp", b_, 2), "gpost"], yk)
                tt(yc, yc, xb[b_][:], ALU.add, yk + [("xb", b_)], yk)
                dma(dst_d[t0 + s * 128:t0 + (s + 1) * 128, :], yc, yk, [xk(s)] if dst_d is xs_d else [("orow", j * 4 + s)], q="pool")
                yield

    def exhaust(gen):
        for _ in gen:
            pass

    import itertools
    for l in range(L):
        layer_setup(l)
        src_d = x_d if l == 0 else xs_d
        dst_d = out_d if l == L - 1 else xs_d
        nprep = iter(prep_list[l + 1]) if l + 1 < L else iter(())
        exhaust(front_gen(l, 0, 0, src_d))
        exhaust(rest_gen(l, 0))
        pending = iter(())
        for j in range(NT):
            par = j % 2
            nxt = front_gen(l, j + 1, 1 - par, src_d) if j + 1 < NT else iter(())
            stream = itertools.chain(pending, nxt)
            attention(l, j, par, pending, stream)
            exhaust(stream)
            for _ in range(2):
                a_ = next(nprep, None)
                if a_ is not None:
                    prep(*a_)
            stage_H(l, j)
            pending = itertools.chain(back_gen(l, j, src_d, dst_d), rest_gen(l, j + 1) if j + 1 < NT else iter(()))
        exhaust(pending)
        for a_ in nprep:
            prep(*a_)

    print("sbuf bytes remaining/partition:", nc.sbuf_bytes_remaining() if callable(nc.sbuf_bytes_remaining) else nc.sbuf_bytes_remaining,
          " ops:", {e: len(v) for e, v in S.ops.items()}, " waits:", S.nwaits)
    S.emit(ctx)
    ctx.close()
    return nc


_NC_CACHE = {}


def run(inp, T, L, n_cores=8):
    f = lambda a: np.ascontiguousarray(np.asarray(a, dtype=np.float32))
    x = f(inp["x"])
    B = x.shape[0]
    key = (T, L)
    if key not in _NC_CACHE:
        _NC_CACHE[key] = build(T, L)
    nc = _NC_CACHE[key]
    shared = host_consts(T)
    shared.update(host_weights(L, f(inp["w_in"]), f(inp["w_out"]), f(inp["cmp_w1_k"]), f(inp["cmp_w1_v"]),
                               f(inp["cmp_w2_k"]), f(inp["cmp_w2_v"]), f(inp["cmp_pos_k"]), f(inp["cmp_pos_v"]),
                               f(inp["w_mem_kv"]), f(inp["conv_w"]), f(inp["conv_b"])))
    shared["gpre"] = f(inp["pre_norm_g"])
    shared["gpost"] = f(inp["post_norm_g"])
    shared["gmem"] = f(inp["mem_norm_g"])
    shared["bgate"] = f(inp["b_gate"])
    mem = f(inp["mem"])
    in_maps = []
    for c in range(n_cores):
        b = c % B
        m = dict(shared)
        m["x"] = np.ascontiguousarray(x[b])
        m["mem"] = np.ascontiguousarray(mem[b])
        in_maps.append(m)
    res = run_bass_kernel_spmd(nc, in_maps, core_ids=list(range(n_cores)))
    out = np.stack([np.asarray(res.results[b]["out"], dtype=np.float32) for b in range(B)], axis=0)
    return out


def kernel(**inputs):
    return run(inputs, 8192, 4)
```
